# Optimizing a Trainium2 kernel written in Bass

```python
import math
import jax, jax.numpy as jnp
from jax import lax
import numpy as np

D_MODEL = 1024
BATCH = 16
SEQ = 2048
DEPTH = 1

D_SSM = D_MODEL // 2
SSM_GROUP = 16
N_SSM_GROUPS = D_SSM // SSM_GROUP
SSM_STATE = 64
D_ATTN = D_MODEL - D_SSM
HEAD_DIM = 64
N_HEADS = D_ATTN // HEAD_DIM
N_KV_HEADS = 2
D_KV = N_KV_HEADS * HEAD_DIM
IDX_HEADS = 8
IDX_DIM = 64
TOPK_MAX = 256
Q_BLOCK = 128
D_FF = 4 * D_MODEL
EPS = 1e-6
DT_MIN = 1e-3
DT_MAX = 1e-1
IDX_SCALE = (IDX_DIM ** -0.5) * (IDX_HEADS ** -0.5)
D_IN = D_SSM + D_ATTN + 2 * D_KV + IDX_HEADS * IDX_DIM + IDX_DIM + IDX_HEADS

kernel_name = "hymba_s5_dsa_hybrid_layer"


def rms_norm(x, g):
    xf = x.astype(jnp.float32)
    y = xf * lax.rsqrt(jnp.mean(xf * xf, axis=-1, keepdims=True) + EPS)
    return (y * g.astype(jnp.float32)).astype(x.dtype)


def s5_mixer(u, lam_re, lam_im, log_dt, b_re, b_im, c_re, c_im, d_skip, w_glu, b_glu):
    bsz, seq, _ = u.shape
    uf = u.astype(jnp.float32)
    ug = uf.reshape(bsz, seq, N_SSM_GROUPS, SSM_GROUP)
    lam = lax.complex(lam_re.astype(jnp.float32), lam_im.astype(jnp.float32))
    dt = jnp.exp(log_dt.astype(jnp.float32))[:, None]
    lam_bar = jnp.exp(lam * dt)
    b_c = lax.complex(b_re.astype(jnp.float32), b_im.astype(jnp.float32))
    b_bar = ((lam_bar - 1.0) / lam)[..., None] * b_c
    bu = jnp.einsum('gpc,blgc->blgp', b_bar, ug.astype(jnp.complex64))
    a = jnp.broadcast_to(lam_bar[None, None], (1, seq, N_SSM_GROUPS, SSM_STATE))

    def combine(e1, e2):
        a1, s1 = e1
        a2, s2 = e2
        return a1 * a2, a2 * s1 + s2

    _, states = lax.associative_scan(combine, (a, bu), axis=1)
    c_c = lax.complex(c_re.astype(jnp.float32), c_im.astype(jnp.float32))
    y = jnp.real(jnp.einsum('gcp,blgp->blgc', c_c, states)).reshape(bsz, seq, D_SSM)
    y = y + d_skip.astype(jnp.float32) * uf
    z = jax.nn.gelu(y)
    out = z * jax.nn.sigmoid(z @ w_glu.astype(jnp.float32) + b_glu.astype(jnp.float32))
    return out.astype(u.dtype)


def dsa_attention(q, k, v, q_idx, k_idx, w_idx, q_gain, k_gain):
    bsz, seq = q.shape[0], q.shape[1]
    q = rms_norm(q, q_gain)
    k = rms_norm(k, k_gain)
    topk = min(TOPK_MAX, seq // 4)
    n_blocks = seq // Q_BLOCK
    key_pos = jnp.arange(seq)
    bidx = jnp.arange(bsz)[:, None, None]
    k_idx_f = k_idx.astype(jnp.float32)

    def block(start):
        qb = lax.dynamic_slice_in_dim(q, start, Q_BLOCK, axis=1)
        qib = lax.dynamic_slice_in_dim(q_idx, start, Q_BLOCK, axis=1).astype(jnp.float32)
        wb = lax.dynamic_slice_in_dim(w_idx, start, Q_BLOCK, axis=1).astype(jnp.float32)
        q_pos = start + jnp.arange(Q_BLOCK)
        causal = key_pos[None, :] <= q_pos[:, None]
        logits = jax.nn.relu(jnp.einsum('bthd,bsd->bths', qib, k_idx_f))
        score = jnp.einsum('bth,bths->bts', wb, logits) * IDX_SCALE
        score = jnp.where(causal[None], score, -jnp.inf)
        _, sel = lax.top_k(score, topk)
        sel_ok = sel <= q_pos[None, :, None]
        k_sel = k[bidx, sel].astype(jnp.float32)
        v_sel = v[bidx, sel].astype(jnp.float32)
        qg = qb.astype(jnp.float32).reshape(bsz, Q_BLOCK, N_KV_HEADS, N_HEADS // N_KV_HEADS, HEAD_DIM)
        s = jnp.einsum('btkgd,btnkd->btkgn', qg, k_sel) * (HEAD_DIM ** -0.5)
        s = jnp.where(sel_ok[:, :, None, None, :], s, -jnp.inf)
        p = jax.nn.softmax(s, axis=-1)
        o = jnp.einsum('btkgn,btnkd->btkgd', p, v_sel).reshape(bsz, Q_BLOCK, N_HEADS * HEAD_DIM)
        return o.astype(q.dtype)

    outs = lax.map(block, jnp.arange(n_blocks) * Q_BLOCK)
    return outs.transpose(1, 0, 2, 3).reshape(bsz, seq, N_HEADS * HEAD_DIM)


def setup_inputs(seed: int = 0) -> dict:
    key = jax.random.key(seed)
    ks = jax.random.split(key, 26)
    f32 = jnp.float32
    G, P, C = N_SSM_GROUPS, SSM_STATE, SSM_GROUP

    def nrm(k, shape, scale):
        return jax.random.normal(k, shape, f32) * scale

    x = jax.random.normal(ks[0], (BATCH, SEQ, D_MODEL), f32)
    c = jax.random.normal(ks[1], (BATCH, D_MODEL), f32)
    norm1_g = 1.0 + nrm(ks[2], (DEPTH, D_MODEL), 0.02)
    norm2_g = 1.0 + nrm(ks[3], (DEPTH, D_MODEL), 0.02)
    w_ada = nrm(ks[4], (DEPTH, D_MODEL, 6 * D_MODEL), 0.5 * D_MODEL ** -0.5)
    b_ada = nrm(ks[5], (DEPTH, 6 * D_MODEL), 0.02)
    w_in = nrm(ks[6], (DEPTH, D_MODEL, D_IN), D_MODEL ** -0.5)
    lam_re = -0.5 + nrm(ks[7], (DEPTH, G, P), 0.01)
    lam_im = jnp.pi * jnp.arange(P, dtype=f32)[None, None, :] + nrm(ks[8], (DEPTH, G, P), 0.01)
    log_dt = jax.random.uniform(ks[9], (DEPTH, G), f32, math.log(DT_MIN), math.log(DT_MAX))
    ssm_b_re = nrm(ks[10], (DEPTH, G, P, C), (2.0 * C) ** -0.5)
    ssm_b_im = nrm(ks[11], (DEPTH, G, P, C), (2.0 * C) ** -0.5)
    ssm_c_re = nrm(ks[12], (DEPTH, G, C, P), (2.0 * P) ** -0.5)
    ssm_c_im = nrm(ks[13], (DEPTH, G, C, P), (2.0 * P) ** -0.5)
    d_skip = nrm(ks[14], (DEPTH, D_SSM), 1.0)
    w_glu = nrm(ks[15], (DEPTH, D_SSM, D_SSM), D_SSM ** -0.5)
    b_glu = nrm(ks[16], (DEPTH, D_SSM), 0.02)
    q_gain = 1.0 + nrm(ks[17], (DEPTH, HEAD_DIM), 0.02)
    k_gain = 1.0 + nrm(ks[18], (DEPTH, HEAD_DIM), 0.02)
    gn_ssm = 1.0 + nrm(ks[19], (DEPTH, D_SSM), 0.02)
    gn_attn = 1.0 + nrm(ks[20], (DEPTH, D_ATTN), 0.02)
    w_out = nrm(ks[21], (DEPTH, D_SSM + D_ATTN, D_MODEL), (D_SSM + D_ATTN) ** -0.5)
    w_ff1 = nrm(ks[22], (DEPTH, D_MODEL, D_FF), D_MODEL ** -0.5)
    w_ff2 = nrm(ks[23], (DEPTH, D_FF, D_MODEL), D_FF ** -0.5)
    return {"x": x, "c": c, "norm1_g": norm1_g, "norm2_g": norm2_g, "w_ada": w_ada, "b_ada": b_ada,
            "w_in": w_in, "lam_re": lam_re, "lam_im": lam_im, "log_dt": log_dt,
            "ssm_b_re": ssm_b_re, "ssm_b_im": ssm_b_im, "ssm_c_re": ssm_c_re, "ssm_c_im": ssm_c_im,
            "d_skip": d_skip, "w_glu": w_glu, "b_glu": b_glu, "q_gain": q_gain, "k_gain": k_gain,
            "gn_ssm": gn_ssm, "gn_attn": gn_attn, "w_out": w_out, "w_ff1": w_ff1, "w_ff2": w_ff2}


def reference(x, c, norm1_g, norm2_g, w_ada, b_ada, w_in, lam_re, lam_im, log_dt,
              ssm_b_re, ssm_b_im, ssm_c_re, ssm_c_im, d_skip, w_glu, b_glu, q_gain, k_gain,
              gn_ssm, gn_attn, w_out, w_ff1, w_ff2):
    bsz, seq, _ = x.shape
    split_points = list(np.cumsum([D_SSM, D_ATTN, D_KV, D_KV, IDX_HEADS * IDX_DIM, IDX_DIM]))
    silu_c = jax.nn.silu(c)
    for i in range(DEPTH):
        mod = silu_c @ w_ada[i] + b_ada[i]
        sh1, sc1, g1, sh2, sc2, g2 = [m[:, None, :] for m in jnp.split(mod, 6, axis=-1)]

        h = rms_norm(x, norm1_g[i]) * (1.0 + sc1) + sh1
        proj = h @ w_in[i]
        u_ssm, q, k, v, q_idx, k_idx, w_idx = jnp.split(proj, split_points, axis=-1)
        y_ssm = s5_mixer(u_ssm, lam_re[i], lam_im[i], log_dt[i], ssm_b_re[i], ssm_b_im[i],
                         ssm_c_re[i], ssm_c_im[i], d_skip[i], w_glu[i], b_glu[i])
        y_att = dsa_attention(q.reshape(bsz, seq, N_HEADS, HEAD_DIM),
                              k.reshape(bsz, seq, N_KV_HEADS, HEAD_DIM),
                              v.reshape(bsz, seq, N_KV_HEADS, HEAD_DIM),
                              q_idx.reshape(bsz, seq, IDX_HEADS, IDX_DIM),
                              k_idx, w_idx, q_gain[i], k_gain[i])
        mixed = jnp.concatenate([rms_norm(y_ssm, gn_ssm[i]), rms_norm(y_att, gn_attn[i])], axis=-1)
        x = x + g1 * (mixed @ w_out[i])

        h2 = rms_norm(x, norm2_g[i]) * (1.0 + sc2) + sh2
        ff = jnp.square(jax.nn.relu(h2 @ w_ff1[i])) @ w_ff2[i]
        x = x + g2 * ff
    return x
```

```python
import math
from contextlib import ExitStack

import numpy as np
import concourse.bass as bass
import concourse.mybir as mybir
from concourse.alu_op_type import AluOpType as ALU
from concourse.bass_utils import run_bass_kernel_spmd

F32 = mybir.dt.float32
BF16 = mybir.dt.bfloat16
I32 = mybir.dt.int32
AF = mybir.ActivationFunctionType
AX = mybir.AxisListType

NCORES = 8
D = 1024
SEQ = 2048
NSEQ = 2
DIN = 1864
DFF = 4096
EPS = 1e-6
IDX_SCALE = (64 ** -0.5) * (8 ** -0.5)
TOPK = 256
NITER = 18
BIG = 30000.0
EXPS = list(range(-7, 9)) + [16, 32, 64, 128, 256, 512, 1024]
NE = len(EXPS)
EIDX = {m: i for i, m in enumerate(EXPS)}
TWO_PI = 2.0 * math.pi


class Buf:
    __slots__ = ("w", "r", "ex")

    def __init__(self, ex=False):
        self.w = None
        self.r = {}
        self.ex = ex


class Sched:
    NDS = 24

    def __init__(self, nc, st):
        self.nc = nc
        self.eng = {"pe": nc.tensor, "act": nc.scalar, "dve": nc.vector, "pool": nc.gpsimd, "sp": nc.sync}
        self.sem = {k: st.enter_context(nc.semaphore("s_" + k)) for k in self.eng}
        self.cnt = {k: 0 for k in self.eng}
        self.waited = {k: {} for k in self.eng}
        self.dsem = [st.enter_context(nc.semaphore("d%d" % i)) for i in range(self.NDS)]
        self.dcnt = [0] * self.NDS
        self.dpool = {"sp": list(range(0, 16)), "pool": list(range(16, 24))}
        self.dnext = {"sp": 0, "pool": 0}

    def _semof(self, key):
        return self.sem[key[1]] if key[0] == "e" else self.dsem[key[1]]

    def _wait(self, e, key, val):
        if key[0] == "e" and key[1] == e and e == "pe":
            return
        if self.waited[e].get(key, 0) >= val:
            return
        self.eng[e].wait_ge(self._semof(key), val)
        self.waited[e][key] = val

    def _deps(self, e, reads, writes):
        for b in reads:
            if b.w is not None:
                self._wait(e, b.w[0], b.w[1])
            if b.ex:
                for k, v in b.r.items():
                    if k != ("e", e):
                        self._wait(e, k, v)
        for b in writes:
            if b.w is not None:
                self._wait(e, b.w[0], b.w[1])
            for k, v in b.r.items():
                self._wait(e, k, v)

    def _mark(self, key, val, reads, writes):
        for b in reads:
            if b.r.get(key, 0) < val:
                b.r[key] = val
        for b in writes:
            b.w = (key, val)
            b.r = {}

    def op(self, e, fn, reads=(), writes=()):
        self._deps(e, reads, writes)
        ins = fn(self.eng[e])
        self.cnt[e] += 1
        ins.then_inc(self.sem[e], 1)
        self._mark(("e", e), self.cnt[e], reads, writes)

    def dma(self, e, out, in_, reads=(), writes=(), **kw):
        self._deps(e, reads, writes)
        pl = self.dpool[e]
        j = pl[self.dnext[e] % len(pl)]
        self.dnext[e] += 1
        if self.dcnt[j] > 0:
            self._wait(e, ("d", j), self.dcnt[j])
        ins = self.eng[e].dma_start(out=out, in_=in_, **kw)
        self.dcnt[j] += 16
        ins.then_inc(self.dsem[j], 16)
        self._mark(("d", j), self.dcnt[j], reads, writes)

    def barrier(self):
        for e in self.eng:
            for k in self.eng:
                if k != e and self.cnt[k] > 0:
                    self._wait(e, ("e", k), self.cnt[k])
            for j in range(self.NDS):
                if self.dcnt[j] > 0:
                    self._wait(e, ("d", j), self.dcnt[j])


class _Stop(Exception):
    pass


def build_program(stop=None):
    nc = bass.Bass("TRN2", target_bir_lowering=False)

    def din(name, shape, dt=F32):
        return nc.dram_tensor(name, list(shape), dt, kind="ExternalInput").ap()

    x_d = din("x", [NSEQ * SEQ, D])
    cT_d = din("cT", [128, 8, NSEQ, 128])
    wada_d = din("w_ada", [128, 8, 6 * D])
    bada_d = din("b_ada_b", [128, 6 * D])
    win_d = din("w_in", [128, 8, DIN])
    wout_d = din("w_out", [128, 8, D])
    wglu_d = din("w_glu", [128, 4, 512])
    wff1_d = din("w_ff1", [128, 8, DFF])
    wff2_d = din("w_ff2", [128, 32, D])
    n1g_d = din("norm1_g_b", [128, D])
    n2g_d = din("norm2_g_b", [128, D])
    bglu_d = din("b_glu_b", [128, 512])
    gns_d = din("gn_ssm_b", [128, 512])
    gna_d = din("gn_attn_b", [128, 512])
    gqb_d = din("gq_b", [128, 64])
    gkb_d = din("gk_b", [128, 64])
    gq2_d = din("gq2", [128, 1])
    gk2_d = din("gk2", [128, 1])
    lre_d = din("lamre_q", [128, 16])
    lim_d = din("lamim_q", [128, 16])
    ldt_d = din("logdt_q", [128, 16])
    bre_d = din("bre_q", [128, 16, 16])
    bim_d = din("bim_q", [128, 16, 16])
    cre_d = din("cre_q", [128, 16, 16])
    cim_d = din("cim_q", [128, 16, 16])
    dsk_d = din("dsk_b", [128, 32, 128])
    ident_d = din("ident", [128, 128])
    causal_d = din("causal", [128, 128])
    negi4_d = din("negi4", [128, 512])
    bmask_d = din("bmask", [128, 128])
    dmask_d = din("dmask", [128, 128])
    onesblk_d = din("onesblk", [128, 128])
    mtab_d = din("mtab", [128, 16, NE])
    pow2_d = din("pow2", [128, NITER + 2])
    zmask_d = din("zmask", [128, 2])
    out_d = nc.dram_tensor("out", [NSEQ * SEQ, D], F32, kind="ExternalOutput").ap()

    try:
      with ExitStack() as top:
        S = Sched(nc, top)

        def chk(name):
            if stop == name:
                S.barrier()
                raise _Stop()

        uid = [0]

        def sb(st, name, shape, dt):
            uid[0] += 1
            return st.enter_context(nc.sbuf_tensor("sb%d_%s" % (uid[0], name), list(shape), dt))

        def ps(st, name, shape, dt):
            uid[0] += 1
            return st.enter_context(nc.psum_tensor("ps%d_%s" % (uid[0], name), list(shape), dt))

        pt = [ps(top, "pt%d" % i, [128, 1024], BF16) for i in range(2)]
        ptb = [Buf(ex=True) for _ in range(2)]
        pf = [ps(top, "pf%d" % i, [128, 512], F32) for i in range(6)]
        pfb = [Buf(ex=True) for _ in range(6)]
        rot = {"pt": 0, "pw": 0}

        def next_pt():
            i = rot["pt"]
            rot["pt"] = (i + 1) % 2
            return pt[i], ptb[i]

        def next_pw(n=4):
            i = rot["pw"] % n
            rot["pw"] += 1
            return pf[i], pfb[i]

        ident_bf = sb(top, "ident_bf", [128, 128], BF16)
        ident_f = sb(top, "ident_f", [128, 128], F32)
        causal = sb(top, "causal", [128, 128], F32)
        negi4 = sb(top, "negi4", [128, 512], BF16)
        onesblk = sb(top, "onesblk", [128, 128], BF16)
        zeros_bf = sb(top, "zeros_bf", [128, 260], BF16)
        bglu_b = sb(top, "bglu_b", [128, 512], F32)
        gns_b = sb(top, "gns_b", [128, 512], F32)
        gna_b = sb(top, "gna_b", [128, 512], F32)
        pow2 = sb(top, "pow2", [128, NITER + 2], F32)
        G2 = sb(top, "G2", [128, 1], F32)
        negM = sb(top, "negM", [128, 1], F32)
        taufix = sb(top, "taufix", [128, 1], F32)
        siluT = sb(top, "siluT", [128, 8, NSEQ, 128], BF16)
        cst = Buf()
        for t_, d_ in ((ident_bf, ident_d), (ident_f, ident_d), (causal, causal_d), (negi4, negi4_d),
                       (onesblk, onesblk_d), (bglu_b, bglu_d), (gns_b, gns_d), (gna_b, gna_d), (pow2, pow2_d)):
            S.dma("pool", t_[:], d_, writes=[cst])
        S.op("dve", lambda e: e.memset(zeros_bf[:], 0.0), writes=[cst])
        S.op("dve", lambda e: e.memset(taufix[:], 1.0e29), writes=[cst])

        with ExitStack() as st0:
            gqb = sb(st0, "gqb", [128, 64], F32)
            gkb = sb(st0, "gkb", [128, 64], F32)
            gq2 = sb(st0, "gq2", [128, 1], F32)
            gk2 = sb(st0, "gk2", [128, 1], F32)
            cTf = sb(st0, "cTf", [128, 8 * NSEQ * 128], F32)
            tb = Buf()
            S.dma("sp", gqb[:], gqb_d, writes=[tb])
            S.dma("sp", gkb[:], gkb_d, writes=[tb])
            S.dma("sp", gq2[:], gq2_d, writes=[tb])
            S.dma("sp", gk2[:], gk2_d, writes=[tb])
            S.dma("sp", cTf[:], cT_d.rearrange("p a b c -> p (a b c)"), writes=[tb])
            S.op("dve", lambda e: e.scalar_tensor_tensor(out=G2[:], in0=gq2[:], scalar=0.125, in1=gk2[:],
                                                          op0=ALU.mult, op1=ALU.mult), reads=[tb], writes=[cst])
            S.op("dve", lambda e: e.scalar_tensor_tensor(out=gqb[:], in0=gqb[:], scalar=0.125, in1=gkb[:],
                                                          op0=ALU.mult, op1=ALU.mult), reads=[tb], writes=[tb])
            S.op("dve", lambda e: e.tensor_reduce(out=negM[:], in_=gqb[:], axis=AX.X, op=ALU.max,
                                                  apply_absolute_value=True), reads=[tb], writes=[cst])
            S.op("dve", lambda e: e.tensor_scalar(out=negM[:], in0=negM[:], scalar1=-64.0, scalar2=None,
                                                  op0=ALU.mult), reads=[cst], writes=[cst])
            S.op("act", lambda e: e.activation(out=siluT[:].rearrange("p a b c -> p (a b c)"), in_=cTf[:],
                                               func=AF.Silu), reads=[tb], writes=[cst])
            S.barrier()

        chk('consts')
        def adaln(tabs, tabs_b, seqs, first_j, ng_d, st):
            wst = [sb(st, "wst%d" % i, [128, 8, 512], BF16) for i in range(2)]
            wstb = [Buf(), Buf()]
            bst = [sb(st, "bst%d" % i, [128, 512], F32) for i in range(2)]
            gst = [sb(st, "gst%d" % i, [128, 512], F32) for i in range(2)]
            tmp = [sb(st, "adat%d" % i, [128, 512], F32) for i in range(2)]
            tmpb = [Buf(), Buf()]
            it = 0
            for jj in range(3):
                for hc in range(2):
                    c0 = (first_j + jj) * D + hc * 512
                    b = it % 2
                    it += 1
                    S.dma("pool", wst[b][:], wada_d[:, :, c0:c0 + 512], writes=[wstb[b]])
                    S.dma("sp", bst[b][:], bada_d[:, c0:c0 + 512], writes=[wstb[b]])
                    if jj == 1:
                        S.dma("sp", gst[b][:], ng_d[:, hc * 512:(hc + 1) * 512], writes=[wstb[b]])
                    for n, s in enumerate(seqs):
                        p, pb = next_pw()
                        for kt in range(8):
                            S.op("pe", lambda e, p=p, b=b, kt=kt, s=s: e.matmul(
                                p[:], siluT[:, kt, s, :], wst[b][:, kt, :], start=(kt == 0), stop=(kt == 7)),
                                reads=[cst, wstb[b]], writes=[pb])
                        dst = tabs[n][:, jj, hc * 512:(hc + 1) * 512]
                        if jj == 1:
                            tb_ = tmpb[n]
                            S.op("dve", lambda e, p=p, b=b, n=n: e.tensor_tensor(
                                out=tmp[n][:], in0=p[:], in1=bst[b][:], op=ALU.add),
                                reads=[pb, wstb[b]], writes=[tb_])
                            S.op("dve", lambda e, b=b, n=n, dst=dst: e.scalar_tensor_tensor(
                                out=dst, in0=tmp[n][:], scalar=1.0, in1=gst[b][:], op0=ALU.add, op1=ALU.mult),
                                reads=[tb_, wstb[b]], writes=[tabs_b[n]])
                        else:
                            S.op("dve", lambda e, p=p, b=b, dst=dst: e.tensor_tensor(
                                out=dst, in0=p[:], in1=bst[b][:], op=ALU.add),
                                reads=[pb, wstb[b]], writes=[tabs_b[n]])

        with ExitStack() as stA:
            tabsA1 = sb(stA, "tabsA", [128, 3, D], F32)
            tabsA = [tabsA1, tabsA1]
            tabsA_b1 = Buf()
            tabsA_b = [tabsA_b1, tabsA_b1]

            ssmA_d = nc.dram_tensor("ssmA_scr", [128, 32, 128], BF16, kind="Internal").ap()
            ssmB_d = nc.dram_tensor("ssmB_scr", [128, 32, 128], BF16, kind="Internal").ap()
            ssmC_d = nc.dram_tensor("ssmC_scr", [128, 32, 2, 128], BF16, kind="Internal").ap()
            ssmd_b = Buf()
            ak = sb(stA, "ak", [128, 16, 8], F32)
            ck = sb(stA, "ck", [128, 16, 8], F32)
            nck = sb(stA, "nck", [128, 16, 8], F32)
            ssm_b = Buf()
            with ExitStack() as st1:
                A_sb = sb(st1, "A_sb", [128, 32, 128], BF16)
                Bm_sb = sb(st1, "Bm_sb", [128, 32, 128], BF16)
                Cmz = sb(st1, "Cmz", [128, 32, 2, 128], BF16)

                def t3(name, n):
                    return sb(st1, name, [128, 16, n], F32)
                lre = t3("lre", 1); lim = t3("lim", 1); ldt = t3("ldt", 1)
                bre = t3("bre", 16); bim = t3("bim", 16); cre = t3("cre", 16); cim = t3("cim", 16)
                mtab = t3("mtab", NE)
                bmask = sb(st1, "bmask", [128, 128], F32)
                dmask = sb(st1, "dmask", [128, 128], F32)
                zmask = sb(st1, "zmask", [128, 2], F32)
                dsk = sb(st1, "dsk", [128, 32, 128], F32)
                ib = Buf()
                for t_, d_ in ((lre, lre_d), (lim, lim_d), (ldt, ldt_d)):
                    S.dma("sp", t_[:, :, 0], d_, writes=[ib])
                for t_, d_ in ((bre, bre_d), (bim, bim_d), (cre, cre_d), (cim, cim_d), (mtab, mtab_d),
                               (bmask, bmask_d), (dmask, dmask_d), (dsk, dsk_d), (zmask, zmask_d)):
                    S.dma("sp", t_[:], d_, writes=[ib])
                chk('ssm_a')
                dt_ = t3("dt_", 1); aa = t3("aa", 1); th = t3("th", 1)
                ang = t3("ang", NE); lmag = t3("lmag", NE); mag = t3("mag", NE)
                tq = t3("tq", NE); tqi = sb(st1, "tqi", [128, 16, NE], I32); tqf = t3("tqf", NE)
                wr = t3("wr", NE); sn = t3("sn", NE); cs = t3("cs", NE)
                pr_ = t3("pr_", NE); pi_ = t3("pi_", NE)
                wb = Buf()

                def V(fn, reads=(), writes=(wb,)):
                    S.op("dve", fn, reads=[ib, wb] + list(reads), writes=list(writes))

                def ACT(fn, reads=(), writes=(wb,)):
                    S.op("act", fn, reads=[ib, wb] + list(reads), writes=list(writes))

                ACT(lambda e: e.activation(out=dt_[:], in_=ldt[:], func=AF.Exp))
                V(lambda e: e.tensor_tensor(out=aa[:], in0=lre[:], in1=dt_[:], op=ALU.mult))
                V(lambda e: e.tensor_tensor(out=th[:], in0=lim[:], in1=dt_[:], op=ALU.mult))
                V(lambda e: e.tensor_tensor(out=lmag[:], in0=mtab[:], in1=aa[:].to_broadcast([128, 16, NE]), op=ALU.mult))
                V(lambda e: e.tensor_tensor(out=ang[:], in0=mtab[:], in1=th[:].to_broadcast([128, 16, NE]), op=ALU.mult))
                ACT(lambda e: e.activation(out=mag[:], in_=lmag[:], func=AF.Exp))
                V(lambda e: e.tensor_scalar(out=tq[:], in0=ang[:], scalar1=1.0 / TWO_PI, scalar2=None, op0=ALU.mult))
                V(lambda e: e.tensor_copy(out=tqi[:], in_=tq[:]))
                V(lambda e: e.tensor_copy(out=tqf[:], in_=tqi[:]))
                V(lambda e: e.scalar_tensor_tensor(out=wr[:], in0=tqf[:], scalar=-TWO_PI, in1=ang[:],
                                                   op0=ALU.mult, op1=ALU.add))
                wt_ = t3("wt_", NE)
                for t_, shift in ((sn, 0.0), (cs, math.pi / 2)):
                    V(lambda e, t_=t_, shift=shift: e.tensor_scalar(out=t_[:], in0=wr[:], scalar1=shift, scalar2=None, op0=ALU.add))
                    V(lambda e, t_=t_: e.tensor_scalar(out=wt_[:], in0=t_[:], scalar1=math.pi, scalar2=-TWO_PI,
                                                       op0=ALU.is_gt, op1=ALU.mult))
                    V(lambda e, t_=t_: e.tensor_scalar(out=tq[:], in0=t_[:], scalar1=-math.pi, scalar2=TWO_PI,
                                                       op0=ALU.is_lt, op1=ALU.mult))
                    V(lambda e, t_=t_: e.tensor_tensor(out=t_[:], in0=t_[:], in1=wt_[:], op=ALU.add))
                    V(lambda e, t_=t_: e.tensor_tensor(out=t_[:], in0=t_[:], in1=tq[:], op=ALU.add))
                for t_ in (sn, cs):
                    V(lambda e, t_=t_: e.tensor_scalar(out=t_[:], in0=t_[:], scalar1=3.14159, scalar2=-3.14159,
                                                       op0=ALU.min, op1=ALU.max))
                ACT(lambda e: e.activation(out=sn[:], in_=sn[:], func=AF.Sin))
                ACT(lambda e: e.activation(out=cs[:], in_=cs[:], func=AF.Sin))
                V(lambda e: e.tensor_tensor(out=pr_[:], in0=mag[:], in1=cs[:], op=ALU.mult))
                V(lambda e: e.tensor_tensor(out=pi_[:], in0=mag[:], in1=sn[:], op=ALU.mult))
                chk('ssm_b')
                i1 = EIDX[1]
                nr = t3("nr", 1); den = t3("den", 1); t1_ = t3("t1_", 1); gr = t3("gr", 1); gi = t3("gi", 1)
                V(lambda e: e.tensor_scalar(out=nr[:], in0=pr_[:, :, i1:i1 + 1], scalar1=-1.0, scalar2=None, op0=ALU.add))
                V(lambda e: e.tensor_tensor(out=den[:], in0=lre[:], in1=lre[:], op=ALU.mult))
                V(lambda e: e.tensor_tensor(out=t1_[:], in0=lim[:], in1=lim[:], op=ALU.mult))
                V(lambda e: e.tensor_tensor(out=den[:], in0=den[:], in1=t1_[:], op=ALU.add))
                V(lambda e: e.reciprocal(out=den[:], in_=den[:]))
                V(lambda e: e.tensor_tensor(out=gr[:], in0=nr[:], in1=lre[:], op=ALU.mult))
                V(lambda e: e.tensor_tensor(out=t1_[:], in0=pi_[:, :, i1:i1 + 1], in1=lim[:], op=ALU.mult))
                V(lambda e: e.tensor_tensor(out=gr[:], in0=gr[:], in1=t1_[:], op=ALU.add))
                V(lambda e: e.tensor_tensor(out=gr[:], in0=gr[:], in1=den[:], op=ALU.mult))
                V(lambda e: e.tensor_tensor(out=gi[:], in0=pi_[:, :, i1:i1 + 1], in1=lre[:], op=ALU.mult))
                V(lambda e: e.tensor_tensor(out=t1_[:], in0=nr[:], in1=lim[:], op=ALU.mult))
                V(lambda e: e.tensor_tensor(out=gi[:], in0=gi[:], in1=t1_[:], op=ALU.subtract))
                V(lambda e: e.tensor_tensor(out=gi[:], in0=gi[:], in1=den[:], op=ALU.mult))
                for k in range(8):
                    ii = EIDX[8 * (2 ** k)]
                    V(lambda e, k=k, ii=ii: e.tensor_copy(out=ak[:, :, k:k + 1], in_=pr_[:, :, ii:ii + 1]), writes=[wb, ssm_b])
                    V(lambda e, k=k, ii=ii: e.tensor_copy(out=ck[:, :, k:k + 1], in_=pi_[:, :, ii:ii + 1]), writes=[wb, ssm_b])
                    V(lambda e, k=k, ii=ii: e.tensor_scalar(out=nck[:, :, k:k + 1], in0=pi_[:, :, ii:ii + 1], scalar1=-1.0,
                                                            scalar2=None, op0=ALU.mult), writes=[wb, ssm_b])
                PBr = t3("PBr", 8); PBi = t3("PBi", 8); tt8 = t3("tt8", 8)
                e7 = EIDX[0]
                sl07 = slice(e7, e7 + 8)
                V(lambda e: e.tensor_tensor(out=PBr[:], in0=pr_[:, :, sl07], in1=gr[:].to_broadcast([128, 16, 8]), op=ALU.mult))
                V(lambda e: e.tensor_tensor(out=tt8[:], in0=pi_[:, :, sl07], in1=gi[:].to_broadcast([128, 16, 8]), op=ALU.mult))
                V(lambda e: e.tensor_tensor(out=PBr[:], in0=PBr[:], in1=tt8[:], op=ALU.subtract))
                V(lambda e: e.tensor_tensor(out=PBi[:], in0=pr_[:, :, sl07], in1=gi[:].to_broadcast([128, 16, 8]), op=ALU.mult))
                V(lambda e: e.tensor_tensor(out=tt8[:], in0=pi_[:, :, sl07], in1=gr[:].to_broadcast([128, 16, 8]), op=ALU.mult))
                V(lambda e: e.tensor_tensor(out=PBi[:], in0=PBi[:], in1=tt8[:], op=ALU.add))
                BmTr = sb(st1, "BmTr", [128, 16, 8, 16], F32)
                BmTi = sb(st1, "BmTi", [128, 16, 8, 16], F32)
                t816 = sb(st1, "t816", [128, 16, 8, 16], F32)
                for i in range(8):
                    m = 7 - i
                    def bc(t_, m=m):
                        return t_[:, :, m:m + 1].to_broadcast([128, 16, 16])
                    V(lambda e, i=i, bc=bc: e.tensor_tensor(out=BmTr[:, :, i, :], in0=bre[:], in1=bc(PBr), op=ALU.mult))
                    V(lambda e, i=i, bc=bc: e.tensor_tensor(out=t816[:, :, i, :], in0=bim[:], in1=bc(PBi), op=ALU.mult))
                    V(lambda e, i=i, bc=bc: e.tensor_tensor(out=BmTi[:, :, i, :], in0=bim[:], in1=bc(PBr), op=ALU.mult))
                V(lambda e: e.tensor_tensor(out=BmTr[:], in0=BmTr[:], in1=t816[:], op=ALU.subtract))
                for i in range(8):
                    m = 7 - i
                    V(lambda e, i=i, m=m: e.tensor_tensor(out=t816[:, :, i, :], in0=bre[:],
                                                          in1=PBi[:, :, m:m + 1].to_broadcast([128, 16, 16]), op=ALU.mult))
                V(lambda e: e.tensor_tensor(out=BmTi[:], in0=BmTi[:], in1=t816[:], op=ALU.add))
                Wcr = sb(st1, "Wcr", [128, 16, 8, 16], F32)
                Wci = sb(st1, "Wci", [128, 16, 8, 16], F32)
                Cmr = sb(st1, "Cmr", [128, 16, 8, 16], F32)
                Cmi = sb(st1, "Cmi", [128, 16, 8, 16], F32)
                for (dr, di, off) in ((Wcr, Wci, -7), (Cmr, Cmi, 1)):
                    for j in range(8):
                        ii = EIDX[j + off]
                        def bc2(t_, ii=ii):
                            return t_[:, :, ii:ii + 1].to_broadcast([128, 16, 16])
                        V(lambda e, j=j, bc2=bc2, dr=dr: e.tensor_tensor(out=dr[:, :, j, :], in0=cre[:], in1=bc2(pr_), op=ALU.mult))
                        V(lambda e, j=j, bc2=bc2: e.tensor_tensor(out=t816[:, :, j, :], in0=cim[:], in1=bc2(pi_), op=ALU.mult))
                        V(lambda e, j=j, bc2=bc2, di=di: e.tensor_tensor(out=di[:, :, j, :], in0=cre[:], in1=bc2(pi_), op=ALU.mult))
                    V(lambda e, dr=dr: e.tensor_tensor(out=dr[:], in0=dr[:], in1=t816[:], op=ALU.subtract))
                    for j in range(8):
                        ii = EIDX[j + off]
                        V(lambda e, j=j, ii=ii: e.tensor_tensor(out=t816[:, :, j, :], in0=cim[:],
                                                                in1=pr_[:, :, ii:ii + 1].to_broadcast([128, 16, 16]), op=ALU.mult))
                    V(lambda e, di=di: e.tensor_tensor(out=di[:], in0=di[:], in1=t816[:], op=ALU.add))
                    V(lambda e, di=di: e.tensor_scalar(out=di[:], in0=di[:], scalar1=-1.0, scalar2=None, op0=ALU.mult))
                chk('ssm_c')
                for pr in range(16):
                    for gp in range(2):
                        g = 2 * pr + gp
                        for ri, src in ((0, Cmr), (1, Cmi)):
                            V(lambda e, g=g, ri=ri, src=src, pr=pr, gp=gp: e.tensor_scalar(
                                out=Cmz[:, g, ri, :], in0=src[:, pr, :, :].rearrange("p a b -> p (a b)"),
                                scalar1=zmask[:, gp:gp + 1], scalar2=None, op0=ALU.mult), writes=[wb, ssm_b])
                chk('ssm_d')
                Bz = [sb(st1, "Bz%d" % i, [128, 2, 128], BF16) for i in range(2)]
                Wz = [sb(st1, "Wz%d" % i, [128, 2, 128], BF16) for i in range(2)]
                Bzb = [Buf(), Buf()]
                At = [sb(st1, "At%d" % i, [128, 128], F32) for i in range(2)]
                Atb = [Buf(), Buf()]
                for pr in range(16):
                    for gp in range(2):
                        g = 2 * pr + gp
                        b = g % 2
                        for ri, src in ((0, BmTr), (1, BmTi)):
                            V(lambda e, b=b, ri=ri, src=src, pr=pr, gp=gp: e.tensor_scalar(
                                out=Bz[b][:, ri, :], in0=src[:, pr, :, :].rearrange("p a b -> p (a b)"),
                                scalar1=zmask[:, gp:gp + 1], scalar2=None, op0=ALU.mult), writes=[wb, Bzb[b]])
                        for ri, src in ((0, Wcr), (1, Wci)):
                            V(lambda e, b=b, ri=ri, src=src, pr=pr, gp=gp: e.tensor_scalar(
                                out=Wz[b][:, ri, :], in0=src[:, pr, :, :].rearrange("p a b -> p (a b)"),
                                scalar1=zmask[:, gp:gp + 1], scalar2=None, op0=ALU.mult), writes=[wb, Bzb[b]])
                        ptt, ptb_ = next_pt()
                        for ri in range(2):
                            S.op("pe", lambda e, ptt=ptt, b=b, ri=ri: e.transpose(
                                out=ptt[:, ri * 128:(ri + 1) * 128], in_=Bz[b][:, ri, :], identity=ident_bf[:]),
                                reads=[Bzb[b], cst], writes=[ptb_])
                        for ri in range(2):
                            S.op("act", lambda e, ptt=ptt, g=g, ri=ri, gp=gp: e.copy(
                                out=Bm_sb[:, g, ri * 64:(ri + 1) * 64],
                                in_=ptt[:, ri * 128 + gp * 64: ri * 128 + gp * 64 + 64]),
                                reads=[ptb_], writes=[ssm_b])
                        p, pb = next_pw()
                        for ri in range(2):
                            S.op("pe", lambda e, p=p, b=b, ri=ri: e.matmul(
                                p[:, 0:128], Bz[b][:, ri, :], Wz[b][:, ri, :], start=(ri == 0), stop=(ri == 1)),
                                reads=[Bzb[b]], writes=[pb])
                        S.op("dve", lambda e, p=p, b=b: e.tensor_tensor(out=At[b][:], in0=p[:, 0:128], in1=bmask[:], op=ALU.mult),
                             reads=[pb, ib], writes=[Atb[b]])
                        S.op("dve", lambda e, b=b, g=g: e.tensor_tensor(out=dsk[:, g, :], in0=dsk[:, g, :], in1=dmask[:], op=ALU.mult),
                             reads=[ib], writes=[ib])
                        S.op("dve", lambda e, b=b, g=g: e.tensor_tensor(out=A_sb[:, g, :], in0=At[b][:], in1=dsk[:, g, :], op=ALU.add),
                             reads=[Atb[b], ib], writes=[ssm_b])
                chk('ssm_e')
                S.dma("sp", ssmA_d, A_sb[:], reads=[ssm_b], writes=[ssmd_b])
                S.dma("sp", ssmB_d, Bm_sb[:], reads=[ssm_b], writes=[ssmd_b])
                S.dma("sp", ssmC_d, Cmz[:], reads=[ssm_b], writes=[ssmd_b])
                S.barrier()

            chk('ssmsetup')
            for s in range(NSEQ):
                r0 = s * SEQ
                with ExitStack() as stt:
                    adaln([tabsA1], [tabsA_b1], [s], 0, n1g_d, stt)
                    S.barrier()
                chk('adaln')
                with ExitStack() as stS:
                    qT = sb(stS, "qT", [128, 4, SEQ], BF16)
                    kTz = sb(stS, "kTz", [128, 2, 2, SEQ], BF16)
                    qiT = sb(stS, "qiT", [128, 4, SEQ], BF16)
                    kiTz = sb(stS, "kiTz", [128, 2, SEQ], BF16)
                    Vp = sb(stS, "Vp", [128, 16, 2, 65], BF16)
                    wi = sb(stS, "wi", [128, 16, 8], F32)
                    mixT_s = sb(stS, "mixT_s", [128, 4, SEQ], BF16)
                    qT_b, kT_b, qiT_b, kiT_b, Vp_b, wi_b, mixT_sb = (Buf() for _ in range(7))
                    S.op("pool", lambda e: e.memset(kTz[:].rearrange("p a b c -> p (a b c)"), 0.0), writes=[kT_b])
                    S.op("pool", lambda e: e.memset(kiTz[:].rearrange("p a c -> p (a c)"), 0.0), writes=[kiT_b])
                    S.op("pool", lambda e: e.memset(Vp[:].rearrange("p a b c -> p (a b c)"), 1.0), writes=[Vp_b])

                    with ExitStack() as st12:
                        U8 = sb(st12, "U8", [128, 2, 32, 8, 16], BF16)
                        U8_b = Buf()
                        with ExitStack() as st1:
                            w_in = sb(st1, "w_in", [128, 8, DIN], BF16)
                            w_in_b = Buf()
                            for kt in range(8):
                                S.dma("pool", w_in[:, kt, :], win_d[:, kt, :], writes=[w_in_b])
                            hT = sb(st1, "hT", [128, 8, SEQ], BF16)
                            hT_b = Buf()
                            st1a = ExitStack()
                            xt = [sb(st1a, "xt%d" % i, [128, D], F32) for i in range(2)]
                            xt_b = [Buf(), Buf()]
                            junk = sb(st1a, "junk", [128, D], BF16)
                            junk_b = Buf()
                            t1 = sb(st1a, "t1", [128, D], F32)
                            hb = [sb(st1a, "hb%d" % i, [128, D], BF16) for i in range(2)]
                            hb_b = [Buf(), Buf()]
                            st_ = [sb(st1a, "st%d" % i, [128, 4], F32) for i in range(2)]
                            st_b = [Buf(), Buf()]
                            t1_b = Buf()
                            for t in range(16):
                                b = t % 2
                                S.dma("sp", xt[b][:], x_d[r0 + t * 128: r0 + (t + 1) * 128, :], writes=[xt_b[b]])
                                S.op("act", lambda e, b=b: e.activation(out=junk[:], in_=xt[b][:], func=AF.Square,
                                                                        accum_out=st_[b][:, 0:1]),
                                     reads=[xt_b[b]], writes=[junk_b, st_b[b]])
                                S.op("act", lambda e, b=b: e.activation(out=st_[b][:, 1:2], in_=st_[b][:, 0:1], func=AF.Sqrt,
                                                                        bias=EPS, scale=1.0 / D),
                                     reads=[st_b[b]], writes=[st_b[b]])
                                S.op("dve", lambda e, b=b: e.reciprocal(out=st_[b][:, 2:3], in_=st_[b][:, 1:2]),
                                     reads=[st_b[b]], writes=[st_b[b]])
                                S.op("dve", lambda e, b=b: e.scalar_tensor_tensor(
                                    out=t1[:], in0=xt[b][:], scalar=st_[b][:, 2:3], in1=tabsA[s][:, 1, :],
                                    op0=ALU.mult, op1=ALU.mult), reads=[xt_b[b], st_b[b], tabsA_b[s]], writes=[t1_b])
                                S.op("dve", lambda e, b=b: e.tensor_tensor(out=hb[b][:], in0=t1[:], in1=tabsA[s][:, 0, :], op=ALU.add),
                                     reads=[t1_b, tabsA_b[s]], writes=[hb_b[b]])
                                ptt, ptb_ = next_pt()
                                for kt in range(8):
                                    S.op("pe", lambda e, ptt=ptt, b=b, kt=kt: e.transpose(
                                        out=ptt[:, kt * 128:(kt + 1) * 128], in_=hb[b][:, kt * 128:(kt + 1) * 128],
                                        identity=ident_bf[:]), reads=[hb_b[b], cst], writes=[ptb_])
                                S.op("act", lambda e, ptt=ptt, t=t: e.copy(
                                    out=hT[:, :, t * 128:(t + 1) * 128], in_=ptt[:].rearrange("p (a b) -> p a b", b=128)),
                                    reads=[ptb_], writes=[hT_b])
                            S.barrier()
                            st1a.close()
                            sq = [sb(st1, "sq%d" % i, [128, 512], BF16) for i in range(2)]
                            sq_b = [Buf(), Buf()]
                            sd = [sb(st1, "sd%d" % i, [128, 512], F32) for i in range(2)]
                            sd_b = [Buf(), Buf()]
                            groups = []
                            for j in range(4):
                                groups.append(("q", j, [(512 + j * 128, 128, 0)]))
                            groups.append(("kA", 0, [(1024, 128, 0)]))
                            groups.append(("kB", 0, [(1088, 64, 0), (1024, 64, 64)]))
                            for j in range(4):
                                groups.append(("qi", j, [(1280 + j * 128, 128, 0)]))
                            groups.append(("ki", 0, [(1792, 64, 0), (1792, 64, 64)]))
                            it = 0
                            for kind, j, parts in groups:
                                for c in range(4):
                                    cs_ = slice(c * 512, (c + 1) * 512)
                                    p, pb = next_pw()
                                    for (c0, m, po) in parts:
                                        for kt in range(8):
                                            S.op("pe", lambda e, p=p, c0=c0, m=m, po=po, kt=kt, cs_=cs_: e.matmul(
                                                p[po:po + m, :], w_in[:, kt, c0:c0 + m], hT[:, kt, cs_],
                                                start=(kt == 0), stop=(kt == 7)),
                                                reads=[w_in_b, hT_b], writes=[pb])
                                    if kind == "qi":
                                        S.op("act", lambda e, p=p, j=j, cs_=cs_: e.copy(out=qiT[:, j, cs_], in_=p[:]),
                                             reads=[pb], writes=[qiT_b])
                                    elif kind == "ki":
                                        for half in range(2):
                                            rs = slice(half * 64, half * 64 + 64)
                                            S.op("act", lambda e, p=p, half=half, rs=rs, cs_=cs_: e.copy(
                                                out=kiTz[rs, half, cs_], in_=p[rs, :]), reads=[pb], writes=[kiT_b])
                                    else:
                                        b = it % 2
                                        it += 1
                                        S.op("act", lambda e, p=p, b=b: e.activation(out=sq[b][:], in_=p[:], func=AF.Square),
                                             reads=[pb], writes=[sq_b[b]])
                                        p2, p2b = next_pw()
                                        S.op("pe", lambda e, p2=p2, b=b: e.matmul(p2[:], onesblk[:], sq[b][:], start=True, stop=True),
                                             reads=[sq_b[b], cst], writes=[p2b])
                                        S.op("act", lambda e, p2=p2, b=b: e.activation(out=sd[b][:], in_=p2[:], func=AF.Sqrt,
                                                                                      bias=EPS, scale=1.0 / 64),
                                             reads=[p2b], writes=[sd_b[b]])
                                        S.op("dve", lambda e, b=b: e.reciprocal(out=sd[b][:], in_=sd[b][:]),
                                             reads=[sd_b[b]], writes=[sd_b[b]])
                                        if kind == "q":
                                            S.op("dve", lambda e, p=p, b=b, j=j, cs_=cs_: e.scalar_tensor_tensor(
                                                out=qT[:, j, cs_], in0=p[:], scalar=G2[:, 0:1], in1=sd[b][:],
                                                op0=ALU.mult, op1=ALU.mult), reads=[pb, sd_b[b], cst], writes=[qT_b])
                                        else:
                                            kvs = (0, 1) if kind == "kA" else (1, 0)
                                            for half in range(2):
                                                rs = slice(half * 64, half * 64 + 64)
                                                kv = kvs[half]
                                                S.op("dve", lambda e, p=p, b=b, rs=rs, kv=kv, half=half, cs_=cs_: e.tensor_tensor(
                                                    out=kTz[rs, kv, half, cs_], in0=p[rs, :], in1=sd[b][rs, :], op=ALU.mult),
                                                    reads=[pb, sd_b[b]], writes=[kT_b])
                            for t in range(16):
                                ts_ = slice(t * 128, (t + 1) * 128)
                                p, pb = next_pw()
                                for kt in range(8):
                                    S.op("pe", lambda e, p=p, kt=kt, ts_=ts_: e.matmul(
                                        p[:, 0:128], hT[:, kt, ts_], w_in[:, kt, 1152:1280], start=(kt == 0), stop=(kt == 7)),
                                        reads=[w_in_b, hT_b], writes=[pb])
                                for kt in range(8):
                                    S.op("pe", lambda e, p=p, kt=kt, ts_=ts_: e.matmul(
                                        p[:, 128:136], hT[:, kt, ts_], w_in[:, kt, 1856:1864], start=(kt == 0), stop=(kt == 7)),
                                        reads=[w_in_b, hT_b], writes=[pb])
                                S.op("act", lambda e, p=p, t=t: e.copy(
                                    out=Vp[:, t, :, 0:64], in_=p[:, 0:128].rearrange("p (a b) -> p a b", b=64)),
                                    reads=[pb], writes=[Vp_b])
                                S.op("act", lambda e, p=p, t=t: e.mul(out=wi[:, t, :], in_=p[:, 128:136], mul=IDX_SCALE),
                                     reads=[pb], writes=[wi_b])
                            for sp in range(2):
                                for i in range(8):
                                    p, pb = next_pw()
                                    for kt in range(8):
                                        lhs = hT[:, kt, sp * 1024:(sp + 1) * 1024].rearrange("p (b i) -> p i b", i=8)[:, i, :]
                                        S.op("pe", lambda e, p=p, lhs=lhs, kt=kt: e.matmul(
                                            p[:], lhs, w_in[:, kt, 0:512], start=(kt == 0), stop=(kt == 7)),
                                            reads=[w_in_b, hT_b], writes=[pb])
                                    S.op("act", lambda e, p=p, sp=sp, i=i: e.copy(
                                        out=U8[:, sp, :, i, :], in_=p[:].rearrange("p (g c) -> p g c", c=16)),
                                         reads=[pb], writes=[U8_b])
                            S.barrier()

                        chk('s1')
                        with ExitStack() as st2:
                            w_glu = sb(st2, "w_glu", [128, 4, 512], BF16)
                            w_glu_b = Buf()
                            S.dma("pool", w_glu[:], wglu_d, writes=[w_glu_b])
                            Ytok = sb(st2, "Ytok", [128, 2, 8, 512], F32)
                            Ytok_b = Buf()
                            U8T = [sb(st2, "U8T%d" % i, [128, 2, 256], BF16) for i in range(2)]
                            U8T_b = [Buf(), Buf()]
                            XA = sb(st2, "XA", [128, 2, 384], F32)
                            XB = sb(st2, "XB", [128, 2, 384], F32)
                            XA_b, XB_b = Buf(), Buf()
                            TM = sb(st2, "TM", [128, 2, 256], F32)
                            TM_b = Buf()
                            Xst = [sb(st2, "Xst%d" % i, [128, 2, 258], BF16) for i in range(2)]
                            Xst_b = [Buf(), Buf()]
                            Ysb = [sb(st2, "Ysb%d" % i, [128, 256], F32) for i in range(2)]
                            Ysb_b = [Buf(), Buf()]
                            S.op("pool", lambda e: e.memset(XA[:].rearrange("p a b -> p (a b)"), 0.0), writes=[XA_b])
                            S.op("pool", lambda e: e.memset(XB[:].rearrange("p a b -> p (a b)"), 0.0), writes=[XB_b])
                            for i in range(2):
                                S.op("pool", lambda e, i=i: e.memset(Xst[i][:].rearrange("p a b -> p (a b)"), 0.0), writes=[Xst_b[i]])
                            PAD = 128
                            A2 = [sb(st2, "A2_%d" % i, [128, 2, 128], BF16) for i in range(2)]
                            B2 = [sb(st2, "B2_%d" % i, [128, 2, 128], BF16) for i in range(2)]
                            C2 = [sb(st2, "C2_%d" % i, [128, 2, 2, 128], BF16) for i in range(2)]
                            M2_b = [Buf(), Buf()]
                            for pr in range(16):
                                ub = pr % 2
                                S.dma("sp", A2[ub][:], ssmA_d[:, 2 * pr:2 * pr + 2, :], reads=[ssmd_b], writes=[M2_b[ub]])
                                S.dma("sp", B2[ub][:], ssmB_d[:, 2 * pr:2 * pr + 2, :], reads=[ssmd_b], writes=[M2_b[ub]])
                                S.dma("sp", C2[ub][:], ssmC_d[:, 2 * pr:2 * pr + 2, :, :], reads=[ssmd_b], writes=[M2_b[ub]])
                                ptt, ptb_ = next_pt()
                                for gp in range(2):
                                    g = 2 * pr + gp
                                    for sp in range(2):
                                        S.op("pe", lambda e, ptt=ptt, gp=gp, sp=sp, g=g: e.transpose(
                                            out=ptt[:, (gp * 2 + sp) * 128:(gp * 2 + sp + 1) * 128],
                                            in_=U8[:, sp, g, :, :].rearrange("p a b -> p (a b)"), identity=ident_bf[:]),
                                            reads=[U8_b, cst], writes=[ptb_])
                                S.op("act", lambda e, ptt=ptt, ub=ub: e.copy(
                                    out=U8T[ub][:].rearrange("p a b -> p (a b)"), in_=ptt[:, 0:512]),
                                    reads=[ptb_], writes=[U8T_b[ub]])
                                p, pb = next_pw()
                                for gp in range(2):
                                    g = 2 * pr + gp
                                    for ri in range(2):
                                        S.op("pe", lambda e, p=p, gp=gp, g=g, ri=ri, ub=ub: e.matmul(
                                            p[gp * 64:(gp + 1) * 64, ri * 256:(ri + 1) * 256],
                                            B2[ub][:, gp, ri * 64:(ri + 1) * 64], U8T[ub][:, gp, :], start=True, stop=True),
                                            reads=[M2_b[ub], U8T_b[ub]], writes=[pb])
                                S.op("act", lambda e, p=p: e.copy(out=XA[:, :, PAD:PAD + 256],
                                                                  in_=p[:].rearrange("p (a b) -> p a b", b=256)),
                                     reads=[pb], writes=[XA_b])
                                cur, curb, nxt, nxtb = XA, XA_b, XB, XB_b
                                xs = Xst[ub]
                                for k in range(8):
                                    sft = 2 ** k
                                    last = (k == 7)
                                    a_ = ak[:, pr, k:k + 1]
                                    c_ = ck[:, pr, k:k + 1]
                                    nc_ = nck[:, pr, k:k + 1]
                                    sh = slice(PAD - sft, PAD - sft + 256)
                                    ce = slice(PAD, PAD + 256)
                                    outr = xs[:, 0, 1:257] if last else nxt[:, 0, ce]
                                    outi = xs[:, 1, 1:257] if last else nxt[:, 1, ce]
                                    ob = Xst_b[ub] if last else nxtb
                                    S.op("dve", lambda e, cur=cur, a_=a_, sh=sh, ce=ce: e.scalar_tensor_tensor(
                                        out=TM[:, 0, :], in0=cur[:, 0, sh], scalar=a_, in1=cur[:, 0, ce], op0=ALU.mult, op1=ALU.add),
                                        reads=[curb, ssm_b], writes=[TM_b])
                                    S.op("dve", lambda e, cur=cur, nc_=nc_, sh=sh, outr=outr: e.scalar_tensor_tensor(
                                        out=outr, in0=cur[:, 1, sh], scalar=nc_, in1=TM[:, 0, :], op0=ALU.mult, op1=ALU.add),
                                        reads=[curb, TM_b, ssm_b], writes=[ob])
                                    S.op("dve", lambda e, cur=cur, a_=a_, sh=sh, ce=ce: e.scalar_tensor_tensor(
                                        out=TM[:, 1, :], in0=cur[:, 1, sh], scalar=a_, in1=cur[:, 1, ce], op0=ALU.mult, op1=ALU.add),
                                        reads=[curb, ssm_b], writes=[TM_b])
                                    S.op("dve", lambda e, cur=cur, c_=c_, sh=sh, outi=outi: e.scalar_tensor_tensor(
                                        out=outi, in0=cur[:, 0, sh], scalar=c_, in1=TM[:, 1, :], op0=ALU.mult, op1=ALU.add),
                                        reads=[curb, TM_b, ssm_b], writes=[ob])
                                    cur, curb, nxt, nxtb = nxt, nxtb, cur, curb
                                for gp in range(2):
                                    g = 2 * pr + gp
                                    yb = g % 2
                                    p, pb = next_pw()
                                    S.op("pe", lambda e, p=p, g=g, gp=gp, ub=ub: e.matmul(
                                        p[:, 0:256], A2[ub][:, gp, :], U8T[ub][:, gp, :], start=True, stop=False),
                                        reads=[M2_b[ub], U8T_b[ub]], writes=[pb])
                                    for ri in range(2):
                                        S.op("pe", lambda e, p=p, g=g, ri=ri, xs=xs: e.matmul(
                                            p[:, 0:256], C2[ub][:, gp, ri, :], xs[:, ri, 0:256], start=False, stop=(ri == 1)),
                                            reads=[M2_b[ub], Xst_b[ub]], writes=[pb])
                                    S.op("act", lambda e, p=p, yb=yb: e.copy(out=Ysb[yb][:], in_=p[:, 0:256]),
                                         reads=[pb], writes=[Ysb_b[yb]])
                                    p2, p2b = next_pw()
                                    for sp in range(2):
                                        S.op("pe", lambda e, p2=p2, sp=sp, yb=yb: e.transpose(
                                            out=p2[:, sp * 128:(sp + 1) * 128], in_=Ysb[yb][:, sp * 128:(sp + 1) * 128],
                                            identity=ident_f[:]), reads=[Ysb_b[yb], cst], writes=[p2b])
                                    for sp in range(2):
                                        S.op("act", lambda e, p2=p2, sp=sp, g=g: e.copy(
                                            out=Ytok[:, sp, :, g * 16:(g + 1) * 16],
                                            in_=p2[:, sp * 128:(sp + 1) * 128].rearrange("p (a b) -> p a b", b=16)),
                                            reads=[p2b], writes=[Ytok_b])
                            g1 = [sb(st2, "g1_%d" % i, [128, 512], F32) for i in range(2)]
                            g2_ = [sb(st2, "g2_%d" % i, [128, 512], F32) for i in range(2)]
                            zf = [sb(st2, "zf%d" % i, [128, 512], F32) for i in range(2)]
                            zb = [sb(st2, "zb%d" % i, [128, 512], BF16) for i in range(2)]
                            zT = [sb(st2, "zT%d" % i, [128, 4, 128], BF16) for i in range(2)]
                            sg = [sb(st2, "sg%d" % i, [128, 512], F32) for i in range(2)]
                            mb_ = [sb(st2, "mb%d" % i, [128, 512], BF16) for i in range(2)]
                            sst = [sb(st2, "sst%d" % i, [128, 4], F32) for i in range(2)]
                            gb = [[Buf() for _ in range(8)] for _ in range(2)]
                            KG = 2.0 * math.sqrt(2.0 / math.pi)
                            for sp in range(2):
                                for i in range(8):
                                    b = i % 2
                                    B = gb[b]
                                    y = Ytok[:, sp, i, :]
                                    S.op("act", lambda e, b=b, y=y: e.activation(out=g1[b][:], in_=y, func=AF.Square),
                                         reads=[Ytok_b], writes=[B[0]])
                                    S.op("dve", lambda e, b=b: e.tensor_scalar(out=g1[b][:], in0=g1[b][:], scalar1=0.044715,
                                                                               scalar2=1.0, op0=ALU.mult, op1=ALU.add),
                                         reads=[B[0]], writes=[B[0]])
                                    S.op("dve", lambda e, b=b, y=y: e.tensor_tensor(out=g2_[b][:], in0=g1[b][:], in1=y, op=ALU.mult),
                                         reads=[B[0], Ytok_b], writes=[B[1]])
                                    S.op("act", lambda e, b=b: e.activation(out=g2_[b][:], in_=g2_[b][:], func=AF.Sigmoid, scale=KG),
                                         reads=[B[1]], writes=[B[1]])
                                    S.op("dve", lambda e, b=b, y=y: e.tensor_tensor(out=zf[b][:], in0=g2_[b][:], in1=y, op=ALU.mult),
                                         reads=[B[1], Ytok_b], writes=[B[2]])
                                    S.op("act", lambda e, b=b: e.copy(out=zb[b][:], in_=zf[b][:]), reads=[B[2]], writes=[B[3]])
                                    ptt, ptb_ = next_pt()
                                    for ft in range(4):
                                        S.op("pe", lambda e, ptt=ptt, b=b, ft=ft: e.transpose(
                                            out=ptt[:, ft * 128:(ft + 1) * 128], in_=zb[b][:, ft * 128:(ft + 1) * 128],
                                            identity=ident_bf[:]), reads=[B[3], cst], writes=[ptb_])
                                    S.op("act", lambda e, ptt=ptt, b=b: e.copy(out=zT[b][:].rearrange("p a b -> p (a b)"),
                                                                               in_=ptt[:, 0:512]), reads=[ptb_], writes=[B[4]])
                                    p, pb = next_pw()
                                    for ft in range(4):
                                        S.op("pe", lambda e, p=p, b=b, ft=ft: e.matmul(
                                            p[:], zT[b][:, ft, :], w_glu[:, ft, :], start=(ft == 0), stop=(ft == 3)),
                                            reads=[B[4], w_glu_b], writes=[pb])
                                    S.op("dve", lambda e, p=p, b=b: e.tensor_tensor(out=sg[b][:], in0=p[:], in1=bglu_b[:], op=ALU.add),
                                         reads=[pb, cst], writes=[B[5]])
                                    S.op("act", lambda e, b=b: e.activation(out=sg[b][:], in_=sg[b][:], func=AF.Sigmoid),
                                         reads=[B[5]], writes=[B[5]])
                                    S.op("dve", lambda e, b=b: e.tensor_tensor(out=sg[b][:], in0=sg[b][:], in1=zf[b][:], op=ALU.mult),
                                         reads=[B[5], B[2]], writes=[B[5]])
                                    S.op("act", lambda e, b=b: e.activation(out=g1[b][:], in_=sg[b][:], func=AF.Square,
                                                                            accum_out=sst[b][:, 0:1]),
                                         reads=[B[5], B[0]], writes=[B[0], B[6]])
                                    S.op("act", lambda e, b=b: e.activation(out=sst[b][:, 1:2], in_=sst[b][:, 0:1], func=AF.Sqrt,
                                                                            bias=EPS, scale=1.0 / 512), reads=[B[6]], writes=[B[6]])
                                    S.op("dve", lambda e, b=b: e.reciprocal(out=sst[b][:, 2:3], in_=sst[b][:, 1:2]),
                                         reads=[B[6]], writes=[B[6]])
                                    S.op("dve", lambda e, b=b: e.scalar_tensor_tensor(
                                        out=mb_[b][:], in0=sg[b][:], scalar=sst[b][:, 2:3], in1=gns_b[:], op0=ALU.mult, op1=ALU.mult),
                                        reads=[B[5], B[6], cst], writes=[B[7]])
                                    ptt, ptb_ = next_pt()
                                    for ft in range(4):
                                        S.op("pe", lambda e, ptt=ptt, b=b, ft=ft: e.transpose(
                                            out=ptt[:, ft * 128:(ft + 1) * 128], in_=mb_[b][:, ft * 128:(ft + 1) * 128],
                                            identity=ident_bf[:]), reads=[B[7], cst], writes=[ptb_])
                                    dst = mixT_s[:, :, sp * 1024:(sp + 1) * 1024].rearrange("p a (b i) -> p a i b", i=8)[:, :, i, :]
                                    S.op("act", lambda e, ptt=ptt, dst=dst: e.copy(
                                        out=dst, in_=ptt[:, 0:512].rearrange("p (a b) -> p a b", b=128)),
                                        reads=[ptb_], writes=[mixT_sb])
                            S.barrier()

                    chk('s2')
                    with ExitStack() as st3:
                        w_out = sb(st3, "w_out", [128, 8, D], BF16)
                        w_out_b = Buf()
                        S.dma("pool", w_out[:], wout_d, writes=[w_out_b])
                        score = [sb(st3, "score%d" % i, [128, SEQ], F32) for i in range(2)]
                        score_b = [Buf(), Buf()]
                        nm = [sb(st3, "nm%d" % i, [128, SEQ], BF16) for i in range(2)]
                        nm_b = [Buf(), Buf()]
                        rbuf = [sb(st3, "rbuf%d" % i, [128, 8, 512], BF16) for i in range(2)]
                        rbuf_b = [Buf(), Buf()]
                        dg = [sb(st3, "dg%d" % i, [128, 8, 128], BF16) for i in range(2)]
                        dg_b = [Buf(), Buf()]
                        sjunk = sb(st3, "sjunk", [128, SEQ], BF16)
                        sjunk_b = Buf()
                        bs = [sb(st3, "bs%d" % i, [128, 16 + 2 * NITER], F32) for i in range(2)]
                        bs_b = [Buf(), Buf()]
                        PT = [sb(st3, "PT%d" % i, [128, 512], BF16) for i in range(3)]
                        PT_b = [Buf() for _ in range(3)]
                        yatt = sb(st3, "yatt", [128, 512], F32)
                        yatt_b = Buf()
                        rden = sb(st3, "rden", [128, 8], F32)
                        ajunk = sb(st3, "ajunk", [128, 512], BF16)
                        ast = sb(st3, "ast", [128, 4], F32)
                        mixa = sb(st3, "mixa", [128, 512], BF16)
                        mixa_b = Buf()
                        mixTa = sb(st3, "mixTa", [128, 4, 128], BF16)
                        mixTa_b = Buf()
                        xr = [sb(st3, "xr%d" % i, [128, D], F32) for i in range(2)]
                        xr_b = [Buf(), Buf()]
                        x1t = [sb(st3, "x1t%d" % i, [128, 512], F32) for i in range(2)]
                        x1t_b = [Buf(), Buf()]
                        ptc = 0
                        for qt in range(16):
                            sbi = qt % 2
                            nk = qt + 1
                            SK = nk * 128
                            qs = slice(qt * 128, (qt + 1) * 128)
                            sc, scb = score[sbi], score_b[sbi]
                            BS, BSb = bs[sbi], bs_b[sbi]
                            S.op("dve", lambda e, sbi=sbi, qt=qt: e.tensor_tensor(
                                out=dg[sbi][:], in0=ident_bf[:].unsqueeze(1).to_broadcast([128, 8, 128]),
                                in1=wi[:, qt, :].unsqueeze(2).to_broadcast([128, 8, 128]), op=ALU.mult),
                                reads=[cst, wi_b], writes=[dg_b[sbi]])
                            nch = (SK + 511) // 512
                            for c in range(nch):
                                cw = min(512, SK - c * 512)
                                ks = slice(c * 512, c * 512 + cw)
                                rb = c % 2
                                for h in range(8):
                                    j, half = h // 2, h % 2
                                    p, pb = next_pw()
                                    S.op("pe", lambda e, p=p, j=j, half=half, ks=ks, cw=cw: e.matmul(
                                        p[:, 0:cw], qiT[:, j, qs], kiTz[:, half, ks], start=True, stop=True),
                                        reads=[qiT_b, kiT_b], writes=[pb])
                                    S.op("act", lambda e, p=p, rb=rb, h=h, cw=cw: e.activation(
                                        out=rbuf[rb][:, h, 0:cw], in_=p[:, 0:cw], func=AF.Relu),
                                        reads=[pb], writes=[rbuf_b[rb]])
                                p, pb = next_pw()
                                for h in range(8):
                                    S.op("pe", lambda e, p=p, h=h, rb=rb, cw=cw, sbi=sbi: e.matmul(
                                        p[:, 0:cw], dg[sbi][:, h, :], rbuf[rb][:, h, 0:cw], start=(h == 0), stop=(h == 7)),
                                        reads=[dg_b[sbi], rbuf_b[rb]], writes=[pb])
                                S.op("act", lambda e, p=p, sc=sc, ks=ks, cw=cw: e.copy(out=sc[:, ks], in_=p[:, 0:cw]),
                                     reads=[pb], writes=[scb])
                                S.op("dve", lambda e, sc=sc, BS=BS, c=c, ks=ks: e.tensor_reduce(
                                    out=BS[:, c:c + 1], in_=sc[:, ks], axis=AX.X, op=ALU.max, apply_absolute_value=True),
                                    reads=[scb], writes=[BSb])
                            S.op("dve", lambda e, sc=sc: e.tensor_tensor(out=sc[:, qs], in0=sc[:, qs], in1=causal[:], op=ALU.add),
                                 reads=[scb, cst], writes=[scb])
                            if qt >= 2:
                                S.op("dve", lambda e, BS=BS, nch=nch: e.tensor_reduce(
                                    out=BS[:, 4:5], in_=BS[:, 0:nch], axis=AX.X, op=ALU.max), reads=[BSb], writes=[BSb])
                                S.op("dve", lambda e, BS=BS: e.tensor_scalar(
                                    out=BS[:, 8:8 + NITER + 1], in0=pow2[:, 0:NITER + 1], scalar1=BS[:, 4:5], scalar2=None,
                                    op0=ALU.mult), reads=[BSb, cst], writes=[BSb])
                                S.op("dve", lambda e, BS=BS: e.memset(BS[:, 5:6], 0.0), reads=[BSb], writes=[BSb])
                                thr = float(2 * TOPK - SK) - 0.5
                                for n in range(NITER):
                                    S.op("act", lambda e, sc=sc, BS=BS, SK=SK: e.activation(
                                        out=sjunk[:, 0:SK], in_=sc[:, 0:SK], func=AF.Sign, bias=BS[:, 5:6], scale=1.0,
                                        accum_out=BS[:, 6:7]), reads=[scb, BSb], writes=[sjunk_b, BSb])
                                    S.op("dve", lambda e, BS=BS, n=n, thr=thr: e.tensor_scalar(
                                        out=BS[:, 7:8], in0=BS[:, 6:7], scalar1=thr, scalar2=BS[:, 8 + n:9 + n],
                                        op0=ALU.is_lt, op1=ALU.mult), reads=[BSb], writes=[BSb])
                                    S.op("dve", lambda e, BS=BS, n=n: e.scalar_tensor_tensor(
                                        out=BS[:, 5:6], in0=BS[:, 7:8], scalar=BS[:, 9 + n:10 + n], in1=BS[:, 5:6],
                                        op0=ALU.subtract, op1=ALU.add), reads=[BSb], writes=[BSb])
                                S.op("dve", lambda e, BS=BS: e.tensor_tensor(
                                    out=BS[:, 5:6], in0=BS[:, 5:6], in1=BS[:, 8 + NITER:9 + NITER], op=ALU.add),
                                    reads=[BSb], writes=[BSb])
                                ntau = BS[:, 5:6]
                            else:
                                ntau = taufix[:, 0:1]
                            S.op("dve", lambda e, sc=sc, sbi=sbi, SK=SK, ntau=ntau: e.tensor_scalar(
                                out=nm[sbi][:, 0:SK], in0=sc[:, 0:SK], scalar1=ntau, scalar2=0.0, op0=ALU.add, op1=ALU.is_lt),
                                reads=[scb, BSb, cst], writes=[nm_b[sbi]])
                            O = [pf[4], pf[5]]
                            Ob = [pfb[4], pfb[5]]
                            for kv in range(2):
                                S.op("pe", lambda e, kv=kv: e.matmul(O[kv][:, 0:260], zeros_bf[:, 0:128], zeros_bf[:, 0:260],
                                                                     start=True, stop=True), reads=[cst], writes=[Ob[kv]])
                            for kt in range(nk):
                                ksl = slice(kt * 128, (kt + 1) * 128)
                                for kv in range(2):
                                    p, pb = next_pw()
                                    S.op("pe", lambda e, p=p, sbi=sbi, ksl=ksl: e.matmul(
                                        p[:], nm[sbi][:, ksl], negi4[:], start=True, stop=True),
                                        reads=[nm_b[sbi], cst], writes=[pb])
                                    for hh in range(4):
                                        head = kv * 4 + hh
                                        j, half = head // 2, head % 2
                                        S.op("pe", lambda e, p=p, hh=hh, kv=kv, half=half, j=j, ksl=ksl: e.matmul(
                                            p[:, hh * 128:(hh + 1) * 128], kTz[:, kv, half, ksl], qT[:, j, qs],
                                            start=False, stop=True, skip_group_check=True),
                                            reads=[kT_b, qT_b], writes=[pb])
                                    pi3 = ptc % 3
                                    ptc += 1
                                    S.op("act", lambda e, p=p, pi3=pi3: e.activation(
                                        out=PT[pi3][:], in_=p[:], func=AF.Exp, bias=negM[:, 0:1], scale=1.0),
                                        reads=[pb, cst], writes=[PT_b[pi3]])
                                    for hh in range(4):
                                        S.op("pe", lambda e, kv=kv, hh=hh, pi3=pi3, kt=kt: e.matmul(
                                            O[kv][:, hh * 65:(hh + 1) * 65], PT[pi3][:, hh * 128:(hh + 1) * 128], Vp[:, kt, kv, :],
                                            start=False, stop=True, skip_group_check=True),
                                            reads=[PT_b[pi3], Vp_b], writes=[Ob[kv]])
                            for kv in range(2):
                                ov = O[kv][:, 0:260].rearrange("p (h e) -> p h e", e=65)
                                S.op("dve", lambda e, kv=kv, ov=ov: e.reciprocal(out=rden[:, kv * 4:(kv + 1) * 4], in_=ov[:, :, 64]),
                                     reads=[Ob[kv]], writes=[yatt_b])
                                S.op("dve", lambda e, kv=kv, ov=ov: e.tensor_tensor(
                                    out=yatt[:, kv * 256:(kv + 1) * 256].rearrange("p (h d) -> p h d", d=64), in0=ov[:, :, 0:64],
                                    in1=rden[:, kv * 4:(kv + 1) * 4].unsqueeze(2).to_broadcast([128, 4, 64]), op=ALU.mult),
                                    reads=[Ob[kv], yatt_b], writes=[yatt_b])
                            S.op("act", lambda e: e.activation(out=ajunk[:], in_=yatt[:], func=AF.Square, accum_out=ast[:, 0:1]),
                                 reads=[yatt_b], writes=[yatt_b])
                            S.op("act", lambda e: e.activation(out=ast[:, 1:2], in_=ast[:, 0:1], func=AF.Sqrt, bias=EPS, scale=1.0 / 512),
                                 reads=[yatt_b], writes=[yatt_b])
                            S.op("dve", lambda e: e.reciprocal(out=ast[:, 2:3], in_=ast[:, 1:2]), reads=[yatt_b], writes=[yatt_b])
                            S.op("dve", lambda e: e.scalar_tensor_tensor(out=mixa[:], in0=yatt[:], scalar=ast[:, 2:3], in1=gna_b[:],
                                                                          op0=ALU.mult, op1=ALU.mult),
                                 reads=[yatt_b, cst], writes=[mixa_b])
                            ptt, ptb_ = next_pt()
                            for ft in range(4):
                                S.op("pe", lambda e, ptt=ptt, ft=ft: e.transpose(
                                    out=ptt[:, ft * 128:(ft + 1) * 128], in_=mixa[:, ft * 128:(ft + 1) * 128], identity=ident_bf[:]),
                                    reads=[mixa_b, cst], writes=[ptb_])
                            S.op("act", lambda e, ptt=ptt: e.copy(out=mixTa[:].rearrange("p a b -> p (a b)"), in_=ptt[:, 0:512]),
                                 reads=[ptb_], writes=[mixTa_b])
                            xb = qt % 2
                            rows = slice(r0 + qt * 128, r0 + (qt + 1) * 128)
                            S.dma("sp", xr[xb][:], x_d[rows, :], writes=[xr_b[xb]])
                            for hf in range(2):
                                p, pb = next_pw()
                                for k8 in range(8):
                                    lhs = mixT_s[:, k8, qs] if k8 < 4 else mixTa[:, k8 - 4, :]
                                    S.op("pe", lambda e, p=p, lhs=lhs, k8=k8, hf=hf: e.matmul(
                                        p[:], lhs, w_out[:, k8, hf * 512:(hf + 1) * 512], start=(k8 == 0), stop=(k8 == 7)),
                                        reads=[mixT_sb, mixTa_b, w_out_b], writes=[pb])
                                hs = slice(hf * 512, (hf + 1) * 512)
                                S.op("dve", lambda e, p=p, hf=hf, hs=hs: e.tensor_tensor(
                                    out=x1t[hf][:], in0=p[:], in1=tabsA[s][:, 2, hs], op=ALU.mult),
                                    reads=[pb, tabsA_b[s]], writes=[x1t_b[hf]])
                                S.op("dve", lambda e, xb=xb, hf=hf, hs=hs: e.tensor_tensor(
                                    out=xr[xb][:, hs], in0=x1t[hf][:], in1=xr[xb][:, hs], op=ALU.add),
                                    reads=[x1t_b[hf], xr_b[xb]], writes=[xr_b[xb]])
                            S.dma("sp", out_d[rows, :], xr[xb][:], reads=[xr_b[xb]])
                        S.barrier()

        chk('s3')
        NB = 256
        NTT = NB // 128
        with ExitStack() as stB:
            tabB_d = nc.dram_tensor("tabB_scr", [NSEQ, 128, 3 * D], F32, kind="Internal").ap()
            tabBd_b = Buf()
            with ExitStack() as stt:
                tl = [sb(stt, "tabsBt%d" % s, [128, 3, D], F32) for s in range(NSEQ)]
                tlb = [Buf() for _ in range(NSEQ)]
                adaln(tl, tlb, list(range(NSEQ)), 3, n2g_d, stt)
                for s in range(NSEQ):
                    S.dma("sp", tabB_d[s], tl[s][:].rearrange("p a b -> p (a b)"), reads=[tlb[s]], writes=[tabBd_b])
                S.barrier()
            tabsB1 = sb(stB, "tabsB", [128, 3, D], F32)
            tabsB_b1 = Buf()
            W1 = sb(stB, "W1", [128, 8, DFF], BF16)
            W2 = sb(stB, "W2", [128, 32, D], BF16)
            W_b = Buf()
            for kt in range(8):
                S.dma("pool", W1[:, kt, :], wff1_d[:, kt, :], writes=[W_b])
            for ft in range(0, 32, 4):
                S.dma("pool", W2[:, ft:ft + 4, :], wff2_d[:, ft:ft + 4, :], writes=[W_b])
            hidT = sb(stB, "hidT", [128, 32, NB], BF16)
            hid_b = [Buf() for _ in range(32)]
            rl = [sb(stB, "rl%d" % i, [128, NB], BF16) for i in range(2)]
            rl_b = [Buf(), Buf()]
            h2T = sb(stB, "h2T", [128, 8, NB], BF16)
            h2T_b = Buf()
            x1 = [sb(stB, "x1_%d" % i, [128, D], F32) for i in range(2)]
            x1_b = [Buf() for _ in range(2)]
            junkB = sb(stB, "junkB", [128, D], BF16)
            junkB_b = Buf()
            tB = sb(stB, "tB", [128, D], F32)
            tB_b = Buf()
            h2 = [sb(stB, "h2_%d" % i, [128, D], BF16) for i in range(2)]
            h2_b = [Buf(), Buf()]
            stb = [sb(stB, "stb%d" % i, [128, 4], F32) for i in range(2)]
            stb_b = [Buf(), Buf()]
            ot = [sb(stB, "ot%d" % i, [128, D], F32) for i in range(2)]
            ot_b = [Buf(), Buf()]
            oc = 0
            nblk = NSEQ * SEQ // NB
            for blk in range(nblk):
                s = (blk * NB) // SEQ
                if (blk * NB) % SEQ == 0:
                    S.dma("sp", tabsB1[:].rearrange("p a b -> p (a b)"), tabB_d[s], reads=[tabBd_b], writes=[tabsB_b1])
                for tt in range(NTT):
                    rows = slice(blk * NB + tt * 128, blk * NB + (tt + 1) * 128)
                    b = tt % 2
                    S.dma("sp", x1[b][:], out_d[rows, :], writes=[x1_b[b]])
                    S.op("act", lambda e, b=b: e.activation(out=junkB[:], in_=x1[b][:], func=AF.Square,
                                                            accum_out=stb[b][:, 0:1]),
                         reads=[x1_b[b]], writes=[junkB_b, stb_b[b]])
                    S.op("act", lambda e, b=b: e.activation(out=stb[b][:, 1:2], in_=stb[b][:, 0:1], func=AF.Sqrt,
                                                            bias=EPS, scale=1.0 / D), reads=[stb_b[b]], writes=[stb_b[b]])
                    S.op("dve", lambda e, b=b: e.reciprocal(out=stb[b][:, 2:3], in_=stb[b][:, 1:2]),
                         reads=[stb_b[b]], writes=[stb_b[b]])
                    S.op("dve", lambda e, b=b: e.scalar_tensor_tensor(
                        out=tB[:], in0=x1[b][:], scalar=stb[b][:, 2:3], in1=tabsB1[:, 1, :], op0=ALU.mult, op1=ALU.mult),
                        reads=[x1_b[b], stb_b[b], tabsB_b1], writes=[tB_b])
                    S.op("dve", lambda e, b=b: e.tensor_tensor(out=h2[b][:], in0=tB[:], in1=tabsB1[:, 0, :], op=ALU.add),
                         reads=[tB_b, tabsB_b1], writes=[h2_b[b]])
                    ptt, ptb_ = next_pt()
                    for kt in range(8):
                        S.op("pe", lambda e, ptt=ptt, b=b, kt=kt: e.transpose(
                            out=ptt[:, kt * 128:(kt + 1) * 128], in_=h2[b][:, kt * 128:(kt + 1) * 128], identity=ident_bf[:]),
                            reads=[h2_b[b], cst], writes=[ptb_])
                    S.op("act", lambda e, ptt=ptt, tt=tt: e.copy(
                        out=h2T[:, :, tt * 128:(tt + 1) * 128], in_=ptt[:].rearrange("p (a b) -> p a b", b=128)),
                        reads=[ptb_], writes=[h2T_b])
                for ft in range(32):
                    p, pb = next_pw(2)
                    for kt in range(8):
                        S.op("pe", lambda e, p=p, kt=kt, ft=ft: e.matmul(
                            p[:, 0:NB], W1[:, kt, ft * 128:(ft + 1) * 128], h2T[:, kt, :], start=(kt == 0), stop=(kt == 7)),
                            reads=[W_b, h2T_b], writes=[pb])
                    b = ft % 2
                    S.op("act", lambda e, p=p, b=b: e.activation(out=rl[b][:], in_=p[:, 0:NB], func=AF.Relu),
                         reads=[pb], writes=[rl_b[b]])
                    S.op("pool", lambda e, b=b, ft=ft: e.tensor_tensor(out=hidT[:, ft, :], in0=rl[b][:], in1=rl[b][:], op=ALU.mult),
                         reads=[rl_b[b]], writes=[hid_b[ft]])
                for tt in range(NTT):
                    rows = slice(blk * NB + tt * 128, blk * NB + (tt + 1) * 128)
                    ob = oc % 2
                    oc += 1
                    S.dma("sp", ot[ob][:], out_d[rows, :], writes=[ot_b[ob]])
                    for hf in range(2):
                        bi = 2 + (2 * tt + hf) % 4
                        p, pb = pf[bi], pfb[bi]
                        for ft in range(32):
                            S.op("pe", lambda e, p=p, ft=ft, tt=tt, hf=hf: e.matmul(
                                p[:], hidT[:, ft, tt * 128:(tt + 1) * 128], W2[:, ft, hf * 512:(hf + 1) * 512],
                                start=(ft == 0), stop=(ft == 31)), reads=[hid_b[ft], W_b], writes=[pb])
                        hs = slice(hf * 512, (hf + 1) * 512)
                        S.op("dve", lambda e, p=p, hs=hs: e.tensor_tensor(
                            out=tB[:, hs], in0=p[:], in1=tabsB1[:, 2, hs], op=ALU.mult),
                            reads=[pb, tabsB_b1], writes=[tB_b])
                        S.op("dve", lambda e, ob=ob, hs=hs: e.tensor_tensor(
                            out=ot[ob][:, hs], in0=tB[:, hs], in1=ot[ob][:, hs], op=ALU.add),
                            reads=[tB_b, ot_b[ob]], writes=[ot_b[ob]])
                    S.dma("sp", out_d[rows, :], ot[ob][:], reads=[ot_b[ob]])
            S.barrier()
    except _Stop:
        pass
    return nc


_PROGRAM = None


def _prep_shared(inp):
    f = np.float32
    def kt_layout(w):
        K, N = w.shape
        return np.ascontiguousarray(w.reshape(K // 128, 128, N).transpose(1, 0, 2)).astype(f)
    def bc(v, n=128):
        return np.ascontiguousarray(np.broadcast_to(np.asarray(v, f).reshape(1, -1), (n, np.asarray(v).size)))
    def qlay(a):
        a = np.asarray(a, f)
        rest = a.shape[2:]
        return np.ascontiguousarray(a.reshape((16, 2, 64) + rest).transpose((1, 2, 0) + tuple(range(3, 3 + len(rest))))
                                    .reshape((128, 16) + rest))
    sh = {}
    sh["w_ada"] = kt_layout(inp["w_ada"][0])
    sh["b_ada_b"] = bc(inp["b_ada"][0])
    sh["w_in"] = kt_layout(inp["w_in"][0])
    sh["w_out"] = kt_layout(inp["w_out"][0])
    sh["w_glu"] = kt_layout(inp["w_glu"][0])
    sh["w_ff1"] = kt_layout(inp["w_ff1"][0])
    sh["w_ff2"] = kt_layout(inp["w_ff2"][0])
    sh["norm1_g_b"] = bc(inp["norm1_g"][0])
    sh["norm2_g_b"] = bc(inp["norm2_g"][0])
    sh["b_glu_b"] = bc(inp["b_glu"][0])
    sh["gn_ssm_b"] = bc(inp["gn_ssm"][0])
    sh["gn_attn_b"] = bc(inp["gn_attn"][0])
    sh["gq_b"] = bc(inp["q_gain"][0])
    sh["gk_b"] = bc(inp["k_gain"][0])
    sh["gq2"] = np.ascontiguousarray(np.tile(np.asarray(inp["q_gain"][0], f), 2).reshape(128, 1))
    sh["gk2"] = np.ascontiguousarray(np.tile(np.asarray(inp["k_gain"][0], f), 2).reshape(128, 1))
    sh["lamre_q"] = qlay(inp["lam_re"][0])
    sh["lamim_q"] = qlay(inp["lam_im"][0])
    sh["logdt_q"] = qlay(np.broadcast_to(np.asarray(inp["log_dt"][0], f)[:, None], (32, 64)))
    sh["bre_q"] = qlay(inp["ssm_b_re"][0])
    sh["bim_q"] = qlay(inp["ssm_b_im"][0])
    sh["cre_q"] = qlay(np.asarray(inp["ssm_c_re"][0]).transpose(0, 2, 1))
    sh["cim_q"] = qlay(np.asarray(inp["ssm_c_im"][0]).transpose(0, 2, 1))
    dsk = np.asarray(inp["d_skip"][0], f).reshape(32, 16)
    sh["dsk_b"] = np.ascontiguousarray(np.broadcast_to(np.tile(dsk, (1, 8))[None], (128, 32, 128))).astype(f)
    sh["ident"] = np.eye(128, dtype=f)
    r = np.arange(128)
    sh["causal"] = np.where(r[None, :] <= r[:, None], 0.0, -1.0e30).astype(f)
    sh["negi4"] = np.tile(-BIG * np.eye(128, dtype=f), (1, 4)).astype(f)
    ib, jb = r // 16, r // 16
    sh["bmask"] = (jb[None, :] >= ib[:, None]).astype(f)
    sh["dmask"] = (r[None, :] == r[:, None]).astype(f)
    sh["onesblk"] = ((r[None, :] // 64) == (r[:, None] // 64)).astype(f)
    sh["mtab"] = np.ascontiguousarray(np.broadcast_to(np.asarray(EXPS, f)[None, None, :], (128, 16, NE)))
    sh["pow2"] = np.ascontiguousarray(np.broadcast_to((2.0 ** -np.arange(NITER + 2)).astype(f)[None], (128, NITER + 2)))
    zm = np.zeros((128, 2), f)
    zm[:64, 0] = 1.0
    zm[64:, 1] = 1.0
    sh["zmask"] = zm
    return sh


def kernel(**inputs):
    global _PROGRAM
    inp = {k: np.asarray(v) for k, v in inputs.items()}
    if _PROGRAM is None:
        _PROGRAM = build_program()
    nc = _PROGRAM
    shared = _prep_shared(inp)
    x = np.asarray(inp["x"], np.float32)
    c = np.asarray(inp["c"], np.float32)
    in_maps = []
    for core in range(NCORES):
        m = dict(shared)
        m["x"] = np.ascontiguousarray(x[NSEQ * core: NSEQ * (core + 1)].reshape(NSEQ * SEQ, D))
        cc = c[NSEQ * core: NSEQ * (core + 1)]
        cT = cc.reshape(NSEQ, 8, 128).transpose(2, 1, 0)
        m["cT"] = np.ascontiguousarray(np.broadcast_to(cT[:, :, :, None], (128, 8, NSEQ, 128))).astype(np.float32)
        in_maps.append(m)
    res = run_bass_kernel_spmd(nc, in_maps, core_ids=list(range(NCORES)))
    outs = [np.asarray(r["out"], np.float32).reshape(NSEQ, SEQ, D) for r in res.results]
    return np.concatenate(outs, axis=0)
```

```python
import math
from contextlib import ExitStack

import numpy as np
import concourse.bass as bass
import concourse.mybir as mybir
from concourse.alu_op_type import AluOpType as ALU
from concourse.bass_utils import run_bass_kernel_spmd

F32 = mybir.dt.float32
BF16 = mybir.dt.bfloat16
I32 = mybir.dt.int32
AF = mybir.ActivationFunctionType
AX = mybir.AxisListType

NCORES = 8
D = 1024
SEQ = 2048
NSEQ = 2
DIN = 1864
DFF = 4096
EPS = 1e-6
IDX_SCALE = (64 ** -0.5) * (8 ** -0.5)
TOPK = 256
NITER = 18
BIG = 30000.0
EXPS = list(range(-7, 9)) + [16, 32, 64, 128, 256, 512, 1024]
NE = len(EXPS)
EIDX = {m: i for i, m in enumerate(EXPS)}
TWO_PI = 2.0 * math.pi


class Buf:
    __slots__ = ("w", "r", "ex")

    def __init__(self, ex=False):
        self.w = None
        self.r = {}
        self.ex = ex


class Sched:
    NDS = 24

    def __init__(self, nc, st):
        self.nc = nc
        self.eng = {"pe": nc.tensor, "act": nc.scalar, "dve": nc.vector, "pool": nc.gpsimd, "sp": nc.sync}
        self.sem = {k: st.enter_context(nc.semaphore("s_" + k)) for k in self.eng}
        self.cnt = {k: 0 for k in self.eng}
        self.waited = {k: {} for k in self.eng}
        self.dsem = [st.enter_context(nc.semaphore("d%d" % i)) for i in range(self.NDS)]
        self.dcnt = [0] * self.NDS
        self.dpool = {"sp": list(range(0, 16)), "pool": list(range(16, 24))}
        self.dnext = {"sp": 0, "pool": 0}

    def _semof(self, key):
        return self.sem[key[1]] if key[0] == "e" else self.dsem[key[1]]

    def _wait(self, e, key, val):
        if key[0] == "e" and key[1] == e and e == "pe":
            return
        if self.waited[e].get(key, 0) >= val:
            return
        self.eng[e].wait_ge(self._semof(key), val)
        self.waited[e][key] = val

    def _deps(self, e, reads, writes):
        for b in reads:
            if b.w is not None:
                self._wait(e, b.w[0], b.w[1])
            if b.ex:
                for k, v in b.r.items():
                    if k != ("e", e):
                        self._wait(e, k, v)
        for b in writes:
            if b.w is not None:
                self._wait(e, b.w[0], b.w[1])
            for k, v in b.r.items():
                self._wait(e, k, v)

    def _mark(self, key, val, reads, writes):
        for b in reads:
            if b.r.get(key, 0) < val:
                b.r[key] = val
        for b in writes:
            b.w = (key, val)
            b.r = {}

    def op(self, e, fn, reads=(), writes=()):
        self._deps(e, reads, writes)
        ins = fn(self.eng[e])
        self.cnt[e] += 1
        ins.then_inc(self.sem[e], 1)
        self._mark(("e", e), self.cnt[e], reads, writes)

    def dma(self, e, out, in_, reads=(), writes=(), **kw):
        self._deps(e, reads, writes)
        pl = self.dpool[e]
        j = pl[self.dnext[e] % len(pl)]
        self.dnext[e] += 1
        if self.dcnt[j] > 0:
            self._wait(e, ("d", j), self.dcnt[j])
        ins = self.eng[e].dma_start(out=out, in_=in_, **kw)
        self.dcnt[j] += 16
        ins.then_inc(self.dsem[j], 16)
        self._mark(("d", j), self.dcnt[j], reads, writes)

    def barrier(self):
        for e in self.eng:
            for k in self.eng:
                if k != e and self.cnt[k] > 0:
                    self._wait(e, ("e", k), self.cnt[k])
            for j in range(self.NDS):
                if self.dcnt[j] > 0:
                    self._wait(e, ("d", j), self.dcnt[j])


class _Stop(Exception):
    pass


def build_program(stop=None):
    nc = bass.Bass("TRN2", target_bir_lowering=False)

    def din(name, shape, dt=F32):
        return nc.dram_tensor(name, list(shape), dt, kind="ExternalInput").ap()

    x_d = din("x", [NSEQ * SEQ, D])
    cT_d = din("cT", [128, 8, NSEQ, 128])
    wada_d = din("w_ada", [128, 8, 6 * D])
    bada_d = din("b_ada_b", [128, 6 * D])
    win_d = din("w_in", [128, 8, DIN])
    wout_d = din("w_out", [128, 8, D])
    wglu_d = din("w_glu", [128, 4, 512])
    wff1_d = din("w_ff1", [128, 8, DFF])
    wff2_d = din("w_ff2", [128, 32, D])
    n1g_d = din("norm1_g_b", [128, D])
    n2g_d = din("norm2_g_b", [128, D])
    bglu_d = din("b_glu_b", [128, 512])
    gns_d = din("gn_ssm_b", [128, 512])
    gna_d = din("gn_attn_b", [128, 512])
    gqb_d = din("gq_b", [128, 64])
    gkb_d = din("gk_b", [128, 64])
    gq2_d = din("gq2", [128, 1])
    gk2_d = din("gk2", [128, 1])
    lre_d = din("lamre_q", [128, 16])
    lim_d = din("lamim_q", [128, 16])
    ldt_d = din("logdt_q", [128, 16])
    bre_d = din("bre_q", [128, 16, 16])
    bim_d = din("bim_q", [128, 16, 16])
    cre_d = din("cre_q", [128, 16, 16])
    cim_d = din("cim_q", [128, 16, 16])
    dsk_d = din("dsk_b", [128, 32, 128])
    ident_d = din("ident", [128, 128])
    causal_d = din("causal", [128, 128])
    negi4_d = din("negi4", [128, 512])
    bmask_d = din("bmask", [128, 128])
    dmask_d = din("dmask", [128, 128])
    onesblk_d = din("onesblk", [128, 128])
    mtab_d = din("mtab", [128, 16, NE])
    pow2_d = din("pow2", [128, NITER + 2])
    zmask_d = din("zmask", [128, 2])
    out_d = nc.dram_tensor("out", [NSEQ * SEQ, D], F32, kind="ExternalOutput").ap()

    try:
      with ExitStack() as top:
        S = Sched(nc, top)

        def chk(name):
            if stop == name:
                S.barrier()
                raise _Stop()

        uid = [0]

        def sb(st, name, shape, dt):
            uid[0] += 1
            return st.enter_context(nc.sbuf_tensor("sb%d_%s" % (uid[0], name), list(shape), dt))

        def ps(st, name, shape, dt):
            uid[0] += 1
            return st.enter_context(nc.psum_tensor("ps%d_%s" % (uid[0], name), list(shape), dt))

        pt = [ps(top, "pt%d" % i, [128, 1024], BF16) for i in range(2)]
        ptb = [Buf(ex=True) for _ in range(2)]
        pf = [ps(top, "pf%d" % i, [128, 512], F32) for i in range(6)]
        pfb = [Buf(ex=True) for _ in range(6)]
        rot = {"pt": 0, "pw": 0}

        def next_pt():
            i = rot["pt"]
            rot["pt"] = (i + 1) % 2
            return pt[i], ptb[i]

        def next_pw(n=4):
            i = rot["pw"] % n
            rot["pw"] += 1
            return pf[i], pfb[i]

        ident_bf = sb(top, "ident_bf", [128, 128], BF16)
        ident_f = sb(top, "ident_f", [128, 128], F32)
        causal = sb(top, "causal", [128, 128], F32)
        negi4 = sb(top, "negi4", [128, 512], BF16)
        onesblk = sb(top, "onesblk", [128, 128], BF16)
        zeros_bf = sb(top, "zeros_bf", [128, 260], BF16)
        bglu_b = sb(top, "bglu_b", [128, 512], F32)
        gns_b = sb(top, "gns_b", [128, 512], F32)
        gna_b = sb(top, "gna_b", [128, 512], F32)
        pow2 = sb(top, "pow2", [128, NITER + 2], F32)
        G2 = sb(top, "G2", [128, 1], F32)
        negM = sb(top, "negM", [128, 1], F32)
        taufix = sb(top, "taufix", [128, 1], F32)
        siluT = sb(top, "siluT", [128, 8, NSEQ, 128], BF16)
        cst = Buf()
        for t_, d_ in ((ident_bf, ident_d), (ident_f, ident_d), (causal, causal_d), (negi4, negi4_d),
                       (onesblk, onesblk_d), (bglu_b, bglu_d), (gns_b, gns_d), (gna_b, gna_d), (pow2, pow2_d)):
            S.dma("pool", t_[:], d_, writes=[cst])
        S.op("dve", lambda e: e.memset(zeros_bf[:], 0.0), writes=[cst])
        S.op("dve", lambda e: e.memset(taufix[:], 1.0e29), writes=[cst])

        with ExitStack() as st0:
            gqb = sb(st0, "gqb", [128, 64], F32)
            gkb = sb(st0, "gkb", [128, 64], F32)
            gq2 = sb(st0, "gq2", [128, 1], F32)
            gk2 = sb(st0, "gk2", [128, 1], F32)
            cTf = sb(st0, "cTf", [128, 8 * NSEQ * 128], F32)
            tb = Buf()
            S.dma("sp", gqb[:], gqb_d, writes=[tb])
            S.dma("sp", gkb[:], gkb_d, writes=[tb])
            S.dma("sp", gq2[:], gq2_d, writes=[tb])
            S.dma("sp", gk2[:], gk2_d, writes=[tb])
            S.dma("sp", cTf[:], cT_d.rearrange("p a b c -> p (a b c)"), writes=[tb])
            S.op("dve", lambda e: e.scalar_tensor_tensor(out=G2[:], in0=gq2[:], scalar=0.125, in1=gk2[:],
                                                          op0=ALU.mult, op1=ALU.mult), reads=[tb], writes=[cst])
            S.op("dve", lambda e: e.scalar_tensor_tensor(out=gqb[:], in0=gqb[:], scalar=0.125, in1=gkb[:],
                                                          op0=ALU.mult, op1=ALU.mult), reads=[tb], writes=[tb])
            S.op("dve", lambda e: e.tensor_reduce(out=negM[:], in_=gqb[:], axis=AX.X, op=ALU.max,
                                                  apply_absolute_value=True), reads=[tb], writes=[cst])
            S.op("dve", lambda e: e.tensor_scalar(out=negM[:], in0=negM[:], scalar1=-64.0, scalar2=None,
                                                  op0=ALU.mult), reads=[cst], writes=[cst])
            S.op("act", lambda e: e.activation(out=siluT[:].rearrange("p a b c -> p (a b c)"), in_=cTf[:],
                                               func=AF.Silu), reads=[tb], writes=[cst])
            S.barrier()

        chk('consts')
        def adaln(tabs, tabs_b, seqs, first_j, ng_d, st):
            wst = [sb(st, "wst%d" % i, [128, 8, 512], BF16) for i in range(2)]
            wstb = [Buf(), Buf()]
            bst = [sb(st, "bst%d" % i, [128, 512], F32) for i in range(2)]
            gst = [sb(st, "gst%d" % i, [128, 512], F32) for i in range(2)]
            tmp = [sb(st, "adat%d" % i, [128, 512], F32) for i in range(2)]
            tmpb = [Buf(), Buf()]
            it = 0
            for jj in range(3):
                for hc in range(2):
                    c0 = (first_j + jj) * D + hc * 512
                    b = it % 2
                    it += 1
                    S.dma("pool", wst[b][:], wada_d[:, :, c0:c0 + 512], writes=[wstb[b]])
                    S.dma("sp", bst[b][:], bada_d[:, c0:c0 + 512], writes=[wstb[b]])
                    if jj == 1:
                        S.dma("sp", gst[b][:], ng_d[:, hc * 512:(hc + 1) * 512], writes=[wstb[b]])
                    for n, s in enumerate(seqs):
                        p, pb = next_pw()
                        for kt in range(8):
                            S.op("pe", lambda e, p=p, b=b, kt=kt, s=s: e.matmul(
                                p[:], siluT[:, kt, s, :], wst[b][:, kt, :], start=(kt == 0), stop=(kt == 7)),
                                reads=[cst, wstb[b]], writes=[pb])
                        dst = tabs[n][:, jj, hc * 512:(hc + 1) * 512]
                        if jj == 1:
                            tb_ = tmpb[n]
                            S.op("dve", lambda e, p=p, b=b, n=n: e.tensor_tensor(
                                out=tmp[n][:], in0=p[:], in1=bst[b][:], op=ALU.add),
                                reads=[pb, wstb[b]], writes=[tb_])
                            S.op("dve", lambda e, b=b, n=n, dst=dst: e.scalar_tensor_tensor(
                                out=dst, in0=tmp[n][:], scalar=1.0, in1=gst[b][:], op0=ALU.add, op1=ALU.mult),
                                reads=[tb_, wstb[b]], writes=[tabs_b[n]])
                        else:
                            S.op("dve", lambda e, p=p, b=b, dst=dst: e.tensor_tensor(
                                out=dst, in0=p[:], in1=bst[b][:], op=ALU.add),
                                reads=[pb, wstb[b]], writes=[tabs_b[n]])

        with ExitStack() as stA:
            tabsA1 = sb(stA, "tabsA", [128, 3, D], F32)
            tabsA = [tabsA1, tabsA1]
            tabsA_b1 = Buf()
            tabsA_b = [tabsA_b1, tabsA_b1]

            ssmA_d = nc.dram_tensor("ssmA_scr", [128, 32, 128], BF16, kind="Internal").ap()
            ssmB_d = nc.dram_tensor("ssmB_scr", [128, 32, 128], BF16, kind="Internal").ap()
            ssmC_d = nc.dram_tensor("ssmC_scr", [128, 32, 2, 128], BF16, kind="Internal").ap()
            ssmd_b = Buf()
            ak = sb(stA, "ak", [128, 16, 8], F32)
            ck = sb(stA, "ck", [128, 16, 8], F32)
            nck = sb(stA, "nck", [128, 16, 8], F32)
            ssm_b = Buf()
            with ExitStack() as st1:
                A_sb = sb(st1, "A_sb", [128, 32, 128], BF16)
                Bm_sb = sb(st1, "Bm_sb", [128, 32, 128], BF16)
                Cmz = sb(st1, "Cmz", [128, 32, 2, 128], BF16)

                def t3(name, n):
                    return sb(st1, name, [128, 16, n], F32)
                lre = t3("lre", 1); lim = t3("lim", 1); ldt = t3("ldt", 1)
                bre = t3("bre", 16); bim = t3("bim", 16); cre = t3("cre", 16); cim = t3("cim", 16)
                mtab = t3("mtab", NE)
                bmask = sb(st1, "bmask", [128, 128], F32)
                dmask = sb(st1, "dmask", [128, 128], F32)
                zmask = sb(st1, "zmask", [128, 2], F32)
                dsk = sb(st1, "dsk", [128, 32, 128], F32)
                ib = Buf()
                for t_, d_ in ((lre, lre_d), (lim, lim_d), (ldt, ldt_d)):
                    S.dma("sp", t_[:, :, 0], d_, writes=[ib])
                for t_, d_ in ((bre, bre_d), (bim, bim_d), (cre, cre_d), (cim, cim_d), (mtab, mtab_d),
                               (bmask, bmask_d), (dmask, dmask_d), (dsk, dsk_d), (zmask, zmask_d)):
                    S.dma("sp", t_[:], d_, writes=[ib])
                chk('ssm_a')
                dt_ = t3("dt_", 1); aa = t3("aa", 1); th = t3("th", 1)
                ang = t3("ang", NE); lmag = t3("lmag", NE); mag = t3("mag", NE)
                tq = t3("tq", NE); tqi = sb(st1, "tqi", [128, 16, NE], I32); tqf = t3("tqf", NE)
                wr = t3("wr", NE); sn = t3("sn", NE); cs = t3("cs", NE)
                pr_ = t3("pr_", NE); pi_ = t3("pi_", NE)
                wb = Buf()

                def V(fn, reads=(), writes=(wb,)):
                    S.op("dve", fn, reads=[ib, wb] + list(reads), writes=list(writes))

                def ACT(fn, reads=(), writes=(wb,)):
                    S.op("act", fn, reads=[ib, wb] + list(reads), writes=list(writes))

                ACT(lambda e: e.activation(out=dt_[:], in_=ldt[:], func=AF.Exp))
                V(lambda e: e.tensor_tensor(out=aa[:], in0=lre[:], in1=dt_[:], op=ALU.mult))
                V(lambda e: e.tensor_tensor(out=th[:], in0=lim[:], in1=dt_[:], op=ALU.mult))
                V(lambda e: e.tensor_tensor(out=lmag[:], in0=mtab[:], in1=aa[:].to_broadcast([128, 16, NE]), op=ALU.mult))
                V(lambda e: e.tensor_tensor(out=ang[:], in0=mtab[:], in1=th[:].to_broadcast([128, 16, NE]), op=ALU.mult))
                ACT(lambda e: e.activation(out=mag[:], in_=lmag[:], func=AF.Exp))
                V(lambda e: e.tensor_scalar(out=tq[:], in0=ang[:], scalar1=1.0 / TWO_PI, scalar2=None, op0=ALU.mult))
                V(lambda e: e.tensor_copy(out=tqi[:], in_=tq[:]))
                V(lambda e: e.tensor_copy(out=tqf[:], in_=tqi[:]))
                V(lambda e: e.scalar_tensor_tensor(out=wr[:], in0=tqf[:], scalar=-TWO_PI, in1=ang[:],
                                                   op0=ALU.mult, op1=ALU.add))
                wt_ = t3("wt_", NE)
                for t_, shift in ((sn, 0.0), (cs, math.pi / 2)):
                    V(lambda e, t_=t_, shift=shift: e.tensor_scalar(out=t_[:], in0=wr[:], scalar1=shift, scalar2=None, op0=ALU.add))
                    V(lambda e, t_=t_: e.tensor_scalar(out=wt_[:], in0=t_[:], scalar1=math.pi, scalar2=-TWO_PI,
                                                       op0=ALU.is_gt, op1=ALU.mult))
                    V(lambda e, t_=t_: e.tensor_scalar(out=tq[:], in0=t_[:], scalar1=-math.pi, scalar2=TWO_PI,
                                                       op0=ALU.is_lt, op1=ALU.mult))
                    V(lambda e, t_=t_: e.tensor_tensor(out=t_[:], in0=t_[:], in1=wt_[:], op=ALU.add))
                    V(lambda e, t_=t_: e.tensor_tensor(out=t_[:], in0=t_[:], in1=tq[:], op=ALU.add))
                for t_ in (sn, cs):
                    V(lambda e, t_=t_: e.tensor_scalar(out=t_[:], in0=t_[:], scalar1=3.14159, scalar2=-3.14159,
                                                       op0=ALU.min, op1=ALU.max))
                ACT(lambda e: e.activation(out=sn[:], in_=sn[:], func=AF.Sin))
                ACT(lambda e: e.activation(out=cs[:], in_=cs[:], func=AF.Sin))
                V(lambda e: e.tensor_tensor(out=pr_[:], in0=mag[:], in1=cs[:], op=ALU.mult))
                V(lambda e: e.tensor_tensor(out=pi_[:], in0=mag[:], in1=sn[:], op=ALU.mult))
                chk('ssm_b')
                i1 = EIDX[1]
                nr = t3("nr", 1); den = t3("den", 1); t1_ = t3("t1_", 1); gr = t3("gr", 1); gi = t3("gi", 1)
                V(lambda e: e.tensor_scalar(out=nr[:], in0=pr_[:, :, i1:i1 + 1], scalar1=-1.0, scalar2=None, op0=ALU.add))
                V(lambda e: e.tensor_tensor(out=den[:], in0=lre[:], in1=lre[:], op=ALU.mult))
                V(lambda e: e.tensor_tensor(out=t1_[:], in0=lim[:], in1=lim[:], op=ALU.mult))
                V(lambda e: e.tensor_tensor(out=den[:], in0=den[:], in1=t1_[:], op=ALU.add))
                V(lambda e: e.reciprocal(out=den[:], in_=den[:]))
                V(lambda e: e.tensor_tensor(out=gr[:], in0=nr[:], in1=lre[:], op=ALU.mult))
                V(lambda e: e.tensor_tensor(out=t1_[:], in0=pi_[:, :, i1:i1 + 1], in1=lim[:], op=ALU.mult))
                V(lambda e: e.tensor_tensor(out=gr[:], in0=gr[:], in1=t1_[:], op=ALU.add))
                V(lambda e: e.tensor_tensor(out=gr[:], in0=gr[:], in1=den[:], op=ALU.mult))
                V(lambda e: e.tensor_tensor(out=gi[:], in0=pi_[:, :, i1:i1 + 1], in1=lre[:], op=ALU.mult))
                V(lambda e: e.tensor_tensor(out=t1_[:], in0=nr[:], in1=lim[:], op=ALU.mult))
                V(lambda e: e.tensor_tensor(out=gi[:], in0=gi[:], in1=t1_[:], op=ALU.subtract))
                V(lambda e: e.tensor_tensor(out=gi[:], in0=gi[:], in1=den[:], op=ALU.mult))
                for k in range(8):
                    ii = EIDX[8 * (2 ** k)]
                    V(lambda e, k=k, ii=ii: e.tensor_copy(out=ak[:, :, k:k + 1], in_=pr_[:, :, ii:ii + 1]), writes=[wb, ssm_b])
                    V(lambda e, k=k, ii=ii: e.tensor_copy(out=ck[:, :, k:k + 1], in_=pi_[:, :, ii:ii + 1]), writes=[wb, ssm_b])
                    V(lambda e, k=k, ii=ii: e.tensor_scalar(out=nck[:, :, k:k + 1], in0=pi_[:, :, ii:ii + 1], scalar1=-1.0,
                                                            scalar2=None, op0=ALU.mult), writes=[wb, ssm_b])
                PBr = t3("PBr", 8); PBi = t3("PBi", 8); tt8 = t3("tt8", 8)
                e7 = EIDX[0]
                sl07 = slice(e7, e7 + 8)
                V(lambda e: e.tensor_tensor(out=PBr[:], in0=pr_[:, :, sl07], in1=gr[:].to_broadcast([128, 16, 8]), op=ALU.mult))
                V(lambda e: e.tensor_tensor(out=tt8[:], in0=pi_[:, :, sl07], in1=gi[:].to_broadcast([128, 16, 8]), op=ALU.mult))
                V(lambda e: e.tensor_tensor(out=PBr[:], in0=PBr[:], in1=tt8[:], op=ALU.subtract))
                V(lambda e: e.tensor_tensor(out=PBi[:], in0=pr_[:, :, sl07], in1=gi[:].to_broadcast([128, 16, 8]), op=ALU.mult))
                V(lambda e: e.tensor_tensor(out=tt8[:], in0=pi_[:, :, sl07], in1=gr[:].to_broadcast([128, 16, 8]), op=ALU.mult))
                V(lambda e: e.tensor_tensor(out=PBi[:], in0=PBi[:], in1=tt8[:], op=ALU.add))
                BmTr = sb(st1, "BmTr", [128, 16, 8, 16], F32)
                BmTi = sb(st1, "BmTi", [128, 16, 8, 16], F32)
                t816 = sb(st1, "t816", [128, 16, 8, 16], F32)
                for i in range(8):
                    m = 7 - i
                    def bc(t_, m=m):
                        return t_[:, :, m:m + 1].to_broadcast([128, 16, 16])
                    V(lambda e, i=i, bc=bc: e.tensor_tensor(out=BmTr[:, :, i, :], in0=bre[:], in1=bc(PBr), op=ALU.mult))
                    V(lambda e, i=i, bc=bc: e.tensor_tensor(out=t816[:, :, i, :], in0=bim[:], in1=bc(PBi), op=ALU.mult))
                    V(lambda e, i=i, bc=bc: e.tensor_tensor(out=BmTi[:, :, i, :], in0=bim[:], in1=bc(PBr), op=ALU.mult))
                V(lambda e: e.tensor_tensor(out=BmTr[:], in0=BmTr[:], in1=t816[:], op=ALU.subtract))
                for i in range(8):
                    m = 7 - i
                    V(lambda e, i=i, m=m: e.tensor_tensor(out=t816[:, :, i, :], in0=bre[:],
                                                          in1=PBi[:, :, m:m + 1].to_broadcast([128, 16, 16]), op=ALU.mult))
                V(lambda e: e.tensor_tensor(out=BmTi[:], in0=BmTi[:], in1=t816[:], op=ALU.add))
                Wcr = sb(st1, "Wcr", [128, 16, 8, 16], F32)
                Wci = sb(st1, "Wci", [128, 16, 8, 16], F32)
                Cmr = sb(st1, "Cmr", [128, 16, 8, 16], F32)
                Cmi = sb(st1, "Cmi", [128, 16, 8, 16], F32)
                for (dr, di, off) in ((Wcr, Wci, -7), (Cmr, Cmi, 1)):
                    for j in range(8):
                        ii = EIDX[j + off]
                        def bc2(t_, ii=ii):
                            return t_[:, :, ii:ii + 1].to_broadcast([128, 16, 16])
                        V(lambda e, j=j, bc2=bc2, dr=dr: e.tensor_tensor(out=dr[:, :, j, :], in0=cre[:], in1=bc2(pr_), op=ALU.mult))
                        V(lambda e, j=j, bc2=bc2: e.tensor_tensor(out=t816[:, :, j, :], in0=cim[:], in1=bc2(pi_), op=ALU.mult))
                        V(lambda e, j=j, bc2=bc2, di=di: e.tensor_tensor(out=di[:, :, j, :], in0=cre[:], in1=bc2(pi_), op=ALU.mult))
                    V(lambda e, dr=dr: e.tensor_tensor(out=dr[:], in0=dr[:], in1=t816[:], op=ALU.subtract))
                    for j in range(8):
                        ii = EIDX[j + off]
                        V(lambda e, j=j, ii=ii: e.tensor_tensor(out=t816[:, :, j, :], in0=cim[:],
                                                                in1=pr_[:, :, ii:ii + 1].to_broadcast([128, 16, 16]), op=ALU.mult))
                    V(lambda e, di=di: e.tensor_tensor(out=di[:], in0=di[:], in1=t816[:], op=ALU.add))
                    V(lambda e, di=di: e.tensor_scalar(out=di[:], in0=di[:], scalar1=-1.0, scalar2=None, op0=ALU.mult))
                chk('ssm_c')
                for pr in range(16):
                    for gp in range(2):
                        g = 2 * pr + gp
                        for ri, src in ((0, Cmr), (1, Cmi)):
                            V(lambda e, g=g, ri=ri, src=src, pr=pr, gp=gp: e.tensor_scalar(
                                out=Cmz[:, g, ri, :], in0=src[:, pr, :, :].rearrange("p a b -> p (a b)"),
                                scalar1=zmask[:, gp:gp + 1], scalar2=None, op0=ALU.mult), writes=[wb, ssm_b])
                chk('ssm_d')
                Bz = [sb(st1, "Bz%d" % i, [128, 2, 128], BF16) for i in range(2)]
                Wz = [sb(st1, "Wz%d" % i, [128, 2, 128], BF16) for i in range(2)]
                Bzb = [Buf(), Buf()]
                At = [sb(st1, "At%d" % i, [128, 128], F32) for i in range(2)]
                Atb = [Buf(), Buf()]
                for pr in range(16):
                    for gp in range(2):
                        g = 2 * pr + gp
                        b = g % 2
                        for ri, src in ((0, BmTr), (1, BmTi)):
                            V(lambda e, b=b, ri=ri, src=src, pr=pr, gp=gp: e.tensor_scalar(
                                out=Bz[b][:, ri, :], in0=src[:, pr, :, :].rearrange("p a b -> p (a b)"),
                                scalar1=zmask[:, gp:gp + 1], scalar2=None, op0=ALU.mult), writes=[wb, Bzb[b]])
                        for ri, src in ((0, Wcr), (1, Wci)):
                            V(lambda e, b=b, ri=ri, src=src, pr=pr, gp=gp: e.tensor_scalar(
                                out=Wz[b][:, ri, :], in0=src[:, pr, :, :].rearrange("p a b -> p (a b)"),
                                scalar1=zmask[:, gp:gp + 1], scalar2=None, op0=ALU.mult), writes=[wb, Bzb[b]])
                        ptt, ptb_ = next_pt()
                        for ri in range(2):
                            S.op("pe", lambda e, ptt=ptt, b=b, ri=ri: e.transpose(
                                out=ptt[:, ri * 128:(ri + 1) * 128], in_=Bz[b][:, ri, :], identity=ident_bf[:]),
                                reads=[Bzb[b], cst], writes=[ptb_])
                        for ri in range(2):
                            S.op("act", lambda e, ptt=ptt, g=g, ri=ri, gp=gp: e.copy(
                                out=Bm_sb[:, g, ri * 64:(ri + 1) * 64],
                                in_=ptt[:, ri * 128 + gp * 64: ri * 128 + gp * 64 + 64]),
                                reads=[ptb_], writes=[ssm_b])
                        p, pb = next_pw()
                        for ri in range(2):
                            S.op("pe", lambda e, p=p, b=b, ri=ri: e.matmul(
                                p[:, 0:128], Bz[b][:, ri, :], Wz[b][:, ri, :], start=(ri == 0), stop=(ri == 1)),
                                reads=[Bzb[b]], writes=[pb])
                        S.op("dve", lambda e, p=p, b=b: e.tensor_tensor(out=At[b][:], in0=p[:, 0:128], in1=bmask[:], op=ALU.mult),
                             reads=[pb, ib], writes=[Atb[b]])
                        S.op("dve", lambda e, b=b, g=g: e.tensor_tensor(out=dsk[:, g, :], in0=dsk[:, g, :], in1=dmask[:], op=ALU.mult),
                             reads=[ib], writes=[ib])
                        S.op("dve", lambda e, b=b, g=g: e.tensor_tensor(out=A_sb[:, g, :], in0=At[b][:], in1=dsk[:, g, :], op=ALU.add),
                             reads=[Atb[b], ib], writes=[ssm_b])
                chk('ssm_e')
                S.dma("sp", ssmA_d, A_sb[:], reads=[ssm_b], writes=[ssmd_b])
                S.dma("sp", ssmB_d, Bm_sb[:], reads=[ssm_b], writes=[ssmd_b])
                S.dma("sp", ssmC_d, Cmz[:], reads=[ssm_b], writes=[ssmd_b])
                S.barrier()

            chk('ssmsetup')
            for s in range(NSEQ):
                r0 = s * SEQ
                with ExitStack() as stt:
                    adaln([tabsA1], [tabsA_b1], [s], 0, n1g_d, stt)
                    S.barrier()
                chk('adaln')
                with ExitStack() as stS:
                    qT = sb(stS, "qT", [128, 4, SEQ], BF16)
                    kTz = sb(stS, "kTz", [128, 2, 2, SEQ], BF16)
                    qiT = sb(stS, "qiT", [128, 4, SEQ], BF16)
                    kiTz = sb(stS, "kiTz", [128, 2, SEQ], BF16)
                    Vp = sb(stS, "Vp", [128, 16, 2, 65], BF16)
                    wi = sb(stS, "wi", [128, 16, 8], F32)
                    mixT_s = sb(stS, "mixT_s", [128, 4, SEQ], BF16)
                    qT_b, kT_b, qiT_b, kiT_b, Vp_b, wi_b, mixT_sb = (Buf() for _ in range(7))
                    S.op("pool", lambda e: e.memset(kTz[:].rearrange("p a b c -> p (a b c)"), 0.0), writes=[kT_b])
                    S.op("pool", lambda e: e.memset(kiTz[:].rearrange("p a c -> p (a c)"), 0.0), writes=[kiT_b])
                    S.op("pool", lambda e: e.memset(Vp[:].rearrange("p a b c -> p (a b c)"), 1.0), writes=[Vp_b])

                    with ExitStack() as st12:
                        U8 = sb(st12, "U8", [128, 2, 32, 8, 16], BF16)
                        U8_b = Buf()
                        with ExitStack() as st1:
                            w_in = sb(st1, "w_in", [128, 8, DIN], BF16)
                            w_in_b = Buf()
                            for kt in range(8):
                                S.dma("pool", w_in[:, kt, :], win_d[:, kt, :], writes=[w_in_b])
                            hT = sb(st1, "hT", [128, 8, SEQ], BF16)
                            hT_b = Buf()
                            st1a = ExitStack()
                            xt = [sb(st1a, "xt%d" % i, [128, D], F32) for i in range(2)]
                            xt_b = [Buf(), Buf()]
                            junk = sb(st1a, "junk", [128, D], BF16)
                            junk_b = Buf()
                            t1 = sb(st1a, "t1", [128, D], F32)
                            hb = [sb(st1a, "hb%d" % i, [128, D], BF16) for i in range(2)]
                            hb_b = [Buf(), Buf()]
                            st_ = [sb(st1a, "st%d" % i, [128, 4], F32) for i in range(2)]
                            st_b = [Buf(), Buf()]
                            t1_b = Buf()
                            for t in range(16):
                                b = t % 2
                                S.dma("sp", xt[b][:], x_d[r0 + t * 128: r0 + (t + 1) * 128, :], writes=[xt_b[b]])
                                S.op("act", lambda e, b=b: e.activation(out=junk[:], in_=xt[b][:], func=AF.Square,
                                                                        accum_out=st_[b][:, 0:1]),
                                     reads=[xt_b[b]], writes=[junk_b, st_b[b]])
                                S.op("act", lambda e, b=b: e.activation(out=st_[b][:, 1:2], in_=st_[b][:, 0:1], func=AF.Sqrt,
                                                                        bias=EPS, scale=1.0 / D),
                                     reads=[st_b[b]], writes=[st_b[b]])
                                S.op("dve", lambda e, b=b: e.reciprocal(out=st_[b][:, 2:3], in_=st_[b][:, 1:2]),
                                     reads=[st_b[b]], writes=[st_b[b]])
                                S.op("dve", lambda e, b=b: e.scalar_tensor_tensor(
                                    out=t1[:], in0=xt[b][:], scalar=st_[b][:, 2:3], in1=tabsA[s][:, 1, :],
                                    op0=ALU.mult, op1=ALU.mult), reads=[xt_b[b], st_b[b], tabsA_b[s]], writes=[t1_b])
                                S.op("dve", lambda e, b=b: e.tensor_tensor(out=hb[b][:], in0=t1[:], in1=tabsA[s][:, 0, :], op=ALU.add),
                                     reads=[t1_b, tabsA_b[s]], writes=[hb_b[b]])
                                ptt, ptb_ = next_pt()
                                for kt in range(8):
                                    S.op("pe", lambda e, ptt=ptt, b=b, kt=kt: e.transpose(
                                        out=ptt[:, kt * 128:(kt + 1) * 128], in_=hb[b][:, kt * 128:(kt + 1) * 128],
                                        identity=ident_bf[:]), reads=[hb_b[b], cst], writes=[ptb_])
                                S.op("act", lambda e, ptt=ptt, t=t: e.copy(
                                    out=hT[:, :, t * 128:(t + 1) * 128], in_=ptt[:].rearrange("p (a b) -> p a b", b=128)),
                                    reads=[ptb_], writes=[hT_b])
                            S.barrier()
                            st1a.close()
                            sq = [sb(st1, "sq%d" % i, [128, 512], BF16) for i in range(2)]
                            sq_b = [Buf(), Buf()]
                            sd = [sb(st1, "sd%d" % i, [128, 512], F32) for i in range(2)]
                            sd_b = [Buf(), Buf()]
                            groups = []
                            for j in range(4):
                                groups.append(("q", j, [(512 + j * 128, 128, 0)]))
                            groups.append(("kA", 0, [(1024, 128, 0)]))
                            groups.append(("kB", 0, [(1088, 64, 0), (1024, 64, 64)]))
                            for j in range(4):
                                groups.append(("qi", j, [(1280 + j * 128, 128, 0)]))
                            groups.append(("ki", 0, [(1792, 64, 0), (1792, 64, 64)]))
                            it = 0
                            for kind, j, parts in groups:
                                for c in range(4):
                                    cs_ = slice(c * 512, (c + 1) * 512)
                                    p, pb = next_pw()
                                    for (c0, m, po) in parts:
                                        for kt in range(8):
                                            S.op("pe", lambda e, p=p, c0=c0, m=m, po=po, kt=kt, cs_=cs_: e.matmul(
                                                p[po:po + m, :], w_in[:, kt, c0:c0 + m], hT[:, kt, cs_],
                                                start=(kt == 0), stop=(kt == 7)),
                                                reads=[w_in_b, hT_b], writes=[pb])
                                    if kind == "qi":
                                        S.op("act", lambda e, p=p, j=j, cs_=cs_: e.copy(out=qiT[:, j, cs_], in_=p[:]),
                                             reads=[pb], writes=[qiT_b])
                                    elif kind == "ki":
                                        for half in range(2):
                                            rs = slice(half * 64, half * 64 + 64)
                                            S.op("act", lambda e, p=p, half=half, rs=rs, cs_=cs_: e.copy(
                                                out=kiTz[rs, half, cs_], in_=p[rs, :]), reads=[pb], writes=[kiT_b])
                                    else:
                                        b = it % 2
                                        it += 1
                                        S.op("act", lambda e, p=p, b=b: e.activation(out=sq[b][:], in_=p[:], func=AF.Square),
                                             reads=[pb], writes=[sq_b[b]])
                                        p2, p2b = next_pw()
                                        S.op("pe", lambda e, p2=p2, b=b: e.matmul(p2[:], onesblk[:], sq[b][:], start=True, stop=True),
                                             reads=[sq_b[b], cst], writes=[p2b])
                                        S.op("act", lambda e, p2=p2, b=b: e.activation(out=sd[b][:], in_=p2[:], func=AF.Sqrt,
                                                                                      bias=EPS, scale=1.0 / 64),
                                             reads=[p2b], writes=[sd_b[b]])
                                        S.op("dve", lambda e, b=b: e.reciprocal(out=sd[b][:], in_=sd[b][:]),
                                             reads=[sd_b[b]], writes=[sd_b[b]])
                                        if kind == "q":
                                            S.op("dve", lambda e, p=p, b=b, j=j, cs_=cs_: e.scalar_tensor_tensor(
                                                out=qT[:, j, cs_], in0=p[:], scalar=G2[:, 0:1], in1=sd[b][:],
                                                op0=ALU.mult, op1=ALU.mult), reads=[pb, sd_b[b], cst], writes=[qT_b])
                                        else:
                                            kvs = (0, 1) if kind == "kA" else (1, 0)
                                            for half in range(2):
                                                rs = slice(half * 64, half * 64 + 64)
                                                kv = kvs[half]
                                                S.op("dve", lambda e, p=p, b=b, rs=rs, kv=kv, half=half, cs_=cs_: e.tensor_tensor(
                                                    out=kTz[rs, kv, half, cs_], in0=p[rs, :], in1=sd[b][rs, :], op=ALU.mult),
                                                    reads=[pb, sd_b[b]], writes=[kT_b])
                            for t in range(16):
                                ts_ = slice(t * 128, (t + 1) * 128)
                                p, pb = next_pw()
                                for kt in range(8):
                                    S.op("pe", lambda e, p=p, kt=kt, ts_=ts_: e.matmul(
                                        p[:, 0:128], hT[:, kt, ts_], w_in[:, kt, 1152:1280], start=(kt == 0), stop=(kt == 7)),
                                        reads=[w_in_b, hT_b], writes=[pb])
                                for kt in range(8):
                                    S.op("pe", lambda e, p=p, kt=kt, ts_=ts_: e.matmul(
                                        p[:, 128:136], hT[:, kt, ts_], w_in[:, kt, 1856:1864], start=(kt == 0), stop=(kt == 7)),
                                        reads=[w_in_b, hT_b], writes=[pb])
                                S.op("act", lambda e, p=p, t=t: e.copy(
                                    out=Vp[:, t, :, 0:64], in_=p[:, 0:128].rearrange("p (a b) -> p a b", b=64)),
                                    reads=[pb], writes=[Vp_b])
                                S.op("act", lambda e, p=p, t=t: e.mul(out=wi[:, t, :], in_=p[:, 128:136], mul=IDX_SCALE),
                                     reads=[pb], writes=[wi_b])
                            for sp in range(2):
                                for i in range(8):
                                    p, pb = next_pw()
                                    for kt in range(8):
                                        lhs = hT[:, kt, sp * 1024:(sp + 1) * 1024].rearrange("p (b i) -> p i b", i=8)[:, i, :]
                                        S.op("pe", lambda e, p=p, lhs=lhs, kt=kt: e.matmul(
                                            p[:], lhs, w_in[:, kt, 0:512], start=(kt == 0), stop=(kt == 7)),
                                            reads=[w_in_b, hT_b], writes=[pb])
                                    S.op("act", lambda e, p=p, sp=sp, i=i: e.copy(
                                        out=U8[:, sp, :, i, :], in_=p[:].rearrange("p (g c) -> p g c", c=16)),
                                         reads=[pb], writes=[U8_b])
                            S.barrier()

                        chk('s1')
                        with ExitStack() as st2:
                            w_glu = sb(st2, "w_glu", [128, 4, 512], BF16)
                            w_glu_b = Buf()
                            S.dma("pool", w_glu[:], wglu_d, writes=[w_glu_b])
                            Ytok = sb(st2, "Ytok", [128, 2, 8, 512], F32)
                            Ytok_b = Buf()
                            U8T = [sb(st2, "U8T%d" % i, [128, 2, 256], BF16) for i in range(2)]
                            U8T_b = [Buf(), Buf()]
                            XA = sb(st2, "XA", [128, 2, 384], F32)
                            XB = sb(st2, "XB", [128, 2, 384], F32)
                            XA_b, XB_b = Buf(), Buf()
                            TM = sb(st2, "TM", [128, 2, 256], F32)
                            TM_b = Buf()
                            Xst = [sb(st2, "Xst%d" % i, [128, 2, 258], BF16) for i in range(2)]
                            Xst_b = [Buf(), Buf()]
                            Ysb = [sb(st2, "Ysb%d" % i, [128, 256], F32) for i in range(2)]
                            Ysb_b = [Buf(), Buf()]
                            S.op("pool", lambda e: e.memset(XA[:].rearrange("p a b -> p (a b)"), 0.0), writes=[XA_b])
                            S.op("pool", lambda e: e.memset(XB[:].rearrange("p a b -> p (a b)"), 0.0), writes=[XB_b])
                            for i in range(2):
                                S.op("pool", lambda e, i=i: e.memset(Xst[i][:].rearrange("p a b -> p (a b)"), 0.0), writes=[Xst_b[i]])
                            PAD = 128
                            A2 = [sb(st2, "A2_%d" % i, [128, 2, 128], BF16) for i in range(2)]
                            B2 = [sb(st2, "B2_%d" % i, [128, 2, 128], BF16) for i in range(2)]
                            C2 = [sb(st2, "C2_%d" % i, [128, 2, 2, 128], BF16) for i in range(2)]
                            M2_b = [Buf(), Buf()]
                            for pr in range(16):
                                ub = pr % 2
                                S.dma("sp", A2[ub][:], ssmA_d[:, 2 * pr:2 * pr + 2, :], reads=[ssmd_b], writes=[M2_b[ub]])
                                S.dma("sp", B2[ub][:], ssmB_d[:, 2 * pr:2 * pr + 2, :], reads=[ssmd_b], writes=[M2_b[ub]])
                                S.dma("sp", C2[ub][:], ssmC_d[:, 2 * pr:2 * pr + 2, :, :], reads=[ssmd_b], writes=[M2_b[ub]])
                                ptt, ptb_ = next_pt()
                                for gp in range(2):
                                    g = 2 * pr + gp
                                    for sp in range(2):
                                        S.op("pe", lambda e, ptt=ptt, gp=gp, sp=sp, g=g: e.transpose(
                                            out=ptt[:, (gp * 2 + sp) * 128:(gp * 2 + sp + 1) * 128],
                                            in_=U8[:, sp, g, :, :].rearrange("p a b -> p (a b)"), identity=ident_bf[:]),
                                            reads=[U8_b, cst], writes=[ptb_])
                                S.op("act", lambda e, ptt=ptt, ub=ub: e.copy(
                                    out=U8T[ub][:].rearrange("p a b -> p (a b)"), in_=ptt[:, 0:512]),
                                    reads=[ptb_], writes=[U8T_b[ub]])
                                p, pb = next_pw()
                                for gp in range(2):
                                    g = 2 * pr + gp
                                    for ri in range(2):
                                        S.op("pe", lambda e, p=p, gp=gp, g=g, ri=ri, ub=ub: e.matmul(
                                            p[gp * 64:(gp + 1) * 64, ri * 256:(ri + 1) * 256],
                                            B2[ub][:, gp, ri * 64:(ri + 1) * 64], U8T[ub][:, gp, :], start=True, stop=True),
                                            reads=[M2_b[ub], U8T_b[ub]], writes=[pb])
                                S.op("act", lambda e, p=p: e.copy(out=XA[:, :, PAD:PAD + 256],
                                                                  in_=p[:].rearrange("p (a b) -> p a b", b=256)),
                                     reads=[pb], writes=[XA_b])
                                cur, curb, nxt, nxtb = XA, XA_b, XB, XB_b
                                xs = Xst[ub]
                                for k in range(8):
                                    sft = 2 ** k
                                    last = (k == 7)
                                    a_ = ak[:, pr, k:k + 1]
                                    c_ = ck[:, pr, k:k + 1]
                                    nc_ = nck[:, pr, k:k + 1]
                                    sh = slice(PAD - sft, PAD - sft + 256)
                                    ce = slice(PAD, PAD + 256)
                                    outr = xs[:, 0, 1:257] if last else nxt[:, 0, ce]
                                    outi = xs[:, 1, 1:257] if last else nxt[:, 1, ce]
                                    ob = Xst_b[ub] if last else nxtb
                                    S.op("dve", lambda e, cur=cur, a_=a_, sh=sh, ce=ce: e.scalar_tensor_tensor(
                                        out=TM[:, 0, :], in0=cur[:, 0, sh], scalar=a_, in1=cur[:, 0, ce], op0=ALU.mult, op1=ALU.add),
                                        reads=[curb, ssm_b], writes=[TM_b])
                                    S.op("dve", lambda e, cur=cur, nc_=nc_, sh=sh, outr=outr: e.scalar_tensor_tensor(
                                        out=outr, in0=cur[:, 1, sh], scalar=nc_, in1=TM[:, 0, :], op0=ALU.mult, op1=ALU.add),
                                        reads=[curb, TM_b, ssm_b], writes=[ob])
                                    S.op("dve", lambda e, cur=cur, a_=a_, sh=sh, ce=ce: e.scalar_tensor_tensor(
                                        out=TM[:, 1, :], in0=cur[:, 1, sh], scalar=a_, in1=cur[:, 1, ce], op0=ALU.mult, op1=ALU.add),
                                        reads=[curb, ssm_b], writes=[TM_b])
                                    S.op("dve", lambda e, cur=cur, c_=c_, sh=sh, outi=outi: e.scalar_tensor_tensor(
                                        out=outi, in0=cur[:, 0, sh], scalar=c_, in1=TM[:, 1, :], op0=ALU.mult, op1=ALU.add),
                                        reads=[curb, TM_b, ssm_b], writes=[ob])
                                    cur, curb, nxt, nxtb = nxt, nxtb, cur, curb
                                for gp in range(2):
                                    g = 2 * pr + gp
                                    yb = g % 2
                                    p, pb = next_pw()
                                    S.op("pe", lambda e, p=p, g=g, gp=gp, ub=ub: e.matmul(
                                        p[:, 0:256], A2[ub][:, gp, :], U8T[ub][:, gp, :], start=True, stop=False),
                                        reads=[M2_b[ub], U8T_b[ub]], writes=[pb])
                                    for ri in range(2):
                                        S.op("pe", lambda e, p=p, g=g, ri=ri, xs=xs: e.matmul(
                                            p[:, 0:256], C2[ub][:, gp, ri, :], xs[:, ri, 0:256], start=False, stop=(ri == 1)),
                                            reads=[M2_b[ub], Xst_b[ub]], writes=[pb])
                                    S.op("act", lambda e, p=p, yb=yb: e.copy(out=Ysb[yb][:], in_=p[:, 0:256]),
                                         reads=[pb], writes=[Ysb_b[yb]])
                                    p2, p2b = next_pw()
                                    for sp in range(2):
                                        S.op("pe", lambda e, p2=p2, sp=sp, yb=yb: e.transpose(
                                            out=p2[:, sp * 128:(sp + 1) * 128], in_=Ysb[yb][:, sp * 128:(sp + 1) * 128],
                                            identity=ident_f[:]), reads=[Ysb_b[yb], cst], writes=[p2b])
                                    for sp in range(2):
                                        S.op("act", lambda e, p2=p2, sp=sp, g=g: e.copy(
                                            out=Ytok[:, sp, :, g * 16:(g + 1) * 16],
                                            in_=p2[:, sp * 128:(sp + 1) * 128].rearrange("p (a b) -> p a b", b=16)),
                                            reads=[p2b], writes=[Ytok_b])
                            g1 = [sb(st2, "g1_%d" % i, [128, 512], F32) for i in range(2)]
                            g2_ = [sb(st2, "g2_%d" % i, [128, 512], F32) for i in range(2)]
                            zf = [sb(st2, "zf%d" % i, [128, 512], F32) for i in range(2)]
                            zb = [sb(st2, "zb%d" % i, [128, 512], BF16) for i in range(2)]
                            zT = [sb(st2, "zT%d" % i, [128, 4, 128], BF16) for i in range(2)]
                            sg = [sb(st2, "sg%d" % i, [128, 512], F32) for i in range(2)]
                            mb_ = [sb(st2, "mb%d" % i, [128, 512], BF16) for i in range(2)]
                            sst = [sb(st2, "sst%d" % i, [128, 4], F32) for i in range(2)]
                            gb = [[Buf() for _ in range(8)] for _ in range(2)]
                            KG = 2.0 * math.sqrt(2.0 / math.pi)
                            for sp in range(2):
                                for i in range(8):
                                    b = i % 2
                                    B = gb[b]
                                    y = Ytok[:, sp, i, :]
                                    S.op("act", lambda e, b=b, y=y: e.activation(out=g1[b][:], in_=y, func=AF.Square),
                                         reads=[Ytok_b], writes=[B[0]])
                                    S.op("dve", lambda e, b=b: e.tensor_scalar(out=g1[b][:], in0=g1[b][:], scalar1=0.044715,
                                                                               scalar2=1.0, op0=ALU.mult, op1=ALU.add),
                                         reads=[B[0]], writes=[B[0]])
                                    S.op("dve", lambda e, b=b, y=y: e.tensor_tensor(out=g2_[b][:], in0=g1[b][:], in1=y, op=ALU.mult),
                                         reads=[B[0], Ytok_b], writes=[B[1]])
                                    S.op("act", lambda e, b=b: e.activation(out=g2_[b][:], in_=g2_[b][:], func=AF.Sigmoid, scale=KG),
                                         reads=[B[1]], writes=[B[1]])
                                    S.op("dve", lambda e, b=b, y=y: e.tensor_tensor(out=zf[b][:], in0=g2_[b][:], in1=y, op=ALU.mult),
                                         reads=[B[1], Ytok_b], writes=[B[2]])
                                    S.op("act", lambda e, b=b: e.copy(out=zb[b][:], in_=zf[b][:]), reads=[B[2]], writes=[B[3]])
                                    ptt, ptb_ = next_pt()
                                    for ft in range(4):
                                        S.op("pe", lambda e, ptt=ptt, b=b, ft=ft: e.transpose(
                                            out=ptt[:, ft * 128:(ft + 1) * 128], in_=zb[b][:, ft * 128:(ft + 1) * 128],
                                            identity=ident_bf[:]), reads=[B[3], cst], writes=[ptb_])
                                    S.op("act", lambda e, ptt=ptt, b=b: e.copy(out=zT[b][:].rearrange("p a b -> p (a b)"),
                                                                               in_=ptt[:, 0:512]), reads=[ptb_], writes=[B[4]])
                                    p, pb = next_pw()
                                    for ft in range(4):
                                        S.op("pe", lambda e, p=p, b=b, ft=ft: e.matmul(
                                            p[:], zT[b][:, ft, :], w_glu[:, ft, :], start=(ft == 0), stop=(ft == 3)),
                                            reads=[B[4], w_glu_b], writes=[pb])
                                    S.op("dve", lambda e, p=p, b=b: e.tensor_tensor(out=sg[b][:], in0=p[:], in1=bglu_b[:], op=ALU.add),
                                         reads=[pb, cst], writes=[B[5]])
                                    S.op("act", lambda e, b=b: e.activation(out=sg[b][:], in_=sg[b][:], func=AF.Sigmoid),
                                         reads=[B[5]], writes=[B[5]])
                                    S.op("dve", lambda e, b=b: e.tensor_tensor(out=sg[b][:], in0=sg[b][:], in1=zf[b][:], op=ALU.mult),
                                         reads=[B[5], B[2]], writes=[B[5]])
                                    S.op("act", lambda e, b=b: e.activation(out=g1[b][:], in_=sg[b][:], func=AF.Square,
                                                                            accum_out=sst[b][:, 0:1]),
                                         reads=[B[5], B[0]], writes=[B[0], B[6]])
                                    S.op("act", lambda e, b=b: e.activation(out=sst[b][:, 1:2], in_=sst[b][:, 0:1], func=AF.Sqrt,
                                                                            bias=EPS, scale=1.0 / 512), reads=[B[6]], writes=[B[6]])
                                    S.op("dve", lambda e, b=b: e.reciprocal(out=sst[b][:, 2:3], in_=sst[b][:, 1:2]),
                                         reads=[B[6]], writes=[B[6]])
                                    S.op("dve", lambda e, b=b: e.scalar_tensor_tensor(
                                        out=mb_[b][:], in0=sg[b][:], scalar=sst[b][:, 2:3], in1=gns_b[:], op0=ALU.mult, op1=ALU.mult),
                                        reads=[B[5], B[6], cst], writes=[B[7]])
                                    ptt, ptb_ = next_pt()
                                    for ft in range(4):
                                        S.op("pe", lambda e, ptt=ptt, b=b, ft=ft: e.transpose(
                                            out=ptt[:, ft * 128:(ft + 1) * 128], in_=mb_[b][:, ft * 128:(ft + 1) * 128],
                                            identity=ident_bf[:]), reads=[B[7], cst], writes=[ptb_])
                                    dst = mixT_s[:, :, sp * 1024:(sp + 1) * 1024].rearrange("p a (b i) -> p a i b", i=8)[:, :, i, :]
                                    S.op("act", lambda e, ptt=ptt, dst=dst: e.copy(
                                        out=dst, in_=ptt[:, 0:512].rearrange("p (a b) -> p a b", b=128)),
                                        reads=[ptb_], writes=[mixT_sb])
                            S.barrier()

                    chk('s2')
                    with ExitStack() as st3:
                        w_out = sb(st3, "w_out", [128, 8, D], BF16)
                        w_out_b = Buf()
                        S.dma("pool", w_out[:], wout_d, writes=[w_out_b])
                        score = [sb(st3, "score%d" % i, [128, SEQ], F32) for i in range(2)]
                        score_b = [Buf(), Buf()]
                        nm = [sb(st3, "nm%d" % i, [128, SEQ], BF16) for i in range(3)]
                        nm_b = [Buf() for _ in range(3)]
                        rbuf = [sb(st3, "rbuf%d" % i, [128, 8, 512], BF16) for i in range(2)]
                        rbuf_b = [Buf(), Buf()]
                        dg = [sb(st3, "dg%d" % i, [128, 8, 128], BF16) for i in range(2)]
                        dg_b = [Buf(), Buf()]
                        bs = [sb(st3, "bs%d" % i, [128, 16 + 2 * NITER], F32) for i in range(2)]
                        bs_b = [Buf(), Buf()]
                        PT = [sb(st3, "PT%d" % i, [128, 512], BF16) for i in range(3)]
                        PT_b = [Buf() for _ in range(3)]
                        yatt = sb(st3, "yatt", [128, 512], F32)
                        yatt_b = Buf()
                        rden = sb(st3, "rden", [128, 8], F32)
                        ajunk = sb(st3, "ajunk", [128, 512], BF16)
                        ast = sb(st3, "ast", [128, 4], F32)
                        mixa = sb(st3, "mixa", [128, 512], BF16)
                        mixa_b = Buf()
                        mixTa = sb(st3, "mixTa", [128, 4, 128], BF16)
                        mixTa_b = Buf()
                        xr = [sb(st3, "xr%d" % i, [128, D], F32) for i in range(2)]
                        xr_b = [Buf(), Buf()]
                        x1t = [sb(st3, "x1t%d" % i, [128, 512], F32) for i in range(2)]
                        x1t_b = [Buf(), Buf()]
                        prot = {"I": 0, "A": 0, "pt": 0}

                        def pwI():
                            i = prot["I"] % 2
                            prot["I"] += 1
                            return pf[i], pfb[i]

                        def pwA():
                            i = 2 + prot["A"] % 2
                            prot["A"] += 1
                            return pf[i], pfb[i]

                        def gen_I(qt):
                            sl = qt % 2
                            nsl = qt % 3
                            nk = qt + 1
                            SK = nk * 128
                            qs = slice(qt * 128, (qt + 1) * 128)
                            sc, scb = score[sl], score_b[sl]
                            BS, BSb = bs[sl], bs_b[sl]
                            S.op("dve", lambda e: e.tensor_tensor(
                                out=dg[sl][:], in0=ident_bf[:].unsqueeze(1).to_broadcast([128, 8, 128]),
                                in1=wi[:, qt, :].unsqueeze(2).to_broadcast([128, 8, 128]), op=ALU.mult),
                                reads=[cst, wi_b], writes=[dg_b[sl]])
                            nch = (SK + 511) // 512
                            for c in range(nch):
                                cw = min(512, SK - c * 512)
                                ks = slice(c * 512, c * 512 + cw)
                                for h in range(8):
                                    j, half = h // 2, h % 2
                                    p, pb = pwI()
                                    S.op("pe", lambda e, p=p, j=j, half=half: e.matmul(
                                        p[:, 0:cw], qiT[:, j, qs], kiTz[:, half, ks], start=True, stop=True),
                                        reads=[qiT_b, kiT_b], writes=[pb])
                                    if h % 2 == 0:
                                        S.op("act", lambda e, p=p, h=h: e.activation(
                                            out=rbuf[sl][:, h, 0:cw], in_=p[:, 0:cw], func=AF.Relu),
                                            reads=[pb], writes=[rbuf_b[sl]])
                                    else:
                                        S.op("dve", lambda e, p=p, h=h: e.tensor_scalar(
                                            out=rbuf[sl][:, h, 0:cw], in0=p[:, 0:cw], scalar1=0.0, scalar2=None, op0=ALU.max),
                                            reads=[pb], writes=[rbuf_b[sl]])
                                p, pb = pwI()
                                for h in range(8):
                                    S.op("pe", lambda e, p=p, h=h: e.matmul(
                                        p[:, 0:cw], dg[sl][:, h, :], rbuf[sl][:, h, 0:cw], start=(h == 0), stop=(h == 7)),
                                        reads=[dg_b[sl], rbuf_b[sl]], writes=[pb])
                                S.op("act", lambda e, p=p: e.copy(out=sc[:, ks], in_=p[:, 0:cw]),
                                     reads=[pb], writes=[scb])
                                S.op("dve", lambda e, c=c: e.tensor_reduce(
                                    out=BS[:, c:c + 1], in_=sc[:, ks], axis=AX.X, op=ALU.max, apply_absolute_value=True),
                                    reads=[scb], writes=[BSb])
                                yield
                            S.op("dve", lambda e: e.tensor_tensor(out=sc[:, qs], in0=sc[:, qs], in1=causal[:], op=ALU.add),
                                 reads=[scb, cst], writes=[scb])
                            if qt >= 2:
                                S.op("dve", lambda e: e.tensor_reduce(
                                    out=BS[:, 4:5], in_=BS[:, 0:nch], axis=AX.X, op=ALU.max), reads=[BSb], writes=[BSb])
                                S.op("dve", lambda e: e.tensor_scalar(
                                    out=BS[:, 8:8 + NITER + 1], in0=pow2[:, 0:NITER + 1], scalar1=BS[:, 4:5], scalar2=None,
                                    op0=ALU.mult), reads=[BSb, cst], writes=[BSb])
                                S.op("dve", lambda e: e.memset(BS[:, 5:6], 0.0), reads=[BSb], writes=[BSb])
                                thr = float(2 * TOPK - SK) - 0.5
                                for n in range(NITER):
                                    S.op("act", lambda e: e.activation(
                                        out=nm[nsl][:, 0:SK], in_=sc[:, 0:SK], func=AF.Sign, bias=BS[:, 5:6], scale=1.0,
                                        accum_out=BS[:, 6:7]), reads=[scb, BSb], writes=[nm_b[nsl], BSb])
                                    S.op("dve", lambda e, n=n: e.tensor_scalar(
                                        out=BS[:, 7:8], in0=BS[:, 6:7], scalar1=thr, scalar2=BS[:, 8 + n:9 + n],
                                        op0=ALU.is_lt, op1=ALU.mult), reads=[BSb], writes=[BSb])
                                    S.op("dve", lambda e, n=n: e.scalar_tensor_tensor(
                                        out=BS[:, 5:6], in0=BS[:, 7:8], scalar=BS[:, 9 + n:10 + n], in1=BS[:, 5:6],
                                        op0=ALU.subtract, op1=ALU.add), reads=[BSb], writes=[BSb])
                                    yield
                                S.op("dve", lambda e: e.tensor_tensor(
                                    out=BS[:, 5:6], in0=BS[:, 5:6], in1=BS[:, 8 + NITER:9 + NITER], op=ALU.add),
                                    reads=[BSb], writes=[BSb])
                                ntau = BS[:, 5:6]
                            else:
                                ntau = taufix[:, 0:1]
                            S.op("dve", lambda e: e.tensor_scalar(
                                out=nm[nsl][:, 0:SK], in0=sc[:, 0:SK], scalar1=ntau, scalar2=0.0, op0=ALU.add, op1=ALU.is_lt),
                                reads=[scb, BSb, cst], writes=[nm_b[nsl]])
                            yield

                        def gen_A(qt):
                            nsl = qt % 3
                            nk = qt + 1
                            qs = slice(qt * 128, (qt + 1) * 128)
                            O = [pf[4], pf[5]]
                            Ob = [pfb[4], pfb[5]]
                            for kv in range(2):
                                S.op("pe", lambda e, kv=kv: e.matmul(O[kv][:, 0:260], zeros_bf[:, 0:128], zeros_bf[:, 0:260],
                                                                     start=True, stop=True), reads=[cst], writes=[Ob[kv]])
                            for kt in range(nk):
                                ksl = slice(kt * 128, (kt + 1) * 128)
                                for kv in range(2):
                                    p, pb = pwA()
                                    S.op("pe", lambda e, p=p: e.matmul(
                                        p[:], nm[nsl][:, ksl], negi4[:], start=True, stop=True),
                                        reads=[nm_b[nsl], cst], writes=[pb])
                                    for hh in range(4):
                                        head = kv * 4 + hh
                                        j, half = head // 2, head % 2
                                        S.op("pe", lambda e, p=p, hh=hh, kv=kv, half=half, j=j: e.matmul(
                                            p[:, hh * 128:(hh + 1) * 128], kTz[:, kv, half, ksl], qT[:, j, qs],
                                            start=False, stop=True, skip_group_check=True),
                                            reads=[kT_b, qT_b], writes=[pb])
                                    pi3 = prot["pt"] % 3
                                    prot["pt"] += 1
                                    S.op("act", lambda e, p=p, pi3=pi3: e.activation(
                                        out=PT[pi3][:], in_=p[:], func=AF.Exp, bias=negM[:, 0:1], scale=1.0),
                                        reads=[pb, cst], writes=[PT_b[pi3]])
                                    for hh in range(4):
                                        S.op("pe", lambda e, kv=kv, hh=hh, pi3=pi3, kt=kt: e.matmul(
                                            O[kv][:, hh * 65:(hh + 1) * 65], PT[pi3][:, hh * 128:(hh + 1) * 128], Vp[:, kt, kv, :],
                                            start=False, stop=True, skip_group_check=True),
                                            reads=[PT_b[pi3], Vp_b], writes=[Ob[kv]])
                                    yield
                            for kv in range(2):
                                ov = O[kv][:, 0:260].rearrange("p (h e) -> p h e", e=65)
                                S.op("dve", lambda e, kv=kv, ov=ov: e.reciprocal(out=rden[:, kv * 4:(kv + 1) * 4], in_=ov[:, :, 64]),
                                     reads=[Ob[kv]], writes=[yatt_b])
                                S.op("dve", lambda e, kv=kv, ov=ov: e.tensor_tensor(
                                    out=yatt[:, kv * 256:(kv + 1) * 256].rearrange("p (h d) -> p h d", d=64), in0=ov[:, :, 0:64],
                                    in1=rden[:, kv * 4:(kv + 1) * 4].unsqueeze(2).to_broadcast([128, 4, 64]), op=ALU.mult),
                                    reads=[Ob[kv], yatt_b], writes=[yatt_b])
                            S.op("act", lambda e: e.activation(out=ajunk[:], in_=yatt[:], func=AF.Square, accum_out=ast[:, 0:1]),
                                 reads=[yatt_b], writes=[yatt_b])
                            S.op("act", lambda e: e.activation(out=ast[:, 1:2], in_=ast[:, 0:1], func=AF.Sqrt, bias=EPS, scale=1.0 / 512),
                                 reads=[yatt_b], writes=[yatt_b])
                            S.op("dve", lambda e: e.reciprocal(out=ast[:, 2:3], in_=ast[:, 1:2]), reads=[yatt_b], writes=[yatt_b])
                            S.op("dve", lambda e: e.scalar_tensor_tensor(out=mixa[:], in0=yatt[:], scalar=ast[:, 2:3], in1=gna_b[:],
                                                                          op0=ALU.mult, op1=ALU.mult),
                                 reads=[yatt_b, cst], writes=[mixa_b])
                            ptt, ptb_ = next_pt()
                            for ft in range(4):
                                S.op("pe", lambda e, ptt=ptt, ft=ft: e.transpose(
                                    out=ptt[:, ft * 128:(ft + 1) * 128], in_=mixa[:, ft * 128:(ft + 1) * 128], identity=ident_bf[:]),
                                    reads=[mixa_b, cst], writes=[ptb_])
                            S.op("act", lambda e, ptt=ptt: e.copy(out=mixTa[:].rearrange("p a b -> p (a b)"), in_=ptt[:, 0:512]),
                                 reads=[ptb_], writes=[mixTa_b])
                            yield
                            xb = qt % 2
                            rows = slice(r0 + qt * 128, r0 + (qt + 1) * 128)
                            S.dma("sp", xr[xb][:], x_d[rows, :], writes=[xr_b[xb]])
                            for hf in range(2):
                                p, pb = pwA()
                                for k8 in range(8):
                                    lhs = mixT_s[:, k8, qs] if k8 < 4 else mixTa[:, k8 - 4, :]
                                    S.op("pe", lambda e, p=p, lhs=lhs, k8=k8, hf=hf: e.matmul(
                                        p[:], lhs, w_out[:, k8, hf * 512:(hf + 1) * 512], start=(k8 == 0), stop=(k8 == 7)),
                                        reads=[mixT_sb, mixTa_b, w_out_b], writes=[pb])
                                hs = slice(hf * 512, (hf + 1) * 512)
                                S.op("dve", lambda e, p=p, hf=hf, hs=hs: e.tensor_tensor(
                                    out=x1t[hf][:], in0=p[:], in1=tabsA[s][:, 2, hs], op=ALU.mult),
                                    reads=[pb, tabsA_b[s]], writes=[x1t_b[hf]])
                                S.op("dve", lambda e, xb=xb, hf=hf, hs=hs: e.tensor_tensor(
                                    out=xr[xb][:, hs], in0=x1t[hf][:], in1=xr[xb][:, hs], op=ALU.add),
                                    reads=[x1t_b[hf], xr_b[xb]], writes=[xr_b[xb]])
                            S.dma("sp", out_d[rows, :], xr[xb][:], reads=[xr_b[xb]])
                            yield

                        NQ = 16
                        actI = []
                        actA = None
                        nextI, nextA = 0, 0
                        doneI = set()
                        while nextA < NQ or actA is not None:
                            if actA is None and nextA in doneI:
                                actA = (nextA, gen_A(nextA))
                                nextA += 1
                            firstA = actA[0] if actA is not None else nextA
                            while (nextI < NQ and len(actI) < 2 and nextI - 3 < firstA
                                   and (nextI < 2 or (nextI - 2) in doneI)):
                                actI.append((nextI, gen_I(nextI)))
                                nextI += 1
                            for item in list(actI):
                                try:
                                    next(item[1])
                                except StopIteration:
                                    actI.remove(item)
                                    doneI.add(item[0])
                            if actA is not None:
                                try:
                                    next(actA[1])
                                except StopIteration:
                                    actA = None
                        S.barrier()

        chk('s3')
        NB = 256
        NTT = NB // 128
        with ExitStack() as stB:
            tabB_d = nc.dram_tensor("tabB_scr", [NSEQ, 128, 3 * D], F32, kind="Internal").ap()
            tabBd_b = Buf()
            with ExitStack() as stt:
                tl = [sb(stt, "tabsBt%d" % s, [128, 3, D], F32) for s in range(NSEQ)]
                tlb = [Buf() for _ in range(NSEQ)]
                adaln(tl, tlb, list(range(NSEQ)), 3, n2g_d, stt)
                for s in range(NSEQ):
                    S.dma("sp", tabB_d[s], tl[s][:].rearrange("p a b -> p (a b)"), reads=[tlb[s]], writes=[tabBd_b])
                S.barrier()
            tabsB1 = sb(stB, "tabsB", [128, 3, D], F32)
            tabsB_b1 = Buf()
            W1 = sb(stB, "W1", [128, 8, DFF], BF16)
            W2 = sb(stB, "W2", [128, 32, D], BF16)
            W_b = Buf()
            for kt in range(8):
                S.dma("pool", W1[:, kt, :], wff1_d[:, kt, :], writes=[W_b])
            for ft in range(0, 32, 4):
                S.dma("pool", W2[:, ft:ft + 4, :], wff2_d[:, ft:ft + 4, :], writes=[W_b])
            hidT = sb(stB, "hidT", [128, 32, NB], BF16)
            hid_b = [Buf() for _ in range(32)]
            rl = [sb(stB, "rl%d" % i, [128, NB], BF16) for i in range(2)]
            rl_b = [Buf(), Buf()]
            h2T = sb(stB, "h2T", [128, 8, NB], BF16)
            h2T_b = Buf()
            x1 = [sb(stB, "x1_%d" % i, [128, D], F32) for i in range(2)]
            x1_b = [Buf() for _ in range(2)]
            junkB = sb(stB, "junkB", [128, D], BF16)
            junkB_b = Buf()
            tB = sb(stB, "tB", [128, D], F32)
            tB_b = Buf()
            h2 = [sb(stB, "h2_%d" % i, [128, D], BF16) for i in range(2)]
            h2_b = [Buf(), Buf()]
            stb = [sb(stB, "stb%d" % i, [128, 4], F32) for i in range(2)]
            stb_b = [Buf(), Buf()]
            ot = [sb(stB, "ot%d" % i, [128, D], F32) for i in range(2)]
            ot_b = [Buf(), Buf()]
            oc = 0
            nblk = NSEQ * SEQ // NB
            for blk in range(nblk):
                s = (blk * NB) // SEQ
                if (blk * NB) % SEQ == 0:
                    S.dma("sp", tabsB1[:].rearrange("p a b -> p (a b)"), tabB_d[s], reads=[tabBd_b], writes=[tabsB_b1])
                for tt in range(NTT):
                    rows = slice(blk * NB + tt * 128, blk * NB + (tt + 1) * 128)
                    b = tt % 2
                    S.dma("sp", x1[b][:], out_d[rows, :], writes=[x1_b[b]])
                    S.op("act", lambda e, b=b: e.activation(out=junkB[:], in_=x1[b][:], func=AF.Square,
                                                            accum_out=stb[b][:, 0:1]),
                         reads=[x1_b[b]], writes=[junkB_b, stb_b[b]])
                    S.op("act", lambda e, b=b: e.activation(out=stb[b][:, 1:2], in_=stb[b][:, 0:1], func=AF.Sqrt,
                                                            bias=EPS, scale=1.0 / D), reads=[stb_b[b]], writes=[stb_b[b]])
                    S.op("dve", lambda e, b=b: e.reciprocal(out=stb[b][:, 2:3], in_=stb[b][:, 1:2]),
                         reads=[stb_b[b]], writes=[stb_b[b]])
                    S.op("dve", lambda e, b=b: e.scalar_tensor_tensor(
                        out=tB[:], in0=x1[b][:], scalar=stb[b][:, 2:3], in1=tabsB1[:, 1, :], op0=ALU.mult, op1=ALU.mult),
                        reads=[x1_b[b], stb_b[b], tabsB_b1], writes=[tB_b])
                    S.op("dve", lambda e, b=b: e.tensor_tensor(out=h2[b][:], in0=tB[:], in1=tabsB1[:, 0, :], op=ALU.add),
                         reads=[tB_b, tabsB_b1], writes=[h2_b[b]])
                    ptt, ptb_ = next_pt()
                    for kt in range(8):
                        S.op("pe", lambda e, ptt=ptt, b=b, kt=kt: e.transpose(
                            out=ptt[:, kt * 128:(kt + 1) * 128], in_=h2[b][:, kt * 128:(kt + 1) * 128], identity=ident_bf[:]),
                            reads=[h2_b[b], cst], writes=[ptb_])
                    S.op("act", lambda e, ptt=ptt, tt=tt: e.copy(
                        out=h2T[:, :, tt * 128:(tt + 1) * 128], in_=ptt[:].rearrange("p (a b) -> p a b", b=128)),
                        reads=[ptb_], writes=[h2T_b])
                for ft in range(32):
                    p, pb = next_pw(2)
                    for kt in range(8):
                        S.op("pe", lambda e, p=p, kt=kt, ft=ft: e.matmul(
                            p[:, 0:NB], W1[:, kt, ft * 128:(ft + 1) * 128], h2T[:, kt, :], start=(kt == 0), stop=(kt == 7)),
                            reads=[W_b, h2T_b], writes=[pb])
                    b = ft % 2
                    S.op("act", lambda e, p=p, b=b: e.activation(out=rl[b][:], in_=p[:, 0:NB], func=AF.Relu),
                         reads=[pb], writes=[rl_b[b]])
                    S.op("pool", lambda e, b=b, ft=ft: e.tensor_tensor(out=hidT[:, ft, :], in0=rl[b][:], in1=rl[b][:], op=ALU.mult),
                         reads=[rl_b[b]], writes=[hid_b[ft]])
                for tt in range(NTT):
                    rows = slice(blk * NB + tt * 128, blk * NB + (tt + 1) * 128)
                    ob = oc % 2
                    oc += 1
                    S.dma("sp", ot[ob][:], out_d[rows, :], writes=[ot_b[ob]])
                    for hf in range(2):
                        bi = 2 + (2 * tt + hf) % 4
                        p, pb = pf[bi], pfb[bi]
                        for ft in range(32):
                            S.op("pe", lambda e, p=p, ft=ft, tt=tt, hf=hf: e.matmul(
                                p[:], hidT[:, ft, tt * 128:(tt + 1) * 128], W2[:, ft, hf * 512:(hf + 1) * 512],
                                start=(ft == 0), stop=(ft == 31)), reads=[hid_b[ft], W_b], writes=[pb])
                        hs = slice(hf * 512, (hf + 1) * 512)
                        S.op("dve", lambda e, p=p, hs=hs: e.tensor_tensor(
                            out=tB[:, hs], in0=p[:], in1=tabsB1[:, 2, hs], op=ALU.mult),
                            reads=[pb, tabsB_b1], writes=[tB_b])
                        S.op("dve", lambda e, ob=ob, hs=hs: e.tensor_tensor(
                            out=ot[ob][:, hs], in0=tB[:, hs], in1=ot[ob][:, hs], op=ALU.add),
                            reads=[tB_b, ot_b[ob]], writes=[ot_b[ob]])
                    S.dma("sp", out_d[rows, :], ot[ob][:], reads=[ot_b[ob]])
            S.barrier()
    except _Stop:
        pass
    return nc


_PROGRAM = None


def _prep_shared(inp):
    f = np.float32
    def kt_layout(w):
        K, N = w.shape
        return np.ascontiguousarray(w.reshape(K // 128, 128, N).transpose(1, 0, 2)).astype(f)
    def bc(v, n=128):
        return np.ascontiguousarray(np.broadcast_to(np.asarray(v, f).reshape(1, -1), (n, np.asarray(v).size)))
    def qlay(a):
        a = np.asarray(a, f)
        rest = a.shape[2:]
        return np.ascontiguousarray(a.reshape((16, 2, 64) + rest).transpose((1, 2, 0) + tuple(range(3, 3 + len(rest))))
                                    .reshape((128, 16) + rest))
    sh = {}
    sh["w_ada"] = kt_layout(inp["w_ada"][0])
    sh["b_ada_b"] = bc(inp["b_ada"][0])
    sh["w_in"] = kt_layout(inp["w_in"][0])
    sh["w_out"] = kt_layout(inp["w_out"][0])
    sh["w_glu"] = kt_layout(inp["w_glu"][0])
    sh["w_ff1"] = kt_layout(inp["w_ff1"][0])
    sh["w_ff2"] = kt_layout(inp["w_ff2"][0])
    sh["norm1_g_b"] = bc(inp["norm1_g"][0])
    sh["norm2_g_b"] = bc(inp["norm2_g"][0])
    sh["b_glu_b"] = bc(inp["b_glu"][0])
    sh["gn_ssm_b"] = bc(inp["gn_ssm"][0])
    sh["gn_attn_b"] = bc(inp["gn_attn"][0])
    sh["gq_b"] = bc(inp["q_gain"][0])
    sh["gk_b"] = bc(inp["k_gain"][0])
    sh["gq2"] = np.ascontiguousarray(np.tile(np.asarray(inp["q_gain"][0], f), 2).reshape(128, 1))
    sh["gk2"] = np.ascontiguousarray(np.tile(np.asarray(inp["k_gain"][0], f), 2).reshape(128, 1))
    sh["lamre_q"] = qlay(inp["lam_re"][0])
    sh["lamim_q"] = qlay(inp["lam_im"][0])
    sh["logdt_q"] = qlay(np.broadcast_to(np.asarray(inp["log_dt"][0], f)[:, None], (32, 64)))
    sh["bre_q"] = qlay(inp["ssm_b_re"][0])
    sh["bim_q"] = qlay(inp["ssm_b_im"][0])
    sh["cre_q"] = qlay(np.asarray(inp["ssm_c_re"][0]).transpose(0, 2, 1))
    sh["cim_q"] = qlay(np.asarray(inp["ssm_c_im"][0]).transpose(0, 2, 1))
    dsk = np.asarray(inp["d_skip"][0], f).reshape(32, 16)
    sh["dsk_b"] = np.ascontiguousarray(np.broadcast_to(np.tile(dsk, (1, 8))[None], (128, 32, 128))).astype(f)
    sh["ident"] = np.eye(128, dtype=f)
    r = np.arange(128)
    sh["causal"] = np.where(r[None, :] <= r[:, None], 0.0, -1.0e30).astype(f)
    sh["negi4"] = np.tile(-BIG * np.eye(128, dtype=f), (1, 4)).astype(f)
    ib, jb = r // 16, r // 16
    sh["bmask"] = (jb[None, :] >= ib[:, None]).astype(f)
    sh["dmask"] = (r[None, :] == r[:, None]).astype(f)
    sh["onesblk"] = ((r[None, :] // 64) == (r[:, None] // 64)).astype(f)
    sh["mtab"] = np.ascontiguousarray(np.broadcast_to(np.asarray(EXPS, f)[None, None, :], (128, 16, NE)))
    sh["pow2"] = np.ascontiguousarray(np.broadcast_to((2.0 ** -np.arange(NITER + 2)).astype(f)[None], (128, NITER + 2)))
    zm = np.zeros((128, 2), f)
    zm[:64, 0] = 1.0
    zm[64:, 1] = 1.0
    sh["zmask"] = zm
    return sh


def kernel(**inputs):
    global _PROGRAM
    inp = {k: np.asarray(v) for k, v in inputs.items()}
    if _PROGRAM is None:
        _PROGRAM = build_program()
    nc = _PROGRAM
    shared = _prep_shared(inp)
    x = np.asarray(inp["x"], np.float32)
    c = np.asarray(inp["c"], np.float32)
    in_maps = []
    for core in range(NCORES):
        m = dict(shared)
        m["x"] = np.ascontiguousarray(x[NSEQ * core: NSEQ * (core + 1)].reshape(NSEQ * SEQ, D))
        cc = c[NSEQ * core: NSEQ * (core + 1)]
        cT = cc.reshape(NSEQ, 8, 128).transpose(2, 1, 0)
        m["cT"] = np.ascontiguousarray(np.broadcast_to(cT[:, :, :, None], (128, 8, NSEQ, 128))).astype(np.float32)
        in_maps.append(m)
    res = run_bass_kernel_spmd(nc, in_maps, core_ids=list(range(NCORES)))
    outs = [np.asarray(r["out"], np.float32).reshape(NSEQ, SEQ, D) for r in res.results]
    return np.concatenate(outs, axis=0)
```

```python
import math
from contextlib import ExitStack

import numpy as np
import concourse.bass as bass
import concourse.mybir as mybir
from concourse.alu_op_type import AluOpType as ALU
from concourse.bass_utils import run_bass_kernel_spmd

F32 = mybir.dt.float32
BF16 = mybir.dt.bfloat16
I32 = mybir.dt.int32
AF = mybir.ActivationFunctionType
AX = mybir.AxisListType

NCORES = 8
D = 1024
SEQ = 2048
NSEQ = 2
DIN = 1864
DFF = 4096
EPS = 1e-6
IDX_SCALE = (64 ** -0.5) * (8 ** -0.5)
TOPK = 256
NITER = 18
BIG = 30000.0
EXPS = list(range(-7, 9)) + [16, 32, 64, 128, 256, 512, 1024]
NE = len(EXPS)
EIDX = {m: i for i, m in enumerate(EXPS)}
TWO_PI = 2.0 * math.pi


class Buf:
    __slots__ = ("w", "r", "ex")

    def __init__(self, ex=False):
        self.w = None
        self.r = {}
        self.ex = ex


class Sched:
    NDS = 24

    def __init__(self, nc, st):
        self.nc = nc
        self.eng = {"pe": nc.tensor, "act": nc.scalar, "dve": nc.vector, "pool": nc.gpsimd, "sp": nc.sync}
        self.sem = {k: st.enter_context(nc.semaphore("s_" + k)) for k in self.eng}
        self.cnt = {k: 0 for k in self.eng}
        self.waited = {k: {} for k in self.eng}
        self.dsem = [st.enter_context(nc.semaphore("d%d" % i)) for i in range(self.NDS)]
        self.dcnt = [0] * self.NDS
        self.dpool = {"sp": list(range(0, 16)), "pool": list(range(16, 24))}
        self.dnext = {"sp": 0, "pool": 0}

    def _semof(self, key):
        return self.sem[key[1]] if key[0] == "e" else self.dsem[key[1]]

    def _wait(self, e, key, val):
        if key[0] == "e" and key[1] == e and e == "pe":
            return
        if self.waited[e].get(key, 0) >= val:
            return
        self.eng[e].wait_ge(self._semof(key), val)
        self.waited[e][key] = val

    def _deps(self, e, reads, writes):
        for b in reads:
            if b.w is not None:
                self._wait(e, b.w[0], b.w[1])
            if b.ex:
                for k, v in b.r.items():
                    if k != ("e", e):
                        self._wait(e, k, v)
        for b in writes:
            if b.w is not None:
                self._wait(e, b.w[0], b.w[1])
            for k, v in b.r.items():
                self._wait(e, k, v)

    def _mark(self, key, val, reads, writes):
        for b in reads:
            if b.r.get(key, 0) < val:
                b.r[key] = val
        for b in writes:
            b.w = (key, val)
            b.r = {}

    def op(self, e, fn, reads=(), writes=()):
        self._deps(e, reads, writes)
        ins = fn(self.eng[e])
        self.cnt[e] += 1
        ins.then_inc(self.sem[e], 1)
        self._mark(("e", e), self.cnt[e], reads, writes)

    def dma(self, e, out, in_, reads=(), writes=(), **kw):
        self._deps(e, reads, writes)
        pl = self.dpool[e]
        j = pl[self.dnext[e] % len(pl)]
        self.dnext[e] += 1
        if self.dcnt[j] > 0:
            self._wait(e, ("d", j), self.dcnt[j])
        ins = self.eng[e].dma_start(out=out, in_=in_, **kw)
        self.dcnt[j] += 16
        ins.then_inc(self.dsem[j], 16)
        self._mark(("d", j), self.dcnt[j], reads, writes)

    def barrier(self):
        for e in self.eng:
            for k in self.eng:
                if k != e and self.cnt[k] > 0:
                    self._wait(e, ("e", k), self.cnt[k])
            for j in range(self.NDS):
                if self.dcnt[j] > 0:
                    self._wait(e, ("d", j), self.dcnt[j])


class _Stop(Exception):
    pass


def build_program(stop=None):
    nc = bass.Bass("TRN2", target_bir_lowering=False)

    def din(name, shape, dt=F32):
        return nc.dram_tensor(name, list(shape), dt, kind="ExternalInput").ap()

    x_d = din("x", [NSEQ * SEQ, D])
    cT_d = din("cT", [128, 8, NSEQ, 128])
    wada_d = din("w_ada", [128, 8, 6 * D])
    bada_d = din("b_ada_b", [128, 6 * D])
    win_d = din("w_in", [128, 8, DIN])
    wout_d = din("w_out", [128, 8, D])
    wglu_d = din("w_glu", [128, 4, 512])
    wff1_d = din("w_ff1", [128, 8, DFF])
    wff2_d = din("w_ff2", [128, 32, D])
    n1g_d = din("norm1_g_b", [128, D])
    n2g_d = din("norm2_g_b", [128, D])
    bglu_d = din("b_glu_b", [128, 512])
    gns_d = din("gn_ssm_b", [128, 512])
    gna_d = din("gn_attn_b", [128, 512])
    gqb_d = din("gq_b", [128, 64])
    gkb_d = din("gk_b", [128, 64])
    gq2_d = din("gq2", [128, 1])
    gk2_d = din("gk2", [128, 1])
    lre_d = din("lamre_q", [128, 16])
    lim_d = din("lamim_q", [128, 16])
    ldt_d = din("logdt_q", [128, 16])
    bre_d = din("bre_q", [128, 16, 16])
    bim_d = din("bim_q", [128, 16, 16])
    cre_d = din("cre_q", [128, 16, 16])
    cim_d = din("cim_q", [128, 16, 16])
    dsk_d = din("dsk_b", [128, 32, 128])
    ident_d = din("ident", [128, 128])
    causal_d = din("causal", [128, 128])
    negi4_d = din("negi4", [128, 512])
    bmask_d = din("bmask", [128, 128])
    dmask_d = din("dmask", [128, 128])
    onesblk_d = din("onesblk", [128, 128])
    mtab_d = din("mtab", [128, 16, NE])
    pow2_d = din("pow2", [128, NITER + 2])
    zmask_d = din("zmask", [128, 2])
    out_d = nc.dram_tensor("out", [NSEQ * SEQ, D], F32, kind="ExternalOutput").ap()

    try:
      with ExitStack() as top:
        S = Sched(nc, top)

        def chk(name):
            if stop == name:
                S.barrier()
                raise _Stop()

        uid = [0]

        def sb(st, name, shape, dt):
            uid[0] += 1
            return st.enter_context(nc.sbuf_tensor("sb%d_%s" % (uid[0], name), list(shape), dt))

        def ps(st, name, shape, dt):
            uid[0] += 1
            return st.enter_context(nc.psum_tensor("ps%d_%s" % (uid[0], name), list(shape), dt))

        pt = [ps(top, "pt%d" % i, [128, 1024], BF16) for i in range(2)]
        ptb = [Buf(ex=True) for _ in range(2)]
        pf = [ps(top, "pf%d" % i, [128, 512], F32) for i in range(6)]
        pfb = [Buf(ex=True) for _ in range(6)]
        rot = {"pt": 0, "pw": 0}

        def next_pt():
            i = rot["pt"]
            rot["pt"] = (i + 1) % 2
            return pt[i], ptb[i]

        def next_pw(n=4):
            i = rot["pw"] % n
            rot["pw"] += 1
            return pf[i], pfb[i]

        ident_bf = sb(top, "ident_bf", [128, 128], BF16)
        ident_f = sb(top, "ident_f", [128, 128], F32)
        causal = sb(top, "causal", [128, 128], F32)
        negi4 = sb(top, "negi4", [128, 512], BF16)
        onesblk = sb(top, "onesblk", [128, 128], BF16)
        zeros_bf = sb(top, "zeros_bf", [128, 260], BF16)
        bglu_b = sb(top, "bglu_b", [128, 512], F32)
        gns_b = sb(top, "gns_b", [128, 512], F32)
        gna_b = sb(top, "gna_b", [128, 512], F32)
        pow2 = sb(top, "pow2", [128, NITER + 2], F32)
        G2 = sb(top, "G2", [128, 1], F32)
        negM = sb(top, "negM", [128, 1], F32)
        taufix = sb(top, "taufix", [128, 1], F32)
        siluT = sb(top, "siluT", [128, 8, NSEQ, 128], BF16)
        cst = Buf()
        for t_, d_ in ((ident_bf, ident_d), (ident_f, ident_d), (causal, causal_d), (negi4, negi4_d),
                       (onesblk, onesblk_d), (bglu_b, bglu_d), (gns_b, gns_d), (gna_b, gna_d), (pow2, pow2_d)):
            S.dma("pool", t_[:], d_, writes=[cst])
        S.op("dve", lambda e: e.memset(zeros_bf[:], 0.0), writes=[cst])
        S.op("dve", lambda e: e.memset(taufix[:], 1.0e29), writes=[cst])

        with ExitStack() as st0:
            gqb = sb(st0, "gqb", [128, 64], F32)
            gkb = sb(st0, "gkb", [128, 64], F32)
            gq2 = sb(st0, "gq2", [128, 1], F32)
            gk2 = sb(st0, "gk2", [128, 1], F32)
            cTf = sb(st0, "cTf", [128, 8 * NSEQ * 128], F32)
            tb = Buf()
            S.dma("sp", gqb[:], gqb_d, writes=[tb])
            S.dma("sp", gkb[:], gkb_d, writes=[tb])
            S.dma("sp", gq2[:], gq2_d, writes=[tb])
            S.dma("sp", gk2[:], gk2_d, writes=[tb])
            S.dma("sp", cTf[:], cT_d.rearrange("p a b c -> p (a b c)"), writes=[tb])
            S.op("dve", lambda e: e.scalar_tensor_tensor(out=G2[:], in0=gq2[:], scalar=0.125, in1=gk2[:],
                                                          op0=ALU.mult, op1=ALU.mult), reads=[tb], writes=[cst])
            S.op("dve", lambda e: e.scalar_tensor_tensor(out=gqb[:], in0=gqb[:], scalar=0.125, in1=gkb[:],
                                                          op0=ALU.mult, op1=ALU.mult), reads=[tb], writes=[tb])
            S.op("dve", lambda e: e.tensor_reduce(out=negM[:], in_=gqb[:], axis=AX.X, op=ALU.max,
                                                  apply_absolute_value=True), reads=[tb], writes=[cst])
            S.op("dve", lambda e: e.tensor_scalar(out=negM[:], in0=negM[:], scalar1=-64.0, scalar2=None,
                                                  op0=ALU.mult), reads=[cst], writes=[cst])
            S.op("act", lambda e: e.activation(out=siluT[:].rearrange("p a b c -> p (a b c)"), in_=cTf[:],
                                               func=AF.Silu), reads=[tb], writes=[cst])
            S.barrier()

        chk('consts')
        def adaln(tabs, tabs_b, seqs, first_j, ng_d, st):
            wst = [sb(st, "wst%d" % i, [128, 8, 512], BF16) for i in range(2)]
            wstb = [Buf(), Buf()]
            bst = [sb(st, "bst%d" % i, [128, 512], F32) for i in range(2)]
            gst = [sb(st, "gst%d" % i, [128, 512], F32) for i in range(2)]
            tmp = [sb(st, "adat%d" % i, [128, 512], F32) for i in range(2)]
            tmpb = [Buf(), Buf()]
            it = 0
            for jj in range(3):
                for hc in range(2):
                    c0 = (first_j + jj) * D + hc * 512
                    b = it % 2
                    it += 1
                    S.dma("pool", wst[b][:], wada_d[:, :, c0:c0 + 512], writes=[wstb[b]])
                    S.dma("sp", bst[b][:], bada_d[:, c0:c0 + 512], writes=[wstb[b]])
                    if jj == 1:
                        S.dma("sp", gst[b][:], ng_d[:, hc * 512:(hc + 1) * 512], writes=[wstb[b]])
                    for n, s in enumerate(seqs):
                        p, pb = next_pw()
                        for kt in range(8):
                            S.op("pe", lambda e, p=p, b=b, kt=kt, s=s: e.matmul(
                                p[:], siluT[:, kt, s, :], wst[b][:, kt, :], start=(kt == 0), stop=(kt == 7)),
                                reads=[cst, wstb[b]], writes=[pb])
                        dst = tabs[n][:, jj, hc * 512:(hc + 1) * 512]
                        if jj == 1:
                            tb_ = tmpb[n]
                            S.op("dve", lambda e, p=p, b=b, n=n: e.tensor_tensor(
                                out=tmp[n][:], in0=p[:], in1=bst[b][:], op=ALU.add),
                                reads=[pb, wstb[b]], writes=[tb_])
                            S.op("dve", lambda e, b=b, n=n, dst=dst: e.scalar_tensor_tensor(
                                out=dst, in0=tmp[n][:], scalar=1.0, in1=gst[b][:], op0=ALU.add, op1=ALU.mult),
                                reads=[tb_, wstb[b]], writes=[tabs_b[n]])
                        else:
                            S.op("dve", lambda e, p=p, b=b, dst=dst: e.tensor_tensor(
                                out=dst, in0=p[:], in1=bst[b][:], op=ALU.add),
                                reads=[pb, wstb[b]], writes=[tabs_b[n]])

        with ExitStack() as stA:
            tabsA1 = sb(stA, "tabsA", [128, 3, D], F32)
            tabsA = [tabsA1, tabsA1]
            tabsA_b1 = Buf()
            tabsA_b = [tabsA_b1, tabsA_b1]

            ssmA_d = nc.dram_tensor("ssmA_scr", [128, 32, 128], BF16, kind="Internal").ap()
            ssmB_d = nc.dram_tensor("ssmB_scr", [128, 32, 128], BF16, kind="Internal").ap()
            ssmC_d = nc.dram_tensor("ssmC_scr", [128, 32, 2, 128], BF16, kind="Internal").ap()
            ssmd_b = Buf()
            ak = sb(stA, "ak", [128, 16, 8], F32)
            ck = sb(stA, "ck", [128, 16, 8], F32)
            nck = sb(stA, "nck", [128, 16, 8], F32)
            ssm_b = Buf()
            with ExitStack() as st1:
                A_sb = sb(st1, "A_sb", [128, 32, 128], BF16)
                Bm_sb = sb(st1, "Bm_sb", [128, 32, 128], BF16)
                Cmz = sb(st1, "Cmz", [128, 32, 2, 128], BF16)

                def t3(name, n):
                    return sb(st1, name, [128, 16, n], F32)
                lre = t3("lre", 1); lim = t3("lim", 1); ldt = t3("ldt", 1)
                bre = t3("bre", 16); bim = t3("bim", 16); cre = t3("cre", 16); cim = t3("cim", 16)
                mtab = t3("mtab", NE)
                bmask = sb(st1, "bmask", [128, 128], F32)
                dmask = sb(st1, "dmask", [128, 128], F32)
                zmask = sb(st1, "zmask", [128, 2], F32)
                dsk = sb(st1, "dsk", [128, 32, 128], F32)
                ib = Buf()
                for t_, d_ in ((lre, lre_d), (lim, lim_d), (ldt, ldt_d)):
                    S.dma("sp", t_[:, :, 0], d_, writes=[ib])
                for t_, d_ in ((bre, bre_d), (bim, bim_d), (cre, cre_d), (cim, cim_d), (mtab, mtab_d),
                               (bmask, bmask_d), (dmask, dmask_d), (dsk, dsk_d), (zmask, zmask_d)):
                    S.dma("sp", t_[:], d_, writes=[ib])
                chk('ssm_a')
                dt_ = t3("dt_", 1); aa = t3("aa", 1); th = t3("th", 1)
                ang = t3("ang", NE); lmag = t3("lmag", NE); mag = t3("mag", NE)
                tq = t3("tq", NE); tqi = sb(st1, "tqi", [128, 16, NE], I32); tqf = t3("tqf", NE)
                wr = t3("wr", NE); sn = t3("sn", NE); cs = t3("cs", NE)
                pr_ = t3("pr_", NE); pi_ = t3("pi_", NE)
                wb = Buf()

                def V(fn, reads=(), writes=(wb,)):
                    S.op("dve", fn, reads=[ib, wb] + list(reads), writes=list(writes))

                def ACT(fn, reads=(), writes=(wb,)):
                    S.op("act", fn, reads=[ib, wb] + list(reads), writes=list(writes))

                ACT(lambda e: e.activation(out=dt_[:], in_=ldt[:], func=AF.Exp))
                V(lambda e: e.tensor_tensor(out=aa[:], in0=lre[:], in1=dt_[:], op=ALU.mult))
                V(lambda e: e.tensor_tensor(out=th[:], in0=lim[:], in1=dt_[:], op=ALU.mult))
                V(lambda e: e.tensor_tensor(out=lmag[:], in0=mtab[:], in1=aa[:].to_broadcast([128, 16, NE]), op=ALU.mult))
                V(lambda e: e.tensor_tensor(out=ang[:], in0=mtab[:], in1=th[:].to_broadcast([128, 16, NE]), op=ALU.mult))
                ACT(lambda e: e.activation(out=mag[:], in_=lmag[:], func=AF.Exp))
                V(lambda e: e.tensor_scalar(out=tq[:], in0=ang[:], scalar1=1.0 / TWO_PI, scalar2=None, op0=ALU.mult))
                V(lambda e: e.tensor_copy(out=tqi[:], in_=tq[:]))
                V(lambda e: e.tensor_copy(out=tqf[:], in_=tqi[:]))
                V(lambda e: e.scalar_tensor_tensor(out=wr[:], in0=tqf[:], scalar=-TWO_PI, in1=ang[:],
                                                   op0=ALU.mult, op1=ALU.add))
                wt_ = t3("wt_", NE)
                for t_, shift in ((sn, 0.0), (cs, math.pi / 2)):
                    V(lambda e, t_=t_, shift=shift: e.tensor_scalar(out=t_[:], in0=wr[:], scalar1=shift, scalar2=None, op0=ALU.add))
                    V(lambda e, t_=t_: e.tensor_scalar(out=wt_[:], in0=t_[:], scalar1=math.pi, scalar2=-TWO_PI,
                                                       op0=ALU.is_gt, op1=ALU.mult))
                    V(lambda e, t_=t_: e.tensor_scalar(out=tq[:], in0=t_[:], scalar1=-math.pi, scalar2=TWO_PI,
                                                       op0=ALU.is_lt, op1=ALU.mult))
                    V(lambda e, t_=t_: e.tensor_tensor(out=t_[:], in0=t_[:], in1=wt_[:], op=ALU.add))
                    V(lambda e, t_=t_: e.tensor_tensor(out=t_[:], in0=t_[:], in1=tq[:], op=ALU.add))
                for t_ in (sn, cs):
                    V(lambda e, t_=t_: e.tensor_scalar(out=t_[:], in0=t_[:], scalar1=3.14159, scalar2=-3.14159,
                                                       op0=ALU.min, op1=ALU.max))
                ACT(lambda e: e.activation(out=sn[:], in_=sn[:], func=AF.Sin))
                ACT(lambda e: e.activation(out=cs[:], in_=cs[:], func=AF.Sin))
                V(lambda e: e.tensor_tensor(out=pr_[:], in0=mag[:], in1=cs[:], op=ALU.mult))
                V(lambda e: e.tensor_tensor(out=pi_[:], in0=mag[:], in1=sn[:], op=ALU.mult))
                chk('ssm_b')
                i1 = EIDX[1]
                nr = t3("nr", 1); den = t3("den", 1); t1_ = t3("t1_", 1); gr = t3("gr", 1); gi = t3("gi", 1)
                V(lambda e: e.tensor_scalar(out=nr[:], in0=pr_[:, :, i1:i1 + 1], scalar1=-1.0, scalar2=None, op0=ALU.add))
                V(lambda e: e.tensor_tensor(out=den[:], in0=lre[:], in1=lre[:], op=ALU.mult))
                V(lambda e: e.tensor_tensor(out=t1_[:], in0=lim[:], in1=lim[:], op=ALU.mult))
                V(lambda e: e.tensor_tensor(out=den[:], in0=den[:], in1=t1_[:], op=ALU.add))
                V(lambda e: e.reciprocal(out=den[:], in_=den[:]))
                V(lambda e: e.tensor_tensor(out=gr[:], in0=nr[:], in1=lre[:], op=ALU.mult))
                V(lambda e: e.tensor_tensor(out=t1_[:], in0=pi_[:, :, i1:i1 + 1], in1=lim[:], op=ALU.mult))
                V(lambda e: e.tensor_tensor(out=gr[:], in0=gr[:], in1=t1_[:], op=ALU.add))
                V(lambda e: e.tensor_tensor(out=gr[:], in0=gr[:], in1=den[:], op=ALU.mult))
                V(lambda e: e.tensor_tensor(out=gi[:], in0=pi_[:, :, i1:i1 + 1], in1=lre[:], op=ALU.mult))
                V(lambda e: e.tensor_tensor(out=t1_[:], in0=nr[:], in1=lim[:], op=ALU.mult))
                V(lambda e: e.tensor_tensor(out=gi[:], in0=gi[:], in1=t1_[:], op=ALU.subtract))
                V(lambda e: e.tensor_tensor(out=gi[:], in0=gi[:], in1=den[:], op=ALU.mult))
                for k in range(8):
                    ii = EIDX[8 * (2 ** k)]
                    V(lambda e, k=k, ii=ii: e.tensor_copy(out=ak[:, :, k:k + 1], in_=pr_[:, :, ii:ii + 1]), writes=[wb, ssm_b])
                    V(lambda e, k=k, ii=ii: e.tensor_copy(out=ck[:, :, k:k + 1], in_=pi_[:, :, ii:ii + 1]), writes=[wb, ssm_b])
                    V(lambda e, k=k, ii=ii: e.tensor_scalar(out=nck[:, :, k:k + 1], in0=pi_[:, :, ii:ii + 1], scalar1=-1.0,
                                                            scalar2=None, op0=ALU.mult), writes=[wb, ssm_b])
                PBr = t3("PBr", 8); PBi = t3("PBi", 8); tt8 = t3("tt8", 8)
                e7 = EIDX[0]
                sl07 = slice(e7, e7 + 8)
                V(lambda e: e.tensor_tensor(out=PBr[:], in0=pr_[:, :, sl07], in1=gr[:].to_broadcast([128, 16, 8]), op=ALU.mult))
                V(lambda e: e.tensor_tensor(out=tt8[:], in0=pi_[:, :, sl07], in1=gi[:].to_broadcast([128, 16, 8]), op=ALU.mult))
                V(lambda e: e.tensor_tensor(out=PBr[:], in0=PBr[:], in1=tt8[:], op=ALU.subtract))
                V(lambda e: e.tensor_tensor(out=PBi[:], in0=pr_[:, :, sl07], in1=gi[:].to_broadcast([128, 16, 8]), op=ALU.mult))
                V(lambda e: e.tensor_tensor(out=tt8[:], in0=pi_[:, :, sl07], in1=gr[:].to_broadcast([128, 16, 8]), op=ALU.mult))
                V(lambda e: e.tensor_tensor(out=PBi[:], in0=PBi[:], in1=tt8[:], op=ALU.add))
                BmTr = sb(st1, "BmTr", [128, 16, 8, 16], F32)
                BmTi = sb(st1, "BmTi", [128, 16, 8, 16], F32)
                t816 = sb(st1, "t816", [128, 16, 8, 16], F32)
                for i in range(8):
                    m = 7 - i
                    def bc(t_, m=m):
                        return t_[:, :, m:m + 1].to_broadcast([128, 16, 16])
                    V(lambda e, i=i, bc=bc: e.tensor_tensor(out=BmTr[:, :, i, :], in0=bre[:], in1=bc(PBr), op=ALU.mult))
                    V(lambda e, i=i, bc=bc: e.tensor_tensor(out=t816[:, :, i, :], in0=bim[:], in1=bc(PBi), op=ALU.mult))
                    V(lambda e, i=i, bc=bc: e.tensor_tensor(out=BmTi[:, :, i, :], in0=bim[:], in1=bc(PBr), op=ALU.mult))
                V(lambda e: e.tensor_tensor(out=BmTr[:], in0=BmTr[:], in1=t816[:], op=ALU.subtract))
                for i in range(8):
                    m = 7 - i
                    V(lambda e, i=i, m=m: e.tensor_tensor(out=t816[:, :, i, :], in0=bre[:],
                                                          in1=PBi[:, :, m:m + 1].to_broadcast([128, 16, 16]), op=ALU.mult))
                V(lambda e: e.tensor_tensor(out=BmTi[:], in0=BmTi[:], in1=t816[:], op=ALU.add))
                Wcr = sb(st1, "Wcr", [128, 16, 8, 16], F32)
                Wci = sb(st1, "Wci", [128, 16, 8, 16], F32)
                Cmr = sb(st1, "Cmr", [128, 16, 8, 16], F32)
                Cmi = sb(st1, "Cmi", [128, 16, 8, 16], F32)
                for (dr, di, off) in ((Wcr, Wci, -7), (Cmr, Cmi, 1)):
                    for j in range(8):
                        ii = EIDX[j + off]
                        def bc2(t_, ii=ii):
                            return t_[:, :, ii:ii + 1].to_broadcast([128, 16, 16])
                        V(lambda e, j=j, bc2=bc2, dr=dr: e.tensor_tensor(out=dr[:, :, j, :], in0=cre[:], in1=bc2(pr_), op=ALU.mult))
                        V(lambda e, j=j, bc2=bc2: e.tensor_tensor(out=t816[:, :, j, :], in0=cim[:], in1=bc2(pi_), op=ALU.mult))
                        V(lambda e, j=j, bc2=bc2, di=di: e.tensor_tensor(out=di[:, :, j, :], in0=cre[:], in1=bc2(pi_), op=ALU.mult))
                    V(lambda e, dr=dr: e.tensor_tensor(out=dr[:], in0=dr[:], in1=t816[:], op=ALU.subtract))
                    for j in range(8):
                        ii = EIDX[j + off]
                        V(lambda e, j=j, ii=ii: e.tensor_tensor(out=t816[:, :, j, :], in0=cim[:],
                                                                in1=pr_[:, :, ii:ii + 1].to_broadcast([128, 16, 16]), op=ALU.mult))
                    V(lambda e, di=di: e.tensor_tensor(out=di[:], in0=di[:], in1=t816[:], op=ALU.add))
                    V(lambda e, di=di: e.tensor_scalar(out=di[:], in0=di[:], scalar1=-1.0, scalar2=None, op0=ALU.mult))
                chk('ssm_c')
                for pr in range(16):
                    for gp in range(2):
                        g = 2 * pr + gp
                        for ri, src in ((0, Cmr), (1, Cmi)):
                            V(lambda e, g=g, ri=ri, src=src, pr=pr, gp=gp: e.tensor_scalar(
                                out=Cmz[:, g, ri, :], in0=src[:, pr, :, :].rearrange("p a b -> p (a b)"),
                                scalar1=zmask[:, gp:gp + 1], scalar2=None, op0=ALU.mult), writes=[wb, ssm_b])
                chk('ssm_d')
                Bz = [sb(st1, "Bz%d" % i, [128, 2, 128], BF16) for i in range(2)]
                Wz = [sb(st1, "Wz%d" % i, [128, 2, 128], BF16) for i in range(2)]
                Bzb = [Buf(), Buf()]
                At = [sb(st1, "At%d" % i, [128, 128], F32) for i in range(2)]
                Atb = [Buf(), Buf()]
                for pr in range(16):
                    for gp in range(2):
                        g = 2 * pr + gp
                        b = g % 2
                        for ri, src in ((0, BmTr), (1, BmTi)):
                            V(lambda e, b=b, ri=ri, src=src, pr=pr, gp=gp: e.tensor_scalar(
                                out=Bz[b][:, ri, :], in0=src[:, pr, :, :].rearrange("p a b -> p (a b)"),
                                scalar1=zmask[:, gp:gp + 1], scalar2=None, op0=ALU.mult), writes=[wb, Bzb[b]])
                        for ri, src in ((0, Wcr), (1, Wci)):
                            V(lambda e, b=b, ri=ri, src=src, pr=pr, gp=gp: e.tensor_scalar(
                                out=Wz[b][:, ri, :], in0=src[:, pr, :, :].rearrange("p a b -> p (a b)"),
                                scalar1=zmask[:, gp:gp + 1], scalar2=None, op0=ALU.mult), writes=[wb, Bzb[b]])
                        ptt, ptb_ = next_pt()
                        for ri in range(2):
                            S.op("pe", lambda e, ptt=ptt, b=b, ri=ri: e.transpose(
                                out=ptt[:, ri * 128:(ri + 1) * 128], in_=Bz[b][:, ri, :], identity=ident_bf[:]),
                                reads=[Bzb[b], cst], writes=[ptb_])
                        for ri in range(2):
                            S.op("act", lambda e, ptt=ptt, g=g, ri=ri, gp=gp: e.copy(
                                out=Bm_sb[:, g, ri * 64:(ri + 1) * 64],
                                in_=ptt[:, ri * 128 + gp * 64: ri * 128 + gp * 64 + 64]),
                                reads=[ptb_], writes=[ssm_b])
                        p, pb = next_pw()
                        for ri in range(2):
                            S.op("pe", lambda e, p=p, b=b, ri=ri: e.matmul(
                                p[:, 0:128], Bz[b][:, ri, :], Wz[b][:, ri, :], start=(ri == 0), stop=(ri == 1)),
                                reads=[Bzb[b]], writes=[pb])
                        S.op("dve", lambda e, p=p, b=b: e.tensor_tensor(out=At[b][:], in0=p[:, 0:128], in1=bmask[:], op=ALU.mult),
                             reads=[pb, ib], writes=[Atb[b]])
                        S.op("dve", lambda e, b=b, g=g: e.tensor_tensor(out=dsk[:, g, :], in0=dsk[:, g, :], in1=dmask[:], op=ALU.mult),
                             reads=[ib], writes=[ib])
                        S.op("dve", lambda e, b=b, g=g: e.tensor_tensor(out=A_sb[:, g, :], in0=At[b][:], in1=dsk[:, g, :], op=ALU.add),
                             reads=[Atb[b], ib], writes=[ssm_b])
                chk('ssm_e')
                S.dma("sp", ssmA_d, A_sb[:], reads=[ssm_b], writes=[ssmd_b])
                S.dma("sp", ssmB_d, Bm_sb[:], reads=[ssm_b], writes=[ssmd_b])
                S.dma("sp", ssmC_d, Cmz[:], reads=[ssm_b], writes=[ssmd_b])
                S.barrier()

            chk('ssmsetup')
            for s in range(NSEQ):
                r0 = s * SEQ
                with ExitStack() as stt:
                    adaln([tabsA1], [tabsA_b1], [s], 0, n1g_d, stt)
                    S.barrier()
                chk('adaln')
                with ExitStack() as stS:
                    qT = sb(stS, "qT", [128, 4, SEQ], BF16)
                    kTz = sb(stS, "kTz", [128, 2, 2, SEQ], BF16)
                    qiT = sb(stS, "qiT", [128, 4, SEQ], BF16)
                    kiTz = sb(stS, "kiTz", [128, 2, SEQ], BF16)
                    Vp = sb(stS, "Vp", [128, 16, 2, 65], BF16)
                    wi = sb(stS, "wi", [128, 16, 8], F32)
                    mixT_s = sb(stS, "mixT_s", [128, 4, SEQ], BF16)
                    qT_b, kT_b, qiT_b, kiT_b, Vp_b, wi_b, mixT_sb = (Buf() for _ in range(7))
                    S.op("pool", lambda e: e.memset(kTz[:].rearrange("p a b c -> p (a b c)"), 0.0), writes=[kT_b])
                    S.op("pool", lambda e: e.memset(kiTz[:].rearrange("p a c -> p (a c)"), 0.0), writes=[kiT_b])
                    S.op("pool", lambda e: e.memset(Vp[:].rearrange("p a b c -> p (a b c)"), 1.0), writes=[Vp_b])

                    with ExitStack() as st12:
                        U8 = sb(st12, "U8", [128, 2, 32, 8, 16], BF16)
                        U8_b = Buf()
                        with ExitStack() as st1:
                            w_in = sb(st1, "w_in", [128, 8, DIN], BF16)
                            w_in_b = Buf()
                            for kt in range(8):
                                S.dma("pool", w_in[:, kt, :], win_d[:, kt, :], writes=[w_in_b])
                            hT = sb(st1, "hT", [128, 8, SEQ], BF16)
                            hT_b = Buf()
                            st1a = ExitStack()
                            xt = [sb(st1a, "xt%d" % i, [128, D], F32) for i in range(2)]
                            xt_b = [Buf(), Buf()]
                            junk = sb(st1a, "junk", [128, D], BF16)
                            junk_b = Buf()
                            t1 = sb(st1a, "t1", [128, D], F32)
                            hb = [sb(st1a, "hb%d" % i, [128, D], BF16) for i in range(2)]
                            hb_b = [Buf(), Buf()]
                            st_ = [sb(st1a, "st%d" % i, [128, 4], F32) for i in range(2)]
                            st_b = [Buf(), Buf()]
                            t1_b = Buf()
                            for t in range(16):
                                b = t % 2
                                S.dma("sp", xt[b][:], x_d[r0 + t * 128: r0 + (t + 1) * 128, :], writes=[xt_b[b]])
                                S.op("act", lambda e, b=b: e.activation(out=junk[:], in_=xt[b][:], func=AF.Square,
                                                                        accum_out=st_[b][:, 0:1]),
                                     reads=[xt_b[b]], writes=[junk_b, st_b[b]])
                                S.op("act", lambda e, b=b: e.activation(out=st_[b][:, 1:2], in_=st_[b][:, 0:1], func=AF.Sqrt,
                                                                        bias=EPS, scale=1.0 / D),
                                     reads=[st_b[b]], writes=[st_b[b]])
                                S.op("dve", lambda e, b=b: e.reciprocal(out=st_[b][:, 2:3], in_=st_[b][:, 1:2]),
                                     reads=[st_b[b]], writes=[st_b[b]])
                                S.op("dve", lambda e, b=b: e.scalar_tensor_tensor(
                                    out=t1[:], in0=xt[b][:], scalar=st_[b][:, 2:3], in1=tabsA[s][:, 1, :],
                                    op0=ALU.mult, op1=ALU.mult), reads=[xt_b[b], st_b[b], tabsA_b[s]], writes=[t1_b])
                                S.op("dve", lambda e, b=b: e.tensor_tensor(out=hb[b][:], in0=t1[:], in1=tabsA[s][:, 0, :], op=ALU.add),
                                     reads=[t1_b, tabsA_b[s]], writes=[hb_b[b]])
                                ptt, ptb_ = next_pt()
                                for kt in range(8):
                                    S.op("pe", lambda e, ptt=ptt, b=b, kt=kt: e.transpose(
                                        out=ptt[:, kt * 128:(kt + 1) * 128], in_=hb[b][:, kt * 128:(kt + 1) * 128],
                                        identity=ident_bf[:]), reads=[hb_b[b], cst], writes=[ptb_])
                                S.op("act", lambda e, ptt=ptt, t=t: e.copy(
                                    out=hT[:, :, t * 128:(t + 1) * 128], in_=ptt[:].rearrange("p (a b) -> p a b", b=128)),
                                    reads=[ptb_], writes=[hT_b])
                            S.barrier()
                            st1a.close()
                            sq = [sb(st1, "sq%d" % i, [128, 512], BF16) for i in range(2)]
                            sq_b = [Buf(), Buf()]
                            sd = [sb(st1, "sd%d" % i, [128, 512], F32) for i in range(2)]
                            sd_b = [Buf(), Buf()]
                            groups = []
                            for j in range(4):
                                groups.append(("q", j, [(512 + j * 128, 128, 0)]))
                            groups.append(("kA", 0, [(1024, 128, 0)]))
                            groups.append(("kB", 0, [(1088, 64, 0), (1024, 64, 64)]))
                            for j in range(4):
                                groups.append(("qi", j, [(1280 + j * 128, 128, 0)]))
                            groups.append(("ki", 0, [(1792, 64, 0), (1792, 64, 64)]))
                            it = 0
                            for kind, j, parts in groups:
                                for c in range(4):
                                    cs_ = slice(c * 512, (c + 1) * 512)
                                    p, pb = next_pw()
                                    for (c0, m, po) in parts:
                                        for kt in range(8):
                                            S.op("pe", lambda e, p=p, c0=c0, m=m, po=po, kt=kt, cs_=cs_: e.matmul(
                                                p[po:po + m, :], w_in[:, kt, c0:c0 + m], hT[:, kt, cs_],
                                                start=(kt == 0), stop=(kt == 7)),
                                                reads=[w_in_b, hT_b], writes=[pb])
                                    if kind == "qi":
                                        S.op("act", lambda e, p=p, j=j, cs_=cs_: e.copy(out=qiT[:, j, cs_], in_=p[:]),
                                             reads=[pb], writes=[qiT_b])
                                    elif kind == "ki":
                                        for half in range(2):
                                            rs = slice(half * 64, half * 64 + 64)
                                            S.op("act", lambda e, p=p, half=half, rs=rs, cs_=cs_: e.copy(
                                                out=kiTz[rs, half, cs_], in_=p[rs, :]), reads=[pb], writes=[kiT_b])
                                    else:
                                        b = it % 2
                                        it += 1
                                        S.op("act", lambda e, p=p, b=b: e.activation(out=sq[b][:], in_=p[:], func=AF.Square),
                                             reads=[pb], writes=[sq_b[b]])
                                        p2, p2b = next_pw()
                                        S.op("pe", lambda e, p2=p2, b=b: e.matmul(p2[:], onesblk[:], sq[b][:], start=True, stop=True),
                                             reads=[sq_b[b], cst], writes=[p2b])
                                        S.op("act", lambda e, p2=p2, b=b: e.activation(out=sd[b][:], in_=p2[:], func=AF.Sqrt,
                                                                                      bias=EPS, scale=1.0 / 64),
                                             reads=[p2b], writes=[sd_b[b]])
                                        S.op("dve", lambda e, b=b: e.reciprocal(out=sd[b][:], in_=sd[b][:]),
                                             reads=[sd_b[b]], writes=[sd_b[b]])
                                        if kind == "q":
                                            S.op("dve", lambda e, p=p, b=b, j=j, cs_=cs_: e.scalar_tensor_tensor(
                                                out=qT[:, j, cs_], in0=p[:], scalar=G2[:, 0:1], in1=sd[b][:],
                                                op0=ALU.mult, op1=ALU.mult), reads=[pb, sd_b[b], cst], writes=[qT_b])
                                        else:
                                            kvs = (0, 1) if kind == "kA" else (1, 0)
                                            for half in range(2):
                                                rs = slice(half * 64, half * 64 + 64)
                                                kv = kvs[half]
                                                S.op("dve", lambda e, p=p, b=b, rs=rs, kv=kv, half=half, cs_=cs_: e.tensor_tensor(
                                                    out=kTz[rs, kv, half, cs_], in0=p[rs, :], in1=sd[b][rs, :], op=ALU.mult),
                                                    reads=[pb, sd_b[b]], writes=[kT_b])
                            for t in range(16):
                                ts_ = slice(t * 128, (t + 1) * 128)
                                p, pb = next_pw()
                                for kt in range(8):
                                    S.op("pe", lambda e, p=p, kt=kt, ts_=ts_: e.matmul(
                                        p[:, 0:128], hT[:, kt, ts_], w_in[:, kt, 1152:1280], start=(kt == 0), stop=(kt == 7)),
                                        reads=[w_in_b, hT_b], writes=[pb])
                                for kt in range(8):
                                    S.op("pe", lambda e, p=p, kt=kt, ts_=ts_: e.matmul(
                                        p[:, 128:136], hT[:, kt, ts_], w_in[:, kt, 1856:1864], start=(kt == 0), stop=(kt == 7)),
                                        reads=[w_in_b, hT_b], writes=[pb])
                                S.op("act", lambda e, p=p, t=t: e.copy(
                                    out=Vp[:, t, :, 0:64], in_=p[:, 0:128].rearrange("p (a b) -> p a b", b=64)),
                                    reads=[pb], writes=[Vp_b])
                                S.op("act", lambda e, p=p, t=t: e.mul(out=wi[:, t, :], in_=p[:, 128:136], mul=IDX_SCALE),
                                     reads=[pb], writes=[wi_b])
                            for sp in range(2):
                                for i in range(8):
                                    p, pb = next_pw()
                                    for kt in range(8):
                                        lhs = hT[:, kt, sp * 1024:(sp + 1) * 1024].rearrange("p (b i) -> p i b", i=8)[:, i, :]
                                        S.op("pe", lambda e, p=p, lhs=lhs, kt=kt: e.matmul(
                                            p[:], lhs, w_in[:, kt, 0:512], start=(kt == 0), stop=(kt == 7)),
                                            reads=[w_in_b, hT_b], writes=[pb])
                                    S.op("act", lambda e, p=p, sp=sp, i=i: e.copy(
                                        out=U8[:, sp, :, i, :], in_=p[:].rearrange("p (g c) -> p g c", c=16)),
                                         reads=[pb], writes=[U8_b])
                            S.barrier()

                        chk('s1')
                        with ExitStack() as st2:
                            w_glu = sb(st2, "w_glu", [128, 4, 512], BF16)
                            w_glu_b = Buf()
                            S.dma("pool", w_glu[:], wglu_d, writes=[w_glu_b])
                            Ytok = sb(st2, "Ytok", [128, 2, 8, 512], F32)
                            Ytok_b = Buf()
                            U8T = [sb(st2, "U8T%d" % i, [128, 2, 256], BF16) for i in range(2)]
                            U8T_b = [Buf(), Buf()]
                            XA = [sb(st2, "XA%d" % i, [128, 2, 384], F32) for i in range(2)]
                            XB = [sb(st2, "XB%d" % i, [128, 2, 384], F32) for i in range(2)]
                            XA_b, XB_b = [Buf(), Buf()], [Buf(), Buf()]
                            TM = [sb(st2, "TM%d" % i, [128, 2, 256], F32) for i in range(2)]
                            TM_b = [Buf(), Buf()]
                            Xst = [sb(st2, "Xst%d" % i, [128, 2, 258], BF16) for i in range(2)]
                            Xst_b = [Buf(), Buf()]
                            Ysb = [sb(st2, "Ysb%d" % i, [128, 256], F32) for i in range(2)]
                            Ysb_b = [Buf(), Buf()]
                            for i in range(2):
                                S.op("pool", lambda e, i=i: e.memset(XA[i][:].rearrange("p a b -> p (a b)"), 0.0), writes=[XA_b[i]])
                                S.op("pool", lambda e, i=i: e.memset(XB[i][:].rearrange("p a b -> p (a b)"), 0.0), writes=[XB_b[i]])
                                S.op("pool", lambda e, i=i: e.memset(Xst[i][:].rearrange("p a b -> p (a b)"), 0.0), writes=[Xst_b[i]])
                            PAD = 128
                            A2 = [sb(st2, "A2_%d" % i, [128, 2, 128], BF16) for i in range(2)]
                            B2 = [sb(st2, "B2_%d" % i, [128, 2, 128], BF16) for i in range(2)]
                            C2 = [sb(st2, "C2_%d" % i, [128, 2, 2, 128], BF16) for i in range(2)]
                            M2_b = [Buf(), Buf()]

                            def gen_pair(pr):
                                ub = pr % 2
                                S.dma("sp", A2[ub][:], ssmA_d[:, 2 * pr:2 * pr + 2, :], reads=[ssmd_b], writes=[M2_b[ub]])
                                S.dma("sp", B2[ub][:], ssmB_d[:, 2 * pr:2 * pr + 2, :], reads=[ssmd_b], writes=[M2_b[ub]])
                                S.dma("sp", C2[ub][:], ssmC_d[:, 2 * pr:2 * pr + 2, :, :], reads=[ssmd_b], writes=[M2_b[ub]])
                                ptt, ptb_ = next_pt()
                                for gp in range(2):
                                    g = 2 * pr + gp
                                    for sp in range(2):
                                        S.op("pe", lambda e, ptt=ptt, gp=gp, sp=sp, g=g: e.transpose(
                                            out=ptt[:, (gp * 2 + sp) * 128:(gp * 2 + sp + 1) * 128],
                                            in_=U8[:, sp, g, :, :].rearrange("p a b -> p (a b)"), identity=ident_bf[:]),
                                            reads=[U8_b, cst], writes=[ptb_])
                                S.op("act", lambda e, ptt=ptt, ub=ub: e.copy(
                                    out=U8T[ub][:].rearrange("p a b -> p (a b)"), in_=ptt[:, 0:512]),
                                    reads=[ptb_], writes=[U8T_b[ub]])
                                p, pb = next_pw()
                                for gp in range(2):
                                    g = 2 * pr + gp
                                    for ri in range(2):
                                        S.op("pe", lambda e, p=p, gp=gp, g=g, ri=ri, ub=ub: e.matmul(
                                            p[gp * 64:(gp + 1) * 64, ri * 256:(ri + 1) * 256],
                                            B2[ub][:, gp, ri * 64:(ri + 1) * 64], U8T[ub][:, gp, :], start=True, stop=True),
                                            reads=[M2_b[ub], U8T_b[ub]], writes=[pb])
                                S.op("act", lambda e, p=p: e.copy(out=XA[ub][:, :, PAD:PAD + 256],
                                                                  in_=p[:].rearrange("p (a b) -> p a b", b=256)),
                                     reads=[pb], writes=[XA_b[ub]])
                                yield
                                cur, curb, nxt, nxtb = XA[ub], XA_b[ub], XB[ub], XB_b[ub]
                                xs = Xst[ub]
                                tm, tmb = TM[ub], TM_b[ub]
                                for k in range(8):
                                    sft = 2 ** k
                                    last = (k == 7)
                                    a_ = ak[:, pr, k:k + 1]
                                    c_ = ck[:, pr, k:k + 1]
                                    nc_ = nck[:, pr, k:k + 1]
                                    sh = slice(PAD - sft, PAD - sft + 256)
                                    ce = slice(PAD, PAD + 256)
                                    outr = xs[:, 0, 1:257] if last else nxt[:, 0, ce]
                                    outi = xs[:, 1, 1:257] if last else nxt[:, 1, ce]
                                    ob = Xst_b[ub] if last else nxtb
                                    S.op("dve", lambda e, cur=cur, a_=a_, sh=sh, ce=ce: e.scalar_tensor_tensor(
                                        out=tm[:, 0, :], in0=cur[:, 0, sh], scalar=a_, in1=cur[:, 0, ce], op0=ALU.mult, op1=ALU.add),
                                        reads=[curb, ssm_b], writes=[tmb])
                                    S.op("dve", lambda e, cur=cur, a_=a_, sh=sh, ce=ce: e.scalar_tensor_tensor(
                                        out=tm[:, 1, :], in0=cur[:, 1, sh], scalar=a_, in1=cur[:, 1, ce], op0=ALU.mult, op1=ALU.add),
                                        reads=[curb, ssm_b], writes=[tmb])
                                    yield
                                    S.op("dve", lambda e, cur=cur, nc_=nc_, sh=sh, outr=outr: e.scalar_tensor_tensor(
                                        out=outr, in0=cur[:, 1, sh], scalar=nc_, in1=tm[:, 0, :], op0=ALU.mult, op1=ALU.add),
                                        reads=[curb, tmb, ssm_b], writes=[ob])
                                    S.op("dve", lambda e, cur=cur, c_=c_, sh=sh, outi=outi: e.scalar_tensor_tensor(
                                        out=outi, in0=cur[:, 0, sh], scalar=c_, in1=tm[:, 1, :], op0=ALU.mult, op1=ALU.add),
                                        reads=[curb, tmb, ssm_b], writes=[ob])
                                    yield
                                    cur, curb, nxt, nxtb = nxt, nxtb, cur, curb
                                for gp in range(2):
                                    g = 2 * pr + gp
                                    yb = g % 2
                                    p, pb = next_pw()
                                    S.op("pe", lambda e, p=p, g=g, gp=gp, ub=ub: e.matmul(
                                        p[:, 0:256], A2[ub][:, gp, :], U8T[ub][:, gp, :], start=True, stop=False),
                                        reads=[M2_b[ub], U8T_b[ub]], writes=[pb])
                                    for ri in range(2):
                                        S.op("pe", lambda e, p=p, g=g, ri=ri, xs=xs: e.matmul(
                                            p[:, 0:256], C2[ub][:, gp, ri, :], xs[:, ri, 0:256], start=False, stop=(ri == 1)),
                                            reads=[M2_b[ub], Xst_b[ub]], writes=[pb])
                                    S.op("act", lambda e, p=p, yb=yb: e.copy(out=Ysb[yb][:], in_=p[:, 0:256]),
                                         reads=[pb], writes=[Ysb_b[yb]])
                                    p2, p2b = next_pw()
                                    for sp in range(2):
                                        S.op("pe", lambda e, p2=p2, sp=sp, yb=yb: e.transpose(
                                            out=p2[:, sp * 128:(sp + 1) * 128], in_=Ysb[yb][:, sp * 128:(sp + 1) * 128],
                                            identity=ident_f[:]), reads=[Ysb_b[yb], cst], writes=[p2b])
                                    for sp in range(2):
                                        S.op("act", lambda e, p2=p2, sp=sp, g=g: e.copy(
                                            out=Ytok[:, sp, :, g * 16:(g + 1) * 16],
                                            in_=p2[:, sp * 128:(sp + 1) * 128].rearrange("p (a b) -> p a b", b=16)),
                                            reads=[p2b], writes=[Ytok_b])
                                    yield

                            act_p = []
                            nxt_p = 0
                            steps = 0
                            while nxt_p < 16 or act_p:
                                if nxt_p < 16 and len(act_p) < 2 and (nxt_p == 0 or steps >= 6):
                                    if not act_p or act_p[-1][0] % 2 != nxt_p % 2:
                                        act_p.append((nxt_p, gen_pair(nxt_p)))
                                        nxt_p += 1
                                for item in list(act_p):
                                    try:
                                        next(item[1])
                                    except StopIteration:
                                        act_p.remove(item)
                                steps += 1
                            g1 = [sb(st2, "g1_%d" % i, [128, 512], F32) for i in range(2)]
                            g2_ = [sb(st2, "g2_%d" % i, [128, 512], F32) for i in range(2)]
                            zf = [sb(st2, "zf%d" % i, [128, 512], F32) for i in range(2)]
                            zb = [sb(st2, "zb%d" % i, [128, 512], BF16) for i in range(2)]
                            zT = [sb(st2, "zT%d" % i, [128, 4, 128], BF16) for i in range(2)]
                            sg = [sb(st2, "sg%d" % i, [128, 512], F32) for i in range(2)]
                            mb_ = [sb(st2, "mb%d" % i, [128, 512], BF16) for i in range(2)]
                            sst = [sb(st2, "sst%d" % i, [128, 4], F32) for i in range(2)]
                            gb = [[Buf() for _ in range(8)] for _ in range(2)]
                            KG = 2.0 * math.sqrt(2.0 / math.pi)
                            for sp in range(2):
                                for i in range(8):
                                    b = i % 2
                                    B = gb[b]
                                    y = Ytok[:, sp, i, :]
                                    S.op("act", lambda e, b=b, y=y: e.activation(out=g1[b][:], in_=y, func=AF.Square),
                                         reads=[Ytok_b], writes=[B[0]])
                                    S.op("dve", lambda e, b=b: e.tensor_scalar(out=g1[b][:], in0=g1[b][:], scalar1=0.044715,
                                                                               scalar2=1.0, op0=ALU.mult, op1=ALU.add),
                                         reads=[B[0]], writes=[B[0]])
                                    S.op("dve", lambda e, b=b, y=y: e.tensor_tensor(out=g2_[b][:], in0=g1[b][:], in1=y, op=ALU.mult),
                                         reads=[B[0], Ytok_b], writes=[B[1]])
                                    S.op("act", lambda e, b=b: e.activation(out=g2_[b][:], in_=g2_[b][:], func=AF.Sigmoid, scale=KG),
                                         reads=[B[1]], writes=[B[1]])
                                    S.op("dve", lambda e, b=b, y=y: e.tensor_tensor(out=zf[b][:], in0=g2_[b][:], in1=y, op=ALU.mult),
                                         reads=[B[1], Ytok_b], writes=[B[2]])
                                    S.op("act", lambda e, b=b: e.copy(out=zb[b][:], in_=zf[b][:]), reads=[B[2]], writes=[B[3]])
                                    ptt, ptb_ = next_pt()
                                    for ft in range(4):
                                        S.op("pe", lambda e, ptt=ptt, b=b, ft=ft: e.transpose(
                                            out=ptt[:, ft * 128:(ft + 1) * 128], in_=zb[b][:, ft * 128:(ft + 1) * 128],
                                            identity=ident_bf[:]), reads=[B[3], cst], writes=[ptb_])
                                    S.op("act", lambda e, ptt=ptt, b=b: e.copy(out=zT[b][:].rearrange("p a b -> p (a b)"),
                                                                               in_=ptt[:, 0:512]), reads=[ptb_], writes=[B[4]])
                                    p, pb = next_pw()
                                    for ft in range(4):
                                        S.op("pe", lambda e, p=p, b=b, ft=ft: e.matmul(
                                            p[:], zT[b][:, ft, :], w_glu[:, ft, :], start=(ft == 0), stop=(ft == 3)),
                                            reads=[B[4], w_glu_b], writes=[pb])
                                    S.op("dve", lambda e, p=p, b=b: e.tensor_tensor(out=sg[b][:], in0=p[:], in1=bglu_b[:], op=ALU.add),
                                         reads=[pb, cst], writes=[B[5]])
                                    S.op("act", lambda e, b=b: e.activation(out=sg[b][:], in_=sg[b][:], func=AF.Sigmoid),
                                         reads=[B[5]], writes=[B[5]])
                                    S.op("dve", lambda e, b=b: e.tensor_tensor(out=sg[b][:], in0=sg[b][:], in1=zf[b][:], op=ALU.mult),
                                         reads=[B[5], B[2]], writes=[B[5]])
                                    S.op("act", lambda e, b=b: e.activation(out=g1[b][:], in_=sg[b][:], func=AF.Square,
                                                                            accum_out=sst[b][:, 0:1]),
                                         reads=[B[5], B[0]], writes=[B[0], B[6]])
                                    S.op("act", lambda e, b=b: e.activation(out=sst[b][:, 1:2], in_=sst[b][:, 0:1], func=AF.Sqrt,
                                                                            bias=EPS, scale=1.0 / 512), reads=[B[6]], writes=[B[6]])
                                    S.op("dve", lambda e, b=b: e.reciprocal(out=sst[b][:, 2:3], in_=sst[b][:, 1:2]),
                                         reads=[B[6]], writes=[B[6]])
                                    S.op("dve", lambda e, b=b: e.scalar_tensor_tensor(
                                        out=mb_[b][:], in0=sg[b][:], scalar=sst[b][:, 2:3], in1=gns_b[:], op0=ALU.mult, op1=ALU.mult),
                                        reads=[B[5], B[6], cst], writes=[B[7]])
                                    ptt, ptb_ = next_pt()
                                    for ft in range(4):
                                        S.op("pe", lambda e, ptt=ptt, b=b, ft=ft: e.transpose(
                                            out=ptt[:, ft * 128:(ft + 1) * 128], in_=mb_[b][:, ft * 128:(ft + 1) * 128],
                                            identity=ident_bf[:]), reads=[B[7], cst], writes=[ptb_])
                                    dst = mixT_s[:, :, sp * 1024:(sp + 1) * 1024].rearrange("p a (b i) -> p a i b", i=8)[:, :, i, :]
                                    S.op("act", lambda e, ptt=ptt, dst=dst: e.copy(
                                        out=dst, in_=ptt[:, 0:512].rearrange("p (a b) -> p a b", b=128)),
                                        reads=[ptb_], writes=[mixT_sb])
                            S.barrier()

                    chk('s2')
                    with ExitStack() as st3:
                        w_out = sb(st3, "w_out", [128, 8, D], BF16)
                        w_out_b = Buf()
                        S.dma("pool", w_out[:], wout_d, writes=[w_out_b])
                        score = [sb(st3, "score%d" % i, [128, SEQ], F32) for i in range(2)]
                        score_b = [Buf(), Buf()]
                        nm = [sb(st3, "nm%d" % i, [128, SEQ], BF16) for i in range(3)]
                        nm_b = [Buf() for _ in range(3)]
                        rbuf = [sb(st3, "rbuf%d" % i, [128, 8, 512], BF16) for i in range(2)]
                        rbuf_b = [Buf(), Buf()]
                        dg = [sb(st3, "dg%d" % i, [128, 8, 128], BF16) for i in range(2)]
                        dg_b = [Buf(), Buf()]
                        bs = [sb(st3, "bs%d" % i, [128, 16 + 3 * NITER], F32) for i in range(2)]
                        bs_b = [Buf(), Buf()]
                        PT = [sb(st3, "PT%d" % i, [128, 512], BF16) for i in range(3)]
                        PT_b = [Buf() for _ in range(3)]
                        yatt = sb(st3, "yatt", [128, 512], F32)
                        yatt_b = Buf()
                        rden = sb(st3, "rden", [128, 8], F32)
                        ajunk = sb(st3, "ajunk", [128, 512], BF16)
                        ast = sb(st3, "ast", [128, 4], F32)
                        mixa = sb(st3, "mixa", [128, 512], BF16)
                        mixa_b = Buf()
                        mixTa = sb(st3, "mixTa", [128, 4, 128], BF16)
                        mixTa_b = Buf()
                        xr = [sb(st3, "xr%d" % i, [128, D], F32) for i in range(2)]
                        xr_b = [Buf(), Buf()]
                        x1t = [sb(st3, "x1t%d" % i, [128, 512], F32) for i in range(2)]
                        x1t_b = [Buf(), Buf()]
                        prot = {"I": 0, "A": 0, "pt": 0}

                        def pwI():
                            i = prot["I"] % 2
                            prot["I"] += 1
                            return pf[i], pfb[i]

                        def pwA():
                            i = 2 + prot["A"] % 2
                            prot["A"] += 1
                            return pf[i], pfb[i]

                        def gen_I(qt):
                            sl = qt % 2
                            nsl = qt % 3
                            nk = qt + 1
                            SK = nk * 128
                            qs = slice(qt * 128, (qt + 1) * 128)
                            sc, scb = score[sl], score_b[sl]
                            BS, BSb = bs[sl], bs_b[sl]
                            S.op("dve", lambda e: e.tensor_tensor(
                                out=dg[sl][:], in0=ident_bf[:].unsqueeze(1).to_broadcast([128, 8, 128]),
                                in1=wi[:, qt, :].unsqueeze(2).to_broadcast([128, 8, 128]), op=ALU.mult),
                                reads=[cst, wi_b], writes=[dg_b[sl]])
                            nch = (SK + 511) // 512
                            for c in range(nch):
                                cw = min(512, SK - c * 512)
                                ks = slice(c * 512, c * 512 + cw)
                                for h in range(8):
                                    j, half = h // 2, h % 2
                                    p, pb = pwI()
                                    S.op("pe", lambda e, p=p, j=j, half=half: e.matmul(
                                        p[:, 0:cw], qiT[:, j, qs], kiTz[:, half, ks], start=True, stop=True),
                                        reads=[qiT_b, kiT_b], writes=[pb])
                                    if False:
                                        S.op("act", lambda e, p=p, h=h: e.activation(
                                            out=rbuf[sl][:, h, 0:cw], in_=p[:, 0:cw], func=AF.Relu),
                                            reads=[pb], writes=[rbuf_b[sl]])
                                    else:
                                        S.op("dve", lambda e, p=p, h=h: e.tensor_scalar(
                                            out=rbuf[sl][:, h, 0:cw], in0=p[:, 0:cw], scalar1=0.0, scalar2=None, op0=ALU.max),
                                            reads=[pb], writes=[rbuf_b[sl]])
                                p, pb = pwI()
                                for h in range(8):
                                    S.op("pe", lambda e, p=p, h=h: e.matmul(
                                        p[:, 0:cw], dg[sl][:, h, :], rbuf[sl][:, h, 0:cw], start=(h == 0), stop=(h == 7)),
                                        reads=[dg_b[sl], rbuf_b[sl]], writes=[pb])
                                S.op("act", lambda e, p=p: e.copy(out=sc[:, ks], in_=p[:, 0:cw]),
                                     reads=[pb], writes=[scb])
                                S.op("dve", lambda e, c=c: e.tensor_reduce(
                                    out=BS[:, c:c + 1], in_=sc[:, ks], axis=AX.X, op=ALU.max, apply_absolute_value=True),
                                    reads=[scb], writes=[BSb])
                                yield
                            S.op("dve", lambda e: e.tensor_tensor(out=sc[:, qs], in0=sc[:, qs], in1=causal[:], op=ALU.add),
                                 reads=[scb, cst], writes=[scb])
                            if qt >= 2:
                                S.op("dve", lambda e: e.tensor_reduce(
                                    out=BS[:, 4:5], in_=BS[:, 0:nch], axis=AX.X, op=ALU.max), reads=[BSb], writes=[BSb])
                                S.op("dve", lambda e: e.tensor_scalar(
                                    out=BS[:, 8:8 + NITER + 1], in0=pow2[:, 0:NITER + 1], scalar1=BS[:, 4:5], scalar2=None,
                                    op0=ALU.mult), reads=[BSb, cst], writes=[BSb])
                                S.op("dve", lambda e: e.memset(BS[:, 5:6], 0.0), reads=[BSb], writes=[BSb])
                                thr = float(2 * TOPK - SK) - 0.5
                                S.op("dve", lambda e: e.memset(BS[:, 3:4], -thr), reads=[BSb], writes=[BSb])
                                S.op("dve", lambda e: e.tensor_scalar(
                                    out=BS[:, 9 + NITER:10 + 2 * NITER], in0=BS[:, 8:9 + NITER], scalar1=-1.0, scalar2=None,
                                    op0=ALU.mult), reads=[BSb], writes=[BSb])
                                for n in range(NITER):
                                    S.op("act", lambda e: e.activation(
                                        out=nm[nsl][:, 0:SK], in_=sc[:, 0:SK], func=AF.Sign, bias=BS[:, 5:6], scale=1.0,
                                        accum_out=BS[:, 6:7]), reads=[scb, BSb], writes=[nm_b[nsl], BSb])
                                    S.op("act", lambda e: e.activation(
                                        out=BS[:, 7:8], in_=BS[:, 6:7], func=AF.Sign, bias=BS[:, 3:4], scale=1.0),
                                        reads=[BSb], writes=[BSb])
                                    S.op("act", lambda e, n=n: e.activation(
                                        out=BS[:, 5:6], in_=BS[:, 7:8], func=AF.Identity, bias=BS[:, 5:6],
                                        scale=BS[:, 9 + NITER + 1 + n:10 + NITER + 1 + n]),
                                        reads=[BSb], writes=[BSb])
                                    yield
                                S.op("dve", lambda e: e.tensor_tensor(
                                    out=BS[:, 5:6], in0=BS[:, 5:6], in1=BS[:, 8 + NITER:9 + NITER], op=ALU.add),
                                    reads=[BSb], writes=[BSb])
                                ntau = BS[:, 5:6]
                            else:
                                ntau = taufix[:, 0:1]
                            S.op("dve", lambda e: e.tensor_scalar(
                                out=nm[nsl][:, 0:SK], in0=sc[:, 0:SK], scalar1=ntau, scalar2=0.0, op0=ALU.add, op1=ALU.is_lt),
                                reads=[scb, BSb, cst], writes=[nm_b[nsl]])
                            yield

                        def gen_A(qt):
                            nsl = qt % 3
                            nk = qt + 1
                            qs = slice(qt * 128, (qt + 1) * 128)
                            O = [pf[4], pf[5]]
                            Ob = [pfb[4], pfb[5]]
                            for kv in range(2):
                                S.op("pe", lambda e, kv=kv: e.matmul(O[kv][:, 0:260], zeros_bf[:, 0:128], zeros_bf[:, 0:260],
                                                                     start=True, stop=True), reads=[cst], writes=[Ob[kv]])
                            for kt in range(nk):
                                ksl = slice(kt * 128, (kt + 1) * 128)
                                for kv in range(2):
                                    p, pb = pwA()
                                    S.op("pe", lambda e, p=p: e.matmul(
                                        p[:], nm[nsl][:, ksl], negi4[:], start=True, stop=True),
                                        reads=[nm_b[nsl], cst], writes=[pb])
                                    for hh in range(4):
                                        head = kv * 4 + hh
                                        j, half = head // 2, head % 2
                                        S.op("pe", lambda e, p=p, hh=hh, kv=kv, half=half, j=j: e.matmul(
                                            p[:, hh * 128:(hh + 1) * 128], kTz[:, kv, half, ksl], qT[:, j, qs],
                                            start=False, stop=True, skip_group_check=True),
                                            reads=[kT_b, qT_b], writes=[pb])
                                    pi3 = prot["pt"] % 3
                                    prot["pt"] += 1
                                    S.op("act", lambda e, p=p, pi3=pi3: e.activation(
                                        out=PT[pi3][:], in_=p[:], func=AF.Exp, bias=negM[:, 0:1], scale=1.0),
                                        reads=[pb, cst], writes=[PT_b[pi3]])
                                    for hh in range(4):
                                        S.op("pe", lambda e, kv=kv, hh=hh, pi3=pi3, kt=kt: e.matmul(
                                            O[kv][:, hh * 65:(hh + 1) * 65], PT[pi3][:, hh * 128:(hh + 1) * 128], Vp[:, kt, kv, :],
                                            start=False, stop=True, skip_group_check=True),
                                            reads=[PT_b[pi3], Vp_b], writes=[Ob[kv]])
                                    yield
                            for kv in range(2):
                                ov = O[kv][:, 0:260].rearrange("p (h e) -> p h e", e=65)
                                S.op("dve", lambda e, kv=kv, ov=ov: e.reciprocal(out=rden[:, kv * 4:(kv + 1) * 4], in_=ov[:, :, 64]),
                                     reads=[Ob[kv]], writes=[yatt_b])
                                S.op("dve", lambda e, kv=kv, ov=ov: e.tensor_tensor(
                                    out=yatt[:, kv * 256:(kv + 1) * 256].rearrange("p (h d) -> p h d", d=64), in0=ov[:, :, 0:64],
                                    in1=rden[:, kv * 4:(kv + 1) * 4].unsqueeze(2).to_broadcast([128, 4, 64]), op=ALU.mult),
                                    reads=[Ob[kv], yatt_b], writes=[yatt_b])
                            S.op("act", lambda e: e.activation(out=ajunk[:], in_=yatt[:], func=AF.Square, accum_out=ast[:, 0:1]),
                                 reads=[yatt_b], writes=[yatt_b])
                            S.op("act", lambda e: e.activation(out=ast[:, 1:2], in_=ast[:, 0:1], func=AF.Sqrt, bias=EPS, scale=1.0 / 512),
                                 reads=[yatt_b], writes=[yatt_b])
                            S.op("dve", lambda e: e.reciprocal(out=ast[:, 2:3], in_=ast[:, 1:2]), reads=[yatt_b], writes=[yatt_b])
                            S.op("dve", lambda e: e.scalar_tensor_tensor(out=mixa[:], in0=yatt[:], scalar=ast[:, 2:3], in1=gna_b[:],
                                                                          op0=ALU.mult, op1=ALU.mult),
                                 reads=[yatt_b, cst], writes=[mixa_b])
                            ptt, ptb_ = next_pt()
                            for ft in range(4):
                                S.op("pe", lambda e, ptt=ptt, ft=ft: e.transpose(
                                    out=ptt[:, ft * 128:(ft + 1) * 128], in_=mixa[:, ft * 128:(ft + 1) * 128], identity=ident_bf[:]),
                                    reads=[mixa_b, cst], writes=[ptb_])
                            S.op("act", lambda e, ptt=ptt: e.copy(out=mixTa[:].rearrange("p a b -> p (a b)"), in_=ptt[:, 0:512]),
                                 reads=[ptb_], writes=[mixTa_b])
                            yield
                            xb = qt % 2
                            rows = slice(r0 + qt * 128, r0 + (qt + 1) * 128)
                            S.dma("sp", xr[xb][:], x_d[rows, :], writes=[xr_b[xb]])
                            for hf in range(2):
                                p, pb = pwA()
                                for k8 in range(8):
                                    lhs = mixT_s[:, k8, qs] if k8 < 4 else mixTa[:, k8 - 4, :]
                                    S.op("pe", lambda e, p=p, lhs=lhs, k8=k8, hf=hf: e.matmul(
                                        p[:], lhs, w_out[:, k8, hf * 512:(hf + 1) * 512], start=(k8 == 0), stop=(k8 == 7)),
                                        reads=[mixT_sb, mixTa_b, w_out_b], writes=[pb])
                                hs = slice(hf * 512, (hf + 1) * 512)
                                S.op("dve", lambda e, p=p, hf=hf, hs=hs: e.tensor_tensor(
                                    out=x1t[hf][:], in0=p[:], in1=tabsA[s][:, 2, hs], op=ALU.mult),
                                    reads=[pb, tabsA_b[s]], writes=[x1t_b[hf]])
                                S.op("dve", lambda e, xb=xb, hf=hf, hs=hs: e.tensor_tensor(
                                    out=xr[xb][:, hs], in0=x1t[hf][:], in1=xr[xb][:, hs], op=ALU.add),
                                    reads=[x1t_b[hf], xr_b[xb]], writes=[xr_b[xb]])
                            S.dma("sp", out_d[rows, :], xr[xb][:], reads=[xr_b[xb]])
                            yield

                        NQ = 16
                        actI = []
                        actA = None
                        nextI, nextA = 0, 0
                        doneI = set()
                        while nextA < NQ or actA is not None:
                            if actA is None and nextA in doneI:
                                actA = (nextA, gen_A(nextA))
                                nextA += 1
                            firstA = actA[0] if actA is not None else nextA
                            while (nextI < NQ and len(actI) < 2 and nextI - 3 < firstA
                                   and (nextI < 2 or (nextI - 2) in doneI)):
                                actI.append((nextI, gen_I(nextI)))
                                nextI += 1
                            for item in list(actI):
                                try:
                                    next(item[1])
                                except StopIteration:
                                    actI.remove(item)
                                    doneI.add(item[0])
                            if actA is not None:
                                try:
                                    next(actA[1])
                                except StopIteration:
                                    actA = None
                        S.barrier()

        chk('s3')
        NB = 256
        NTT = NB // 128
        with ExitStack() as stB:
            tabB_d = nc.dram_tensor("tabB_scr", [NSEQ, 128, 3 * D], F32, kind="Internal").ap()
            tabBd_b = Buf()
            with ExitStack() as stt:
                tl = [sb(stt, "tabsBt%d" % s, [128, 3, D], F32) for s in range(NSEQ)]
                tlb = [Buf() for _ in range(NSEQ)]
                adaln(tl, tlb, list(range(NSEQ)), 3, n2g_d, stt)
                for s in range(NSEQ):
                    S.dma("sp", tabB_d[s], tl[s][:].rearrange("p a b -> p (a b)"), reads=[tlb[s]], writes=[tabBd_b])
                S.barrier()
            tabsB1 = sb(stB, "tabsB", [128, 3, D], F32)
            tabsB_b1 = Buf()
            W1 = sb(stB, "W1", [128, 8, DFF], BF16)
            W2 = sb(stB, "W2", [128, 32, D], BF16)
            W_b = Buf()
            for kt in range(8):
                S.dma("pool", W1[:, kt, :], wff1_d[:, kt, :], writes=[W_b])
            for ft in range(0, 32, 4):
                S.dma("pool", W2[:, ft:ft + 4, :], wff2_d[:, ft:ft + 4, :], writes=[W_b])
            hidT = sb(stB, "hidT", [128, 32, NB], BF16)
            hid_b = [Buf() for _ in range(32)]
            rl = [sb(stB, "rl%d" % i, [128, NB], BF16) for i in range(2)]
            rl_b = [Buf(), Buf()]
            h2T = sb(stB, "h2T", [128, 8, NB], BF16)
            h2T_b = Buf()
            x1 = [sb(stB, "x1_%d" % i, [128, D], F32) for i in range(2)]
            x1_b = [Buf() for _ in range(2)]
            junkB = sb(stB, "junkB", [128, D], BF16)
            junkB_b = Buf()
            tB = sb(stB, "tB", [128, D], F32)
            tB_b = Buf()
            h2 = [sb(stB, "h2_%d" % i, [128, D], BF16) for i in range(2)]
            h2_b = [Buf(), Buf()]
            stb = [sb(stB, "stb%d" % i, [128, 4], F32) for i in range(2)]
            stb_b = [Buf(), Buf()]
            ot = [sb(stB, "ot%d" % i, [128, D], F32) for i in range(2)]
            ot_b = [Buf(), Buf()]
            oc = 0
            nblk = NSEQ * SEQ // NB
            for blk in range(nblk):
                s = (blk * NB) // SEQ
                if (blk * NB) % SEQ == 0:
                    S.dma("sp", tabsB1[:].rearrange("p a b -> p (a b)"), tabB_d[s], reads=[tabBd_b], writes=[tabsB_b1])
                for tt in range(NTT):
                    rows = slice(blk * NB + tt * 128, blk * NB + (tt + 1) * 128)
                    b = tt % 2
                    S.dma("sp", x1[b][:], out_d[rows, :], writes=[x1_b[b]])
                    S.op("act", lambda e, b=b: e.activation(out=junkB[:], in_=x1[b][:], func=AF.Square,
                                                            accum_out=stb[b][:, 0:1]),
                         reads=[x1_b[b]], writes=[junkB_b, stb_b[b]])
                    S.op("act", lambda e, b=b: e.activation(out=stb[b][:, 1:2], in_=stb[b][:, 0:1], func=AF.Sqrt,
                                                            bias=EPS, scale=1.0 / D), reads=[stb_b[b]], writes=[stb_b[b]])
                    S.op("dve", lambda e, b=b: e.reciprocal(out=stb[b][:, 2:3], in_=stb[b][:, 1:2]),
                         reads=[stb_b[b]], writes=[stb_b[b]])
                    S.op("dve", lambda e, b=b: e.scalar_tensor_tensor(
                        out=tB[:], in0=x1[b][:], scalar=stb[b][:, 2:3], in1=tabsB1[:, 1, :], op0=ALU.mult, op1=ALU.mult),
                        reads=[x1_b[b], stb_b[b], tabsB_b1], writes=[tB_b])
                    S.op("dve", lambda e, b=b: e.tensor_tensor(out=h2[b][:], in0=tB[:], in1=tabsB1[:, 0, :], op=ALU.add),
                         reads=[tB_b, tabsB_b1], writes=[h2_b[b]])
                    ptt, ptb_ = next_pt()
                    for kt in range(8):
                        S.op("pe", lambda e, ptt=ptt, b=b, kt=kt: e.transpose(
                            out=ptt[:, kt * 128:(kt + 1) * 128], in_=h2[b][:, kt * 128:(kt + 1) * 128], identity=ident_bf[:]),
                            reads=[h2_b[b], cst], writes=[ptb_])
                    S.op("act", lambda e, ptt=ptt, tt=tt: e.copy(
                        out=h2T[:, :, tt * 128:(tt + 1) * 128], in_=ptt[:].rearrange("p (a b) -> p a b", b=128)),
                        reads=[ptb_], writes=[h2T_b])
                for ft in range(32):
                    p, pb = next_pw(2)
                    for kt in range(8):
                        S.op("pe", lambda e, p=p, kt=kt, ft=ft: e.matmul(
                            p[:, 0:NB], W1[:, kt, ft * 128:(ft + 1) * 128], h2T[:, kt, :], start=(kt == 0), stop=(kt == 7)),
                            reads=[W_b, h2T_b], writes=[pb])
                    b = ft % 2
                    S.op("act", lambda e, p=p, b=b: e.activation(out=rl[b][:], in_=p[:, 0:NB], func=AF.Relu),
                         reads=[pb], writes=[rl_b[b]])
                    S.op("pool", lambda e, b=b, ft=ft: e.tensor_tensor(out=hidT[:, ft, :], in0=rl[b][:], in1=rl[b][:], op=ALU.mult),
                         reads=[rl_b[b]], writes=[hid_b[ft]])
                for tt in range(NTT):
                    rows = slice(blk * NB + tt * 128, blk * NB + (tt + 1) * 128)
                    ob = oc % 2
                    oc += 1
                    S.dma("sp", ot[ob][:], out_d[rows, :], writes=[ot_b[ob]])
                    for hf in range(2):
                        bi = 2 + (2 * tt + hf) % 4
                        p, pb = pf[bi], pfb[bi]
                        for ft in range(32):
                            S.op("pe", lambda e, p=p, ft=ft, tt=tt, hf=hf: e.matmul(
                                p[:], hidT[:, ft, tt * 128:(tt + 1) * 128], W2[:, ft, hf * 512:(hf + 1) * 512],
                                start=(ft == 0), stop=(ft == 31)), reads=[hid_b[ft], W_b], writes=[pb])
                        hs = slice(hf * 512, (hf + 1) * 512)
                        S.op("dve", lambda e, p=p, hs=hs: e.tensor_tensor(
                            out=tB[:, hs], in0=p[:], in1=tabsB1[:, 2, hs], op=ALU.mult),
                            reads=[pb, tabsB_b1], writes=[tB_b])
                        S.op("dve", lambda e, ob=ob, hs=hs: e.tensor_tensor(
                            out=ot[ob][:, hs], in0=tB[:, hs], in1=ot[ob][:, hs], op=ALU.add),
                            reads=[tB_b, ot_b[ob]], writes=[ot_b[ob]])
                    S.dma("sp", out_d[rows, :], ot[ob][:], reads=[ot_b[ob]])
            S.barrier()
    except _Stop:
        pass
    return nc


_PROGRAM = None


def _prep_shared(inp):
    f = np.float32
    def kt_layout(w):
        K, N = w.shape
        return np.ascontiguousarray(w.reshape(K // 128, 128, N).transpose(1, 0, 2)).astype(f)
    def bc(v, n=128):
        return np.ascontiguousarray(np.broadcast_to(np.asarray(v, f).reshape(1, -1), (n, np.asarray(v).size)))
    def qlay(a):
        a = np.asarray(a, f)
        rest = a.shape[2:]
        return np.ascontiguousarray(a.reshape((16, 2, 64) + rest).transpose((1, 2, 0) + tuple(range(3, 3 + len(rest))))
                                    .reshape((128, 16) + rest))
    sh = {}
    sh["w_ada"] = kt_layout(inp["w_ada"][0])
    sh["b_ada_b"] = bc(inp["b_ada"][0])
    sh["w_in"] = kt_layout(inp["w_in"][0])
    sh["w_out"] = kt_layout(inp["w_out"][0])
    sh["w_glu"] = kt_layout(inp["w_glu"][0])
    sh["w_ff1"] = kt_layout(inp["w_ff1"][0])
    sh["w_ff2"] = kt_layout(inp["w_ff2"][0])
    sh["norm1_g_b"] = bc(inp["norm1_g"][0])
    sh["norm2_g_b"] = bc(inp["norm2_g"][0])
    sh["b_glu_b"] = bc(inp["b_glu"][0])
    sh["gn_ssm_b"] = bc(inp["gn_ssm"][0])
    sh["gn_attn_b"] = bc(inp["gn_attn"][0])
    sh["gq_b"] = bc(inp["q_gain"][0])
    sh["gk_b"] = bc(inp["k_gain"][0])
    sh["gq2"] = np.ascontiguousarray(np.tile(np.asarray(inp["q_gain"][0], f), 2).reshape(128, 1))
    sh["gk2"] = np.ascontiguousarray(np.tile(np.asarray(inp["k_gain"][0], f), 2).reshape(128, 1))
    sh["lamre_q"] = qlay(inp["lam_re"][0])
    sh["lamim_q"] = qlay(inp["lam_im"][0])
    sh["logdt_q"] = qlay(np.broadcast_to(np.asarray(inp["log_dt"][0], f)[:, None], (32, 64)))
    sh["bre_q"] = qlay(inp["ssm_b_re"][0])
    sh["bim_q"] = qlay(inp["ssm_b_im"][0])
    sh["cre_q"] = qlay(np.asarray(inp["ssm_c_re"][0]).transpose(0, 2, 1))
    sh["cim_q"] = qlay(np.asarray(inp["ssm_c_im"][0]).transpose(0, 2, 1))
    dsk = np.asarray(inp["d_skip"][0], f).reshape(32, 16)
    sh["dsk_b"] = np.ascontiguousarray(np.broadcast_to(np.tile(dsk, (1, 8))[None], (128, 32, 128))).astype(f)
    sh["ident"] = np.eye(128, dtype=f)
    r = np.arange(128)
    sh["causal"] = np.where(r[None, :] <= r[:, None], 0.0, -1.0e30).astype(f)
    sh["negi4"] = np.tile(-BIG * np.eye(128, dtype=f), (1, 4)).astype(f)
    ib, jb = r // 16, r // 16
    sh["bmask"] = (jb[None, :] >= ib[:, None]).astype(f)
    sh["dmask"] = (r[None, :] == r[:, None]).astype(f)
    sh["onesblk"] = ((r[None, :] // 64) == (r[:, None] // 64)).astype(f)
    sh["mtab"] = np.ascontiguousarray(np.broadcast_to(np.asarray(EXPS, f)[None, None, :], (128, 16, NE)))
    sh["pow2"] = np.ascontiguousarray(np.broadcast_to((2.0 ** -np.arange(NITER + 2)).astype(f)[None], (128, NITER + 2)))
    zm = np.zeros((128, 2), f)
    zm[:64, 0] = 1.0
    zm[64:, 1] = 1.0
    sh["zmask"] = zm
    return sh


def kernel(**inputs):
    global _PROGRAM
    inp = {k: np.asarray(v) for k, v in inputs.items()}
    if _PROGRAM is None:
        _PROGRAM = build_program()
    nc = _PROGRAM
    shared = _prep_shared(inp)
    x = np.asarray(inp["x"], np.float32)
    c = np.asarray(inp["c"], np.float32)
    in_maps = []
    for core in range(NCORES):
        m = dict(shared)
        m["x"] = np.ascontiguousarray(x[NSEQ * core: NSEQ * (core + 1)].reshape(NSEQ * SEQ, D))
        cc = c[NSEQ * core: NSEQ * (core + 1)]
        cT = cc.reshape(NSEQ, 8, 128).transpose(2, 1, 0)
        m["cT"] = np.ascontiguousarray(np.broadcast_to(cT[:, :, :, None], (128, 8, NSEQ, 128))).astype(np.float32)
        in_maps.append(m)
    res = run_bass_kernel_spmd(nc, in_maps, core_ids=list(range(NCORES)))
    outs = [np.asarray(r["out"], np.float32).reshape(NSEQ, SEQ, D) for r in res.results]
    return np.concatenate(outs, axis=0)
```

```python
import math
from contextlib import ExitStack

import numpy as np
import concourse.bass as bass
import concourse.mybir as mybir
from concourse.alu_op_type import AluOpType as ALU
from concourse.bass_utils import run_bass_kernel_spmd

F32 = mybir.dt.float32
BF16 = mybir.dt.bfloat16
I32 = mybir.dt.int32
AF = mybir.ActivationFunctionType
AX = mybir.AxisListType

NCORES = 8
D = 1024
SEQ = 2048
NSEQ = 2
DIN = 1864
DFF = 4096
EPS = 1e-6
IDX_SCALE = (64 ** -0.5) * (8 ** -0.5)
TOPK = 256
NITER = 16
BIG = 30000.0
EXPS = list(range(-7, 9)) + [16, 32, 64, 128, 256, 512, 1024]
NE = len(EXPS)
EIDX = {m: i for i, m in enumerate(EXPS)}
TWO_PI = 2.0 * math.pi


class Buf:
    __slots__ = ("w", "r", "ex")

    def __init__(self, ex=False):
        self.w = None
        self.r = {}
        self.ex = ex


class Sched:
    NDS = 24

    def __init__(self, nc, st):
        self.nc = nc
        self.eng = {"pe": nc.tensor, "act": nc.scalar, "dve": nc.vector, "pool": nc.gpsimd, "sp": nc.sync}
        self.sem = {k: st.enter_context(nc.semaphore("s_" + k)) for k in self.eng}
        self.cnt = {k: 0 for k in self.eng}
        self.waited = {k: {} for k in self.eng}
        self.dsem = [st.enter_context(nc.semaphore("d%d" % i)) for i in range(self.NDS)]
        self.dcnt = [0] * self.NDS
        self.dpool = {"sp": list(range(0, 16)), "pool": list(range(16, 24))}
        self.dnext = {"sp": 0, "pool": 0}

    def _semof(self, key):
        return self.sem[key[1]] if key[0] == "e" else self.dsem[key[1]]

    def _wait(self, e, key, val):
        if key[0] == "e" and key[1] == e and e == "pe":
            return
        if self.waited[e].get(key, 0) >= val:
            return
        self.eng[e].wait_ge(self._semof(key), val)
        self.waited[e][key] = val

    def _deps(self, e, reads, writes):
        for b in reads:
            if b.w is not None:
                self._wait(e, b.w[0], b.w[1])
            if b.ex:
                for k, v in b.r.items():
                    if k != ("e", e):
                        self._wait(e, k, v)
        for b in writes:
            if b.w is not None:
                self._wait(e, b.w[0], b.w[1])
            for k, v in b.r.items():
                self._wait(e, k, v)

    def _mark(self, key, val, reads, writes):
        for b in reads:
            if b.r.get(key, 0) < val:
                b.r[key] = val
        for b in writes:
            b.w = (key, val)
            b.r = {}

    def op(self, e, fn, reads=(), writes=()):
        self._deps(e, reads, writes)
        ins = fn(self.eng[e])
        self.cnt[e] += 1
        ins.then_inc(self.sem[e], 1)
        self._mark(("e", e), self.cnt[e], reads, writes)

    def dma(self, e, out, in_, reads=(), writes=(), **kw):
        self._deps(e, reads, writes)
        pl = self.dpool[e]
        j = pl[self.dnext[e] % len(pl)]
        self.dnext[e] += 1
        if self.dcnt[j] > 0:
            self._wait(e, ("d", j), self.dcnt[j])
        ins = self.eng[e].dma_start(out=out, in_=in_, **kw)
        self.dcnt[j] += 16
        ins.then_inc(self.dsem[j], 16)
        self._mark(("d", j), self.dcnt[j], reads, writes)

    def barrier(self):
        for e in self.eng:
            for k in self.eng:
                if k != e and self.cnt[k] > 0:
                    self._wait(e, ("e", k), self.cnt[k])
            for j in range(self.NDS):
                if self.dcnt[j] > 0:
                    self._wait(e, ("d", j), self.dcnt[j])


class _Stop(Exception):
    pass


def build_program(stop=None):
    nc = bass.Bass("TRN2", target_bir_lowering=False)

    def din(name, shape, dt=F32):
        return nc.dram_tensor(name, list(shape), dt, kind="ExternalInput").ap()

    x_d = din("x", [NSEQ * SEQ, D])
    cT_d = din("cT", [128, 8, NSEQ, 128])
    wada_d = din("w_ada", [128, 8, 6 * D])
    bada_d = din("b_ada_b", [128, 6 * D])
    win_d = din("w_in", [128, 8, DIN])
    wout_d = din("w_out", [128, 8, D])
    wglu_d = din("w_glu", [128, 4, 512])
    wff1_d = din("w_ff1", [128, 8, DFF])
    wff2_d = din("w_ff2", [128, 32, D])
    n1g_d = din("norm1_g_b", [128, D])
    n2g_d = din("norm2_g_b", [128, D])
    bglu_d = din("b_glu_b", [128, 512])
    gns_d = din("gn_ssm_b", [128, 512])
    gna_d = din("gn_attn_b", [128, 512])
    gqb_d = din("gq_b", [128, 64])
    gkb_d = din("gk_b", [128, 64])
    gq2_d = din("gq2", [128, 1])
    gk2_d = din("gk2", [128, 1])
    lre_d = din("lamre_q", [128, 16])
    lim_d = din("lamim_q", [128, 16])
    ldt_d = din("logdt_q", [128, 16])
    bre_d = din("bre_q", [128, 16, 16])
    bim_d = din("bim_q", [128, 16, 16])
    cre_d = din("cre_q", [128, 16, 16])
    cim_d = din("cim_q", [128, 16, 16])
    dsk_d = din("dsk_b", [128, 32, 128])
    ident_d = din("ident", [128, 128])
    causal_d = din("causal", [128, 128])
    negi4_d = din("negi4", [128, 512])
    bmask_d = din("bmask", [128, 128])
    dmask_d = din("dmask", [128, 128])
    onesblk_d = din("onesblk", [128, 128])
    mtab_d = din("mtab", [128, 16, NE])
    pow2_d = din("pow2", [128, NITER + 2])
    zmask_d = din("zmask", [128, 2])
    out_d = nc.dram_tensor("out", [NSEQ * SEQ, D], F32, kind="ExternalOutput").ap()

    try:
      with ExitStack() as top:
        S = Sched(nc, top)

        def chk(name):
            if stop == name:
                S.barrier()
                raise _Stop()

        uid = [0]

        def sb(st, name, shape, dt):
            uid[0] += 1
            return st.enter_context(nc.sbuf_tensor("sb%d_%s" % (uid[0], name), list(shape), dt))

        def ps(st, name, shape, dt):
            uid[0] += 1
            return st.enter_context(nc.psum_tensor("ps%d_%s" % (uid[0], name), list(shape), dt))

        pt = [ps(top, "pt%d" % i, [128, 1024], BF16) for i in range(2)]
        ptb = [Buf(ex=True) for _ in range(2)]
        pf = [ps(top, "pf%d" % i, [128, 512], F32) for i in range(6)]
        pfb = [Buf(ex=True) for _ in range(6)]
        rot = {"pt": 0, "pw": 0}

        def next_pt():
            i = rot["pt"]
            rot["pt"] = (i + 1) % 2
            return pt[i], ptb[i]

        def next_pw(n=4):
            i = rot["pw"] % n
            rot["pw"] += 1
            return pf[i], pfb[i]

        ident_bf = sb(top, "ident_bf", [128, 128], BF16)
        ident_f = sb(top, "ident_f", [128, 128], F32)
        causal = sb(top, "causal", [128, 128], F32)
        negi4 = sb(top, "negi4", [128, 512], BF16)
        onesblk = sb(top, "onesblk", [128, 128], BF16)
        zeros_bf = sb(top, "zeros_bf", [128, 260], BF16)
        bglu_b = sb(top, "bglu_b", [128, 512], F32)
        gns_b = sb(top, "gns_b", [128, 512], F32)
        gna_b = sb(top, "gna_b", [128, 512], F32)
        pow2 = sb(top, "pow2", [128, NITER + 2], F32)
        G2 = sb(top, "G2", [128, 1], F32)
        negM = sb(top, "negM", [128, 1], F32)
        taufix = sb(top, "taufix", [128, 1], F32)
        siluT = sb(top, "siluT", [128, 8, NSEQ, 128], BF16)
        cst = Buf()
        for t_, d_ in ((ident_bf, ident_d), (ident_f, ident_d), (causal, causal_d), (negi4, negi4_d),
                       (onesblk, onesblk_d), (bglu_b, bglu_d), (gns_b, gns_d), (gna_b, gna_d), (pow2, pow2_d)):
            S.dma("pool", t_[:], d_, writes=[cst])
        S.op("dve", lambda e: e.memset(zeros_bf[:], 0.0), writes=[cst])
        S.op("dve", lambda e: e.memset(taufix[:], 1.0e29), writes=[cst])

        with ExitStack() as st0:
            gqb = sb(st0, "gqb", [128, 64], F32)
            gkb = sb(st0, "gkb", [128, 64], F32)
            gq2 = sb(st0, "gq2", [128, 1], F32)
            gk2 = sb(st0, "gk2", [128, 1], F32)
            cTf = sb(st0, "cTf", [128, 8 * NSEQ * 128], F32)
            tb = Buf()
            S.dma("sp", gqb[:], gqb_d, writes=[tb])
            S.dma("sp", gkb[:], gkb_d, writes=[tb])
            S.dma("sp", gq2[:], gq2_d, writes=[tb])
            S.dma("sp", gk2[:], gk2_d, writes=[tb])
            S.dma("sp", cTf[:], cT_d.rearrange("p a b c -> p (a b c)"), writes=[tb])
            S.op("dve", lambda e: e.scalar_tensor_tensor(out=G2[:], in0=gq2[:], scalar=0.125, in1=gk2[:],
                                                          op0=ALU.mult, op1=ALU.mult), reads=[tb], writes=[cst])
            S.op("dve", lambda e: e.scalar_tensor_tensor(out=gqb[:], in0=gqb[:], scalar=0.125, in1=gkb[:],
                                                          op0=ALU.mult, op1=ALU.mult), reads=[tb], writes=[tb])
            S.op("dve", lambda e: e.tensor_reduce(out=negM[:], in_=gqb[:], axis=AX.X, op=ALU.max,
                                                  apply_absolute_value=True), reads=[tb], writes=[cst])
            S.op("dve", lambda e: e.tensor_scalar(out=negM[:], in0=negM[:], scalar1=-64.0, scalar2=None,
                                                  op0=ALU.mult), reads=[cst], writes=[cst])
            S.op("act", lambda e: e.activation(out=siluT[:].rearrange("p a b c -> p (a b c)"), in_=cTf[:],
                                               func=AF.Silu), reads=[tb], writes=[cst])
            S.barrier()

        chk('consts')
        def adaln(tabs, tabs_b, seqs, first_j, ng_d, st):
            wst = [sb(st, "wst%d" % i, [128, 8, 512], BF16) for i in range(2)]
            wstb = [Buf(), Buf()]
            bst = [sb(st, "bst%d" % i, [128, 512], F32) for i in range(2)]
            gst = [sb(st, "gst%d" % i, [128, 512], F32) for i in range(2)]
            tmp = [sb(st, "adat%d" % i, [128, 512], F32) for i in range(2)]
            tmpb = [Buf(), Buf()]
            it = 0
            for jj in range(3):
                for hc in range(2):
                    c0 = (first_j + jj) * D + hc * 512
                    b = it % 2
                    it += 1
                    S.dma("pool", wst[b][:], wada_d[:, :, c0:c0 + 512], writes=[wstb[b]])
                    S.dma("sp", bst[b][:], bada_d[:, c0:c0 + 512], writes=[wstb[b]])
                    if jj == 1:
                        S.dma("sp", gst[b][:], ng_d[:, hc * 512:(hc + 1) * 512], writes=[wstb[b]])
                    for n, s in enumerate(seqs):
                        p, pb = next_pw()
                        for kt in range(8):
                            S.op("pe", lambda e, p=p, b=b, kt=kt, s=s: e.matmul(
                                p[:], siluT[:, kt, s, :], wst[b][:, kt, :], start=(kt == 0), stop=(kt == 7)),
                                reads=[cst, wstb[b]], writes=[pb])
                        dst = tabs[n][:, jj, hc * 512:(hc + 1) * 512]
                        if jj == 1:
                            tb_ = tmpb[n]
                            S.op("dve", lambda e, p=p, b=b, n=n: e.tensor_tensor(
                                out=tmp[n][:], in0=p[:], in1=bst[b][:], op=ALU.add),
                                reads=[pb, wstb[b]], writes=[tb_])
                            S.op("dve", lambda e, b=b, n=n, dst=dst: e.scalar_tensor_tensor(
                                out=dst, in0=tmp[n][:], scalar=1.0, in1=gst[b][:], op0=ALU.add, op1=ALU.mult),
                                reads=[tb_, wstb[b]], writes=[tabs_b[n]])
                        else:
                            S.op("dve", lambda e, p=p, b=b, dst=dst: e.tensor_tensor(
                                out=dst, in0=p[:], in1=bst[b][:], op=ALU.add),
                                reads=[pb, wstb[b]], writes=[tabs_b[n]])

        with ExitStack() as stA:
            tabsA1 = sb(stA, "tabsA", [128, 3, D], F32)
            tabsA = [tabsA1, tabsA1]
            tabsA_b1 = Buf()
            tabsA_b = [tabsA_b1, tabsA_b1]

            ssmA_d = nc.dram_tensor("ssmA_scr", [128, 32, 128], BF16, kind="Internal").ap()
            ssmB_d = nc.dram_tensor("ssmB_scr", [128, 32, 128], BF16, kind="Internal").ap()
            ssmC_d = nc.dram_tensor("ssmC_scr", [128, 32, 2, 128], BF16, kind="Internal").ap()
            ssmd_b = Buf()
            ak = sb(stA, "ak", [128, 16, 8], F32)
            ck = sb(stA, "ck", [128, 16, 8], F32)
            nck = sb(stA, "nck", [128, 16, 8], F32)
            ssm_b = Buf()
            with ExitStack() as st1:
                A_sb = sb(st1, "A_sb", [128, 32, 128], BF16)
                Bm_sb = sb(st1, "Bm_sb", [128, 32, 128], BF16)
                Cmz = sb(st1, "Cmz", [128, 32, 2, 128], BF16)

                def t3(name, n):
                    return sb(st1, name, [128, 16, n], F32)
                lre = t3("lre", 1); lim = t3("lim", 1); ldt = t3("ldt", 1)
                bre = t3("bre", 16); bim = t3("bim", 16); cre = t3("cre", 16); cim = t3("cim", 16)
                mtab = t3("mtab", NE)
                bmask = sb(st1, "bmask", [128, 128], F32)
                dmask = sb(st1, "dmask", [128, 128], F32)
                zmask = sb(st1, "zmask", [128, 2], F32)
                dsk = sb(st1, "dsk", [128, 32, 128], F32)
                ib = Buf()
                for t_, d_ in ((lre, lre_d), (lim, lim_d), (ldt, ldt_d)):
                    S.dma("sp", t_[:, :, 0], d_, writes=[ib])
                for t_, d_ in ((bre, bre_d), (bim, bim_d), (cre, cre_d), (cim, cim_d), (mtab, mtab_d),
                               (bmask, bmask_d), (dmask, dmask_d), (dsk, dsk_d), (zmask, zmask_d)):
                    S.dma("sp", t_[:], d_, writes=[ib])
                chk('ssm_a')
                dt_ = t3("dt_", 1); aa = t3("aa", 1); th = t3("th", 1)
                ang = t3("ang", NE); lmag = t3("lmag", NE); mag = t3("mag", NE)
                tq = t3("tq", NE); tqi = sb(st1, "tqi", [128, 16, NE], I32); tqf = t3("tqf", NE)
                wr = t3("wr", NE); sn = t3("sn", NE); cs = t3("cs", NE)
                pr_ = t3("pr_", NE); pi_ = t3("pi_", NE)
                wb = Buf()

                def V(fn, reads=(), writes=(wb,)):
                    S.op("dve", fn, reads=[ib, wb] + list(reads), writes=list(writes))

                def ACT(fn, reads=(), writes=(wb,)):
                    S.op("act", fn, reads=[ib, wb] + list(reads), writes=list(writes))

                ACT(lambda e: e.activation(out=dt_[:], in_=ldt[:], func=AF.Exp))
                V(lambda e: e.tensor_tensor(out=aa[:], in0=lre[:], in1=dt_[:], op=ALU.mult))
                V(lambda e: e.tensor_tensor(out=th[:], in0=lim[:], in1=dt_[:], op=ALU.mult))
                V(lambda e: e.tensor_tensor(out=lmag[:], in0=mtab[:], in1=aa[:].to_broadcast([128, 16, NE]), op=ALU.mult))
                V(lambda e: e.tensor_tensor(out=ang[:], in0=mtab[:], in1=th[:].to_broadcast([128, 16, NE]), op=ALU.mult))
                ACT(lambda e: e.activation(out=mag[:], in_=lmag[:], func=AF.Exp))
                V(lambda e: e.tensor_scalar(out=tq[:], in0=ang[:], scalar1=1.0 / TWO_PI, scalar2=None, op0=ALU.mult))
                V(lambda e: e.tensor_copy(out=tqi[:], in_=tq[:]))
                V(lambda e: e.tensor_copy(out=tqf[:], in_=tqi[:]))
                V(lambda e: e.scalar_tensor_tensor(out=wr[:], in0=tqf[:], scalar=-TWO_PI, in1=ang[:],
                                                   op0=ALU.mult, op1=ALU.add))
                wt_ = t3("wt_", NE)
                for t_, shift in ((sn, 0.0), (cs, math.pi / 2)):
                    V(lambda e, t_=t_, shift=shift: e.tensor_scalar(out=t_[:], in0=wr[:], scalar1=shift, scalar2=None, op0=ALU.add))
                    V(lambda e, t_=t_: e.tensor_scalar(out=wt_[:], in0=t_[:], scalar1=math.pi, scalar2=-TWO_PI,
                                                       op0=ALU.is_gt, op1=ALU.mult))
                    V(lambda e, t_=t_: e.tensor_scalar(out=tq[:], in0=t_[:], scalar1=-math.pi, scalar2=TWO_PI,
                                                       op0=ALU.is_lt, op1=ALU.mult))
                    V(lambda e, t_=t_: e.tensor_tensor(out=t_[:], in0=t_[:], in1=wt_[:], op=ALU.add))
                    V(lambda e, t_=t_: e.tensor_tensor(out=t_[:], in0=t_[:], in1=tq[:], op=ALU.add))
                for t_ in (sn, cs):
                    V(lambda e, t_=t_: e.tensor_scalar(out=t_[:], in0=t_[:], scalar1=3.14159, scalar2=-3.14159,
                                                       op0=ALU.min, op1=ALU.max))
                ACT(lambda e: e.activation(out=sn[:], in_=sn[:], func=AF.Sin))
                ACT(lambda e: e.activation(out=cs[:], in_=cs[:], func=AF.Sin))
                V(lambda e: e.tensor_tensor(out=pr_[:], in0=mag[:], in1=cs[:], op=ALU.mult))
                V(lambda e: e.tensor_tensor(out=pi_[:], in0=mag[:], in1=sn[:], op=ALU.mult))
                chk('ssm_b')
                i1 = EIDX[1]
                nr = t3("nr", 1); den = t3("den", 1); t1_ = t3("t1_", 1); gr = t3("gr", 1); gi = t3("gi", 1)
                V(lambda e: e.tensor_scalar(out=nr[:], in0=pr_[:, :, i1:i1 + 1], scalar1=-1.0, scalar2=None, op0=ALU.add))
                V(lambda e: e.tensor_tensor(out=den[:], in0=lre[:], in1=lre[:], op=ALU.mult))
                V(lambda e: e.tensor_tensor(out=t1_[:], in0=lim[:], in1=lim[:], op=ALU.mult))
                V(lambda e: e.tensor_tensor(out=den[:], in0=den[:], in1=t1_[:], op=ALU.add))
                V(lambda e: e.reciprocal(out=den[:], in_=den[:]))
                V(lambda e: e.tensor_tensor(out=gr[:], in0=nr[:], in1=lre[:], op=ALU.mult))
                V(lambda e: e.tensor_tensor(out=t1_[:], in0=pi_[:, :, i1:i1 + 1], in1=lim[:], op=ALU.mult))
                V(lambda e: e.tensor_tensor(out=gr[:], in0=gr[:], in1=t1_[:], op=ALU.add))
                V(lambda e: e.tensor_tensor(out=gr[:], in0=gr[:], in1=den[:], op=ALU.mult))
                V(lambda e: e.tensor_tensor(out=gi[:], in0=pi_[:, :, i1:i1 + 1], in1=lre[:], op=ALU.mult))
                V(lambda e: e.tensor_tensor(out=t1_[:], in0=nr[:], in1=lim[:], op=ALU.mult))
                V(lambda e: e.tensor_tensor(out=gi[:], in0=gi[:], in1=t1_[:], op=ALU.subtract))
                V(lambda e: e.tensor_tensor(out=gi[:], in0=gi[:], in1=den[:], op=ALU.mult))
                for k in range(8):
                    ii = EIDX[8 * (2 ** k)]
                    V(lambda e, k=k, ii=ii: e.tensor_copy(out=ak[:, :, k:k + 1], in_=pr_[:, :, ii:ii + 1]), writes=[wb, ssm_b])
                    V(lambda e, k=k, ii=ii: e.tensor_copy(out=ck[:, :, k:k + 1], in_=pi_[:, :, ii:ii + 1]), writes=[wb, ssm_b])
                    V(lambda e, k=k, ii=ii: e.tensor_scalar(out=nck[:, :, k:k + 1], in0=pi_[:, :, ii:ii + 1], scalar1=-1.0,
                                                            scalar2=None, op0=ALU.mult), writes=[wb, ssm_b])
                PBr = t3("PBr", 8); PBi = t3("PBi", 8); tt8 = t3("tt8", 8)
                e7 = EIDX[0]
                sl07 = slice(e7, e7 + 8)
                V(lambda e: e.tensor_tensor(out=PBr[:], in0=pr_[:, :, sl07], in1=gr[:].to_broadcast([128, 16, 8]), op=ALU.mult))
                V(lambda e: e.tensor_tensor(out=tt8[:], in0=pi_[:, :, sl07], in1=gi[:].to_broadcast([128, 16, 8]), op=ALU.mult))
                V(lambda e: e.tensor_tensor(out=PBr[:], in0=PBr[:], in1=tt8[:], op=ALU.subtract))
                V(lambda e: e.tensor_tensor(out=PBi[:], in0=pr_[:, :, sl07], in1=gi[:].to_broadcast([128, 16, 8]), op=ALU.mult))
                V(lambda e: e.tensor_tensor(out=tt8[:], in0=pi_[:, :, sl07], in1=gr[:].to_broadcast([128, 16, 8]), op=ALU.mult))
                V(lambda e: e.tensor_tensor(out=PBi[:], in0=PBi[:], in1=tt8[:], op=ALU.add))
                BmTr = sb(st1, "BmTr", [128, 16, 8, 16], F32)
                BmTi = sb(st1, "BmTi", [128, 16, 8, 16], F32)
                t816 = sb(st1, "t816", [128, 16, 8, 16], F32)
                for i in range(8):
                    m = 7 - i
                    def bc(t_, m=m):
                        return t_[:, :, m:m + 1].to_broadcast([128, 16, 16])
                    V(lambda e, i=i, bc=bc: e.tensor_tensor(out=BmTr[:, :, i, :], in0=bre[:], in1=bc(PBr), op=ALU.mult))
                    V(lambda e, i=i, bc=bc: e.tensor_tensor(out=t816[:, :, i, :], in0=bim[:], in1=bc(PBi), op=ALU.mult))
                    V(lambda e, i=i, bc=bc: e.tensor_tensor(out=BmTi[:, :, i, :], in0=bim[:], in1=bc(PBr), op=ALU.mult))
                V(lambda e: e.tensor_tensor(out=BmTr[:], in0=BmTr[:], in1=t816[:], op=ALU.subtract))
                for i in range(8):
                    m = 7 - i
                    V(lambda e, i=i, m=m: e.tensor_tensor(out=t816[:, :, i, :], in0=bre[:],
                                                          in1=PBi[:, :, m:m + 1].to_broadcast([128, 16, 16]), op=ALU.mult))
                V(lambda e: e.tensor_tensor(out=BmTi[:], in0=BmTi[:], in1=t816[:], op=ALU.add))
                Wcr = sb(st1, "Wcr", [128, 16, 8, 16], F32)
                Wci = sb(st1, "Wci", [128, 16, 8, 16], F32)
                Cmr = sb(st1, "Cmr", [128, 16, 8, 16], F32)
                Cmi = sb(st1, "Cmi", [128, 16, 8, 16], F32)
                for (dr, di, off) in ((Wcr, Wci, -7), (Cmr, Cmi, 1)):
                    for j in range(8):
                        ii = EIDX[j + off]
                        def bc2(t_, ii=ii):
                            return t_[:, :, ii:ii + 1].to_broadcast([128, 16, 16])
                        V(lambda e, j=j, bc2=bc2, dr=dr: e.tensor_tensor(out=dr[:, :, j, :], in0=cre[:], in1=bc2(pr_), op=ALU.mult))
                        V(lambda e, j=j, bc2=bc2: e.tensor_tensor(out=t816[:, :, j, :], in0=cim[:], in1=bc2(pi_), op=ALU.mult))
                        V(lambda e, j=j, bc2=bc2, di=di: e.tensor_tensor(out=di[:, :, j, :], in0=cre[:], in1=bc2(pi_), op=ALU.mult))
                    V(lambda e, dr=dr: e.tensor_tensor(out=dr[:], in0=dr[:], in1=t816[:], op=ALU.subtract))
                    for j in range(8):
                        ii = EIDX[j + off]
                        V(lambda e, j=j, ii=ii: e.tensor_tensor(out=t816[:, :, j, :], in0=cim[:],
                                                                in1=pr_[:, :, ii:ii + 1].to_broadcast([128, 16, 16]), op=ALU.mult))
                    V(lambda e, di=di: e.tensor_tensor(out=di[:], in0=di[:], in1=t816[:], op=ALU.add))
                    V(lambda e, di=di: e.tensor_scalar(out=di[:], in0=di[:], scalar1=-1.0, scalar2=None, op0=ALU.mult))
                chk('ssm_c')
                for pr in range(16):
                    for gp in range(2):
                        g = 2 * pr + gp
                        for ri, src in ((0, Cmr), (1, Cmi)):
                            V(lambda e, g=g, ri=ri, src=src, pr=pr, gp=gp: e.tensor_scalar(
                                out=Cmz[:, g, ri, :], in0=src[:, pr, :, :].rearrange("p a b -> p (a b)"),
                                scalar1=zmask[:, gp:gp + 1], scalar2=None, op0=ALU.mult), writes=[wb, ssm_b])
                chk('ssm_d')
                Bz = [sb(st1, "Bz%d" % i, [128, 2, 128], BF16) for i in range(2)]
                Wz = [sb(st1, "Wz%d" % i, [128, 2, 128], BF16) for i in range(2)]
                Bzb = [Buf(), Buf()]
                At = [sb(st1, "At%d" % i, [128, 128], F32) for i in range(2)]
                Atb = [Buf(), Buf()]
                for pr in range(16):
                    for gp in range(2):
                        g = 2 * pr + gp
                        b = g % 2
                        for ri, src in ((0, BmTr), (1, BmTi)):
                            V(lambda e, b=b, ri=ri, src=src, pr=pr, gp=gp: e.tensor_scalar(
                                out=Bz[b][:, ri, :], in0=src[:, pr, :, :].rearrange("p a b -> p (a b)"),
                                scalar1=zmask[:, gp:gp + 1], scalar2=None, op0=ALU.mult), writes=[wb, Bzb[b]])
                        for ri, src in ((0, Wcr), (1, Wci)):
                            V(lambda e, b=b, ri=ri, src=src, pr=pr, gp=gp: e.tensor_scalar(
                                out=Wz[b][:, ri, :], in0=src[:, pr, :, :].rearrange("p a b -> p (a b)"),
                                scalar1=zmask[:, gp:gp + 1], scalar2=None, op0=ALU.mult), writes=[wb, Bzb[b]])
                        ptt, ptb_ = next_pt()
                        for ri in range(2):
                            S.op("pe", lambda e, ptt=ptt, b=b, ri=ri: e.transpose(
                                out=ptt[:, ri * 128:(ri + 1) * 128], in_=Bz[b][:, ri, :], identity=ident_bf[:]),
                                reads=[Bzb[b], cst], writes=[ptb_])
                        for ri in range(2):
                            S.op("act", lambda e, ptt=ptt, g=g, ri=ri, gp=gp: e.copy(
                                out=Bm_sb[:, g, ri * 64:(ri + 1) * 64],
                                in_=ptt[:, ri * 128 + gp * 64: ri * 128 + gp * 64 + 64]),
                                reads=[ptb_], writes=[ssm_b])
                        p, pb = next_pw()
                        for ri in range(2):
                            S.op("pe", lambda e, p=p, b=b, ri=ri: e.matmul(
                                p[:, 0:128], Bz[b][:, ri, :], Wz[b][:, ri, :], start=(ri == 0), stop=(ri == 1)),
                                reads=[Bzb[b]], writes=[pb])
                        S.op("dve", lambda e, p=p, b=b: e.tensor_tensor(out=At[b][:], in0=p[:, 0:128], in1=bmask[:], op=ALU.mult),
                             reads=[pb, ib], writes=[Atb[b]])
                        S.op("dve", lambda e, b=b, g=g: e.tensor_tensor(out=dsk[:, g, :], in0=dsk[:, g, :], in1=dmask[:], op=ALU.mult),
                             reads=[ib], writes=[ib])
                        S.op("dve", lambda e, b=b, g=g: e.tensor_tensor(out=A_sb[:, g, :], in0=At[b][:], in1=dsk[:, g, :], op=ALU.add),
                             reads=[Atb[b], ib], writes=[ssm_b])
                chk('ssm_e')
                S.dma("sp", ssmA_d, A_sb[:], reads=[ssm_b], writes=[ssmd_b])
                S.dma("sp", ssmB_d, Bm_sb[:], reads=[ssm_b], writes=[ssmd_b])
                S.dma("sp", ssmC_d, Cmz[:], reads=[ssm_b], writes=[ssmd_b])
                S.barrier()

            chk('ssmsetup')
            for s in range(NSEQ):
                r0 = s * SEQ
                with ExitStack() as stt:
                    adaln([tabsA1], [tabsA_b1], [s], 0, n1g_d, stt)
                    S.barrier()
                chk('adaln')
                with ExitStack() as stS:
                    qT = sb(stS, "qT", [128, 4, SEQ], BF16)
                    kTz = sb(stS, "kTz", [128, 2, 2, SEQ], BF16)
                    qiT = sb(stS, "qiT", [128, 4, SEQ], BF16)
                    kiTz = sb(stS, "kiTz", [128, 2, SEQ], BF16)
                    Vp = sb(stS, "Vp", [128, 16, 2, 65], BF16)
                    wi = sb(stS, "wi", [128, 16, 8], F32)
                    mixT_s = sb(stS, "mixT_s", [128, 4, SEQ], BF16)
                    qT_b, kT_b, qiT_b, kiT_b, Vp_b, wi_b, mixT_sb = (Buf() for _ in range(7))
                    S.op("pool", lambda e: e.memset(kTz[:].rearrange("p a b c -> p (a b c)"), 0.0), writes=[kT_b])
                    S.op("pool", lambda e: e.memset(kiTz[:].rearrange("p a c -> p (a c)"), 0.0), writes=[kiT_b])
                    S.op("pool", lambda e: e.memset(Vp[:].rearrange("p a b c -> p (a b c)"), 1.0), writes=[Vp_b])

                    with ExitStack() as st12:
                        U8 = sb(st12, "U8", [128, 2, 32, 8, 16], BF16)
                        U8_b = Buf()
                        with ExitStack() as st1:
                            w_in = sb(st1, "w_in", [128, 8, DIN], BF16)
                            w_in_b = Buf()
                            for kt in range(8):
                                S.dma("pool", w_in[:, kt, :], win_d[:, kt, :], writes=[w_in_b])
                            hT = sb(st1, "hT", [128, 8, SEQ], BF16)
                            hT_b = Buf()
                            st1a = ExitStack()
                            xt = [sb(st1a, "xt%d" % i, [128, D], F32) for i in range(2)]
                            xt_b = [Buf(), Buf()]
                            junk = sb(st1a, "junk", [128, D], BF16)
                            junk_b = Buf()
                            t1 = sb(st1a, "t1", [128, D], F32)
                            hb = [sb(st1a, "hb%d" % i, [128, D], BF16) for i in range(2)]
                            hb_b = [Buf(), Buf()]
                            st_ = [sb(st1a, "st%d" % i, [128, 4], F32) for i in range(2)]
                            st_b = [Buf(), Buf()]
                            t1_b = Buf()
                            for t in range(16):
                                b = t % 2
                                S.dma("sp", xt[b][:], x_d[r0 + t * 128: r0 + (t + 1) * 128, :], writes=[xt_b[b]])
                                S.op("act", lambda e, b=b: e.activation(out=junk[:], in_=xt[b][:], func=AF.Square,
                                                                        accum_out=st_[b][:, 0:1]),
                                     reads=[xt_b[b]], writes=[junk_b, st_b[b]])
                                S.op("act", lambda e, b=b: e.activation(out=st_[b][:, 1:2], in_=st_[b][:, 0:1], func=AF.Sqrt,
                                                                        bias=EPS, scale=1.0 / D),
                                     reads=[st_b[b]], writes=[st_b[b]])
                                S.op("dve", lambda e, b=b: e.reciprocal(out=st_[b][:, 2:3], in_=st_[b][:, 1:2]),
                                     reads=[st_b[b]], writes=[st_b[b]])
                                S.op("dve", lambda e, b=b: e.scalar_tensor_tensor(
                                    out=t1[:], in0=xt[b][:], scalar=st_[b][:, 2:3], in1=tabsA[s][:, 1, :],
                                    op0=ALU.mult, op1=ALU.mult), reads=[xt_b[b], st_b[b], tabsA_b[s]], writes=[t1_b])
                                S.op("dve", lambda e, b=b: e.tensor_tensor(out=hb[b][:], in0=t1[:], in1=tabsA[s][:, 0, :], op=ALU.add),
                                     reads=[t1_b, tabsA_b[s]], writes=[hb_b[b]])
                                ptt, ptb_ = next_pt()
                                for kt in range(8):
                                    S.op("pe", lambda e, ptt=ptt, b=b, kt=kt: e.transpose(
                                        out=ptt[:, kt * 128:(kt + 1) * 128], in_=hb[b][:, kt * 128:(kt + 1) * 128],
                                        identity=ident_bf[:]), reads=[hb_b[b], cst], writes=[ptb_])
                                S.op("act", lambda e, ptt=ptt, t=t: e.copy(
                                    out=hT[:, :, t * 128:(t + 1) * 128], in_=ptt[:].rearrange("p (a b) -> p a b", b=128)),
                                    reads=[ptb_], writes=[hT_b])
                            S.barrier()
                            st1a.close()
                            sq = [sb(st1, "sq%d" % i, [128, 512], BF16) for i in range(2)]
                            sq_b = [Buf(), Buf()]
                            sd = [sb(st1, "sd%d" % i, [128, 512], F32) for i in range(2)]
                            sd_b = [Buf(), Buf()]
                            groups = []
                            for j in range(4):
                                groups.append(("q", j, [(512 + j * 128, 128, 0)]))
                            groups.append(("kA", 0, [(1024, 128, 0)]))
                            groups.append(("kB", 0, [(1088, 64, 0), (1024, 64, 64)]))
                            for j in range(4):
                                groups.append(("qi", j, [(1280 + j * 128, 128, 0)]))
                            groups.append(("ki", 0, [(1792, 64, 0), (1792, 64, 64)]))
                            it = 0
                            for kind, j, parts in groups:
                                for c in range(4):
                                    cs_ = slice(c * 512, (c + 1) * 512)
                                    p, pb = next_pw()
                                    for (c0, m, po) in parts:
                                        for kt in range(8):
                                            S.op("pe", lambda e, p=p, c0=c0, m=m, po=po, kt=kt, cs_=cs_: e.matmul(
                                                p[po:po + m, :], w_in[:, kt, c0:c0 + m], hT[:, kt, cs_],
                                                start=(kt == 0), stop=(kt == 7)),
                                                reads=[w_in_b, hT_b], writes=[pb])
                                    if kind == "qi":
                                        S.op("act", lambda e, p=p, j=j, cs_=cs_: e.copy(out=qiT[:, j, cs_], in_=p[:]),
                                             reads=[pb], writes=[qiT_b])
                                    elif kind == "ki":
                                        for half in range(2):
                                            rs = slice(half * 64, half * 64 + 64)
                                            S.op("act", lambda e, p=p, half=half, rs=rs, cs_=cs_: e.copy(
                                                out=kiTz[rs, half, cs_], in_=p[rs, :]), reads=[pb], writes=[kiT_b])
                                    else:
                                        b = it % 2
                                        it += 1
                                        S.op("act", lambda e, p=p, b=b: e.activation(out=sq[b][:], in_=p[:], func=AF.Square),
                                             reads=[pb], writes=[sq_b[b]])
                                        p2, p2b = next_pw()
                                        S.op("pe", lambda e, p2=p2, b=b: e.matmul(p2[:], onesblk[:], sq[b][:], start=True, stop=True),
                                             reads=[sq_b[b], cst], writes=[p2b])
                                        S.op("act", lambda e, p2=p2, b=b: e.activation(out=sd[b][:], in_=p2[:], func=AF.Sqrt,
                                                                                      bias=EPS, scale=1.0 / 64),
                                             reads=[p2b], writes=[sd_b[b]])
                                        S.op("dve", lambda e, b=b: e.reciprocal(out=sd[b][:], in_=sd[b][:]),
                                             reads=[sd_b[b]], writes=[sd_b[b]])
                                        if kind == "q":
                                            S.op("dve", lambda e, p=p, b=b, j=j, cs_=cs_: e.scalar_tensor_tensor(
                                                out=qT[:, j, cs_], in0=p[:], scalar=G2[:, 0:1], in1=sd[b][:],
                                                op0=ALU.mult, op1=ALU.mult), reads=[pb, sd_b[b], cst], writes=[qT_b])
                                        else:
                                            kvs = (0, 1) if kind == "kA" else (1, 0)
                                            for half in range(2):
                                                rs = slice(half * 64, half * 64 + 64)
                                                kv = kvs[half]
                                                S.op("dve", lambda e, p=p, b=b, rs=rs, kv=kv, half=half, cs_=cs_: e.tensor_tensor(
                                                    out=kTz[rs, kv, half, cs_], in0=p[rs, :], in1=sd[b][rs, :], op=ALU.mult),
                                                    reads=[pb, sd_b[b]], writes=[kT_b])
                            for t in range(16):
                                ts_ = slice(t * 128, (t + 1) * 128)
                                p, pb = next_pw()
                                for kt in range(8):
                                    S.op("pe", lambda e, p=p, kt=kt, ts_=ts_: e.matmul(
                                        p[:, 0:128], hT[:, kt, ts_], w_in[:, kt, 1152:1280], start=(kt == 0), stop=(kt == 7)),
                                        reads=[w_in_b, hT_b], writes=[pb])
                                for kt in range(8):
                                    S.op("pe", lambda e, p=p, kt=kt, ts_=ts_: e.matmul(
                                        p[:, 128:136], hT[:, kt, ts_], w_in[:, kt, 1856:1864], start=(kt == 0), stop=(kt == 7)),
                                        reads=[w_in_b, hT_b], writes=[pb])
                                S.op("act", lambda e, p=p, t=t: e.copy(
                                    out=Vp[:, t, :, 0:64], in_=p[:, 0:128].rearrange("p (a b) -> p a b", b=64)),
                                    reads=[pb], writes=[Vp_b])
                                S.op("act", lambda e, p=p, t=t: e.mul(out=wi[:, t, :], in_=p[:, 128:136], mul=IDX_SCALE),
                                     reads=[pb], writes=[wi_b])
                            for sp in range(2):
                                for i in range(8):
                                    p, pb = next_pw()
                                    for kt in range(8):
                                        lhs = hT[:, kt, sp * 1024:(sp + 1) * 1024].rearrange("p (b i) -> p i b", i=8)[:, i, :]
                                        S.op("pe", lambda e, p=p, lhs=lhs, kt=kt: e.matmul(
                                            p[:], lhs, w_in[:, kt, 0:512], start=(kt == 0), stop=(kt == 7)),
                                            reads=[w_in_b, hT_b], writes=[pb])
                                    S.op("act", lambda e, p=p, sp=sp, i=i: e.copy(
                                        out=U8[:, sp, :, i, :], in_=p[:].rearrange("p (g c) -> p g c", c=16)),
                                         reads=[pb], writes=[U8_b])
                            S.barrier()

                        chk('s1')
                        with ExitStack() as st2:
                            w_glu = sb(st2, "w_glu", [128, 4, 512], BF16)
                            w_glu_b = Buf()
                            S.dma("pool", w_glu[:], wglu_d, writes=[w_glu_b])
                            Ytok = sb(st2, "Ytok", [128, 2, 8, 512], F32)
                            Ytok_b = Buf()
                            U8T = [sb(st2, "U8T%d" % i, [128, 2, 256], BF16) for i in range(2)]
                            U8T_b = [Buf(), Buf()]
                            XA = [sb(st2, "XA%d" % i, [128, 2, 384], F32) for i in range(2)]
                            XB = [sb(st2, "XB%d" % i, [128, 2, 384], F32) for i in range(2)]
                            XA_b, XB_b = [Buf(), Buf()], [Buf(), Buf()]
                            TM = [sb(st2, "TM%d" % i, [128, 2, 256], F32) for i in range(2)]
                            TM_b = [Buf(), Buf()]
                            Xst = [sb(st2, "Xst%d" % i, [128, 2, 258], BF16) for i in range(2)]
                            Xst_b = [Buf(), Buf()]
                            Ysb = [sb(st2, "Ysb%d" % i, [128, 256], F32) for i in range(2)]
                            Ysb_b = [Buf(), Buf()]
                            for i in range(2):
                                S.op("pool", lambda e, i=i: e.memset(XA[i][:].rearrange("p a b -> p (a b)"), 0.0), writes=[XA_b[i]])
                                S.op("pool", lambda e, i=i: e.memset(XB[i][:].rearrange("p a b -> p (a b)"), 0.0), writes=[XB_b[i]])
                                S.op("pool", lambda e, i=i: e.memset(Xst[i][:].rearrange("p a b -> p (a b)"), 0.0), writes=[Xst_b[i]])
                            PAD = 128
                            A2 = [sb(st2, "A2_%d" % i, [128, 2, 128], BF16) for i in range(2)]
                            B2 = [sb(st2, "B2_%d" % i, [128, 2, 128], BF16) for i in range(2)]
                            C2 = [sb(st2, "C2_%d" % i, [128, 2, 2, 128], BF16) for i in range(2)]
                            M2_b = [Buf(), Buf()]

                            def gen_pair(pr):
                                ub = pr % 2
                                S.dma("sp", A2[ub][:], ssmA_d[:, 2 * pr:2 * pr + 2, :], reads=[ssmd_b], writes=[M2_b[ub]])
                                S.dma("sp", B2[ub][:], ssmB_d[:, 2 * pr:2 * pr + 2, :], reads=[ssmd_b], writes=[M2_b[ub]])
                                S.dma("sp", C2[ub][:], ssmC_d[:, 2 * pr:2 * pr + 2, :, :], reads=[ssmd_b], writes=[M2_b[ub]])
                                ptt, ptb_ = next_pt()
                                for gp in range(2):
                                    g = 2 * pr + gp
                                    for sp in range(2):
                                        S.op("pe", lambda e, ptt=ptt, gp=gp, sp=sp, g=g: e.transpose(
                                            out=ptt[:, (gp * 2 + sp) * 128:(gp * 2 + sp + 1) * 128],
                                            in_=U8[:, sp, g, :, :].rearrange("p a b -> p (a b)"), identity=ident_bf[:]),
                                            reads=[U8_b, cst], writes=[ptb_])
                                S.op("act", lambda e, ptt=ptt, ub=ub: e.copy(
                                    out=U8T[ub][:].rearrange("p a b -> p (a b)"), in_=ptt[:, 0:512]),
                                    reads=[ptb_], writes=[U8T_b[ub]])
                                p, pb = next_pw()
                                for gp in range(2):
                                    g = 2 * pr + gp
                                    for ri in range(2):
                                        S.op("pe", lambda e, p=p, gp=gp, g=g, ri=ri, ub=ub: e.matmul(
                                            p[gp * 64:(gp + 1) * 64, ri * 256:(ri + 1) * 256],
                                            B2[ub][:, gp, ri * 64:(ri + 1) * 64], U8T[ub][:, gp, :], start=True, stop=True),
                                            reads=[M2_b[ub], U8T_b[ub]], writes=[pb])
                                S.op("act", lambda e, p=p: e.copy(out=XA[ub][:, :, PAD:PAD + 256],
                                                                  in_=p[:].rearrange("p (a b) -> p a b", b=256)),
                                     reads=[pb], writes=[XA_b[ub]])
                                yield
                                cur, curb, nxt, nxtb = XA[ub], XA_b[ub], XB[ub], XB_b[ub]
                                xs = Xst[ub]
                                tm, tmb = TM[ub], TM_b[ub]
                                for k in range(8):
                                    sft = 2 ** k
                                    last = (k == 7)
                                    a_ = ak[:, pr, k:k + 1]
                                    c_ = ck[:, pr, k:k + 1]
                                    nc_ = nck[:, pr, k:k + 1]
                                    sh = slice(PAD - sft, PAD - sft + 256)
                                    ce = slice(PAD, PAD + 256)
                                    outr = xs[:, 0, 1:257] if last else nxt[:, 0, ce]
                                    outi = xs[:, 1, 1:257] if last else nxt[:, 1, ce]
                                    ob = Xst_b[ub] if last else nxtb
                                    S.op("dve", lambda e, cur=cur, a_=a_, sh=sh, ce=ce: e.scalar_tensor_tensor(
                                        out=tm[:, 0, :], in0=cur[:, 0, sh], scalar=a_, in1=cur[:, 0, ce], op0=ALU.mult, op1=ALU.add),
                                        reads=[curb, ssm_b], writes=[tmb])
                                    S.op("dve", lambda e, cur=cur, a_=a_, sh=sh, ce=ce: e.scalar_tensor_tensor(
                                        out=tm[:, 1, :], in0=cur[:, 1, sh], scalar=a_, in1=cur[:, 1, ce], op0=ALU.mult, op1=ALU.add),
                                        reads=[curb, ssm_b], writes=[tmb])
                                    yield
                                    S.op("dve", lambda e, cur=cur, nc_=nc_, sh=sh, outr=outr: e.scalar_tensor_tensor(
                                        out=outr, in0=cur[:, 1, sh], scalar=nc_, in1=tm[:, 0, :], op0=ALU.mult, op1=ALU.add),
                                        reads=[curb, tmb, ssm_b], writes=[ob])
                                    S.op("dve", lambda e, cur=cur, c_=c_, sh=sh, outi=outi: e.scalar_tensor_tensor(
                                        out=outi, in0=cur[:, 0, sh], scalar=c_, in1=tm[:, 1, :], op0=ALU.mult, op1=ALU.add),
                                        reads=[curb, tmb, ssm_b], writes=[ob])
                                    yield
                                    cur, curb, nxt, nxtb = nxt, nxtb, cur, curb
                                for gp in range(2):
                                    g = 2 * pr + gp
                                    yb = g % 2
                                    p, pb = next_pw()
                                    S.op("pe", lambda e, p=p, g=g, gp=gp, ub=ub: e.matmul(
                                        p[:, 0:256], A2[ub][:, gp, :], U8T[ub][:, gp, :], start=True, stop=False),
                                        reads=[M2_b[ub], U8T_b[ub]], writes=[pb])
                                    for ri in range(2):
                                        S.op("pe", lambda e, p=p, g=g, ri=ri, xs=xs: e.matmul(
                                            p[:, 0:256], C2[ub][:, gp, ri, :], xs[:, ri, 0:256], start=False, stop=(ri == 1)),
                                            reads=[M2_b[ub], Xst_b[ub]], writes=[pb])
                                    S.op("act", lambda e, p=p, yb=yb: e.copy(out=Ysb[yb][:], in_=p[:, 0:256]),
                                         reads=[pb], writes=[Ysb_b[yb]])
                                    p2, p2b = next_pw()
                                    for sp in range(2):
                                        S.op("pe", lambda e, p2=p2, sp=sp, yb=yb: e.transpose(
                                            out=p2[:, sp * 128:(sp + 1) * 128], in_=Ysb[yb][:, sp * 128:(sp + 1) * 128],
                                            identity=ident_f[:]), reads=[Ysb_b[yb], cst], writes=[p2b])
                                    for sp in range(2):
                                        S.op("act", lambda e, p2=p2, sp=sp, g=g: e.copy(
                                            out=Ytok[:, sp, :, g * 16:(g + 1) * 16],
                                            in_=p2[:, sp * 128:(sp + 1) * 128].rearrange("p (a b) -> p a b", b=16)),
                                            reads=[p2b], writes=[Ytok_b])
                                    yield

                            act_p = []
                            nxt_p = 0
                            steps = 0
                            while nxt_p < 16 or act_p:
                                if nxt_p < 16 and len(act_p) < 2 and (nxt_p == 0 or steps >= 6):
                                    if not act_p or act_p[-1][0] % 2 != nxt_p % 2:
                                        act_p.append((nxt_p, gen_pair(nxt_p)))
                                        nxt_p += 1
                                for item in list(act_p):
                                    try:
                                        next(item[1])
                                    except StopIteration:
                                        act_p.remove(item)
                                steps += 1
                            g1 = [sb(st2, "g1_%d" % i, [128, 512], F32) for i in range(2)]
                            g2_ = [sb(st2, "g2_%d" % i, [128, 512], F32) for i in range(2)]
                            zf = [sb(st2, "zf%d" % i, [128, 512], F32) for i in range(2)]
                            zb = [sb(st2, "zb%d" % i, [128, 512], BF16) for i in range(2)]
                            zT = [sb(st2, "zT%d" % i, [128, 4, 128], BF16) for i in range(2)]
                            sg = [sb(st2, "sg%d" % i, [128, 512], F32) for i in range(2)]
                            mb_ = [sb(st2, "mb%d" % i, [128, 512], BF16) for i in range(2)]
                            sst = [sb(st2, "sst%d" % i, [128, 4], F32) for i in range(2)]
                            gb = [[Buf() for _ in range(8)] for _ in range(2)]
                            KG = 2.0 * math.sqrt(2.0 / math.pi)
                            for sp in range(2):
                                for i in range(8):
                                    b = i % 2
                                    B = gb[b]
                                    y = Ytok[:, sp, i, :]
                                    S.op("act", lambda e, b=b, y=y: e.activation(out=g1[b][:], in_=y, func=AF.Square),
                                         reads=[Ytok_b], writes=[B[0]])
                                    S.op("dve", lambda e, b=b: e.tensor_scalar(out=g1[b][:], in0=g1[b][:], scalar1=0.044715,
                                                                               scalar2=1.0, op0=ALU.mult, op1=ALU.add),
                                         reads=[B[0]], writes=[B[0]])
                                    S.op("dve", lambda e, b=b, y=y: e.tensor_tensor(out=g2_[b][:], in0=g1[b][:], in1=y, op=ALU.mult),
                                         reads=[B[0], Ytok_b], writes=[B[1]])
                                    S.op("act", lambda e, b=b: e.activation(out=g2_[b][:], in_=g2_[b][:], func=AF.Sigmoid, scale=KG),
                                         reads=[B[1]], writes=[B[1]])
                                    S.op("dve", lambda e, b=b, y=y: e.tensor_tensor(out=zf[b][:], in0=g2_[b][:], in1=y, op=ALU.mult),
                                         reads=[B[1], Ytok_b], writes=[B[2]])
                                    S.op("act", lambda e, b=b: e.copy(out=zb[b][:], in_=zf[b][:]), reads=[B[2]], writes=[B[3]])
                                    ptt, ptb_ = next_pt()
                                    for ft in range(4):
                                        S.op("pe", lambda e, ptt=ptt, b=b, ft=ft: e.transpose(
                                            out=ptt[:, ft * 128:(ft + 1) * 128], in_=zb[b][:, ft * 128:(ft + 1) * 128],
                                            identity=ident_bf[:]), reads=[B[3], cst], writes=[ptb_])
                                    S.op("act", lambda e, ptt=ptt, b=b: e.copy(out=zT[b][:].rearrange("p a b -> p (a b)"),
                                                                               in_=ptt[:, 0:512]), reads=[ptb_], writes=[B[4]])
                                    p, pb = next_pw()
                                    for ft in range(4):
                                        S.op("pe", lambda e, p=p, b=b, ft=ft: e.matmul(
                                            p[:], zT[b][:, ft, :], w_glu[:, ft, :], start=(ft == 0), stop=(ft == 3)),
                                            reads=[B[4], w_glu_b], writes=[pb])
                                    S.op("dve", lambda e, p=p, b=b: e.tensor_tensor(out=sg[b][:], in0=p[:], in1=bglu_b[:], op=ALU.add),
                                         reads=[pb, cst], writes=[B[5]])
                                    S.op("act", lambda e, b=b: e.activation(out=sg[b][:], in_=sg[b][:], func=AF.Sigmoid),
                                         reads=[B[5]], writes=[B[5]])
                                    S.op("dve", lambda e, b=b: e.tensor_tensor(out=sg[b][:], in0=sg[b][:], in1=zf[b][:], op=ALU.mult),
                                         reads=[B[5], B[2]], writes=[B[5]])
                                    S.op("act", lambda e, b=b: e.activation(out=g1[b][:], in_=sg[b][:], func=AF.Square,
                                                                            accum_out=sst[b][:, 0:1]),
                                         reads=[B[5], B[0]], writes=[B[0], B[6]])
                                    S.op("act", lambda e, b=b: e.activation(out=sst[b][:, 1:2], in_=sst[b][:, 0:1], func=AF.Sqrt,
                                                                            bias=EPS, scale=1.0 / 512), reads=[B[6]], writes=[B[6]])
                                    S.op("dve", lambda e, b=b: e.reciprocal(out=sst[b][:, 2:3], in_=sst[b][:, 1:2]),
                                         reads=[B[6]], writes=[B[6]])
                                    S.op("dve", lambda e, b=b: e.scalar_tensor_tensor(
                                        out=mb_[b][:], in0=sg[b][:], scalar=sst[b][:, 2:3], in1=gns_b[:], op0=ALU.mult, op1=ALU.mult),
                                        reads=[B[5], B[6], cst], writes=[B[7]])
                                    ptt, ptb_ = next_pt()
                                    for ft in range(4):
                                        S.op("pe", lambda e, ptt=ptt, b=b, ft=ft: e.transpose(
                                            out=ptt[:, ft * 128:(ft + 1) * 128], in_=mb_[b][:, ft * 128:(ft + 1) * 128],
                                            identity=ident_bf[:]), reads=[B[7], cst], writes=[ptb_])
                                    dst = mixT_s[:, :, sp * 1024:(sp + 1) * 1024].rearrange("p a (b i) -> p a i b", i=8)[:, :, i, :]
                                    S.op("act", lambda e, ptt=ptt, dst=dst: e.copy(
                                        out=dst, in_=ptt[:, 0:512].rearrange("p (a b) -> p a b", b=128)),
                                        reads=[ptb_], writes=[mixT_sb])
                            S.barrier()

                    chk('s2')
                    with ExitStack() as st3:
                        w_out = sb(st3, "w_out", [128, 8, D], BF16)
                        w_out_b = Buf()
                        S.dma("pool", w_out[:], wout_d, writes=[w_out_b])
                        score = [sb(st3, "score%d" % i, [128, SEQ], F32) for i in range(2)]
                        score_b = [Buf(), Buf()]
                        nm = [sb(st3, "nm%d" % i, [128, SEQ], BF16) for i in range(3)]
                        nm_b = [Buf() for _ in range(3)]
                        rbuf = [sb(st3, "rbuf%d" % i, [128, 8, 512], BF16) for i in range(2)]
                        rbuf_b = [Buf(), Buf()]
                        dg = [sb(st3, "dg%d" % i, [128, 8, 128], BF16) for i in range(2)]
                        dg_b = [Buf(), Buf()]
                        bs = [sb(st3, "bs%d" % i, [128, 16 + 3 * NITER], F32) for i in range(2)]
                        bs_b = [Buf(), Buf()]
                        PT = [sb(st3, "PT%d" % i, [128, 512], BF16) for i in range(3)]
                        PT_b = [Buf() for _ in range(3)]
                        yatt = sb(st3, "yatt", [128, 512], F32)
                        yatt_b = Buf()
                        rden = sb(st3, "rden", [128, 8], F32)
                        ajunk = sb(st3, "ajunk", [128, 512], BF16)
                        ast = sb(st3, "ast", [128, 4], F32)
                        mixa = sb(st3, "mixa", [128, 512], BF16)
                        mixa_b = Buf()
                        mixTa = sb(st3, "mixTa", [128, 4, 128], BF16)
                        mixTa_b = Buf()
                        xr = [sb(st3, "xr%d" % i, [128, D], F32) for i in range(2)]
                        xr_b = [Buf(), Buf()]
                        x1t = [sb(st3, "x1t%d" % i, [128, 512], F32) for i in range(2)]
                        x1t_b = [Buf(), Buf()]
                        prot = {"I": 0, "A": 0, "pt": 0}

                        def pwI():
                            i = prot["I"] % 2
                            prot["I"] += 1
                            return pf[i], pfb[i]

                        def pwA():
                            i = 2 + prot["A"] % 2
                            prot["A"] += 1
                            return pf[i], pfb[i]

                        def gen_I(qt):
                            sl = qt % 2
                            nsl = qt % 3
                            nk = qt + 1
                            SK = nk * 128
                            qs = slice(qt * 128, (qt + 1) * 128)
                            sc, scb = score[sl], score_b[sl]
                            BS, BSb = bs[sl], bs_b[sl]
                            S.op("dve", lambda e: e.tensor_tensor(
                                out=dg[sl][:], in0=ident_bf[:].unsqueeze(1).to_broadcast([128, 8, 128]),
                                in1=wi[:, qt, :].unsqueeze(2).to_broadcast([128, 8, 128]), op=ALU.mult),
                                reads=[cst, wi_b], writes=[dg_b[sl]])
                            nch = (SK + 511) // 512
                            for c in range(nch):
                                cw = min(512, SK - c * 512)
                                ks = slice(c * 512, c * 512 + cw)
                                for h in range(8):
                                    j, half = h // 2, h % 2
                                    p, pb = pwI()
                                    S.op("pe", lambda e, p=p, j=j, half=half: e.matmul(
                                        p[:, 0:cw], qiT[:, j, qs], kiTz[:, half, ks], start=True, stop=True),
                                        reads=[qiT_b, kiT_b], writes=[pb])
                                    if False:
                                        S.op("act", lambda e, p=p, h=h: e.activation(
                                            out=rbuf[sl][:, h, 0:cw], in_=p[:, 0:cw], func=AF.Relu),
                                            reads=[pb], writes=[rbuf_b[sl]])
                                    else:
                                        S.op("dve", lambda e, p=p, h=h: e.tensor_scalar(
                                            out=rbuf[sl][:, h, 0:cw], in0=p[:, 0:cw], scalar1=0.0, scalar2=None, op0=ALU.max),
                                            reads=[pb], writes=[rbuf_b[sl]])
                                p, pb = pwI()
                                for h in range(8):
                                    S.op("pe", lambda e, p=p, h=h: e.matmul(
                                        p[:, 0:cw], dg[sl][:, h, :], rbuf[sl][:, h, 0:cw], start=(h == 0), stop=(h == 7)),
                                        reads=[dg_b[sl], rbuf_b[sl]], writes=[pb])
                                S.op("act", lambda e, p=p: e.copy(out=sc[:, ks], in_=p[:, 0:cw]),
                                     reads=[pb], writes=[scb])
                                S.op("dve", lambda e, c=c: e.tensor_reduce(
                                    out=BS[:, c:c + 1], in_=sc[:, ks], axis=AX.X, op=ALU.max, apply_absolute_value=True),
                                    reads=[scb], writes=[BSb])
                                yield
                            S.op("dve", lambda e: e.tensor_tensor(out=sc[:, qs], in0=sc[:, qs], in1=causal[:], op=ALU.add),
                                 reads=[scb, cst], writes=[scb])
                            if qt >= 2:
                                S.op("dve", lambda e: e.tensor_reduce(
                                    out=BS[:, 4:5], in_=BS[:, 0:nch], axis=AX.X, op=ALU.max), reads=[BSb], writes=[BSb])
                                S.op("dve", lambda e: e.tensor_scalar(
                                    out=BS[:, 8:8 + NITER + 1], in0=pow2[:, 0:NITER + 1], scalar1=BS[:, 4:5], scalar2=None,
                                    op0=ALU.mult), reads=[BSb, cst], writes=[BSb])
                                S.op("dve", lambda e: e.memset(BS[:, 5:6], 0.0), reads=[BSb], writes=[BSb])
                                thr = float(2 * TOPK - SK) - 0.5
                                S.op("dve", lambda e: e.memset(BS[:, 3:4], -thr), reads=[BSb], writes=[BSb])
                                S.op("dve", lambda e: e.tensor_scalar(
                                    out=BS[:, 9 + NITER:10 + 2 * NITER], in0=BS[:, 8:9 + NITER], scalar1=-1.0, scalar2=None,
                                    op0=ALU.mult), reads=[BSb], writes=[BSb])
                                for n in range(NITER):
                                    S.op("act", lambda e: e.activation(
                                        out=nm[nsl][:, 0:SK], in_=sc[:, 0:SK], func=AF.Sign, bias=BS[:, 5:6], scale=1.0,
                                        accum_out=BS[:, 6:7]), reads=[scb, BSb], writes=[nm_b[nsl], BSb])
                                    yield
                                    S.op("act", lambda e: e.activation(
                                        out=BS[:, 7:8], in_=BS[:, 6:7], func=AF.Sign, bias=BS[:, 3:4], scale=1.0),
                                        reads=[BSb], writes=[BSb])
                                    yield
                                    S.op("act", lambda e, n=n: e.activation(
                                        out=BS[:, 5:6], in_=BS[:, 7:8], func=AF.Identity, bias=BS[:, 5:6],
                                        scale=BS[:, 9 + NITER + 1 + n:10 + NITER + 1 + n]),
                                        reads=[BSb], writes=[BSb])
                                    yield
                                S.op("dve", lambda e: e.tensor_tensor(
                                    out=BS[:, 5:6], in0=BS[:, 5:6], in1=BS[:, 8 + NITER:9 + NITER], op=ALU.add),
                                    reads=[BSb], writes=[BSb])
                                ntau = BS[:, 5:6]
                            else:
                                ntau = taufix[:, 0:1]
                            S.op("dve", lambda e: e.tensor_scalar(
                                out=nm[nsl][:, 0:SK], in0=sc[:, 0:SK], scalar1=ntau, scalar2=0.0, op0=ALU.add, op1=ALU.is_lt),
                                reads=[scb, BSb, cst], writes=[nm_b[nsl]])
                            yield

                        def gen_A(qt):
                            nsl = qt % 3
                            nk = qt + 1
                            qs = slice(qt * 128, (qt + 1) * 128)
                            O = [pf[4], pf[5]]
                            Ob = [pfb[4], pfb[5]]
                            for kv in range(2):
                                S.op("pe", lambda e, kv=kv: e.matmul(O[kv][:, 0:260], zeros_bf[:, 0:128], zeros_bf[:, 0:260],
                                                                     start=True, stop=True), reads=[cst], writes=[Ob[kv]])
                            for kt in range(nk):
                                ksl = slice(kt * 128, (kt + 1) * 128)
                                for kv in range(2):
                                    p, pb = pwA()
                                    S.op("pe", lambda e, p=p: e.matmul(
                                        p[:], nm[nsl][:, ksl], negi4[:], start=True, stop=True),
                                        reads=[nm_b[nsl], cst], writes=[pb])
                                    for hh in range(4):
                                        head = kv * 4 + hh
                                        j, half = head // 2, head % 2
                                        S.op("pe", lambda e, p=p, hh=hh, kv=kv, half=half, j=j: e.matmul(
                                            p[:, hh * 128:(hh + 1) * 128], kTz[:, kv, half, ksl], qT[:, j, qs],
                                            start=False, stop=True, skip_group_check=True),
                                            reads=[kT_b, qT_b], writes=[pb])
                                    pi3 = prot["pt"] % 3
                                    prot["pt"] += 1
                                    S.op("act", lambda e, p=p, pi3=pi3: e.activation(
                                        out=PT[pi3][:], in_=p[:], func=AF.Exp, bias=negM[:, 0:1], scale=1.0),
                                        reads=[pb, cst], writes=[PT_b[pi3]])
                                    for hh in range(4):
                                        S.op("pe", lambda e, kv=kv, hh=hh, pi3=pi3, kt=kt: e.matmul(
                                            O[kv][:, hh * 65:(hh + 1) * 65], PT[pi3][:, hh * 128:(hh + 1) * 128], Vp[:, kt, kv, :],
                                            start=False, stop=True, skip_group_check=True),
                                            reads=[PT_b[pi3], Vp_b], writes=[Ob[kv]])
                                    yield
                            for kv in range(2):
                                ov = O[kv][:, 0:260].rearrange("p (h e) -> p h e", e=65)
                                S.op("dve", lambda e, kv=kv, ov=ov: e.reciprocal(out=rden[:, kv * 4:(kv + 1) * 4], in_=ov[:, :, 64]),
                                     reads=[Ob[kv]], writes=[yatt_b])
                                S.op("dve", lambda e, kv=kv, ov=ov: e.tensor_tensor(
                                    out=yatt[:, kv * 256:(kv + 1) * 256].rearrange("p (h d) -> p h d", d=64), in0=ov[:, :, 0:64],
                                    in1=rden[:, kv * 4:(kv + 1) * 4].unsqueeze(2).to_broadcast([128, 4, 64]), op=ALU.mult),
                                    reads=[Ob[kv], yatt_b], writes=[yatt_b])
                            S.op("act", lambda e: e.activation(out=ajunk[:], in_=yatt[:], func=AF.Square, accum_out=ast[:, 0:1]),
                                 reads=[yatt_b], writes=[yatt_b])
                            S.op("act", lambda e: e.activation(out=ast[:, 1:2], in_=ast[:, 0:1], func=AF.Sqrt, bias=EPS, scale=1.0 / 512),
                                 reads=[yatt_b], writes=[yatt_b])
                            S.op("dve", lambda e: e.reciprocal(out=ast[:, 2:3], in_=ast[:, 1:2]), reads=[yatt_b], writes=[yatt_b])
                            S.op("dve", lambda e: e.scalar_tensor_tensor(out=mixa[:], in0=yatt[:], scalar=ast[:, 2:3], in1=gna_b[:],
                                                                          op0=ALU.mult, op1=ALU.mult),
                                 reads=[yatt_b, cst], writes=[mixa_b])
                            ptt, ptb_ = next_pt()
                            for ft in range(4):
                                S.op("pe", lambda e, ptt=ptt, ft=ft: e.transpose(
                                    out=ptt[:, ft * 128:(ft + 1) * 128], in_=mixa[:, ft * 128:(ft + 1) * 128], identity=ident_bf[:]),
                                    reads=[mixa_b, cst], writes=[ptb_])
                            S.op("act", lambda e, ptt=ptt: e.copy(out=mixTa[:].rearrange("p a b -> p (a b)"), in_=ptt[:, 0:512]),
                                 reads=[ptb_], writes=[mixTa_b])
                            yield
                            xb = qt % 2
                            rows = slice(r0 + qt * 128, r0 + (qt + 1) * 128)
                            S.dma("sp", xr[xb][:], x_d[rows, :], writes=[xr_b[xb]])
                            for hf in range(2):
                                p, pb = pwA()
                                for k8 in range(8):
                                    lhs = mixT_s[:, k8, qs] if k8 < 4 else mixTa[:, k8 - 4, :]
                                    S.op("pe", lambda e, p=p, lhs=lhs, k8=k8, hf=hf: e.matmul(
                                        p[:], lhs, w_out[:, k8, hf * 512:(hf + 1) * 512], start=(k8 == 0), stop=(k8 == 7)),
                                        reads=[mixT_sb, mixTa_b, w_out_b], writes=[pb])
                                hs = slice(hf * 512, (hf + 1) * 512)
                                S.op("dve", lambda e, p=p, hf=hf, hs=hs: e.tensor_tensor(
                                    out=x1t[hf][:], in0=p[:], in1=tabsA[s][:, 2, hs], op=ALU.mult),
                                    reads=[pb, tabsA_b[s]], writes=[x1t_b[hf]])
                                S.op("dve", lambda e, xb=xb, hf=hf, hs=hs: e.tensor_tensor(
                                    out=xr[xb][:, hs], in0=x1t[hf][:], in1=xr[xb][:, hs], op=ALU.add),
                                    reads=[x1t_b[hf], xr_b[xb]], writes=[xr_b[xb]])
                            S.dma("sp", out_d[rows, :], xr[xb][:], reads=[xr_b[xb]])
                            yield

                        NQ = 16
                        actI = []
                        actA = None
                        nextI, nextA = 0, 0
                        doneI = set()
                        while nextA < NQ or actA is not None:
                            if actA is None and nextA in doneI:
                                actA = (nextA, gen_A(nextA))
                                nextA += 1
                            firstA = actA[0] if actA is not None else nextA
                            while (nextI < NQ and len(actI) < 2 and nextI - 3 < firstA
                                   and (nextI < 2 or (nextI - 2) in doneI)):
                                actI.append((nextI, gen_I(nextI)))
                                nextI += 1
                            for item in list(actI):
                                try:
                                    next(item[1])
                                except StopIteration:
                                    actI.remove(item)
                                    doneI.add(item[0])
                            if actA is not None:
                                try:
                                    next(actA[1])
                                except StopIteration:
                                    actA = None
                        S.barrier()

        chk('s3')
        NB = 256
        NTT = NB // 128
        with ExitStack() as stB:
            tabB_d = nc.dram_tensor("tabB_scr", [NSEQ, 128, 3 * D], F32, kind="Internal").ap()
            tabBd_b = Buf()
            with ExitStack() as stt:
                tl = [sb(stt, "tabsBt%d" % s, [128, 3, D], F32) for s in range(NSEQ)]
                tlb = [Buf() for _ in range(NSEQ)]
                adaln(tl, tlb, list(range(NSEQ)), 3, n2g_d, stt)
                for s in range(NSEQ):
                    S.dma("sp", tabB_d[s], tl[s][:].rearrange("p a b -> p (a b)"), reads=[tlb[s]], writes=[tabBd_b])
                S.barrier()
            tabsB1 = sb(stB, "tabsB", [128, 3, D], F32)
            tabsB_b1 = Buf()
            W1 = sb(stB, "W1", [128, 8, DFF], BF16)
            W2 = sb(stB, "W2", [128, 32, D], BF16)
            W_b = Buf()
            for kt in range(8):
                S.dma("pool", W1[:, kt, :], wff1_d[:, kt, :], writes=[W_b])
            for ft in range(0, 32, 4):
                S.dma("pool", W2[:, ft:ft + 4, :], wff2_d[:, ft:ft + 4, :], writes=[W_b])
            hidT = sb(stB, "hidT", [128, 32, NB], BF16)
            hid_b = [Buf() for _ in range(32)]
            rl = [sb(stB, "rl%d" % i, [128, NB], BF16) for i in range(2)]
            rl_b = [Buf(), Buf()]
            h2T = sb(stB, "h2T", [128, 8, NB], BF16)
            h2T_b = Buf()
            x1 = [sb(stB, "x1_%d" % i, [128, D], F32) for i in range(2)]
            x1_b = [Buf() for _ in range(2)]
            junkB = sb(stB, "junkB", [128, D], BF16)
            junkB_b = Buf()
            tB = sb(stB, "tB", [128, D], F32)
            tB_b = Buf()
            h2 = [sb(stB, "h2_%d" % i, [128, D], BF16) for i in range(2)]
            h2_b = [Buf(), Buf()]
            stb = [sb(stB, "stb%d" % i, [128, 4], F32) for i in range(2)]
            stb_b = [Buf(), Buf()]
            ot = [sb(stB, "ot%d" % i, [128, D], F32) for i in range(2)]
            ot_b = [Buf(), Buf()]
            oc = 0
            nblk = NSEQ * SEQ // NB
            for blk in range(nblk):
                s = (blk * NB) // SEQ
                if (blk * NB) % SEQ == 0:
                    S.dma("sp", tabsB1[:].rearrange("p a b -> p (a b)"), tabB_d[s], reads=[tabBd_b], writes=[tabsB_b1])
                for tt in range(NTT):
                    rows = slice(blk * NB + tt * 128, blk * NB + (tt + 1) * 128)
                    b = tt % 2
                    S.dma("sp", x1[b][:], out_d[rows, :], writes=[x1_b[b]])
                    S.op("act", lambda e, b=b: e.activation(out=junkB[:], in_=x1[b][:], func=AF.Square,
                                                            accum_out=stb[b][:, 0:1]),
                         reads=[x1_b[b]], writes=[junkB_b, stb_b[b]])
                    S.op("act", lambda e, b=b: e.activation(out=stb[b][:, 1:2], in_=stb[b][:, 0:1], func=AF.Sqrt,
                                                            bias=EPS, scale=1.0 / D), reads=[stb_b[b]], writes=[stb_b[b]])
                    S.op("dve", lambda e, b=b: e.reciprocal(out=stb[b][:, 2:3], in_=stb[b][:, 1:2]),
                         reads=[stb_b[b]], writes=[stb_b[b]])
                    S.op("dve", lambda e, b=b: e.scalar_tensor_tensor(
                        out=tB[:], in0=x1[b][:], scalar=stb[b][:, 2:3], in1=tabsB1[:, 1, :], op0=ALU.mult, op1=ALU.mult),
                        reads=[x1_b[b], stb_b[b], tabsB_b1], writes=[tB_b])
                    S.op("dve", lambda e, b=b: e.tensor_tensor(out=h2[b][:], in0=tB[:], in1=tabsB1[:, 0, :], op=ALU.add),
                         reads=[tB_b, tabsB_b1], writes=[h2_b[b]])
                    ptt, ptb_ = next_pt()
                    for kt in range(8):
                        S.op("pe", lambda e, ptt=ptt, b=b, kt=kt: e.transpose(
                            out=ptt[:, kt * 128:(kt + 1) * 128], in_=h2[b][:, kt * 128:(kt + 1) * 128], identity=ident_bf[:]),
                            reads=[h2_b[b], cst], writes=[ptb_])
                    S.op("act", lambda e, ptt=ptt, tt=tt: e.copy(
                        out=h2T[:, :, tt * 128:(tt + 1) * 128], in_=ptt[:].rearrange("p (a b) -> p a b", b=128)),
                        reads=[ptb_], writes=[h2T_b])
                for ft in range(32):
                    p, pb = next_pw(2)
                    for kt in range(8):
                        S.op("pe", lambda e, p=p, kt=kt, ft=ft: e.matmul(
                            p[:, 0:NB], W1[:, kt, ft * 128:(ft + 1) * 128], h2T[:, kt, :], start=(kt == 0), stop=(kt == 7)),
                            reads=[W_b, h2T_b], writes=[pb])
                    b = ft % 2
                    S.op("act", lambda e, p=p, b=b: e.activation(out=rl[b][:], in_=p[:, 0:NB], func=AF.Relu),
                         reads=[pb], writes=[rl_b[b]])
                    S.op("pool", lambda e, b=b, ft=ft: e.tensor_tensor(out=hidT[:, ft, :], in0=rl[b][:], in1=rl[b][:], op=ALU.mult),
                         reads=[rl_b[b]], writes=[hid_b[ft]])
                for tt in range(NTT):
                    rows = slice(blk * NB + tt * 128, blk * NB + (tt + 1) * 128)
                    ob = oc % 2
                    oc += 1
                    S.dma("sp", ot[ob][:], out_d[rows, :], writes=[ot_b[ob]])
                    for hf in range(2):
                        bi = 2 + (2 * tt + hf) % 4
                        p, pb = pf[bi], pfb[bi]
                        for ft in range(32):
                            S.op("pe", lambda e, p=p, ft=ft, tt=tt, hf=hf: e.matmul(
                                p[:], hidT[:, ft, tt * 128:(tt + 1) * 128], W2[:, ft, hf * 512:(hf + 1) * 512],
                                start=(ft == 0), stop=(ft == 31)), reads=[hid_b[ft], W_b], writes=[pb])
                        hs = slice(hf * 512, (hf + 1) * 512)
                        S.op("dve", lambda e, p=p, hs=hs: e.tensor_tensor(
                            out=tB[:, hs], in0=p[:], in1=tabsB1[:, 2, hs], op=ALU.mult),
                            reads=[pb, tabsB_b1], writes=[tB_b])
                        S.op("dve", lambda e, ob=ob, hs=hs: e.tensor_tensor(
                            out=ot[ob][:, hs], in0=tB[:, hs], in1=ot[ob][:, hs], op=ALU.add),
                            reads=[tB_b, ot_b[ob]], writes=[ot_b[ob]])
                    S.dma("sp", out_d[rows, :], ot[ob][:], reads=[ot_b[ob]])
            S.barrier()
    except _Stop:
        pass
    return nc


_PROGRAM = None


def _prep_shared(inp):
    f = np.float32
    def kt_layout(w):
        K, N = w.shape
        return np.ascontiguousarray(w.reshape(K // 128, 128, N).transpose(1, 0, 2)).astype(f)
    def bc(v, n=128):
        return np.ascontiguousarray(np.broadcast_to(np.asarray(v, f).reshape(1, -1), (n, np.asarray(v).size)))
    def qlay(a):
        a = np.asarray(a, f)
        rest = a.shape[2:]
        return np.ascontiguousarray(a.reshape((16, 2, 64) + rest).transpose((1, 2, 0) + tuple(range(3, 3 + len(rest))))
                                    .reshape((128, 16) + rest))
    sh = {}
    sh["w_ada"] = kt_layout(inp["w_ada"][0])
    sh["b_ada_b"] = bc(inp["b_ada"][0])
    sh["w_in"] = kt_layout(inp["w_in"][0])
    sh["w_out"] = kt_layout(inp["w_out"][0])
    sh["w_glu"] = kt_layout(inp["w_glu"][0])
    sh["w_ff1"] = kt_layout(inp["w_ff1"][0])
    sh["w_ff2"] = kt_layout(inp["w_ff2"][0])
    sh["norm1_g_b"] = bc(inp["norm1_g"][0])
    sh["norm2_g_b"] = bc(inp["norm2_g"][0])
    sh["b_glu_b"] = bc(inp["b_glu"][0])
    sh["gn_ssm_b"] = bc(inp["gn_ssm"][0])
    sh["gn_attn_b"] = bc(inp["gn_attn"][0])
    sh["gq_b"] = bc(inp["q_gain"][0])
    sh["gk_b"] = bc(inp["k_gain"][0])
    sh["gq2"] = np.ascontiguousarray(np.tile(np.asarray(inp["q_gain"][0], f), 2).reshape(128, 1))
    sh["gk2"] = np.ascontiguousarray(np.tile(np.asarray(inp["k_gain"][0], f), 2).reshape(128, 1))
    sh["lamre_q"] = qlay(inp["lam_re"][0])
    sh["lamim_q"] = qlay(inp["lam_im"][0])
    sh["logdt_q"] = qlay(np.broadcast_to(np.asarray(inp["log_dt"][0], f)[:, None], (32, 64)))
    sh["bre_q"] = qlay(inp["ssm_b_re"][0])
    sh["bim_q"] = qlay(inp["ssm_b_im"][0])
    sh["cre_q"] = qlay(np.asarray(inp["ssm_c_re"][0]).transpose(0, 2, 1))
    sh["cim_q"] = qlay(np.asarray(inp["ssm_c_im"][0]).transpose(0, 2, 1))
    dsk = np.asarray(inp["d_skip"][0], f).reshape(32, 16)
    sh["dsk_b"] = np.ascontiguousarray(np.broadcast_to(np.tile(dsk, (1, 8))[None], (128, 32, 128))).astype(f)
    sh["ident"] = np.eye(128, dtype=f)
    r = np.arange(128)
    sh["causal"] = np.where(r[None, :] <= r[:, None], 0.0, -1.0e30).astype(f)
    sh["negi4"] = np.tile(-BIG * np.eye(128, dtype=f), (1, 4)).astype(f)
    ib, jb = r // 16, r // 16
    sh["bmask"] = (jb[None, :] >= ib[:, None]).astype(f)
    sh["dmask"] = (r[None, :] == r[:, None]).astype(f)
    sh["onesblk"] = ((r[None, :] // 64) == (r[:, None] // 64)).astype(f)
    sh["mtab"] = np.ascontiguousarray(np.broadcast_to(np.asarray(EXPS, f)[None, None, :], (128, 16, NE)))
    sh["pow2"] = np.ascontiguousarray(np.broadcast_to((2.0 ** -np.arange(NITER + 2)).astype(f)[None], (128, NITER + 2)))
    zm = np.zeros((128, 2), f)
    zm[:64, 0] = 1.0
    zm[64:, 1] = 1.0
    sh["zmask"] = zm
    return sh


def kernel(**inputs):
    global _PROGRAM
    inp = {k: np.asarray(v) for k, v in inputs.items()}
    if _PROGRAM is None:
        _PROGRAM = build_program()
    nc = _PROGRAM
    shared = _prep_shared(inp)
    x = np.asarray(inp["x"], np.float32)
    c = np.asarray(inp["c"], np.float32)
    in_maps = []
    for core in range(NCORES):
        m = dict(shared)
        m["x"] = np.ascontiguousarray(x[NSEQ * core: NSEQ * (core + 1)].reshape(NSEQ * SEQ, D))
        cc = c[NSEQ * core: NSEQ * (core + 1)]
        cT = cc.reshape(NSEQ, 8, 128).transpose(2, 1, 0)
        m["cT"] = np.ascontiguousarray(np.broadcast_to(cT[:, :, :, None], (128, 8, NSEQ, 128))).astype(np.float32)
        in_maps.append(m)
    res = run_bass_kernel_spmd(nc, in_maps, core_ids=list(range(NCORES)))
    outs = [np.asarray(r["out"], np.float32).reshape(NSEQ, SEQ, D) for r in res.results]
    return np.concatenate(outs, axis=0)
```

```python
import math
from contextlib import ExitStack

import numpy as np
import concourse.bass as bass
import concourse.mybir as mybir
from concourse.alu_op_type import AluOpType as ALU
from concourse.bass_utils import run_bass_kernel_spmd

F32 = mybir.dt.float32
BF16 = mybir.dt.bfloat16
I32 = mybir.dt.int32
AF = mybir.ActivationFunctionType
AX = mybir.AxisListType

NCORES = 8
D = 1024
SEQ = 2048
NSEQ = 2
DIN = 1864
DFF = 4096
EPS = 1e-6
IDX_SCALE = (64 ** -0.5) * (8 ** -0.5)
TOPK = 256
NITER = 16
BIG = 30000.0
EXPS = list(range(-7, 9)) + [16, 32, 64, 128, 256, 512, 1024]
NE = len(EXPS)
EIDX = {m: i for i, m in enumerate(EXPS)}
TWO_PI = 2.0 * math.pi


class Buf:
    __slots__ = ("w", "r", "ex")

    def __init__(self, ex=False):
        self.w = None
        self.r = {}
        self.ex = ex


class Sched:
    NDS = 24

    def __init__(self, nc, st):
        self.nc = nc
        self.eng = {"pe": nc.tensor, "act": nc.scalar, "dve": nc.vector, "pool": nc.gpsimd, "sp": nc.sync}
        self.sem = {k: st.enter_context(nc.semaphore("s_" + k)) for k in self.eng}
        self.cnt = {k: 0 for k in self.eng}
        self.waited = {k: {} for k in self.eng}
        self.dsem = [st.enter_context(nc.semaphore("d%d" % i)) for i in range(self.NDS)]
        self.dcnt = [0] * self.NDS
        self.dpool = {"sp": list(range(0, 16)), "pool": list(range(16, 24))}
        self.dnext = {"sp": 0, "pool": 0}

    def _semof(self, key):
        return self.sem[key[1]] if key[0] == "e" else self.dsem[key[1]]

    def _wait(self, e, key, val):
        if key[0] == "e" and key[1] == e and e == "pe":
            return
        if self.waited[e].get(key, 0) >= val:
            return
        self.eng[e].wait_ge(self._semof(key), val)
        self.waited[e][key] = val

    def _deps(self, e, reads, writes):
        for b in reads:
            if b.w is not None:
                self._wait(e, b.w[0], b.w[1])
            if b.ex:
                for k, v in b.r.items():
                    if k != ("e", e):
                        self._wait(e, k, v)
        for b in writes:
            if b.w is not None:
                self._wait(e, b.w[0], b.w[1])
            for k, v in b.r.items():
                self._wait(e, k, v)

    def _mark(self, key, val, reads, writes):
        for b in reads:
            if b.r.get(key, 0) < val:
                b.r[key] = val
        for b in writes:
            b.w = (key, val)
            b.r = {}

    def op(self, e, fn, reads=(), writes=()):
        self._deps(e, reads, writes)
        ins = fn(self.eng[e])
        self.cnt[e] += 1
        ins.then_inc(self.sem[e], 1)
        self._mark(("e", e), self.cnt[e], reads, writes)

    def dma(self, e, out, in_, reads=(), writes=(), **kw):
        self._deps(e, reads, writes)
        pl = self.dpool[e]
        j = pl[self.dnext[e] % len(pl)]
        self.dnext[e] += 1
        if self.dcnt[j] > 0:
            self._wait(e, ("d", j), self.dcnt[j])
        ins = self.eng[e].dma_start(out=out, in_=in_, **kw)
        self.dcnt[j] += 16
        ins.then_inc(self.dsem[j], 16)
        self._mark(("d", j), self.dcnt[j], reads, writes)

    def barrier(self):
        for e in self.eng:
            for k in self.eng:
                if k != e and self.cnt[k] > 0:
                    self._wait(e, ("e", k), self.cnt[k])
            for j in range(self.NDS):
                if self.dcnt[j] > 0:
                    self._wait(e, ("d", j), self.dcnt[j])


class _Stop(Exception):
    pass


def build_program(stop=None):
    nc = bass.Bass("TRN2", target_bir_lowering=False)

    def din(name, shape, dt=F32):
        return nc.dram_tensor(name, list(shape), dt, kind="ExternalInput").ap()

    x_d = din("x", [NSEQ * SEQ, D])
    cT_d = din("cT", [128, 8, NSEQ, 128])
    wada_d = din("w_ada", [128, 8, 6 * D])
    bada_d = din("b_ada_b", [128, 6 * D])
    win_d = din("w_in", [128, 8, DIN])
    wout_d = din("w_out", [128, 8, D])
    wglu_d = din("w_glu", [128, 4, 512])
    wff1_d = din("w_ff1", [128, 8, DFF])
    wff2_d = din("w_ff2", [128, 32, D])
    n1g_d = din("norm1_g_b", [128, D])
    n2g_d = din("norm2_g_b", [128, D])
    bglu_d = din("b_glu_b", [128, 512])
    gns_d = din("gn_ssm_b", [128, 512])
    gna_d = din("gn_attn_b", [128, 512])
    gqb_d = din("gq_b", [128, 64])
    gkb_d = din("gk_b", [128, 64])
    gq2_d = din("gq2", [128, 1])
    gk2_d = din("gk2", [128, 1])
    lre_d = din("lamre_q", [128, 16])
    lim_d = din("lamim_q", [128, 16])
    ldt_d = din("logdt_q", [128, 16])
    bre_d = din("bre_q", [128, 16, 16])
    bim_d = din("bim_q", [128, 16, 16])
    cre_d = din("cre_q", [128, 16, 16])
    cim_d = din("cim_q", [128, 16, 16])
    dsk_d = din("dsk_b", [128, 32, 128])
    ident_d = din("ident", [128, 128])
    causal_d = din("causal", [128, 128])
    negi4_d = din("negi4", [128, 512])
    bmask_d = din("bmask", [128, 128])
    dmask_d = din("dmask", [128, 128])
    onesblk_d = din("onesblk", [128, 128])
    mtab_d = din("mtab", [128, 16, NE])
    pow2_d = din("pow2", [128, NITER + 2])
    zmask_d = din("zmask", [128, 2])
    out_d = nc.dram_tensor("out", [NSEQ * SEQ, D], F32, kind="ExternalOutput").ap()

    try:
      with ExitStack() as top:
        S = Sched(nc, top)

        def chk(name):
            if stop == name:
                S.barrier()
                raise _Stop()

        uid = [0]

        def sb(st, name, shape, dt):
            uid[0] += 1
            return st.enter_context(nc.sbuf_tensor("sb%d_%s" % (uid[0], name), list(shape), dt))

        def ps(st, name, shape, dt):
            uid[0] += 1
            return st.enter_context(nc.psum_tensor("ps%d_%s" % (uid[0], name), list(shape), dt))

        pt = [ps(top, "pt0", [128, 1024], BF16)]
        ptb = [Buf(ex=True)]
        xbank = {}

        def extra_bank(st, as_bf16):
            if as_bf16:
                t = ps(st, "ptx", [128, 1024], BF16)
                del pt[1:], ptb[1:]
                pt.append(t)
                ptb.append(Buf(ex=True))
                rot["pt"] = 0
            else:
                del pt[1:], ptb[1:]
                rot["pt"] = 0
                xbank["t"] = ps(st, "pfx", [128, 512], F32)
                xbank["b"] = Buf(ex=True)
        pf = [ps(top, "pf%d" % i, [128, 512], F32) for i in range(6)]
        pfb = [Buf(ex=True) for _ in range(6)]
        rot = {"pt": 0, "pw": 0}

        def next_pt():
            i = rot["pt"] % len(pt)
            rot["pt"] = i + 1
            return pt[i], ptb[i]

        def next_pw(n=4):
            i = rot["pw"] % n
            rot["pw"] += 1
            return pf[i], pfb[i]

        ident_bf = sb(top, "ident_bf", [128, 128], BF16)
        ident_f = sb(top, "ident_f", [128, 128], F32)
        causal = sb(top, "causal", [128, 128], F32)
        negi4 = sb(top, "negi4", [128, 512], BF16)
        onesblk = sb(top, "onesblk", [128, 128], BF16)
        zeros_bf = sb(top, "zeros_bf", [128, 260], BF16)
        bglu_b = sb(top, "bglu_b", [128, 512], F32)
        gns_b = sb(top, "gns_b", [128, 512], F32)
        gna_b = sb(top, "gna_b", [128, 512], F32)
        pow2 = sb(top, "pow2", [128, NITER + 2], F32)
        G2 = sb(top, "G2", [128, 1], F32)
        negM = sb(top, "negM", [128, 1], F32)
        taufix = sb(top, "taufix", [128, 1], F32)
        siluT = sb(top, "siluT", [128, 8, NSEQ, 128], BF16)
        cst = Buf()
        for t_, d_ in ((ident_bf, ident_d), (ident_f, ident_d), (causal, causal_d), (negi4, negi4_d),
                       (onesblk, onesblk_d), (bglu_b, bglu_d), (gns_b, gns_d), (gna_b, gna_d), (pow2, pow2_d)):
            S.dma("pool", t_[:], d_, writes=[cst])
        S.op("dve", lambda e: e.memset(zeros_bf[:], 0.0), writes=[cst])
        S.op("dve", lambda e: e.memset(taufix[:], 1.0e29), writes=[cst])

        with ExitStack() as st0:
            gqb = sb(st0, "gqb", [128, 64], F32)
            gkb = sb(st0, "gkb", [128, 64], F32)
            gq2 = sb(st0, "gq2", [128, 1], F32)
            gk2 = sb(st0, "gk2", [128, 1], F32)
            cTf = sb(st0, "cTf", [128, 8 * NSEQ * 128], F32)
            tb = Buf()
            S.dma("sp", gqb[:], gqb_d, writes=[tb])
            S.dma("sp", gkb[:], gkb_d, writes=[tb])
            S.dma("sp", gq2[:], gq2_d, writes=[tb])
            S.dma("sp", gk2[:], gk2_d, writes=[tb])
            S.dma("sp", cTf[:], cT_d.rearrange("p a b c -> p (a b c)"), writes=[tb])
            S.op("dve", lambda e: e.scalar_tensor_tensor(out=G2[:], in0=gq2[:], scalar=0.125, in1=gk2[:],
                                                          op0=ALU.mult, op1=ALU.mult), reads=[tb], writes=[cst])
            S.op("dve", lambda e: e.scalar_tensor_tensor(out=gqb[:], in0=gqb[:], scalar=0.125, in1=gkb[:],
                                                          op0=ALU.mult, op1=ALU.mult), reads=[tb], writes=[tb])
            S.op("dve", lambda e: e.tensor_reduce(out=negM[:], in_=gqb[:], axis=AX.X, op=ALU.max,
                                                  apply_absolute_value=True), reads=[tb], writes=[cst])
            S.op("dve", lambda e: e.tensor_scalar(out=negM[:], in0=negM[:], scalar1=-64.0, scalar2=None,
                                                  op0=ALU.mult), reads=[cst], writes=[cst])
            S.op("act", lambda e: e.activation(out=siluT[:].rearrange("p a b c -> p (a b c)"), in_=cTf[:],
                                               func=AF.Silu), reads=[tb], writes=[cst])
            S.barrier()

        chk('consts')
        def adaln(tabs, tabs_b, seqs, first_j, ng_d, st):
            wst = [sb(st, "wst%d" % i, [128, 8, 512], BF16) for i in range(2)]
            wstb = [Buf(), Buf()]
            bst = [sb(st, "bst%d" % i, [128, 512], F32) for i in range(2)]
            gst = [sb(st, "gst%d" % i, [128, 512], F32) for i in range(2)]
            tmp = [sb(st, "adat%d" % i, [128, 512], F32) for i in range(2)]
            tmpb = [Buf(), Buf()]
            it = 0
            for jj in range(3):
                for hc in range(2):
                    c0 = (first_j + jj) * D + hc * 512
                    b = it % 2
                    it += 1
                    S.dma("pool", wst[b][:], wada_d[:, :, c0:c0 + 512], writes=[wstb[b]])
                    S.dma("sp", bst[b][:], bada_d[:, c0:c0 + 512], writes=[wstb[b]])
                    if jj == 1:
                        S.dma("sp", gst[b][:], ng_d[:, hc * 512:(hc + 1) * 512], writes=[wstb[b]])
                    for n, s in enumerate(seqs):
                        p, pb = next_pw()
                        for kt in range(8):
                            S.op("pe", lambda e, p=p, b=b, kt=kt, s=s: e.matmul(
                                p[:], siluT[:, kt, s, :], wst[b][:, kt, :], start=(kt == 0), stop=(kt == 7)),
                                reads=[cst, wstb[b]], writes=[pb])
                        dst = tabs[n][:, jj, hc * 512:(hc + 1) * 512]
                        if jj == 1:
                            tb_ = tmpb[n]
                            S.op("dve", lambda e, p=p, b=b, n=n: e.tensor_tensor(
                                out=tmp[n][:], in0=p[:], in1=bst[b][:], op=ALU.add),
                                reads=[pb, wstb[b]], writes=[tb_])
                            S.op("dve", lambda e, b=b, n=n, dst=dst: e.scalar_tensor_tensor(
                                out=dst, in0=tmp[n][:], scalar=1.0, in1=gst[b][:], op0=ALU.add, op1=ALU.mult),
                                reads=[tb_, wstb[b]], writes=[tabs_b[n]])
                        else:
                            S.op("dve", lambda e, p=p, b=b, dst=dst: e.tensor_tensor(
                                out=dst, in0=p[:], in1=bst[b][:], op=ALU.add),
                                reads=[pb, wstb[b]], writes=[tabs_b[n]])

        with ExitStack() as stA:
            tabsA1 = sb(stA, "tabsA", [128, 3, D], F32)
            tabsA = [tabsA1, tabsA1]
            tabsA_b1 = Buf()
            tabsA_b = [tabsA_b1, tabsA_b1]

            ssmA_d = nc.dram_tensor("ssmA_scr", [128, 32, 128], BF16, kind="Internal").ap()
            ssmB_d = nc.dram_tensor("ssmB_scr", [128, 32, 128], BF16, kind="Internal").ap()
            ssmC_d = nc.dram_tensor("ssmC_scr", [128, 32, 2, 128], BF16, kind="Internal").ap()
            ssmd_b = Buf()
            ak = sb(stA, "ak", [128, 16, 8], F32)
            ck = sb(stA, "ck", [128, 16, 8], F32)
            nck = sb(stA, "nck", [128, 16, 8], F32)
            ssm_b = Buf()
            with ExitStack() as st1:
                extra_bank(st1, True)
                A_sb = sb(st1, "A_sb", [128, 32, 128], BF16)
                Bm_sb = sb(st1, "Bm_sb", [128, 32, 128], BF16)
                Cmz = sb(st1, "Cmz", [128, 32, 2, 128], BF16)

                def t3(name, n):
                    return sb(st1, name, [128, 16, n], F32)
                lre = t3("lre", 1); lim = t3("lim", 1); ldt = t3("ldt", 1)
                bre = t3("bre", 16); bim = t3("bim", 16); cre = t3("cre", 16); cim = t3("cim", 16)
                mtab = t3("mtab", NE)
                bmask = sb(st1, "bmask", [128, 128], F32)
                dmask = sb(st1, "dmask", [128, 128], F32)
                zmask = sb(st1, "zmask", [128, 2], F32)
                dsk = sb(st1, "dsk", [128, 32, 128], F32)
                ib = Buf()
                for t_, d_ in ((lre, lre_d), (lim, lim_d), (ldt, ldt_d)):
                    S.dma("sp", t_[:, :, 0], d_, writes=[ib])
                for t_, d_ in ((bre, bre_d), (bim, bim_d), (cre, cre_d), (cim, cim_d), (mtab, mtab_d),
                               (bmask, bmask_d), (dmask, dmask_d), (dsk, dsk_d), (zmask, zmask_d)):
                    S.dma("sp", t_[:], d_, writes=[ib])
                chk('ssm_a')
                dt_ = t3("dt_", 1); aa = t3("aa", 1); th = t3("th", 1)
                ang = t3("ang", NE); lmag = t3("lmag", NE); mag = t3("mag", NE)
                tq = t3("tq", NE); tqi = sb(st1, "tqi", [128, 16, NE], I32); tqf = t3("tqf", NE)
                wr = t3("wr", NE); sn = t3("sn", NE); cs = t3("cs", NE)
                pr_ = t3("pr_", NE); pi_ = t3("pi_", NE)
                wb = Buf()

                def V(fn, reads=(), writes=(wb,)):
                    S.op("dve", fn, reads=[ib, wb] + list(reads), writes=list(writes))

                def ACT(fn, reads=(), writes=(wb,)):
                    S.op("act", fn, reads=[ib, wb] + list(reads), writes=list(writes))

                ACT(lambda e: e.activation(out=dt_[:], in_=ldt[:], func=AF.Exp))
                V(lambda e: e.tensor_tensor(out=aa[:], in0=lre[:], in1=dt_[:], op=ALU.mult))
                V(lambda e: e.tensor_tensor(out=th[:], in0=lim[:], in1=dt_[:], op=ALU.mult))
                V(lambda e: e.tensor_tensor(out=lmag[:], in0=mtab[:], in1=aa[:].to_broadcast([128, 16, NE]), op=ALU.mult))
                V(lambda e: e.tensor_tensor(out=ang[:], in0=mtab[:], in1=th[:].to_broadcast([128, 16, NE]), op=ALU.mult))
                ACT(lambda e: e.activation(out=mag[:], in_=lmag[:], func=AF.Exp))
                V(lambda e: e.tensor_scalar(out=tq[:], in0=ang[:], scalar1=1.0 / TWO_PI, scalar2=None, op0=ALU.mult))
                V(lambda e: e.tensor_copy(out=tqi[:], in_=tq[:]))
                V(lambda e: e.tensor_copy(out=tqf[:], in_=tqi[:]))
                V(lambda e: e.scalar_tensor_tensor(out=wr[:], in0=tqf[:], scalar=-TWO_PI, in1=ang[:],
                                                   op0=ALU.mult, op1=ALU.add))
                wt_ = t3("wt_", NE)
                for t_, shift in ((sn, 0.0), (cs, math.pi / 2)):
                    V(lambda e, t_=t_, shift=shift: e.tensor_scalar(out=t_[:], in0=wr[:], scalar1=shift, scalar2=None, op0=ALU.add))
                    V(lambda e, t_=t_: e.tensor_scalar(out=wt_[:], in0=t_[:], scalar1=math.pi, scalar2=-TWO_PI,
                                                       op0=ALU.is_gt, op1=ALU.mult))
                    V(lambda e, t_=t_: e.tensor_scalar(out=tq[:], in0=t_[:], scalar1=-math.pi, scalar2=TWO_PI,
                                                       op0=ALU.is_lt, op1=ALU.mult))
                    V(lambda e, t_=t_: e.tensor_tensor(out=t_[:], in0=t_[:], in1=wt_[:], op=ALU.add))
                    V(lambda e, t_=t_: e.tensor_tensor(out=t_[:], in0=t_[:], in1=tq[:], op=ALU.add))
                for t_ in (sn, cs):
                    V(lambda e, t_=t_: e.tensor_scalar(out=t_[:], in0=t_[:], scalar1=3.14159, scalar2=-3.14159,
                                                       op0=ALU.min, op1=ALU.max))
                ACT(lambda e: e.activation(out=sn[:], in_=sn[:], func=AF.Sin))
                ACT(lambda e: e.activation(out=cs[:], in_=cs[:], func=AF.Sin))
                V(lambda e: e.tensor_tensor(out=pr_[:], in0=mag[:], in1=cs[:], op=ALU.mult))
                V(lambda e: e.tensor_tensor(out=pi_[:], in0=mag[:], in1=sn[:], op=ALU.mult))
                chk('ssm_b')
                i1 = EIDX[1]
                nr = t3("nr", 1); den = t3("den", 1); t1_ = t3("t1_", 1); gr = t3("gr", 1); gi = t3("gi", 1)
                V(lambda e: e.tensor_scalar(out=nr[:], in0=pr_[:, :, i1:i1 + 1], scalar1=-1.0, scalar2=None, op0=ALU.add))
                V(lambda e: e.tensor_tensor(out=den[:], in0=lre[:], in1=lre[:], op=ALU.mult))
                V(lambda e: e.tensor_tensor(out=t1_[:], in0=lim[:], in1=lim[:], op=ALU.mult))
                V(lambda e: e.tensor_tensor(out=den[:], in0=den[:], in1=t1_[:], op=ALU.add))
                V(lambda e: e.reciprocal(out=den[:], in_=den[:]))
                V(lambda e: e.tensor_tensor(out=gr[:], in0=nr[:], in1=lre[:], op=ALU.mult))
                V(lambda e: e.tensor_tensor(out=t1_[:], in0=pi_[:, :, i1:i1 + 1], in1=lim[:], op=ALU.mult))
                V(lambda e: e.tensor_tensor(out=gr[:], in0=gr[:], in1=t1_[:], op=ALU.add))
                V(lambda e: e.tensor_tensor(out=gr[:], in0=gr[:], in1=den[:], op=ALU.mult))
                V(lambda e: e.tensor_tensor(out=gi[:], in0=pi_[:, :, i1:i1 + 1], in1=lre[:], op=ALU.mult))
                V(lambda e: e.tensor_tensor(out=t1_[:], in0=nr[:], in1=lim[:], op=ALU.mult))
                V(lambda e: e.tensor_tensor(out=gi[:], in0=gi[:], in1=t1_[:], op=ALU.subtract))
                V(lambda e: e.tensor_tensor(out=gi[:], in0=gi[:], in1=den[:], op=ALU.mult))
                for k in range(8):
                    ii = EIDX[8 * (2 ** k)]
                    V(lambda e, k=k, ii=ii: e.tensor_copy(out=ak[:, :, k:k + 1], in_=pr_[:, :, ii:ii + 1]), writes=[wb, ssm_b])
                    V(lambda e, k=k, ii=ii: e.tensor_copy(out=ck[:, :, k:k + 1], in_=pi_[:, :, ii:ii + 1]), writes=[wb, ssm_b])
                    V(lambda e, k=k, ii=ii: e.tensor_scalar(out=nck[:, :, k:k + 1], in0=pi_[:, :, ii:ii + 1], scalar1=-1.0,
                                                            scalar2=None, op0=ALU.mult), writes=[wb, ssm_b])
                PBr = t3("PBr", 8); PBi = t3("PBi", 8); tt8 = t3("tt8", 8)
                e7 = EIDX[0]
                sl07 = slice(e7, e7 + 8)
                V(lambda e: e.tensor_tensor(out=PBr[:], in0=pr_[:, :, sl07], in1=gr[:].to_broadcast([128, 16, 8]), op=ALU.mult))
                V(lambda e: e.tensor_tensor(out=tt8[:], in0=pi_[:, :, sl07], in1=gi[:].to_broadcast([128, 16, 8]), op=ALU.mult))
                V(lambda e: e.tensor_tensor(out=PBr[:], in0=PBr[:], in1=tt8[:], op=ALU.subtract))
                V(lambda e: e.tensor_tensor(out=PBi[:], in0=pr_[:, :, sl07], in1=gi[:].to_broadcast([128, 16, 8]), op=ALU.mult))
                V(lambda e: e.tensor_tensor(out=tt8[:], in0=pi_[:, :, sl07], in1=gr[:].to_broadcast([128, 16, 8]), op=ALU.mult))
                V(lambda e: e.tensor_tensor(out=PBi[:], in0=PBi[:], in1=tt8[:], op=ALU.add))
                BmTr = sb(st1, "BmTr", [128, 16, 8, 16], F32)
                BmTi = sb(st1, "BmTi", [128, 16, 8, 16], F32)
                t816 = sb(st1, "t816", [128, 16, 8, 16], F32)
                for i in range(8):
                    m = 7 - i
                    def bc(t_, m=m):
                        return t_[:, :, m:m + 1].to_broadcast([128, 16, 16])
                    V(lambda e, i=i, bc=bc: e.tensor_tensor(out=BmTr[:, :, i, :], in0=bre[:], in1=bc(PBr), op=ALU.mult))
                    V(lambda e, i=i, bc=bc: e.tensor_tensor(out=t816[:, :, i, :], in0=bim[:], in1=bc(PBi), op=ALU.mult))
                    V(lambda e, i=i, bc=bc: e.tensor_tensor(out=BmTi[:, :, i, :], in0=bim[:], in1=bc(PBr), op=ALU.mult))
                V(lambda e: e.tensor_tensor(out=BmTr[:], in0=BmTr[:], in1=t816[:], op=ALU.subtract))
                for i in range(8):
                    m = 7 - i
                    V(lambda e, i=i, m=m: e.tensor_tensor(out=t816[:, :, i, :], in0=bre[:],
                                                          in1=PBi[:, :, m:m + 1].to_broadcast([128, 16, 16]), op=ALU.mult))
                V(lambda e: e.tensor_tensor(out=BmTi[:], in0=BmTi[:], in1=t816[:], op=ALU.add))
                Wcr = sb(st1, "Wcr", [128, 16, 8, 16], F32)
                Wci = sb(st1, "Wci", [128, 16, 8, 16], F32)
                Cmr = sb(st1, "Cmr", [128, 16, 8, 16], F32)
                Cmi = sb(st1, "Cmi", [128, 16, 8, 16], F32)
                for (dr, di, off) in ((Wcr, Wci, -7), (Cmr, Cmi, 1)):
                    for j in range(8):
                        ii = EIDX[j + off]
                        def bc2(t_, ii=ii):
                            return t_[:, :, ii:ii + 1].to_broadcast([128, 16, 16])
                        V(lambda e, j=j, bc2=bc2, dr=dr: e.tensor_tensor(out=dr[:, :, j, :], in0=cre[:], in1=bc2(pr_), op=ALU.mult))
                        V(lambda e, j=j, bc2=bc2: e.tensor_tensor(out=t816[:, :, j, :], in0=cim[:], in1=bc2(pi_), op=ALU.mult))
                        V(lambda e, j=j, bc2=bc2, di=di: e.tensor_tensor(out=di[:, :, j, :], in0=cre[:], in1=bc2(pi_), op=ALU.mult))
                    V(lambda e, dr=dr: e.tensor_tensor(out=dr[:], in0=dr[:], in1=t816[:], op=ALU.subtract))
                    for j in range(8):
                        ii = EIDX[j + off]
                        V(lambda e, j=j, ii=ii: e.tensor_tensor(out=t816[:, :, j, :], in0=cim[:],
                                                                in1=pr_[:, :, ii:ii + 1].to_broadcast([128, 16, 16]), op=ALU.mult))
                    V(lambda e, di=di: e.tensor_tensor(out=di[:], in0=di[:], in1=t816[:], op=ALU.add))
                    V(lambda e, di=di: e.tensor_scalar(out=di[:], in0=di[:], scalar1=-1.0, scalar2=None, op0=ALU.mult))
                chk('ssm_c')
                for pr in range(16):
                    for gp in range(2):
                        g = 2 * pr + gp
                        for ri, src in ((0, Cmr), (1, Cmi)):
                            V(lambda e, g=g, ri=ri, src=src, pr=pr, gp=gp: e.tensor_scalar(
                                out=Cmz[:, g, ri, :], in0=src[:, pr, :, :].rearrange("p a b -> p (a b)"),
                                scalar1=zmask[:, gp:gp + 1], scalar2=None, op0=ALU.mult), writes=[wb, ssm_b])
                chk('ssm_d')
                Bz = [sb(st1, "Bz%d" % i, [128, 2, 128], BF16) for i in range(2)]
                Wz = [sb(st1, "Wz%d" % i, [128, 2, 128], BF16) for i in range(2)]
                Bzb = [Buf(), Buf()]
                At = [sb(st1, "At%d" % i, [128, 128], F32) for i in range(2)]
                Atb = [Buf(), Buf()]
                for pr in range(16):
                    for gp in range(2):
                        g = 2 * pr + gp
                        b = g % 2
                        for ri, src in ((0, BmTr), (1, BmTi)):
                            V(lambda e, b=b, ri=ri, src=src, pr=pr, gp=gp: e.tensor_scalar(
                                out=Bz[b][:, ri, :], in0=src[:, pr, :, :].rearrange("p a b -> p (a b)"),
                                scalar1=zmask[:, gp:gp + 1], scalar2=None, op0=ALU.mult), writes=[wb, Bzb[b]])
                        for ri, src in ((0, Wcr), (1, Wci)):
                            V(lambda e, b=b, ri=ri, src=src, pr=pr, gp=gp: e.tensor_scalar(
                                out=Wz[b][:, ri, :], in0=src[:, pr, :, :].rearrange("p a b -> p (a b)"),
                                scalar1=zmask[:, gp:gp + 1], scalar2=None, op0=ALU.mult), writes=[wb, Bzb[b]])
                        ptt, ptb_ = next_pt()
                        for ri in range(2):
                            S.op("pe", lambda e, ptt=ptt, b=b, ri=ri: e.transpose(
                                out=ptt[:, ri * 128:(ri + 1) * 128], in_=Bz[b][:, ri, :], identity=ident_bf[:]),
                                reads=[Bzb[b], cst], writes=[ptb_])
                        for ri in range(2):
                            S.op("act", lambda e, ptt=ptt, g=g, ri=ri, gp=gp: e.copy(
                                out=Bm_sb[:, g, ri * 64:(ri + 1) * 64],
                                in_=ptt[:, ri * 128 + gp * 64: ri * 128 + gp * 64 + 64]),
                                reads=[ptb_], writes=[ssm_b])
                        p, pb = next_pw()
                        for ri in range(2):
                            S.op("pe", lambda e, p=p, b=b, ri=ri: e.matmul(
                                p[:, 0:128], Bz[b][:, ri, :], Wz[b][:, ri, :], start=(ri == 0), stop=(ri == 1)),
                                reads=[Bzb[b]], writes=[pb])
                        S.op("dve", lambda e, p=p, b=b: e.tensor_tensor(out=At[b][:], in0=p[:, 0:128], in1=bmask[:], op=ALU.mult),
                             reads=[pb, ib], writes=[Atb[b]])
                        S.op("dve", lambda e, b=b, g=g: e.tensor_tensor(out=dsk[:, g, :], in0=dsk[:, g, :], in1=dmask[:], op=ALU.mult),
                             reads=[ib], writes=[ib])
                        S.op("dve", lambda e, b=b, g=g: e.tensor_tensor(out=A_sb[:, g, :], in0=At[b][:], in1=dsk[:, g, :], op=ALU.add),
                             reads=[Atb[b], ib], writes=[ssm_b])
                chk('ssm_e')
                S.dma("sp", ssmA_d, A_sb[:], reads=[ssm_b], writes=[ssmd_b])
                S.dma("sp", ssmB_d, Bm_sb[:], reads=[ssm_b], writes=[ssmd_b])
                S.dma("sp", ssmC_d, Cmz[:], reads=[ssm_b], writes=[ssmd_b])
                S.barrier()

            chk('ssmsetup')
            for s in range(NSEQ):
                r0 = s * SEQ
                with ExitStack() as stt:
                    adaln([tabsA1], [tabsA_b1], [s], 0, n1g_d, stt)
                    S.barrier()
                chk('adaln')
                with ExitStack() as stS:
                    qT = sb(stS, "qT", [128, 4, SEQ], BF16)
                    kTz = sb(stS, "kTz", [128, 2, 2, SEQ], BF16)
                    qiT = sb(stS, "qiT", [128, 4, SEQ], BF16)
                    kiTz = sb(stS, "kiTz", [128, 2, SEQ], BF16)
                    Vp = sb(stS, "Vp", [128, 16, 2, 65], BF16)
                    wi = sb(stS, "wi", [128, 16, 8], F32)
                    mixT_s = sb(stS, "mixT_s", [128, 4, SEQ], BF16)
                    qT_b, kT_b, qiT_b, kiT_b, Vp_b, wi_b, mixT_sb = (Buf() for _ in range(7))
                    S.op("pool", lambda e: e.memset(kTz[:].rearrange("p a b c -> p (a b c)"), 0.0), writes=[kT_b])
                    S.op("pool", lambda e: e.memset(kiTz[:].rearrange("p a c -> p (a c)"), 0.0), writes=[kiT_b])
                    S.op("pool", lambda e: e.memset(Vp[:].rearrange("p a b c -> p (a b c)"), 1.0), writes=[Vp_b])

                    with ExitStack() as st12:
                        U8 = sb(st12, "U8", [128, 2, 32, 8, 16], BF16)
                        U8_b = Buf()
                        with ExitStack() as st1:
                            extra_bank(st1, True)
                            w_in = sb(st1, "w_in", [128, 8, DIN], BF16)
                            w_in_b = Buf()
                            for kt in range(8):
                                S.dma("pool", w_in[:, kt, :], win_d[:, kt, :], writes=[w_in_b])
                            hT = sb(st1, "hT", [128, 8, SEQ], BF16)
                            hT_b = Buf()
                            st1a = ExitStack()
                            xt = [sb(st1a, "xt%d" % i, [128, D], F32) for i in range(2)]
                            xt_b = [Buf(), Buf()]
                            junk = sb(st1a, "junk", [128, D], BF16)
                            junk_b = Buf()
                            t1 = sb(st1a, "t1", [128, D], F32)
                            hb = [sb(st1a, "hb%d" % i, [128, D], BF16) for i in range(2)]
                            hb_b = [Buf(), Buf()]
                            st_ = [sb(st1a, "st%d" % i, [128, 4], F32) for i in range(2)]
                            st_b = [Buf(), Buf()]
                            t1_b = Buf()
                            for t in range(16):
                                b = t % 2
                                S.dma("sp", xt[b][:], x_d[r0 + t * 128: r0 + (t + 1) * 128, :], writes=[xt_b[b]])
                                S.op("act", lambda e, b=b: e.activation(out=junk[:], in_=xt[b][:], func=AF.Square,
                                                                        accum_out=st_[b][:, 0:1]),
                                     reads=[xt_b[b]], writes=[junk_b, st_b[b]])
                                S.op("act", lambda e, b=b: e.activation(out=st_[b][:, 1:2], in_=st_[b][:, 0:1], func=AF.Sqrt,
                                                                        bias=EPS, scale=1.0 / D),
                                     reads=[st_b[b]], writes=[st_b[b]])
                                S.op("dve", lambda e, b=b: e.reciprocal(out=st_[b][:, 2:3], in_=st_[b][:, 1:2]),
                                     reads=[st_b[b]], writes=[st_b[b]])
                                S.op("dve", lambda e, b=b: e.scalar_tensor_tensor(
                                    out=t1[:], in0=xt[b][:], scalar=st_[b][:, 2:3], in1=tabsA[s][:, 1, :],
                                    op0=ALU.mult, op1=ALU.mult), reads=[xt_b[b], st_b[b], tabsA_b[s]], writes=[t1_b])
                                S.op("dve", lambda e, b=b: e.tensor_tensor(out=hb[b][:], in0=t1[:], in1=tabsA[s][:, 0, :], op=ALU.add),
                                     reads=[t1_b, tabsA_b[s]], writes=[hb_b[b]])
                                ptt, ptb_ = next_pt()
                                for kt in range(8):
                                    S.op("pe", lambda e, ptt=ptt, b=b, kt=kt: e.transpose(
                                        out=ptt[:, kt * 128:(kt + 1) * 128], in_=hb[b][:, kt * 128:(kt + 1) * 128],
                                        identity=ident_bf[:]), reads=[hb_b[b], cst], writes=[ptb_])
                                S.op("act", lambda e, ptt=ptt, t=t: e.copy(
                                    out=hT[:, :, t * 128:(t + 1) * 128], in_=ptt[:].rearrange("p (a b) -> p a b", b=128)),
                                    reads=[ptb_], writes=[hT_b])
                            S.barrier()
                            st1a.close()
                            sq = [sb(st1, "sq%d" % i, [128, 512], BF16) for i in range(2)]
                            sq_b = [Buf(), Buf()]
                            sd = [sb(st1, "sd%d" % i, [128, 512], F32) for i in range(2)]
                            sd_b = [Buf(), Buf()]
                            groups = []
                            for j in range(4):
                                groups.append(("q", j, [(512 + j * 128, 128, 0)]))
                            groups.append(("kA", 0, [(1024, 128, 0)]))
                            groups.append(("kB", 0, [(1088, 64, 0), (1024, 64, 64)]))
                            for j in range(4):
                                groups.append(("qi", j, [(1280 + j * 128, 128, 0)]))
                            groups.append(("ki", 0, [(1792, 64, 0), (1792, 64, 64)]))
                            it = 0
                            for kind, j, parts in groups:
                                for c in range(4):
                                    cs_ = slice(c * 512, (c + 1) * 512)
                                    p, pb = next_pw()
                                    for (c0, m, po) in parts:
                                        for kt in range(8):
                                            S.op("pe", lambda e, p=p, c0=c0, m=m, po=po, kt=kt, cs_=cs_: e.matmul(
                                                p[po:po + m, :], w_in[:, kt, c0:c0 + m], hT[:, kt, cs_],
                                                start=(kt == 0), stop=(kt == 7)),
                                                reads=[w_in_b, hT_b], writes=[pb])
                                    if kind == "qi":
                                        S.op("act", lambda e, p=p, j=j, cs_=cs_: e.copy(out=qiT[:, j, cs_], in_=p[:]),
                                             reads=[pb], writes=[qiT_b])
                                    elif kind == "ki":
                                        for half in range(2):
                                            rs = slice(half * 64, half * 64 + 64)
                                            S.op("act", lambda e, p=p, half=half, rs=rs, cs_=cs_: e.copy(
                                                out=kiTz[rs, half, cs_], in_=p[rs, :]), reads=[pb], writes=[kiT_b])
                                    else:
                                        b = it % 2
                                        it += 1
                                        S.op("act", lambda e, p=p, b=b: e.activation(out=sq[b][:], in_=p[:], func=AF.Square),
                                             reads=[pb], writes=[sq_b[b]])
                                        p2, p2b = next_pw()
                                        S.op("pe", lambda e, p2=p2, b=b: e.matmul(p2[:], onesblk[:], sq[b][:], start=True, stop=True),
                                             reads=[sq_b[b], cst], writes=[p2b])
                                        S.op("act", lambda e, p2=p2, b=b: e.activation(out=sd[b][:], in_=p2[:], func=AF.Sqrt,
                                                                                      bias=EPS, scale=1.0 / 64),
                                             reads=[p2b], writes=[sd_b[b]])
                                        S.op("dve", lambda e, b=b: e.reciprocal(out=sd[b][:], in_=sd[b][:]),
                                             reads=[sd_b[b]], writes=[sd_b[b]])
                                        if kind == "q":
                                            S.op("dve", lambda e, p=p, b=b, j=j, cs_=cs_: e.scalar_tensor_tensor(
                                                out=qT[:, j, cs_], in0=p[:], scalar=G2[:, 0:1], in1=sd[b][:],
                                                op0=ALU.mult, op1=ALU.mult), reads=[pb, sd_b[b], cst], writes=[qT_b])
                                        else:
                                            kvs = (0, 1) if kind == "kA" else (1, 0)
                                            for half in range(2):
                                                rs = slice(half * 64, half * 64 + 64)
                                                kv = kvs[half]
                                                S.op("dve", lambda e, p=p, b=b, rs=rs, kv=kv, half=half, cs_=cs_: e.tensor_tensor(
                                                    out=kTz[rs, kv, half, cs_], in0=p[rs, :], in1=sd[b][rs, :], op=ALU.mult),
                                                    reads=[pb, sd_b[b]], writes=[kT_b])
                            for t in range(16):
                                ts_ = slice(t * 128, (t + 1) * 128)
                                p, pb = next_pw()
                                for kt in range(8):
                                    S.op("pe", lambda e, p=p, kt=kt, ts_=ts_: e.matmul(
                                        p[:, 0:128], hT[:, kt, ts_], w_in[:, kt, 1152:1280], start=(kt == 0), stop=(kt == 7)),
                                        reads=[w_in_b, hT_b], writes=[pb])
                                for kt in range(8):
                                    S.op("pe", lambda e, p=p, kt=kt, ts_=ts_: e.matmul(
                                        p[:, 128:136], hT[:, kt, ts_], w_in[:, kt, 1856:1864], start=(kt == 0), stop=(kt == 7)),
                                        reads=[w_in_b, hT_b], writes=[pb])
                                S.op("act", lambda e, p=p, t=t: e.copy(
                                    out=Vp[:, t, :, 0:64], in_=p[:, 0:128].rearrange("p (a b) -> p a b", b=64)),
                                    reads=[pb], writes=[Vp_b])
                                S.op("act", lambda e, p=p, t=t: e.mul(out=wi[:, t, :], in_=p[:, 128:136], mul=IDX_SCALE),
                                     reads=[pb], writes=[wi_b])
                            for sp in range(2):
                                for i in range(8):
                                    p, pb = next_pw()
                                    for kt in range(8):
                                        lhs = hT[:, kt, sp * 1024:(sp + 1) * 1024].rearrange("p (b i) -> p i b", i=8)[:, i, :]
                                        S.op("pe", lambda e, p=p, lhs=lhs, kt=kt: e.matmul(
                                            p[:], lhs, w_in[:, kt, 0:512], start=(kt == 0), stop=(kt == 7)),
                                            reads=[w_in_b, hT_b], writes=[pb])
                                    S.op("act", lambda e, p=p, sp=sp, i=i: e.copy(
                                        out=U8[:, sp, :, i, :], in_=p[:].rearrange("p (g c) -> p g c", c=16)),
                                         reads=[pb], writes=[U8_b])
                            S.barrier()

                        chk('s1')
                        with ExitStack() as st2:
                            extra_bank(st2, True)
                            w_glu = sb(st2, "w_glu", [128, 4, 512], BF16)
                            w_glu_b = Buf()
                            S.dma("pool", w_glu[:], wglu_d, writes=[w_glu_b])
                            Ytok = sb(st2, "Ytok", [128, 2, 8, 512], F32)
                            Ytok_b = Buf()
                            U8T = [sb(st2, "U8T%d" % i, [128, 2, 256], BF16) for i in range(2)]
                            U8T_b = [Buf(), Buf()]
                            XA = [sb(st2, "XA%d" % i, [128, 2, 384], F32) for i in range(2)]
                            XB = [sb(st2, "XB%d" % i, [128, 2, 384], F32) for i in range(2)]
                            XA_b, XB_b = [Buf(), Buf()], [Buf(), Buf()]
                            TM = [sb(st2, "TM%d" % i, [128, 2, 256], F32) for i in range(2)]
                            TM_b = [Buf(), Buf()]
                            Xst = [sb(st2, "Xst%d" % i, [128, 2, 258], BF16) for i in range(2)]
                            Xst_b = [Buf(), Buf()]
                            Ysb = [sb(st2, "Ysb%d" % i, [128, 256], F32) for i in range(2)]
                            Ysb_b = [Buf(), Buf()]
                            for i in range(2):
                                S.op("pool", lambda e, i=i: e.memset(XA[i][:].rearrange("p a b -> p (a b)"), 0.0), writes=[XA_b[i]])
                                S.op("pool", lambda e, i=i: e.memset(XB[i][:].rearrange("p a b -> p (a b)"), 0.0), writes=[XB_b[i]])
                                S.op("pool", lambda e, i=i: e.memset(Xst[i][:].rearrange("p a b -> p (a b)"), 0.0), writes=[Xst_b[i]])
                            PAD = 128
                            A2 = [sb(st2, "A2_%d" % i, [128, 2, 128], BF16) for i in range(2)]
                            B2 = [sb(st2, "B2_%d" % i, [128, 2, 128], BF16) for i in range(2)]
                            C2 = [sb(st2, "C2_%d" % i, [128, 2, 2, 128], BF16) for i in range(2)]
                            M2_b = [Buf(), Buf()]

                            def gen_pair(pr):
                                ub = pr % 2
                                S.dma("sp", A2[ub][:], ssmA_d[:, 2 * pr:2 * pr + 2, :], reads=[ssmd_b], writes=[M2_b[ub]])
                                S.dma("sp", B2[ub][:], ssmB_d[:, 2 * pr:2 * pr + 2, :], reads=[ssmd_b], writes=[M2_b[ub]])
                                S.dma("sp", C2[ub][:], ssmC_d[:, 2 * pr:2 * pr + 2, :, :], reads=[ssmd_b], writes=[M2_b[ub]])
                                ptt, ptb_ = next_pt()
                                for gp in range(2):
                                    g = 2 * pr + gp
                                    for sp in range(2):
                                        S.op("pe", lambda e, ptt=ptt, gp=gp, sp=sp, g=g: e.transpose(
                                            out=ptt[:, (gp * 2 + sp) * 128:(gp * 2 + sp + 1) * 128],
                                            in_=U8[:, sp, g, :, :].rearrange("p a b -> p (a b)"), identity=ident_bf[:]),
                                            reads=[U8_b, cst], writes=[ptb_])
                                S.op("act", lambda e, ptt=ptt, ub=ub: e.copy(
                                    out=U8T[ub][:].rearrange("p a b -> p (a b)"), in_=ptt[:, 0:512]),
                                    reads=[ptb_], writes=[U8T_b[ub]])
                                p, pb = next_pw()
                                for gp in range(2):
                                    g = 2 * pr + gp
                                    for ri in range(2):
                                        S.op("pe", lambda e, p=p, gp=gp, g=g, ri=ri, ub=ub: e.matmul(
                                            p[gp * 64:(gp + 1) * 64, ri * 256:(ri + 1) * 256],
                                            B2[ub][:, gp, ri * 64:(ri + 1) * 64], U8T[ub][:, gp, :], start=True, stop=True),
                                            reads=[M2_b[ub], U8T_b[ub]], writes=[pb])
                                S.op("act", lambda e, p=p: e.copy(out=XA[ub][:, :, PAD:PAD + 256],
                                                                  in_=p[:].rearrange("p (a b) -> p a b", b=256)),
                                     reads=[pb], writes=[XA_b[ub]])
                                yield
                                cur, curb, nxt, nxtb = XA[ub], XA_b[ub], XB[ub], XB_b[ub]
                                xs = Xst[ub]
                                tm, tmb = TM[ub], TM_b[ub]
                                for k in range(8):
                                    sft = 2 ** k
                                    last = (k == 7)
                                    a_ = ak[:, pr, k:k + 1]
                                    c_ = ck[:, pr, k:k + 1]
                                    nc_ = nck[:, pr, k:k + 1]
                                    sh = slice(PAD - sft, PAD - sft + 256)
                                    ce = slice(PAD, PAD + 256)
                                    outr = xs[:, 0, 1:257] if last else nxt[:, 0, ce]
                                    outi = xs[:, 1, 1:257] if last else nxt[:, 1, ce]
                                    ob = Xst_b[ub] if last else nxtb
                                    S.op("dve", lambda e, cur=cur, a_=a_, sh=sh, ce=ce: e.scalar_tensor_tensor(
                                        out=tm[:, 0, :], in0=cur[:, 0, sh], scalar=a_, in1=cur[:, 0, ce], op0=ALU.mult, op1=ALU.add),
                                        reads=[curb, ssm_b], writes=[tmb])
                                    S.op("dve", lambda e, cur=cur, a_=a_, sh=sh, ce=ce: e.scalar_tensor_tensor(
                                        out=tm[:, 1, :], in0=cur[:, 1, sh], scalar=a_, in1=cur[:, 1, ce], op0=ALU.mult, op1=ALU.add),
                                        reads=[curb, ssm_b], writes=[tmb])
                                    yield
                                    S.op("dve", lambda e, cur=cur, nc_=nc_, sh=sh, outr=outr: e.scalar_tensor_tensor(
                                        out=outr, in0=cur[:, 1, sh], scalar=nc_, in1=tm[:, 0, :], op0=ALU.mult, op1=ALU.add),
                                        reads=[curb, tmb, ssm_b], writes=[ob])
                                    S.op("dve", lambda e, cur=cur, c_=c_, sh=sh, outi=outi: e.scalar_tensor_tensor(
                                        out=outi, in0=cur[:, 0, sh], scalar=c_, in1=tm[:, 1, :], op0=ALU.mult, op1=ALU.add),
                                        reads=[curb, tmb, ssm_b], writes=[ob])
                                    yield
                                    cur, curb, nxt, nxtb = nxt, nxtb, cur, curb
                                for gp in range(2):
                                    g = 2 * pr + gp
                                    yb = g % 2
                                    p, pb = next_pw()
                                    S.op("pe", lambda e, p=p, g=g, gp=gp, ub=ub: e.matmul(
                                        p[:, 0:256], A2[ub][:, gp, :], U8T[ub][:, gp, :], start=True, stop=False),
                                        reads=[M2_b[ub], U8T_b[ub]], writes=[pb])
                                    for ri in range(2):
                                        S.op("pe", lambda e, p=p, g=g, ri=ri, xs=xs: e.matmul(
                                            p[:, 0:256], C2[ub][:, gp, ri, :], xs[:, ri, 0:256], start=False, stop=(ri == 1)),
                                            reads=[M2_b[ub], Xst_b[ub]], writes=[pb])
                                    S.op("act", lambda e, p=p, yb=yb: e.copy(out=Ysb[yb][:], in_=p[:, 0:256]),
                                         reads=[pb], writes=[Ysb_b[yb]])
                                    p2, p2b = next_pw()
                                    for sp in range(2):
                                        S.op("pe", lambda e, p2=p2, sp=sp, yb=yb: e.transpose(
                                            out=p2[:, sp * 128:(sp + 1) * 128], in_=Ysb[yb][:, sp * 128:(sp + 1) * 128],
                                            identity=ident_f[:]), reads=[Ysb_b[yb], cst], writes=[p2b])
                                    for sp in range(2):
                                        S.op("act", lambda e, p2=p2, sp=sp, g=g: e.copy(
                                            out=Ytok[:, sp, :, g * 16:(g + 1) * 16],
                                            in_=p2[:, sp * 128:(sp + 1) * 128].rearrange("p (a b) -> p a b", b=16)),
                                            reads=[p2b], writes=[Ytok_b])
                                    yield

                            act_p = []
                            nxt_p = 0
                            steps = 0
                            while nxt_p < 16 or act_p:
                                if nxt_p < 16 and len(act_p) < 2 and (nxt_p == 0 or steps >= 6):
                                    if not act_p or act_p[-1][0] % 2 != nxt_p % 2:
                                        act_p.append((nxt_p, gen_pair(nxt_p)))
                                        nxt_p += 1
                                for item in list(act_p):
                                    try:
                                        next(item[1])
                                    except StopIteration:
                                        act_p.remove(item)
                                steps += 1
                            g1 = [sb(st2, "g1_%d" % i, [128, 512], F32) for i in range(2)]
                            g2_ = [sb(st2, "g2_%d" % i, [128, 512], F32) for i in range(2)]
                            zf = [sb(st2, "zf%d" % i, [128, 512], F32) for i in range(2)]
                            zb = [sb(st2, "zb%d" % i, [128, 512], BF16) for i in range(2)]
                            zT = [sb(st2, "zT%d" % i, [128, 4, 128], BF16) for i in range(2)]
                            sg = [sb(st2, "sg%d" % i, [128, 512], F32) for i in range(2)]
                            mb_ = [sb(st2, "mb%d" % i, [128, 512], BF16) for i in range(2)]
                            sst = [sb(st2, "sst%d" % i, [128, 4], F32) for i in range(2)]
                            gb = [[Buf() for _ in range(8)] for _ in range(2)]
                            KG = 2.0 * math.sqrt(2.0 / math.pi)
                            for sp in range(2):
                                for i in range(8):
                                    b = i % 2
                                    B = gb[b]
                                    y = Ytok[:, sp, i, :]
                                    S.op("act", lambda e, b=b, y=y: e.activation(out=g1[b][:], in_=y, func=AF.Square),
                                         reads=[Ytok_b], writes=[B[0]])
                                    S.op("dve", lambda e, b=b: e.tensor_scalar(out=g1[b][:], in0=g1[b][:], scalar1=0.044715,
                                                                               scalar2=1.0, op0=ALU.mult, op1=ALU.add),
                                         reads=[B[0]], writes=[B[0]])
                                    S.op("dve", lambda e, b=b, y=y: e.tensor_tensor(out=g2_[b][:], in0=g1[b][:], in1=y, op=ALU.mult),
                                         reads=[B[0], Ytok_b], writes=[B[1]])
                                    S.op("act", lambda e, b=b: e.activation(out=g2_[b][:], in_=g2_[b][:], func=AF.Sigmoid, scale=KG),
                                         reads=[B[1]], writes=[B[1]])
                                    S.op("dve", lambda e, b=b, y=y: e.tensor_tensor(out=zf[b][:], in0=g2_[b][:], in1=y, op=ALU.mult),
                                         reads=[B[1], Ytok_b], writes=[B[2]])
                                    S.op("act", lambda e, b=b: e.copy(out=zb[b][:], in_=zf[b][:]), reads=[B[2]], writes=[B[3]])
                                    ptt, ptb_ = next_pt()
                                    for ft in range(4):
                                        S.op("pe", lambda e, ptt=ptt, b=b, ft=ft: e.transpose(
                                            out=ptt[:, ft * 128:(ft + 1) * 128], in_=zb[b][:, ft * 128:(ft + 1) * 128],
                                            identity=ident_bf[:]), reads=[B[3], cst], writes=[ptb_])
                                    S.op("act", lambda e, ptt=ptt, b=b: e.copy(out=zT[b][:].rearrange("p a b -> p (a b)"),
                                                                               in_=ptt[:, 0:512]), reads=[ptb_], writes=[B[4]])
                                    p, pb = next_pw()
                                    for ft in range(4):
                                        S.op("pe", lambda e, p=p, b=b, ft=ft: e.matmul(
                                            p[:], zT[b][:, ft, :], w_glu[:, ft, :], start=(ft == 0), stop=(ft == 3)),
                                            reads=[B[4], w_glu_b], writes=[pb])
                                    S.op("dve", lambda e, p=p, b=b: e.tensor_tensor(out=sg[b][:], in0=p[:], in1=bglu_b[:], op=ALU.add),
                                         reads=[pb, cst], writes=[B[5]])
                                    S.op("act", lambda e, b=b: e.activation(out=sg[b][:], in_=sg[b][:], func=AF.Sigmoid),
                                         reads=[B[5]], writes=[B[5]])
                                    S.op("dve", lambda e, b=b: e.tensor_tensor(out=sg[b][:], in0=sg[b][:], in1=zf[b][:], op=ALU.mult),
                                         reads=[B[5], B[2]], writes=[B[5]])
                                    S.op("act", lambda e, b=b: e.activation(out=g1[b][:], in_=sg[b][:], func=AF.Square,
                                                                            accum_out=sst[b][:, 0:1]),
                                         reads=[B[5], B[0]], writes=[B[0], B[6]])
                                    S.op("act", lambda e, b=b: e.activation(out=sst[b][:, 1:2], in_=sst[b][:, 0:1], func=AF.Sqrt,
                                                                            bias=EPS, scale=1.0 / 512), reads=[B[6]], writes=[B[6]])
                                    S.op("dve", lambda e, b=b: e.reciprocal(out=sst[b][:, 2:3], in_=sst[b][:, 1:2]),
                                         reads=[B[6]], writes=[B[6]])
                                    S.op("dve", lambda e, b=b: e.scalar_tensor_tensor(
                                        out=mb_[b][:], in0=sg[b][:], scalar=sst[b][:, 2:3], in1=gns_b[:], op0=ALU.mult, op1=ALU.mult),
                                        reads=[B[5], B[6], cst], writes=[B[7]])
                                    ptt, ptb_ = next_pt()
                                    for ft in range(4):
                                        S.op("pe", lambda e, ptt=ptt, b=b, ft=ft: e.transpose(
                                            out=ptt[:, ft * 128:(ft + 1) * 128], in_=mb_[b][:, ft * 128:(ft + 1) * 128],
                                            identity=ident_bf[:]), reads=[B[7], cst], writes=[ptb_])
                                    dst = mixT_s[:, :, sp * 1024:(sp + 1) * 1024].rearrange("p a (b i) -> p a i b", i=8)[:, :, i, :]
                                    S.op("act", lambda e, ptt=ptt, dst=dst: e.copy(
                                        out=dst, in_=ptt[:, 0:512].rearrange("p (a b) -> p a b", b=128)),
                                        reads=[ptb_], writes=[mixT_sb])
                            S.barrier()

                    chk('s2')
                    with ExitStack() as st3:
                        extra_bank(st3, False)
                        w_out = sb(st3, "w_out", [128, 8, D], BF16)
                        w_out_b = Buf()
                        S.dma("pool", w_out[:], wout_d, writes=[w_out_b])
                        score = [sb(st3, "score%d" % i, [128, SEQ], F32) for i in range(2)]
                        score_b = [Buf(), Buf()]
                        nm = [sb(st3, "nm%d" % i, [128, SEQ], BF16) for i in range(3)]
                        nm_b = [Buf() for _ in range(3)]
                        rbuf = [sb(st3, "rbuf%d" % i, [128, 8, 512], BF16) for i in range(2)]
                        rbuf_b = [Buf(), Buf()]
                        dg = [sb(st3, "dg%d" % i, [128, 8, 128], BF16) for i in range(2)]
                        dg_b = [Buf(), Buf()]
                        bs = [sb(st3, "bs%d" % i, [128, 16 + 3 * NITER], F32) for i in range(2)]
                        bs_b = [Buf(), Buf()]
                        PT = [sb(st3, "PT%d" % i, [128, 512], BF16) for i in range(3)]
                        PT_b = [Buf() for _ in range(3)]
                        yatt = sb(st3, "yatt", [128, 512], F32)
                        yatt_b = Buf()
                        rden = sb(st3, "rden", [128, 8], F32)
                        ajunk = sb(st3, "ajunk", [128, 512], BF16)
                        ast = sb(st3, "ast", [128, 4], F32)
                        mixa = sb(st3, "mixa", [128, 512], BF16)
                        mixa_b = Buf()
                        mixTa = sb(st3, "mixTa", [128, 4, 128], BF16)
                        mixTa_b = Buf()
                        xr = [sb(st3, "xr%d" % i, [128, D], F32) for i in range(2)]
                        xr_b = [Buf(), Buf()]
                        x1t = [sb(st3, "x1t%d" % i, [128, 512], F32) for i in range(2)]
                        x1t_b = [Buf(), Buf()]
                        prot = {"I": 0, "A": 0, "pt": 0}

                        def pwI():
                            i = prot["I"] % 3
                            prot["I"] += 1
                            if i == 2:
                                return xbank["t"], xbank["b"]
                            return pf[i], pfb[i]

                        def pwA():
                            i = 2 + prot["A"] % 2
                            prot["A"] += 1
                            return pf[i], pfb[i]

                        def gen_I(qt):
                            sl = qt % 2
                            nsl = qt % 3
                            nk = qt + 1
                            SK = nk * 128
                            qs = slice(qt * 128, (qt + 1) * 128)
                            sc, scb = score[sl], score_b[sl]
                            BS, BSb = bs[sl], bs_b[sl]
                            S.op("dve", lambda e: e.tensor_tensor(
                                out=dg[sl][:], in0=ident_bf[:].unsqueeze(1).to_broadcast([128, 8, 128]),
                                in1=wi[:, qt, :].unsqueeze(2).to_broadcast([128, 8, 128]), op=ALU.mult),
                                reads=[cst, wi_b], writes=[dg_b[sl]])
                            nch = (SK + 511) // 512
                            for c in range(nch):
                                cw = min(512, SK - c * 512)
                                ks = slice(c * 512, c * 512 + cw)
                                for h in range(8):
                                    j, half = h // 2, h % 2
                                    p, pb = pwI()
                                    S.op("pe", lambda e, p=p, j=j, half=half: e.matmul(
                                        p[:, 0:cw], qiT[:, j, qs], kiTz[:, half, ks], start=True, stop=True),
                                        reads=[qiT_b, kiT_b], writes=[pb])
                                    if False:
                                        S.op("act", lambda e, p=p, h=h: e.activation(
                                            out=rbuf[sl][:, h, 0:cw], in_=p[:, 0:cw], func=AF.Relu),
                                            reads=[pb], writes=[rbuf_b[sl]])
                                    else:
                                        S.op("dve", lambda e, p=p, h=h: e.tensor_scalar(
                                            out=rbuf[sl][:, h, 0:cw], in0=p[:, 0:cw], scalar1=0.0, scalar2=None, op0=ALU.max),
                                            reads=[pb], writes=[rbuf_b[sl]])
                                p, pb = pwI()
                                for h in range(8):
                                    S.op("pe", lambda e, p=p, h=h: e.matmul(
                                        p[:, 0:cw], dg[sl][:, h, :], rbuf[sl][:, h, 0:cw], start=(h == 0), stop=(h == 7)),
                                        reads=[dg_b[sl], rbuf_b[sl]], writes=[pb])
                                S.op("act", lambda e, p=p: e.copy(out=sc[:, ks], in_=p[:, 0:cw]),
                                     reads=[pb], writes=[scb])
                                S.op("dve", lambda e, c=c: e.tensor_reduce(
                                    out=BS[:, c:c + 1], in_=sc[:, ks], axis=AX.X, op=ALU.max, apply_absolute_value=True),
                                    reads=[scb], writes=[BSb])
                                yield
                            S.op("dve", lambda e: e.tensor_tensor(out=sc[:, qs], in0=sc[:, qs], in1=causal[:], op=ALU.add),
                                 reads=[scb, cst], writes=[scb])
                            if qt >= 2:
                                S.op("dve", lambda e: e.tensor_reduce(
                                    out=BS[:, 4:5], in_=BS[:, 0:nch], axis=AX.X, op=ALU.max), reads=[BSb], writes=[BSb])
                                S.op("dve", lambda e: e.tensor_scalar(
                                    out=BS[:, 8:8 + NITER + 1], in0=pow2[:, 0:NITER + 1], scalar1=BS[:, 4:5], scalar2=None,
                                    op0=ALU.mult), reads=[BSb, cst], writes=[BSb])
                                S.op("dve", lambda e: e.memset(BS[:, 5:6], 0.0), reads=[BSb], writes=[BSb])
                                thr = float(2 * TOPK - SK) - 0.5
                                S.op("dve", lambda e: e.memset(BS[:, 3:4], -thr), reads=[BSb], writes=[BSb])
                                S.op("dve", lambda e: e.tensor_scalar(
                                    out=BS[:, 9 + NITER:10 + 2 * NITER], in0=BS[:, 8:9 + NITER], scalar1=-1.0, scalar2=None,
                                    op0=ALU.mult), reads=[BSb], writes=[BSb])
                                for n in range(NITER):
                                    S.op("act", lambda e: e.activation(
                                        out=nm[nsl][:, 0:SK], in_=sc[:, 0:SK], func=AF.Sign, bias=BS[:, 5:6], scale=1.0,
                                        accum_out=BS[:, 6:7]), reads=[scb, BSb], writes=[nm_b[nsl], BSb])
                                    yield
                                    S.op("act", lambda e: e.activation(
                                        out=BS[:, 7:8], in_=BS[:, 6:7], func=AF.Sign, bias=BS[:, 3:4], scale=1.0),
                                        reads=[BSb], writes=[BSb])
                                    yield
                                    S.op("act", lambda e, n=n: e.activation(
                                        out=BS[:, 5:6], in_=BS[:, 7:8], func=AF.Identity, bias=BS[:, 5:6],
                                        scale=BS[:, 9 + NITER + 1 + n:10 + NITER + 1 + n]),
                                        reads=[BSb], writes=[BSb])
                                    yield
                                S.op("dve", lambda e: e.tensor_tensor(
                                    out=BS[:, 5:6], in0=BS[:, 5:6], in1=BS[:, 8 + NITER:9 + NITER], op=ALU.add),
                                    reads=[BSb], writes=[BSb])
                                ntau = BS[:, 5:6]
                            else:
                                ntau = taufix[:, 0:1]
                            S.op("dve", lambda e: e.tensor_scalar(
                                out=nm[nsl][:, 0:SK], in0=sc[:, 0:SK], scalar1=ntau, scalar2=0.0, op0=ALU.add, op1=ALU.is_lt),
                                reads=[scb, BSb, cst], writes=[nm_b[nsl]])
                            yield

                        def gen_A(qt):
                            nsl = qt % 3
                            nk = qt + 1
                            qs = slice(qt * 128, (qt + 1) * 128)
                            O = [pf[4], pf[5]]
                            Ob = [pfb[4], pfb[5]]
                            for kv in range(2):
                                S.op("pe", lambda e, kv=kv: e.matmul(O[kv][:, 0:260], zeros_bf[:, 0:128], zeros_bf[:, 0:260],
                                                                     start=True, stop=True), reads=[cst], writes=[Ob[kv]])
                            steps = [(kt, kv) for kt in range(nk) for kv in range(2)]

                            def emit_L(kt, kv):
                                ksl = slice(kt * 128, (kt + 1) * 128)
                                p, pb = pwA()
                                S.op("pe", lambda e, p=p: e.matmul(
                                    p[:], nm[nsl][:, ksl], negi4[:], start=True, stop=True),
                                    reads=[nm_b[nsl], cst], writes=[pb])
                                for hh in range(4):
                                    head = kv * 4 + hh
                                    j, half = head // 2, head % 2
                                    S.op("pe", lambda e, p=p, hh=hh, half=half, j=j: e.matmul(
                                        p[:, hh * 128:(hh + 1) * 128], kTz[:, kv, half, ksl], qT[:, j, qs],
                                        start=False, stop=True, skip_group_check=True),
                                        reads=[kT_b, qT_b], writes=[pb])
                                return p, pb

                            def emit_PV(kt, kv, p, pb):
                                pi3 = prot["pt"] % 3
                                prot["pt"] += 1
                                S.op("act", lambda e: e.activation(
                                    out=PT[pi3][:], in_=p[:], func=AF.Exp, bias=negM[:, 0:1], scale=1.0),
                                    reads=[pb, cst], writes=[PT_b[pi3]])
                                for hh in range(4):
                                    S.op("pe", lambda e, hh=hh: e.matmul(
                                        O[kv][:, hh * 65:(hh + 1) * 65], PT[pi3][:, hh * 128:(hh + 1) * 128], Vp[:, kt, kv, :],
                                        start=False, stop=True, skip_group_check=True),
                                        reads=[PT_b[pi3], Vp_b], writes=[Ob[kv]])

                            Lcur = emit_L(*steps[0])
                            for i, (kt, kv) in enumerate(steps):
                                Lnext = emit_L(*steps[i + 1]) if i + 1 < len(steps) else None
                                emit_PV(kt, kv, *Lcur)
                                Lcur = Lnext
                                yield
                            for kv in range(2):
                                ov = O[kv][:, 0:260].rearrange("p (h e) -> p h e", e=65)
                                S.op("dve", lambda e, kv=kv, ov=ov: e.reciprocal(out=rden[:, kv * 4:(kv + 1) * 4], in_=ov[:, :, 64]),
                                     reads=[Ob[kv]], writes=[yatt_b])
                                S.op("dve", lambda e, kv=kv, ov=ov: e.tensor_tensor(
                                    out=yatt[:, kv * 256:(kv + 1) * 256].rearrange("p (h d) -> p h d", d=64), in0=ov[:, :, 0:64],
                                    in1=rden[:, kv * 4:(kv + 1) * 4].unsqueeze(2).to_broadcast([128, 4, 64]), op=ALU.mult),
                                    reads=[Ob[kv], yatt_b], writes=[yatt_b])
                            S.op("act", lambda e: e.activation(out=ajunk[:], in_=yatt[:], func=AF.Square, accum_out=ast[:, 0:1]),
                                 reads=[yatt_b], writes=[yatt_b])
                            S.op("act", lambda e: e.activation(out=ast[:, 1:2], in_=ast[:, 0:1], func=AF.Sqrt, bias=EPS, scale=1.0 / 512),
                                 reads=[yatt_b], writes=[yatt_b])
                            S.op("dve", lambda e: e.reciprocal(out=ast[:, 2:3], in_=ast[:, 1:2]), reads=[yatt_b], writes=[yatt_b])
                            S.op("dve", lambda e: e.scalar_tensor_tensor(out=mixa[:], in0=yatt[:], scalar=ast[:, 2:3], in1=gna_b[:],
                                                                          op0=ALU.mult, op1=ALU.mult),
                                 reads=[yatt_b, cst], writes=[mixa_b])
                            ptt, ptb_ = next_pt()
                            for ft in range(4):
                                S.op("pe", lambda e, ptt=ptt, ft=ft: e.transpose(
                                    out=ptt[:, ft * 128:(ft + 1) * 128], in_=mixa[:, ft * 128:(ft + 1) * 128], identity=ident_bf[:]),
                                    reads=[mixa_b, cst], writes=[ptb_])
                            S.op("act", lambda e, ptt=ptt: e.copy(out=mixTa[:].rearrange("p a b -> p (a b)"), in_=ptt[:, 0:512]),
                                 reads=[ptb_], writes=[mixTa_b])
                            yield
                            xb = qt % 2
                            rows = slice(r0 + qt * 128, r0 + (qt + 1) * 128)
                            S.dma("sp", xr[xb][:], x_d[rows, :], writes=[xr_b[xb]])
                            for hf in range(2):
                                p, pb = pwA()
                                for k8 in range(8):
                                    lhs = mixT_s[:, k8, qs] if k8 < 4 else mixTa[:, k8 - 4, :]
                                    S.op("pe", lambda e, p=p, lhs=lhs, k8=k8, hf=hf: e.matmul(
                                        p[:], lhs, w_out[:, k8, hf * 512:(hf + 1) * 512], start=(k8 == 0), stop=(k8 == 7)),
                                        reads=[mixT_sb, mixTa_b, w_out_b], writes=[pb])
                                hs = slice(hf * 512, (hf + 1) * 512)
                                S.op("dve", lambda e, p=p, hf=hf, hs=hs: e.tensor_tensor(
                                    out=x1t[hf][:], in0=p[:], in1=tabsA[s][:, 2, hs], op=ALU.mult),
                                    reads=[pb, tabsA_b[s]], writes=[x1t_b[hf]])
                                S.op("dve", lambda e, xb=xb, hf=hf, hs=hs: e.tensor_tensor(
                                    out=xr[xb][:, hs], in0=x1t[hf][:], in1=xr[xb][:, hs], op=ALU.add),
                                    reads=[x1t_b[hf], xr_b[xb]], writes=[xr_b[xb]])
                            S.dma("sp", out_d[rows, :], xr[xb][:], reads=[xr_b[xb]])
                            yield

                        NQ = 16
                        actI = []
                        actA = None
                        nextI, nextA = 0, 0
                        doneI = set()
                        while nextA < NQ or actA is not None:
                            if actA is None and nextA in doneI:
                                actA = (nextA, gen_A(nextA))
                                nextA += 1
                            firstA = actA[0] if actA is not None else nextA
                            while (nextI < NQ and len(actI) < 2 and nextI - 3 < firstA
                                   and (nextI < 2 or (nextI - 2) in doneI)):
                                actI.append((nextI, gen_I(nextI)))
                                nextI += 1
                            for item in list(actI):
                                try:
                                    next(item[1])
                                except StopIteration:
                                    actI.remove(item)
                                    doneI.add(item[0])
                            if actA is not None:
                                try:
                                    next(actA[1])
                                except StopIteration:
                                    actA = None
                        S.barrier()

        chk('s3')
        NB = 256
        NTT = NB // 128
        with ExitStack() as stB:
            tabB_d = nc.dram_tensor("tabB_scr", [NSEQ, 128, 3 * D], F32, kind="Internal").ap()
            tabBd_b = Buf()
            with ExitStack() as stt:
                tl = [sb(stt, "tabsBt%d" % s, [128, 3, D], F32) for s in range(NSEQ)]
                tlb = [Buf() for _ in range(NSEQ)]
                adaln(tl, tlb, list(range(NSEQ)), 3, n2g_d, stt)
                for s in range(NSEQ):
                    S.dma("sp", tabB_d[s], tl[s][:].rearrange("p a b -> p (a b)"), reads=[tlb[s]], writes=[tabBd_b])
                S.barrier()
            tabsB1 = sb(stB, "tabsB", [128, 3, D], F32)
            tabsB_b1 = Buf()
            extra_bank(stB, True)
            W1 = sb(stB, "W1", [128, 8, DFF], BF16)
            W2 = sb(stB, "W2", [128, 32, D], BF16)
            W_b = Buf()
            for kt in range(8):
                S.dma("pool", W1[:, kt, :], wff1_d[:, kt, :], writes=[W_b])
            for ft in range(0, 32, 4):
                S.dma("pool", W2[:, ft:ft + 4, :], wff2_d[:, ft:ft + 4, :], writes=[W_b])
            hidT = sb(stB, "hidT", [128, 32, NB], BF16)
            hid_b = [Buf() for _ in range(32)]
            rl = [sb(stB, "rl%d" % i, [128, NB], BF16) for i in range(2)]
            rl_b = [Buf(), Buf()]
            h2T = sb(stB, "h2T", [128, 8, NB], BF16)
            h2T_b = Buf()
            x1 = [sb(stB, "x1_%d" % i, [128, D], F32) for i in range(2)]
            x1_b = [Buf() for _ in range(2)]
            junkB = sb(stB, "junkB", [128, D], BF16)
            junkB_b = Buf()
            tB = sb(stB, "tB", [128, D], F32)
            tB_b = Buf()
            h2 = [sb(stB, "h2_%d" % i, [128, D], BF16) for i in range(2)]
            h2_b = [Buf(), Buf()]
            stb = [sb(stB, "stb%d" % i, [128, 4], F32) for i in range(2)]
            stb_b = [Buf(), Buf()]
            ot = [sb(stB, "ot%d" % i, [128, D], F32) for i in range(2)]
            ot_b = [Buf(), Buf()]
            oc = 0
            nblk = NSEQ * SEQ // NB
            for blk in range(nblk):
                s = (blk * NB) // SEQ
                if (blk * NB) % SEQ == 0:
                    S.dma("sp", tabsB1[:].rearrange("p a b -> p (a b)"), tabB_d[s], reads=[tabBd_b], writes=[tabsB_b1])
                for tt in range(NTT):
                    rows = slice(blk * NB + tt * 128, blk * NB + (tt + 1) * 128)
                    b = tt % 2
                    S.dma("sp", x1[b][:], out_d[rows, :], writes=[x1_b[b]])
                    S.op("act", lambda e, b=b: e.activation(out=junkB[:], in_=x1[b][:], func=AF.Square,
                                                            accum_out=stb[b][:, 0:1]),
                         reads=[x1_b[b]], writes=[junkB_b, stb_b[b]])
                    S.op("act", lambda e, b=b: e.activation(out=stb[b][:, 1:2], in_=stb[b][:, 0:1], func=AF.Sqrt,
                                                            bias=EPS, scale=1.0 / D), reads=[stb_b[b]], writes=[stb_b[b]])
                    S.op("dve", lambda e, b=b: e.reciprocal(out=stb[b][:, 2:3], in_=stb[b][:, 1:2]),
                         reads=[stb_b[b]], writes=[stb_b[b]])
                    S.op("dve", lambda e, b=b: e.scalar_tensor_tensor(
                        out=tB[:], in0=x1[b][:], scalar=stb[b][:, 2:3], in1=tabsB1[:, 1, :], op0=ALU.mult, op1=ALU.mult),
                        reads=[x1_b[b], stb_b[b], tabsB_b1], writes=[tB_b])
                    S.op("dve", lambda e, b=b: e.tensor_tensor(out=h2[b][:], in0=tB[:], in1=tabsB1[:, 0, :], op=ALU.add),
                         reads=[tB_b, tabsB_b1], writes=[h2_b[b]])
                    ptt, ptb_ = next_pt()
                    for kt in range(8):
                        S.op("pe", lambda e, ptt=ptt, b=b, kt=kt: e.transpose(
                            out=ptt[:, kt * 128:(kt + 1) * 128], in_=h2[b][:, kt * 128:(kt + 1) * 128], identity=ident_bf[:]),
                            reads=[h2_b[b], cst], writes=[ptb_])
                    S.op("act", lambda e, ptt=ptt, tt=tt: e.copy(
                        out=h2T[:, :, tt * 128:(tt + 1) * 128], in_=ptt[:].rearrange("p (a b) -> p a b", b=128)),
                        reads=[ptb_], writes=[h2T_b])
                for ft in range(32):
                    p, pb = next_pw(2)
                    for kt in range(8):
                        S.op("pe", lambda e, p=p, kt=kt, ft=ft: e.matmul(
                            p[:, 0:NB], W1[:, kt, ft * 128:(ft + 1) * 128], h2T[:, kt, :], start=(kt == 0), stop=(kt == 7)),
                            reads=[W_b, h2T_b], writes=[pb])
                    b = ft % 2
                    S.op("act", lambda e, p=p, b=b: e.activation(out=rl[b][:], in_=p[:, 0:NB], func=AF.Relu),
                         reads=[pb], writes=[rl_b[b]])
                    S.op("pool", lambda e, b=b, ft=ft: e.tensor_tensor(out=hidT[:, ft, :], in0=rl[b][:], in1=rl[b][:], op=ALU.mult),
                         reads=[rl_b[b]], writes=[hid_b[ft]])
                for tt in range(NTT):
                    rows = slice(blk * NB + tt * 128, blk * NB + (tt + 1) * 128)
                    ob = oc % 2
                    oc += 1
                    S.dma("sp", ot[ob][:], out_d[rows, :], writes=[ot_b[ob]])
                    for hf in range(2):
                        bi = 2 + (2 * tt + hf) % 4
                        p, pb = pf[bi], pfb[bi]
                        for ft in range(32):
                            S.op("pe", lambda e, p=p, ft=ft, tt=tt, hf=hf: e.matmul(
                                p[:], hidT[:, ft, tt * 128:(tt + 1) * 128], W2[:, ft, hf * 512:(hf + 1) * 512],
                                start=(ft == 0), stop=(ft == 31)), reads=[hid_b[ft], W_b], writes=[pb])
                        hs = slice(hf * 512, (hf + 1) * 512)
                        S.op("dve", lambda e, p=p, hs=hs: e.tensor_tensor(
                            out=tB[:, hs], in0=p[:], in1=tabsB1[:, 2, hs], op=ALU.mult),
                            reads=[pb, tabsB_b1], writes=[tB_b])
                        S.op("dve", lambda e, ob=ob, hs=hs: e.tensor_tensor(
                            out=ot[ob][:, hs], in0=tB[:, hs], in1=ot[ob][:, hs], op=ALU.add),
                            reads=[tB_b, ot_b[ob]], writes=[ot_b[ob]])
                    S.dma("sp", out_d[rows, :], ot[ob][:], reads=[ot_b[ob]])
            S.barrier()
    except _Stop:
        pass
    return nc


_PROGRAM = None


def _prep_shared(inp):
    f = np.float32
    def kt_layout(w):
        K, N = w.shape
        return np.ascontiguousarray(w.reshape(K // 128, 128, N).transpose(1, 0, 2)).astype(f)
    def bc(v, n=128):
        return np.ascontiguousarray(np.broadcast_to(np.asarray(v, f).reshape(1, -1), (n, np.asarray(v).size)))
    def qlay(a):
        a = np.asarray(a, f)
        rest = a.shape[2:]
        return np.ascontiguousarray(a.reshape((16, 2, 64) + rest).transpose((1, 2, 0) + tuple(range(3, 3 + len(rest))))
                                    .reshape((128, 16) + rest))
    sh = {}
    sh["w_ada"] = kt_layout(inp["w_ada"][0])
    sh["b_ada_b"] = bc(inp["b_ada"][0])
    sh["w_in"] = kt_layout(inp["w_in"][0])
    sh["w_out"] = kt_layout(inp["w_out"][0])
    sh["w_glu"] = kt_layout(inp["w_glu"][0])
    sh["w_ff1"] = kt_layout(inp["w_ff1"][0])
    sh["w_ff2"] = kt_layout(inp["w_ff2"][0])
    sh["norm1_g_b"] = bc(inp["norm1_g"][0])
    sh["norm2_g_b"] = bc(inp["norm2_g"][0])
    sh["b_glu_b"] = bc(inp["b_glu"][0])
    sh["gn_ssm_b"] = bc(inp["gn_ssm"][0])
    sh["gn_attn_b"] = bc(inp["gn_attn"][0])
    sh["gq_b"] = bc(inp["q_gain"][0])
    sh["gk_b"] = bc(inp["k_gain"][0])
    sh["gq2"] = np.ascontiguousarray(np.tile(np.asarray(inp["q_gain"][0], f), 2).reshape(128, 1))
    sh["gk2"] = np.ascontiguousarray(np.tile(np.asarray(inp["k_gain"][0], f), 2).reshape(128, 1))
    sh["lamre_q"] = qlay(inp["lam_re"][0])
    sh["lamim_q"] = qlay(inp["lam_im"][0])
    sh["logdt_q"] = qlay(np.broadcast_to(np.asarray(inp["log_dt"][0], f)[:, None], (32, 64)))
    sh["bre_q"] = qlay(inp["ssm_b_re"][0])
    sh["bim_q"] = qlay(inp["ssm_b_im"][0])
    sh["cre_q"] = qlay(np.asarray(inp["ssm_c_re"][0]).transpose(0, 2, 1))
    sh["cim_q"] = qlay(np.asarray(inp["ssm_c_im"][0]).transpose(0, 2, 1))
    dsk = np.asarray(inp["d_skip"][0], f).reshape(32, 16)
    sh["dsk_b"] = np.ascontiguousarray(np.broadcast_to(np.tile(dsk, (1, 8))[None], (128, 32, 128))).astype(f)
    sh["ident"] = np.eye(128, dtype=f)
    r = np.arange(128)
    sh["causal"] = np.where(r[None, :] <= r[:, None], 0.0, -1.0e30).astype(f)
    sh["negi4"] = np.tile(-BIG * np.eye(128, dtype=f), (1, 4)).astype(f)
    ib, jb = r // 16, r // 16
    sh["bmask"] = (jb[None, :] >= ib[:, None]).astype(f)
    sh["dmask"] = (r[None, :] == r[:, None]).astype(f)
    sh["onesblk"] = ((r[None, :] // 64) == (r[:, None] // 64)).astype(f)
    sh["mtab"] = np.ascontiguousarray(np.broadcast_to(np.asarray(EXPS, f)[None, None, :], (128, 16, NE)))
    sh["pow2"] = np.ascontiguousarray(np.broadcast_to((2.0 ** -np.arange(NITER + 2)).astype(f)[None], (128, NITER + 2)))
    zm = np.zeros((128, 2), f)
    zm[:64, 0] = 1.0
    zm[64:, 1] = 1.0
    sh["zmask"] = zm
    return sh


def kernel(**inputs):
    global _PROGRAM
    inp = {k: np.asarray(v) for k, v in inputs.items()}
    if _PROGRAM is None:
        _PROGRAM = build_program()
    nc = _PROGRAM
    shared = _prep_shared(inp)
    x = np.asarray(inp["x"], np.float32)
    c = np.asarray(inp["c"], np.float32)
    in_maps = []
    for core in range(NCORES):
        m = dict(shared)
        m["x"] = np.ascontiguousarray(x[NSEQ * core: NSEQ * (core + 1)].reshape(NSEQ * SEQ, D))
        cc = c[NSEQ * core: NSEQ * (core + 1)]
        cT = cc.reshape(NSEQ, 8, 128).transpose(2, 1, 0)
        m["cT"] = np.ascontiguousarray(np.broadcast_to(cT[:, :, :, None], (128, 8, NSEQ, 128))).astype(np.float32)
        in_maps.append(m)
    res = run_bass_kernel_spmd(nc, in_maps, core_ids=list(range(NCORES)))
    outs = [np.asarray(r["out"], np.float32).reshape(NSEQ, SEQ, D) for r in res.results]
    return np.concatenate(outs, axis=0)
```

```python
import math
from contextlib import ExitStack

import numpy as np
import concourse.bass as bass
import concourse.mybir as mybir
from concourse.alu_op_type import AluOpType as ALU
from concourse.bass_utils import run_bass_kernel_spmd

F32 = mybir.dt.float32
BF16 = mybir.dt.bfloat16
I32 = mybir.dt.int32
AF = mybir.ActivationFunctionType
AX = mybir.AxisListType

NCORES = 8
D = 1024
SEQ = 2048
NSEQ = 2
DIN = 1864
DFF = 4096
EPS = 1e-6
IDX_SCALE = (64 ** -0.5) * (8 ** -0.5)
TOPK = 256
NITER = 16
BIG = 30000.0
EXPS = list(range(-7, 9)) + [16, 32, 64, 128, 256, 512, 1024]
NE = len(EXPS)
EIDX = {m: i for i, m in enumerate(EXPS)}
TWO_PI = 2.0 * math.pi


class Buf:
    __slots__ = ("w", "r", "ex")

    def __init__(self, ex=False):
        self.w = None
        self.r = {}
        self.ex = ex


class Sched:
    NDS = 24

    def __init__(self, nc, st):
        self.nc = nc
        self.eng = {"pe": nc.tensor, "act": nc.scalar, "dve": nc.vector, "pool": nc.gpsimd, "sp": nc.sync}
        self.sem = {k: st.enter_context(nc.semaphore("s_" + k)) for k in self.eng}
        self.cnt = {k: 0 for k in self.eng}
        self.waited = {k: {} for k in self.eng}
        self.dsem = [st.enter_context(nc.semaphore("d%d" % i)) for i in range(self.NDS)]
        self.dcnt = [0] * self.NDS
        self.dpool = {"sp": list(range(0, 16)), "pool": list(range(16, 24))}
        self.dnext = {"sp": 0, "pool": 0}

    def _semof(self, key):
        return self.sem[key[1]] if key[0] == "e" else self.dsem[key[1]]

    def _wait(self, e, key, val):
        if key[0] == "e" and key[1] == e and e == "pe":
            return
        if self.waited[e].get(key, 0) >= val:
            return
        self.eng[e].wait_ge(self._semof(key), val)
        self.waited[e][key] = val

    def _deps(self, e, reads, writes):
        for b in reads:
            if b.w is not None:
                self._wait(e, b.w[0], b.w[1])
            if b.ex:
                for k, v in b.r.items():
                    if k != ("e", e):
                        self._wait(e, k, v)
        for b in writes:
            if b.w is not None:
                self._wait(e, b.w[0], b.w[1])
            for k, v in b.r.items():
                self._wait(e, k, v)

    def _mark(self, key, val, reads, writes):
        for b in reads:
            if b.r.get(key, 0) < val:
                b.r[key] = val
        for b in writes:
            b.w = (key, val)
            b.r = {}

    def op(self, e, fn, reads=(), writes=()):
        self._deps(e, reads, writes)
        ins = fn(self.eng[e])
        self.cnt[e] += 1
        ins.then_inc(self.sem[e], 1)
        self._mark(("e", e), self.cnt[e], reads, writes)

    def dma(self, e, out, in_, reads=(), writes=(), **kw):
        self._deps(e, reads, writes)
        pl = self.dpool[e]
        j = pl[self.dnext[e] % len(pl)]
        self.dnext[e] += 1
        if self.dcnt[j] > 0:
            self._wait(e, ("d", j), self.dcnt[j])
        ins = self.eng[e].dma_start(out=out, in_=in_, **kw)
        self.dcnt[j] += 16
        ins.then_inc(self.dsem[j], 16)
        self._mark(("d", j), self.dcnt[j], reads, writes)

    def barrier(self):
        for e in self.eng:
            for k in self.eng:
                if k != e and self.cnt[k] > 0:
                    self._wait(e, ("e", k), self.cnt[k])
            for j in range(self.NDS):
                if self.dcnt[j] > 0:
                    self._wait(e, ("d", j), self.dcnt[j])


class _Stop(Exception):
    pass


def build_program(stop=None):
    nc = bass.Bass("TRN2", target_bir_lowering=False)

    def din(name, shape, dt=F32):
        return nc.dram_tensor(name, list(shape), dt, kind="ExternalInput").ap()

    x_d = din("x", [NSEQ * SEQ, D])
    cT_d = din("cT", [128, 8, NSEQ, 128])
    wada_d = din("w_ada", [128, 8, 6 * D])
    bada_d = din("b_ada_b", [128, 6 * D])
    win_d = din("w_in", [128, 8, DIN])
    wout_d = din("w_out", [128, 8, D])
    wglu_d = din("w_glu", [128, 4, 512])
    wff1_d = din("w_ff1", [128, 8, DFF])
    wff2_d = din("w_ff2", [128, 32, D])
    n1g_d = din("norm1_g_b", [128, D])
    n2g_d = din("norm2_g_b", [128, D])
    bglu_d = din("b_glu_b", [128, 512])
    gns_d = din("gn_ssm_b", [128, 512])
    gna_d = din("gn_attn_b", [128, 512])
    gqb_d = din("gq_b", [128, 64])
    gkb_d = din("gk_b", [128, 64])
    gq2_d = din("gq2", [128, 1])
    gk2_d = din("gk2", [128, 1])
    lre_d = din("lamre_q", [128, 16])
    lim_d = din("lamim_q", [128, 16])
    ldt_d = din("logdt_q", [128, 16])
    bre_d = din("bre_q", [128, 16, 16])
    bim_d = din("bim_q", [128, 16, 16])
    cre_d = din("cre_q", [128, 16, 16])
    cim_d = din("cim_q", [128, 16, 16])
    dsk_d = din("dsk_b", [128, 32, 128])
    ident_d = din("ident", [128, 128])
    causal_d = din("causal", [128, 128])
    negi4_d = din("negi4", [128, 512])
    bmask_d = din("bmask", [128, 128])
    dmask_d = din("dmask", [128, 128])
    onesblk_d = din("onesblk", [128, 128])
    mtab_d = din("mtab", [128, 16, NE])
    pow2_d = din("pow2", [128, NITER + 2])
    zmask_d = din("zmask", [128, 2])
    out_d = nc.dram_tensor("out", [NSEQ * SEQ, D], F32, kind="ExternalOutput").ap()

    try:
      with ExitStack() as top:
        S = Sched(nc, top)

        def chk(name):
            if stop == name:
                S.barrier()
                raise _Stop()

        uid = [0]

        def sb(st, name, shape, dt):
            uid[0] += 1
            return st.enter_context(nc.sbuf_tensor("sb%d_%s" % (uid[0], name), list(shape), dt))

        def ps(st, name, shape, dt):
            uid[0] += 1
            return st.enter_context(nc.psum_tensor("ps%d_%s" % (uid[0], name), list(shape), dt))

        pt = [ps(top, "pt0", [128, 1024], BF16)]
        ptb = [Buf(ex=True)]
        xbank = {}

        def extra_bank(st, as_bf16):
            if as_bf16:
                t = ps(st, "ptx", [128, 1024], BF16)
                del pt[1:], ptb[1:]
                pt.append(t)
                ptb.append(Buf(ex=True))
                rot["pt"] = 0
            else:
                del pt[1:], ptb[1:]
                rot["pt"] = 0
                xbank["t"] = ps(st, "pfx", [128, 512], F32)
                xbank["b"] = Buf(ex=True)
        pf = [ps(top, "pf%d" % i, [128, 512], F32) for i in range(6)]
        pfb = [Buf(ex=True) for _ in range(6)]
        rot = {"pt": 0, "pw": 0}

        def next_pt():
            i = rot["pt"] % len(pt)
            rot["pt"] = i + 1
            return pt[i], ptb[i]

        def next_pw(n=4):
            i = rot["pw"] % n
            rot["pw"] += 1
            return pf[i], pfb[i]

        ident_bf = sb(top, "ident_bf", [128, 128], BF16)
        ident_f = sb(top, "ident_f", [128, 128], F32)
        causal = sb(top, "causal", [128, 128], F32)
        negi4 = sb(top, "negi4", [128, 512], BF16)
        onesblk = sb(top, "onesblk", [128, 128], BF16)
        zeros_bf = sb(top, "zeros_bf", [128, 260], BF16)
        bglu_b = sb(top, "bglu_b", [128, 512], F32)
        gns_b = sb(top, "gns_b", [128, 512], F32)
        gna_b = sb(top, "gna_b", [128, 512], F32)
        pow2 = sb(top, "pow2", [128, NITER + 2], F32)
        G2 = sb(top, "G2", [128, 1], F32)
        negM = sb(top, "negM", [128, 1], F32)
        taufix = sb(top, "taufix", [128, 1], F32)
        siluT = sb(top, "siluT", [128, 8, NSEQ, 128], BF16)
        cst = Buf()
        for t_, d_ in ((ident_bf, ident_d), (ident_f, ident_d), (causal, causal_d), (negi4, negi4_d),
                       (onesblk, onesblk_d), (bglu_b, bglu_d), (gns_b, gns_d), (gna_b, gna_d), (pow2, pow2_d)):
            S.dma("pool", t_[:], d_, writes=[cst])
        S.op("dve", lambda e: e.memset(zeros_bf[:], 0.0), writes=[cst])
        S.op("dve", lambda e: e.memset(taufix[:], 1.0e29), writes=[cst])

        with ExitStack() as st0:
            gqb = sb(st0, "gqb", [128, 64], F32)
            gkb = sb(st0, "gkb", [128, 64], F32)
            gq2 = sb(st0, "gq2", [128, 1], F32)
            gk2 = sb(st0, "gk2", [128, 1], F32)
            cTf = sb(st0, "cTf", [128, 8 * NSEQ * 128], F32)
            tb = Buf()
            S.dma("sp", gqb[:], gqb_d, writes=[tb])
            S.dma("sp", gkb[:], gkb_d, writes=[tb])
            S.dma("sp", gq2[:], gq2_d, writes=[tb])
            S.dma("sp", gk2[:], gk2_d, writes=[tb])
            S.dma("sp", cTf[:], cT_d.rearrange("p a b c -> p (a b c)"), writes=[tb])
            S.op("dve", lambda e: e.scalar_tensor_tensor(out=G2[:], in0=gq2[:], scalar=0.125, in1=gk2[:],
                                                          op0=ALU.mult, op1=ALU.mult), reads=[tb], writes=[cst])
            S.op("dve", lambda e: e.scalar_tensor_tensor(out=gqb[:], in0=gqb[:], scalar=0.125, in1=gkb[:],
                                                          op0=ALU.mult, op1=ALU.mult), reads=[tb], writes=[tb])
            S.op("dve", lambda e: e.tensor_reduce(out=negM[:], in_=gqb[:], axis=AX.X, op=ALU.max,
                                                  apply_absolute_value=True), reads=[tb], writes=[cst])
            S.op("dve", lambda e: e.tensor_scalar(out=negM[:], in0=negM[:], scalar1=-64.0, scalar2=None,
                                                  op0=ALU.mult), reads=[cst], writes=[cst])
            S.op("act", lambda e: e.activation(out=siluT[:].rearrange("p a b c -> p (a b c)"), in_=cTf[:],
                                               func=AF.Silu), reads=[tb], writes=[cst])
            S.barrier()

        chk('consts')
        def adaln_scratch(st):
            wst = [sb(st, "wst%d" % i, [128, 8, 512], BF16) for i in range(2)]
            wstb = [Buf(), Buf()]
            bst = [sb(st, "bst%d" % i, [128, 512], F32) for i in range(2)]
            gst = [sb(st, "gst%d" % i, [128, 512], F32) for i in range(2)]
            tmp = [sb(st, "adat%d" % i, [128, 512], F32) for i in range(2)]
            tmpb = [Buf(), Buf()]
            return wst, wstb, bst, gst, tmp, tmpb

        tabAll_d = nc.dram_tensor("tabAll_scr", [2 * NSEQ, 128, 3 * D], F32, kind="Internal").ap()
        tabAll_b = Buf()

        def adaln(tabs, tabs_b, seqs, first_j, ng_d, scr):
            wst, wstb, bst, gst, tmp, tmpb = scr
            it = 0
            for jj in range(3):
                for hc in range(2):
                    c0 = (first_j + jj) * D + hc * 512
                    b = it % 2
                    it += 1
                    S.dma("pool", wst[b][:], wada_d[:, :, c0:c0 + 512], writes=[wstb[b]])
                    S.dma("sp", bst[b][:], bada_d[:, c0:c0 + 512], writes=[wstb[b]])
                    if jj == 1:
                        S.dma("sp", gst[b][:], ng_d[:, hc * 512:(hc + 1) * 512], writes=[wstb[b]])
                    for n, s in enumerate(seqs):
                        p, pb = next_pw()
                        for kt in range(8):
                            S.op("pe", lambda e, p=p, b=b, kt=kt, s=s: e.matmul(
                                p[:], siluT[:, kt, s, :], wst[b][:, kt, :], start=(kt == 0), stop=(kt == 7)),
                                reads=[cst, wstb[b]], writes=[pb])
                        dst = tabs[n][:, jj, hc * 512:(hc + 1) * 512]
                        if jj == 1:
                            tb_ = tmpb[n]
                            S.op("dve", lambda e, p=p, b=b, n=n: e.tensor_tensor(
                                out=tmp[n][:], in0=p[:], in1=bst[b][:], op=ALU.add),
                                reads=[pb, wstb[b]], writes=[tb_])
                            S.op("dve", lambda e, b=b, n=n, dst=dst: e.scalar_tensor_tensor(
                                out=dst, in0=tmp[n][:], scalar=1.0, in1=gst[b][:], op0=ALU.add, op1=ALU.mult),
                                reads=[tb_, wstb[b]], writes=[tabs_b[n]])
                        else:
                            S.op("dve", lambda e, p=p, b=b, dst=dst: e.tensor_tensor(
                                out=dst, in0=p[:], in1=bst[b][:], op=ALU.add),
                                reads=[pb, wstb[b]], writes=[tabs_b[n]])

        with ExitStack() as stA:
            tabsA1 = sb(stA, "tabsA", [128, 3, D], F32)
            tabsA = [tabsA1, tabsA1]
            tabsA_b1 = Buf()
            tabsA_b = [tabsA_b1, tabsA_b1]

            ssmA_d = nc.dram_tensor("ssmA_scr", [128, 32, 128], BF16, kind="Internal").ap()
            ssmB_d = nc.dram_tensor("ssmB_scr", [128, 32, 128], BF16, kind="Internal").ap()
            ssmC_d = nc.dram_tensor("ssmC_scr", [128, 32, 2, 128], BF16, kind="Internal").ap()
            ssmd_b = Buf()
            ak = sb(stA, "ak", [128, 16, 8], F32)
            ck = sb(stA, "ck", [128, 16, 8], F32)
            nck = sb(stA, "nck", [128, 16, 8], F32)
            ssm_b = Buf()
            with ExitStack() as st1:
                extra_bank(st1, True)
                A_sb = sb(st1, "A_sb", [128, 32, 128], BF16)
                Bm_sb = sb(st1, "Bm_sb", [128, 32, 128], BF16)
                Cmz = sb(st1, "Cmz", [128, 32, 2, 128], BF16)

                def t3(name, n):
                    return sb(st1, name, [128, 16, n], F32)
                lre = t3("lre", 1); lim = t3("lim", 1); ldt = t3("ldt", 1)
                bre = t3("bre", 16); bim = t3("bim", 16); cre = t3("cre", 16); cim = t3("cim", 16)
                mtab = t3("mtab", NE)
                bmask = sb(st1, "bmask", [128, 128], F32)
                dmask = sb(st1, "dmask", [128, 128], F32)
                zmask = sb(st1, "zmask", [128, 2], F32)
                dsk = sb(st1, "dsk", [128, 32, 128], F32)
                ib = Buf()
                for t_, d_ in ((lre, lre_d), (lim, lim_d), (ldt, ldt_d)):
                    S.dma("sp", t_[:, :, 0], d_, writes=[ib])
                for t_, d_ in ((bre, bre_d), (bim, bim_d), (cre, cre_d), (cim, cim_d), (mtab, mtab_d),
                               (bmask, bmask_d), (dmask, dmask_d), (dsk, dsk_d), (zmask, zmask_d)):
                    S.dma("sp", t_[:], d_, writes=[ib])
                chk('ssm_a')
                dt_ = t3("dt_", 1); aa = t3("aa", 1); th = t3("th", 1)
                ang = t3("ang", NE); lmag = t3("lmag", NE); mag = t3("mag", NE)
                tq = t3("tq", NE); tqi = sb(st1, "tqi", [128, 16, NE], I32); tqf = t3("tqf", NE)
                wr = t3("wr", NE); sn = t3("sn", NE); cs = t3("cs", NE)
                pr_ = t3("pr_", NE); pi_ = t3("pi_", NE)
                wb = Buf()

                def V(fn, reads=(), writes=(wb,)):
                    S.op("dve", fn, reads=[ib, wb] + list(reads), writes=list(writes))

                def ACT(fn, reads=(), writes=(wb,)):
                    S.op("act", fn, reads=[ib, wb] + list(reads), writes=list(writes))

                ACT(lambda e: e.activation(out=dt_[:], in_=ldt[:], func=AF.Exp))
                V(lambda e: e.tensor_tensor(out=aa[:], in0=lre[:], in1=dt_[:], op=ALU.mult))
                V(lambda e: e.tensor_tensor(out=th[:], in0=lim[:], in1=dt_[:], op=ALU.mult))
                V(lambda e: e.tensor_tensor(out=lmag[:], in0=mtab[:], in1=aa[:].to_broadcast([128, 16, NE]), op=ALU.mult))
                V(lambda e: e.tensor_tensor(out=ang[:], in0=mtab[:], in1=th[:].to_broadcast([128, 16, NE]), op=ALU.mult))
                ACT(lambda e: e.activation(out=mag[:], in_=lmag[:], func=AF.Exp))
                V(lambda e: e.tensor_scalar(out=tq[:], in0=ang[:], scalar1=1.0 / TWO_PI, scalar2=None, op0=ALU.mult))
                V(lambda e: e.tensor_copy(out=tqi[:], in_=tq[:]))
                V(lambda e: e.tensor_copy(out=tqf[:], in_=tqi[:]))
                V(lambda e: e.scalar_tensor_tensor(out=wr[:], in0=tqf[:], scalar=-TWO_PI, in1=ang[:],
                                                   op0=ALU.mult, op1=ALU.add))
                wt_ = t3("wt_", NE)
                for t_, shift in ((sn, 0.0), (cs, math.pi / 2)):
                    V(lambda e, t_=t_, shift=shift: e.tensor_scalar(out=t_[:], in0=wr[:], scalar1=shift, scalar2=None, op0=ALU.add))
                    V(lambda e, t_=t_: e.tensor_scalar(out=wt_[:], in0=t_[:], scalar1=math.pi, scalar2=-TWO_PI,
                                                       op0=ALU.is_gt, op1=ALU.mult))
                    V(lambda e, t_=t_: e.tensor_scalar(out=tq[:], in0=t_[:], scalar1=-math.pi, scalar2=TWO_PI,
                                                       op0=ALU.is_lt, op1=ALU.mult))
                    V(lambda e, t_=t_: e.tensor_tensor(out=t_[:], in0=t_[:], in1=wt_[:], op=ALU.add))
                    V(lambda e, t_=t_: e.tensor_tensor(out=t_[:], in0=t_[:], in1=tq[:], op=ALU.add))
                for t_ in (sn, cs):
                    V(lambda e, t_=t_: e.tensor_scalar(out=t_[:], in0=t_[:], scalar1=3.14159, scalar2=-3.14159,
                                                       op0=ALU.min, op1=ALU.max))
                ACT(lambda e: e.activation(out=sn[:], in_=sn[:], func=AF.Sin))
                ACT(lambda e: e.activation(out=cs[:], in_=cs[:], func=AF.Sin))
                V(lambda e: e.tensor_tensor(out=pr_[:], in0=mag[:], in1=cs[:], op=ALU.mult))
                V(lambda e: e.tensor_tensor(out=pi_[:], in0=mag[:], in1=sn[:], op=ALU.mult))
                chk('ssm_b')
                i1 = EIDX[1]
                nr = t3("nr", 1); den = t3("den", 1); t1_ = t3("t1_", 1); gr = t3("gr", 1); gi = t3("gi", 1)
                V(lambda e: e.tensor_scalar(out=nr[:], in0=pr_[:, :, i1:i1 + 1], scalar1=-1.0, scalar2=None, op0=ALU.add))
                V(lambda e: e.tensor_tensor(out=den[:], in0=lre[:], in1=lre[:], op=ALU.mult))
                V(lambda e: e.tensor_tensor(out=t1_[:], in0=lim[:], in1=lim[:], op=ALU.mult))
                V(lambda e: e.tensor_tensor(out=den[:], in0=den[:], in1=t1_[:], op=ALU.add))
                V(lambda e: e.reciprocal(out=den[:], in_=den[:]))
                V(lambda e: e.tensor_tensor(out=gr[:], in0=nr[:], in1=lre[:], op=ALU.mult))
                V(lambda e: e.tensor_tensor(out=t1_[:], in0=pi_[:, :, i1:i1 + 1], in1=lim[:], op=ALU.mult))
                V(lambda e: e.tensor_tensor(out=gr[:], in0=gr[:], in1=t1_[:], op=ALU.add))
                V(lambda e: e.tensor_tensor(out=gr[:], in0=gr[:], in1=den[:], op=ALU.mult))
                V(lambda e: e.tensor_tensor(out=gi[:], in0=pi_[:, :, i1:i1 + 1], in1=lre[:], op=ALU.mult))
                V(lambda e: e.tensor_tensor(out=t1_[:], in0=nr[:], in1=lim[:], op=ALU.mult))
                V(lambda e: e.tensor_tensor(out=gi[:], in0=gi[:], in1=t1_[:], op=ALU.subtract))
                V(lambda e: e.tensor_tensor(out=gi[:], in0=gi[:], in1=den[:], op=ALU.mult))
                for k in range(8):
                    ii = EIDX[8 * (2 ** k)]
                    V(lambda e, k=k, ii=ii: e.tensor_copy(out=ak[:, :, k:k + 1], in_=pr_[:, :, ii:ii + 1]), writes=[wb, ssm_b])
                    V(lambda e, k=k, ii=ii: e.tensor_copy(out=ck[:, :, k:k + 1], in_=pi_[:, :, ii:ii + 1]), writes=[wb, ssm_b])
                    V(lambda e, k=k, ii=ii: e.tensor_scalar(out=nck[:, :, k:k + 1], in0=pi_[:, :, ii:ii + 1], scalar1=-1.0,
                                                            scalar2=None, op0=ALU.mult), writes=[wb, ssm_b])
                PBr = t3("PBr", 8); PBi = t3("PBi", 8); tt8 = t3("tt8", 8)
                e7 = EIDX[0]
                sl07 = slice(e7, e7 + 8)
                V(lambda e: e.tensor_tensor(out=PBr[:], in0=pr_[:, :, sl07], in1=gr[:].to_broadcast([128, 16, 8]), op=ALU.mult))
                V(lambda e: e.tensor_tensor(out=tt8[:], in0=pi_[:, :, sl07], in1=gi[:].to_broadcast([128, 16, 8]), op=ALU.mult))
                V(lambda e: e.tensor_tensor(out=PBr[:], in0=PBr[:], in1=tt8[:], op=ALU.subtract))
                V(lambda e: e.tensor_tensor(out=PBi[:], in0=pr_[:, :, sl07], in1=gi[:].to_broadcast([128, 16, 8]), op=ALU.mult))
                V(lambda e: e.tensor_tensor(out=tt8[:], in0=pi_[:, :, sl07], in1=gr[:].to_broadcast([128, 16, 8]), op=ALU.mult))
                V(lambda e: e.tensor_tensor(out=PBi[:], in0=PBi[:], in1=tt8[:], op=ALU.add))
                BmTr = sb(st1, "BmTr", [128, 16, 8, 16], F32)
                BmTi = sb(st1, "BmTi", [128, 16, 8, 16], F32)
                t816 = sb(st1, "t816", [128, 16, 8, 16], F32)
                for i in range(8):
                    m = 7 - i
                    def bc(t_, m=m):
                        return t_[:, :, m:m + 1].to_broadcast([128, 16, 16])
                    V(lambda e, i=i, bc=bc: e.tensor_tensor(out=BmTr[:, :, i, :], in0=bre[:], in1=bc(PBr), op=ALU.mult))
                    V(lambda e, i=i, bc=bc: e.tensor_tensor(out=t816[:, :, i, :], in0=bim[:], in1=bc(PBi), op=ALU.mult))
                    V(lambda e, i=i, bc=bc: e.tensor_tensor(out=BmTi[:, :, i, :], in0=bim[:], in1=bc(PBr), op=ALU.mult))
                V(lambda e: e.tensor_tensor(out=BmTr[:], in0=BmTr[:], in1=t816[:], op=ALU.subtract))
                for i in range(8):
                    m = 7 - i
                    V(lambda e, i=i, m=m: e.tensor_tensor(out=t816[:, :, i, :], in0=bre[:],
                                                          in1=PBi[:, :, m:m + 1].to_broadcast([128, 16, 16]), op=ALU.mult))
                V(lambda e: e.tensor_tensor(out=BmTi[:], in0=BmTi[:], in1=t816[:], op=ALU.add))
                Wcr = sb(st1, "Wcr", [128, 16, 8, 16], F32)
                Wci = sb(st1, "Wci", [128, 16, 8, 16], F32)
                Cmr = sb(st1, "Cmr", [128, 16, 8, 16], F32)
                Cmi = sb(st1, "Cmi", [128, 16, 8, 16], F32)
                for (dr, di, off) in ((Wcr, Wci, -7), (Cmr, Cmi, 1)):
                    for j in range(8):
                        ii = EIDX[j + off]
                        def bc2(t_, ii=ii):
                            return t_[:, :, ii:ii + 1].to_broadcast([128, 16, 16])
                        V(lambda e, j=j, bc2=bc2, dr=dr: e.tensor_tensor(out=dr[:, :, j, :], in0=cre[:], in1=bc2(pr_), op=ALU.mult))
                        V(lambda e, j=j, bc2=bc2: e.tensor_tensor(out=t816[:, :, j, :], in0=cim[:], in1=bc2(pi_), op=ALU.mult))
                        V(lambda e, j=j, bc2=bc2, di=di: e.tensor_tensor(out=di[:, :, j, :], in0=cre[:], in1=bc2(pi_), op=ALU.mult))
                    V(lambda e, dr=dr: e.tensor_tensor(out=dr[:], in0=dr[:], in1=t816[:], op=ALU.subtract))
                    for j in range(8):
                        ii = EIDX[j + off]
                        V(lambda e, j=j, ii=ii: e.tensor_tensor(out=t816[:, :, j, :], in0=cim[:],
                                                                in1=pr_[:, :, ii:ii + 1].to_broadcast([128, 16, 16]), op=ALU.mult))
                    V(lambda e, di=di: e.tensor_tensor(out=di[:], in0=di[:], in1=t816[:], op=ALU.add))
                    V(lambda e, di=di: e.tensor_scalar(out=di[:], in0=di[:], scalar1=-1.0, scalar2=None, op0=ALU.mult))
                chk('ssm_c')
                for pr in range(16):
                    for gp in range(2):
                        g = 2 * pr + gp
                        for ri, src in ((0, Cmr), (1, Cmi)):
                            V(lambda e, g=g, ri=ri, src=src, pr=pr, gp=gp: e.tensor_scalar(
                                out=Cmz[:, g, ri, :], in0=src[:, pr, :, :].rearrange("p a b -> p (a b)"),
                                scalar1=zmask[:, gp:gp + 1], scalar2=None, op0=ALU.mult), writes=[wb, ssm_b])
                chk('ssm_d')
                Bz = [sb(st1, "Bz%d" % i, [128, 2, 128], BF16) for i in range(2)]
                Wz = [sb(st1, "Wz%d" % i, [128, 2, 128], BF16) for i in range(2)]
                Bzb = [Buf(), Buf()]
                At = [sb(st1, "At%d" % i, [128, 128], F32) for i in range(2)]
                Atb = [Buf(), Buf()]
                for pr in range(16):
                    for gp in range(2):
                        g = 2 * pr + gp
                        b = g % 2
                        for ri, src in ((0, BmTr), (1, BmTi)):
                            V(lambda e, b=b, ri=ri, src=src, pr=pr, gp=gp: e.tensor_scalar(
                                out=Bz[b][:, ri, :], in0=src[:, pr, :, :].rearrange("p a b -> p (a b)"),
                                scalar1=zmask[:, gp:gp + 1], scalar2=None, op0=ALU.mult), writes=[wb, Bzb[b]])
                        for ri, src in ((0, Wcr), (1, Wci)):
                            V(lambda e, b=b, ri=ri, src=src, pr=pr, gp=gp: e.tensor_scalar(
                                out=Wz[b][:, ri, :], in0=src[:, pr, :, :].rearrange("p a b -> p (a b)"),
                                scalar1=zmask[:, gp:gp + 1], scalar2=None, op0=ALU.mult), writes=[wb, Bzb[b]])
                        ptt, ptb_ = next_pt()
                        for ri in range(2):
                            S.op("pe", lambda e, ptt=ptt, b=b, ri=ri: e.transpose(
                                out=ptt[:, ri * 128:(ri + 1) * 128], in_=Bz[b][:, ri, :], identity=ident_bf[:]),
                                reads=[Bzb[b], cst], writes=[ptb_])
                        for ri in range(2):
                            S.op("act", lambda e, ptt=ptt, g=g, ri=ri, gp=gp: e.copy(
                                out=Bm_sb[:, g, ri * 64:(ri + 1) * 64],
                                in_=ptt[:, ri * 128 + gp * 64: ri * 128 + gp * 64 + 64]),
                                reads=[ptb_], writes=[ssm_b])
                        p, pb = next_pw()
                        for ri in range(2):
                            S.op("pe", lambda e, p=p, b=b, ri=ri: e.matmul(
                                p[:, 0:128], Bz[b][:, ri, :], Wz[b][:, ri, :], start=(ri == 0), stop=(ri == 1)),
                                reads=[Bzb[b]], writes=[pb])
                        S.op("dve", lambda e, p=p, b=b: e.tensor_tensor(out=At[b][:], in0=p[:, 0:128], in1=bmask[:], op=ALU.mult),
                             reads=[pb, ib], writes=[Atb[b]])
                        S.op("dve", lambda e, b=b, g=g: e.tensor_tensor(out=dsk[:, g, :], in0=dsk[:, g, :], in1=dmask[:], op=ALU.mult),
                             reads=[ib], writes=[ib])
                        S.op("dve", lambda e, b=b, g=g: e.tensor_tensor(out=A_sb[:, g, :], in0=At[b][:], in1=dsk[:, g, :], op=ALU.add),
                             reads=[Atb[b], ib], writes=[ssm_b])
                chk('ssm_e')
                S.dma("sp", ssmA_d, A_sb[:], reads=[ssm_b], writes=[ssmd_b])
                S.dma("sp", ssmB_d, Bm_sb[:], reads=[ssm_b], writes=[ssmd_b])
                S.dma("sp", ssmC_d, Cmz[:], reads=[ssm_b], writes=[ssmd_b])
                scr = adaln_scratch(st1)
                tl = [tabsA1, sb(st1, "tabt1", [128, 3, D], F32)]
                tlb = [tabsA_b1, Buf()]
                for half, ngd in ((0, n1g_d), (1, n2g_d)):
                    adaln(tl, tlb, list(range(NSEQ)), 3 * half, ngd, scr)
                    for q in range(NSEQ):
                        S.dma("sp", tabAll_d[NSEQ * half + q], tl[q][:].rearrange("p a b -> p (a b)"),
                              reads=[tlb[q]], writes=[tabAll_b])
                S.barrier()

            chk('ssmsetup')
            for s in range(NSEQ):
                r0 = s * SEQ
                S.dma("sp", tabsA1[:].rearrange("p a b -> p (a b)"), tabAll_d[s], reads=[tabAll_b], writes=[tabsA_b1])
                chk('adaln')
                with ExitStack() as stS:
                    qT = sb(stS, "qT", [128, 4, SEQ], BF16)
                    kTz = sb(stS, "kTz", [128, 2, 2, SEQ], BF16)
                    qiT = sb(stS, "qiT", [128, 4, SEQ], BF16)
                    kiTz = sb(stS, "kiTz", [128, 2, SEQ], BF16)
                    Vp = sb(stS, "Vp", [128, 16, 2, 65], BF16)
                    wi = sb(stS, "wi", [128, 16, 8], F32)
                    mixT_s = sb(stS, "mixT_s", [128, 4, SEQ], BF16)
                    qT_b, kT_b, qiT_b, kiT_b, Vp_b, wi_b, mixT_sb = (Buf() for _ in range(7))
                    S.op("pool", lambda e: e.memset(kTz[:].rearrange("p a b c -> p (a b c)"), 0.0), writes=[kT_b])
                    S.op("pool", lambda e: e.memset(kiTz[:].rearrange("p a c -> p (a c)"), 0.0), writes=[kiT_b])
                    S.op("pool", lambda e: e.memset(Vp[:].rearrange("p a b c -> p (a b c)"), 1.0), writes=[Vp_b])

                    with ExitStack() as st12:
                        U8 = sb(st12, "U8", [128, 2, 32, 8, 16], BF16)
                        U8_b = Buf()
                        with ExitStack() as st1:
                            extra_bank(st1, True)
                            w_in = sb(st1, "w_in", [128, 8, DIN], BF16)
                            w_in_b = Buf()
                            for kt in range(8):
                                S.dma("pool", w_in[:, kt, :], win_d[:, kt, :], writes=[w_in_b])
                            hT = sb(st1, "hT", [128, 8, SEQ], BF16)
                            hT_b = Buf()
                            st1a = ExitStack()
                            xt = [sb(st1a, "xt%d" % i, [128, D], F32) for i in range(2)]
                            xt_b = [Buf(), Buf()]
                            junk = sb(st1a, "junk", [128, D], BF16)
                            junk_b = Buf()
                            t1 = sb(st1a, "t1", [128, D], F32)
                            hb = [sb(st1a, "hb%d" % i, [128, D], BF16) for i in range(2)]
                            hb_b = [Buf(), Buf()]
                            st_ = [sb(st1a, "st%d" % i, [128, 4], F32) for i in range(2)]
                            st_b = [Buf(), Buf()]
                            t1_b = Buf()
                            for t in range(16):
                                b = t % 2
                                S.dma("sp", xt[b][:], x_d[r0 + t * 128: r0 + (t + 1) * 128, :], writes=[xt_b[b]])
                                S.op("act", lambda e, b=b: e.activation(out=junk[:], in_=xt[b][:], func=AF.Square,
                                                                        accum_out=st_[b][:, 0:1]),
                                     reads=[xt_b[b]], writes=[junk_b, st_b[b]])
                                S.op("act", lambda e, b=b: e.activation(out=st_[b][:, 1:2], in_=st_[b][:, 0:1], func=AF.Sqrt,
                                                                        bias=EPS, scale=1.0 / D),
                                     reads=[st_b[b]], writes=[st_b[b]])
                                S.op("dve", lambda e, b=b: e.reciprocal(out=st_[b][:, 2:3], in_=st_[b][:, 1:2]),
                                     reads=[st_b[b]], writes=[st_b[b]])
                                S.op("dve", lambda e, b=b: e.scalar_tensor_tensor(
                                    out=t1[:], in0=xt[b][:], scalar=st_[b][:, 2:3], in1=tabsA[s][:, 1, :],
                                    op0=ALU.mult, op1=ALU.mult), reads=[xt_b[b], st_b[b], tabsA_b[s]], writes=[t1_b])
                                S.op("dve", lambda e, b=b: e.tensor_tensor(out=hb[b][:], in0=t1[:], in1=tabsA[s][:, 0, :], op=ALU.add),
                                     reads=[t1_b, tabsA_b[s]], writes=[hb_b[b]])
                                ptt, ptb_ = next_pt()
                                for kt in range(8):
                                    S.op("pe", lambda e, ptt=ptt, b=b, kt=kt: e.transpose(
                                        out=ptt[:, kt * 128:(kt + 1) * 128], in_=hb[b][:, kt * 128:(kt + 1) * 128],
                                        identity=ident_bf[:]), reads=[hb_b[b], cst], writes=[ptb_])
                                S.op("act", lambda e, ptt=ptt, t=t: e.copy(
                                    out=hT[:, :, t * 128:(t + 1) * 128], in_=ptt[:].rearrange("p (a b) -> p a b", b=128)),
                                    reads=[ptb_], writes=[hT_b])
                            S.barrier()
                            st1a.close()
                            sq = [sb(st1, "sq%d" % i, [128, 512], BF16) for i in range(2)]
                            sq_b = [Buf(), Buf()]
                            sd = [sb(st1, "sd%d" % i, [128, 512], F32) for i in range(2)]
                            sd_b = [Buf(), Buf()]
                            groups = []
                            for j in range(4):
                                groups.append(("q", j, [(512 + j * 128, 128, 0)]))
                            groups.append(("kA", 0, [(1024, 128, 0)]))
                            groups.append(("kB", 0, [(1088, 64, 0), (1024, 64, 64)]))
                            for j in range(4):
                                groups.append(("qi", j, [(1280 + j * 128, 128, 0)]))
                            groups.append(("ki", 0, [(1792, 64, 0), (1792, 64, 64)]))
                            it = 0
                            for kind, j, parts in groups:
                                for c in range(4):
                                    cs_ = slice(c * 512, (c + 1) * 512)
                                    p, pb = next_pw()
                                    for (c0, m, po) in parts:
                                        for kt in range(8):
                                            S.op("pe", lambda e, p=p, c0=c0, m=m, po=po, kt=kt, cs_=cs_: e.matmul(
                                                p[po:po + m, :], w_in[:, kt, c0:c0 + m], hT[:, kt, cs_],
                                                start=(kt == 0), stop=(kt == 7)),
                                                reads=[w_in_b, hT_b], writes=[pb])
                                    if kind == "qi":
                                        S.op("act", lambda e, p=p, j=j, cs_=cs_: e.copy(out=qiT[:, j, cs_], in_=p[:]),
                                             reads=[pb], writes=[qiT_b])
                                    elif kind == "ki":
                                        for half in range(2):
                                            rs = slice(half * 64, half * 64 + 64)
                                            S.op("act", lambda e, p=p, half=half, rs=rs, cs_=cs_: e.copy(
                                                out=kiTz[rs, half, cs_], in_=p[rs, :]), reads=[pb], writes=[kiT_b])
                                    else:
                                        b = it % 2
                                        it += 1
                                        S.op("act", lambda e, p=p, b=b: e.activation(out=sq[b][:], in_=p[:], func=AF.Square),
                                             reads=[pb], writes=[sq_b[b]])
                                        p2, p2b = next_pw()
                                        S.op("pe", lambda e, p2=p2, b=b: e.matmul(p2[:], onesblk[:], sq[b][:], start=True, stop=True),
                                             reads=[sq_b[b], cst], writes=[p2b])
                                        S.op("act", lambda e, p2=p2, b=b: e.activation(out=sd[b][:], in_=p2[:], func=AF.Sqrt,
                                                                                      bias=EPS, scale=1.0 / 64),
                                             reads=[p2b], writes=[sd_b[b]])
                                        S.op("dve", lambda e, b=b: e.reciprocal(out=sd[b][:], in_=sd[b][:]),
                                             reads=[sd_b[b]], writes=[sd_b[b]])
                                        if kind == "q":
                                            S.op("dve", lambda e, p=p, b=b, j=j, cs_=cs_: e.scalar_tensor_tensor(
                                                out=qT[:, j, cs_], in0=p[:], scalar=G2[:, 0:1], in1=sd[b][:],
                                                op0=ALU.mult, op1=ALU.mult), reads=[pb, sd_b[b], cst], writes=[qT_b])
                                        else:
                                            kvs = (0, 1) if kind == "kA" else (1, 0)
                                            for half in range(2):
                                                rs = slice(half * 64, half * 64 + 64)
                                                kv = kvs[half]
                                                S.op("dve", lambda e, p=p, b=b, rs=rs, kv=kv, half=half, cs_=cs_: e.tensor_tensor(
                                                    out=kTz[rs, kv, half, cs_], in0=p[rs, :], in1=sd[b][rs, :], op=ALU.mult),
                                                    reads=[pb, sd_b[b]], writes=[kT_b])
                            for t in range(16):
                                ts_ = slice(t * 128, (t + 1) * 128)
                                p, pb = next_pw()
                                for kt in range(8):
                                    S.op("pe", lambda e, p=p, kt=kt, ts_=ts_: e.matmul(
                                        p[:, 0:128], hT[:, kt, ts_], w_in[:, kt, 1152:1280], start=(kt == 0), stop=(kt == 7)),
                                        reads=[w_in_b, hT_b], writes=[pb])
                                for kt in range(8):
                                    S.op("pe", lambda e, p=p, kt=kt, ts_=ts_: e.matmul(
                                        p[:, 128:136], hT[:, kt, ts_], w_in[:, kt, 1856:1864], start=(kt == 0), stop=(kt == 7)),
                                        reads=[w_in_b, hT_b], writes=[pb])
                                S.op("act", lambda e, p=p, t=t: e.copy(
                                    out=Vp[:, t, :, 0:64], in_=p[:, 0:128].rearrange("p (a b) -> p a b", b=64)),
                                    reads=[pb], writes=[Vp_b])
                                S.op("act", lambda e, p=p, t=t: e.mul(out=wi[:, t, :], in_=p[:, 128:136], mul=IDX_SCALE),
                                     reads=[pb], writes=[wi_b])
                            for sp in range(2):
                                for i in range(8):
                                    p, pb = next_pw()
                                    for kt in range(8):
                                        lhs = hT[:, kt, sp * 1024:(sp + 1) * 1024].rearrange("p (b i) -> p i b", i=8)[:, i, :]
                                        S.op("pe", lambda e, p=p, lhs=lhs, kt=kt: e.matmul(
                                            p[:], lhs, w_in[:, kt, 0:512], start=(kt == 0), stop=(kt == 7)),
                                            reads=[w_in_b, hT_b], writes=[pb])
                                    S.op("act", lambda e, p=p, sp=sp, i=i: e.copy(
                                        out=U8[:, sp, :, i, :], in_=p[:].rearrange("p (g c) -> p g c", c=16)),
                                         reads=[pb], writes=[U8_b])
                            S.barrier()

                        chk('s1')
                        with ExitStack() as st2:
                            extra_bank(st2, True)
                            w_glu = sb(st2, "w_glu", [128, 4, 512], BF16)
                            w_glu_b = Buf()
                            S.dma("pool", w_glu[:], wglu_d, writes=[w_glu_b])
                            Ytok = sb(st2, "Ytok", [128, 2, 8, 512], F32)
                            Ytok_b = Buf()
                            U8T = [sb(st2, "U8T%d" % i, [128, 2, 256], BF16) for i in range(2)]
                            U8T_b = [Buf(), Buf()]
                            XA = [sb(st2, "XA%d" % i, [128, 2, 384], F32) for i in range(2)]
                            XB = [sb(st2, "XB%d" % i, [128, 2, 384], F32) for i in range(2)]
                            XA_b, XB_b = [Buf(), Buf()], [Buf(), Buf()]
                            TM = [sb(st2, "TM%d" % i, [128, 2, 256], F32) for i in range(2)]
                            TM_b = [Buf(), Buf()]
                            Xst = [sb(st2, "Xst%d" % i, [128, 2, 258], BF16) for i in range(2)]
                            Xst_b = [Buf(), Buf()]
                            Ysb = [sb(st2, "Ysb%d" % i, [128, 256], F32) for i in range(2)]
                            Ysb_b = [Buf(), Buf()]
                            for i in range(2):
                                S.op("pool", lambda e, i=i: e.memset(XA[i][:].rearrange("p a b -> p (a b)"), 0.0), writes=[XA_b[i]])
                                S.op("pool", lambda e, i=i: e.memset(XB[i][:].rearrange("p a b -> p (a b)"), 0.0), writes=[XB_b[i]])
                                S.op("pool", lambda e, i=i: e.memset(Xst[i][:].rearrange("p a b -> p (a b)"), 0.0), writes=[Xst_b[i]])
                            PAD = 128
                            A2 = [sb(st2, "A2_%d" % i, [128, 2, 128], BF16) for i in range(2)]
                            B2 = [sb(st2, "B2_%d" % i, [128, 2, 128], BF16) for i in range(2)]
                            C2 = [sb(st2, "C2_%d" % i, [128, 2, 2, 128], BF16) for i in range(2)]
                            M2_b = [Buf(), Buf()]

                            def gen_pair(pr):
                                ub = pr % 2
                                S.dma("sp", A2[ub][:], ssmA_d[:, 2 * pr:2 * pr + 2, :], reads=[ssmd_b], writes=[M2_b[ub]])
                                S.dma("sp", B2[ub][:], ssmB_d[:, 2 * pr:2 * pr + 2, :], reads=[ssmd_b], writes=[M2_b[ub]])
                                S.dma("sp", C2[ub][:], ssmC_d[:, 2 * pr:2 * pr + 2, :, :], reads=[ssmd_b], writes=[M2_b[ub]])
                                ptt, ptb_ = next_pt()
                                for gp in range(2):
                                    g = 2 * pr + gp
                                    for sp in range(2):
                                        S.op("pe", lambda e, ptt=ptt, gp=gp, sp=sp, g=g: e.transpose(
                                            out=ptt[:, (gp * 2 + sp) * 128:(gp * 2 + sp + 1) * 128],
                                            in_=U8[:, sp, g, :, :].rearrange("p a b -> p (a b)"), identity=ident_bf[:]),
                                            reads=[U8_b, cst], writes=[ptb_])
                                S.op("act", lambda e, ptt=ptt, ub=ub: e.copy(
                                    out=U8T[ub][:].rearrange("p a b -> p (a b)"), in_=ptt[:, 0:512]),
                                    reads=[ptb_], writes=[U8T_b[ub]])
                                p, pb = next_pw()
                                for gp in range(2):
                                    g = 2 * pr + gp
                                    for ri in range(2):
                                        S.op("pe", lambda e, p=p, gp=gp, g=g, ri=ri, ub=ub: e.matmul(
                                            p[gp * 64:(gp + 1) * 64, ri * 256:(ri + 1) * 256],
                                            B2[ub][:, gp, ri * 64:(ri + 1) * 64], U8T[ub][:, gp, :], start=True, stop=True),
                                            reads=[M2_b[ub], U8T_b[ub]], writes=[pb])
                                S.op("act", lambda e, p=p: e.copy(out=XA[ub][:, :, PAD:PAD + 256],
                                                                  in_=p[:].rearrange("p (a b) -> p a b", b=256)),
                                     reads=[pb], writes=[XA_b[ub]])
                                yield
                                cur, curb, nxt, nxtb = XA[ub], XA_b[ub], XB[ub], XB_b[ub]
                                xs = Xst[ub]
                                tm, tmb = TM[ub], TM_b[ub]
                                for k in range(8):
                                    sft = 2 ** k
                                    last = (k == 7)
                                    a_ = ak[:, pr, k:k + 1]
                                    c_ = ck[:, pr, k:k + 1]
                                    nc_ = nck[:, pr, k:k + 1]
                                    sh = slice(PAD - sft, PAD - sft + 256)
                                    ce = slice(PAD, PAD + 256)
                                    outr = xs[:, 0, 1:257] if last else nxt[:, 0, ce]
                                    outi = xs[:, 1, 1:257] if last else nxt[:, 1, ce]
                                    ob = Xst_b[ub] if last else nxtb
                                    S.op("dve", lambda e, cur=cur, a_=a_, sh=sh, ce=ce: e.scalar_tensor_tensor(
                                        out=tm[:, 0, :], in0=cur[:, 0, sh], scalar=a_, in1=cur[:, 0, ce], op0=ALU.mult, op1=ALU.add),
                                        reads=[curb, ssm_b], writes=[tmb])
                                    S.op("dve", lambda e, cur=cur, a_=a_, sh=sh, ce=ce: e.scalar_tensor_tensor(
                                        out=tm[:, 1, :], in0=cur[:, 1, sh], scalar=a_, in1=cur[:, 1, ce], op0=ALU.mult, op1=ALU.add),
                                        reads=[curb, ssm_b], writes=[tmb])
                                    yield
                                    S.op("dve", lambda e, cur=cur, nc_=nc_, sh=sh, outr=outr: e.scalar_tensor_tensor(
                                        out=outr, in0=cur[:, 1, sh], scalar=nc_, in1=tm[:, 0, :], op0=ALU.mult, op1=ALU.add),
                                        reads=[curb, tmb, ssm_b], writes=[ob])
                                    S.op("dve", lambda e, cur=cur, c_=c_, sh=sh, outi=outi: e.scalar_tensor_tensor(
                                        out=outi, in0=cur[:, 0, sh], scalar=c_, in1=tm[:, 1, :], op0=ALU.mult, op1=ALU.add),
                                        reads=[curb, tmb, ssm_b], writes=[ob])
                                    yield
                                    cur, curb, nxt, nxtb = nxt, nxtb, cur, curb
                                for gp in range(2):
                                    g = 2 * pr + gp
                                    yb = g % 2
                                    p, pb = next_pw()
                                    S.op("pe", lambda e, p=p, g=g, gp=gp, ub=ub: e.matmul(
                                        p[:, 0:256], A2[ub][:, gp, :], U8T[ub][:, gp, :], start=True, stop=False),
                                        reads=[M2_b[ub], U8T_b[ub]], writes=[pb])
                                    for ri in range(2):
                                        S.op("pe", lambda e, p=p, g=g, ri=ri, xs=xs: e.matmul(
                                            p[:, 0:256], C2[ub][:, gp, ri, :], xs[:, ri, 0:256], start=False, stop=(ri == 1)),
                                            reads=[M2_b[ub], Xst_b[ub]], writes=[pb])
                                    S.op("act", lambda e, p=p, yb=yb: e.copy(out=Ysb[yb][:], in_=p[:, 0:256]),
                                         reads=[pb], writes=[Ysb_b[yb]])
                                    p2, p2b = next_pw()
                                    for sp in range(2):
                                        S.op("pe", lambda e, p2=p2, sp=sp, yb=yb: e.transpose(
                                            out=p2[:, sp * 128:(sp + 1) * 128], in_=Ysb[yb][:, sp * 128:(sp + 1) * 128],
                                            identity=ident_f[:]), reads=[Ysb_b[yb], cst], writes=[p2b])
                                    for sp in range(2):
                                        S.op("act", lambda e, p2=p2, sp=sp, g=g: e.copy(
                                            out=Ytok[:, sp, :, g * 16:(g + 1) * 16],
                                            in_=p2[:, sp * 128:(sp + 1) * 128].rearrange("p (a b) -> p a b", b=16)),
                                            reads=[p2b], writes=[Ytok_b])
                                    yield

                            act_p = []
                            nxt_p = 0
                            steps = 0
                            while nxt_p < 16 or act_p:
                                if nxt_p < 16 and len(act_p) < 2 and (nxt_p == 0 or steps >= 6):
                                    if not act_p or act_p[-1][0] % 2 != nxt_p % 2:
                                        act_p.append((nxt_p, gen_pair(nxt_p)))
                                        nxt_p += 1
                                for item in list(act_p):
                                    try:
                                        next(item[1])
                                    except StopIteration:
                                        act_p.remove(item)
                                steps += 1
                            g1 = [sb(st2, "g1_%d" % i, [128, 512], F32) for i in range(2)]
                            g2_ = [sb(st2, "g2_%d" % i, [128, 512], F32) for i in range(2)]
                            zf = [sb(st2, "zf%d" % i, [128, 512], F32) for i in range(2)]
                            zb = [sb(st2, "zb%d" % i, [128, 512], BF16) for i in range(2)]
                            zT = [sb(st2, "zT%d" % i, [128, 4, 128], BF16) for i in range(2)]
                            sg = [sb(st2, "sg%d" % i, [128, 512], F32) for i in range(2)]
                            mb_ = [sb(st2, "mb%d" % i, [128, 512], BF16) for i in range(2)]
                            sst = [sb(st2, "sst%d" % i, [128, 4], F32) for i in range(2)]
                            gb = [[Buf() for _ in range(8)] for _ in range(2)]
                            KG = 2.0 * math.sqrt(2.0 / math.pi)
                            for sp in range(2):
                                for i in range(8):
                                    b = i % 2
                                    B = gb[b]
                                    y = Ytok[:, sp, i, :]
                                    S.op("act", lambda e, b=b, y=y: e.activation(out=g1[b][:], in_=y, func=AF.Square),
                                         reads=[Ytok_b], writes=[B[0]])
                                    S.op("dve", lambda e, b=b: e.tensor_scalar(out=g1[b][:], in0=g1[b][:], scalar1=0.044715,
                                                                               scalar2=1.0, op0=ALU.mult, op1=ALU.add),
                                         reads=[B[0]], writes=[B[0]])
                                    S.op("dve", lambda e, b=b, y=y: e.tensor_tensor(out=g2_[b][:], in0=g1[b][:], in1=y, op=ALU.mult),
                                         reads=[B[0], Ytok_b], writes=[B[1]])
                                    S.op("act", lambda e, b=b: e.activation(out=g2_[b][:], in_=g2_[b][:], func=AF.Sigmoid, scale=KG),
                                         reads=[B[1]], writes=[B[1]])
                                    S.op("dve", lambda e, b=b, y=y: e.tensor_tensor(out=zf[b][:], in0=g2_[b][:], in1=y, op=ALU.mult),
                                         reads=[B[1], Ytok_b], writes=[B[2]])
                                    S.op("act", lambda e, b=b: e.copy(out=zb[b][:], in_=zf[b][:]), reads=[B[2]], writes=[B[3]])
                                    ptt, ptb_ = next_pt()
                                    for ft in range(4):
                                        S.op("pe", lambda e, ptt=ptt, b=b, ft=ft: e.transpose(
                                            out=ptt[:, ft * 128:(ft + 1) * 128], in_=zb[b][:, ft * 128:(ft + 1) * 128],
                                            identity=ident_bf[:]), reads=[B[3], cst], writes=[ptb_])
                                    S.op("act", lambda e, ptt=ptt, b=b: e.copy(out=zT[b][:].rearrange("p a b -> p (a b)"),
                                                                               in_=ptt[:, 0:512]), reads=[ptb_], writes=[B[4]])
                                    p, pb = next_pw()
                                    for ft in range(4):
                                        S.op("pe", lambda e, p=p, b=b, ft=ft: e.matmul(
                                            p[:], zT[b][:, ft, :], w_glu[:, ft, :], start=(ft == 0), stop=(ft == 3)),
                                            reads=[B[4], w_glu_b], writes=[pb])
                                    S.op("dve", lambda e, p=p, b=b: e.tensor_tensor(out=sg[b][:], in0=p[:], in1=bglu_b[:], op=ALU.add),
                                         reads=[pb, cst], writes=[B[5]])
                                    S.op("act", lambda e, b=b: e.activation(out=sg[b][:], in_=sg[b][:], func=AF.Sigmoid),
                                         reads=[B[5]], writes=[B[5]])
                                    S.op("dve", lambda e, b=b: e.tensor_tensor(out=sg[b][:], in0=sg[b][:], in1=zf[b][:], op=ALU.mult),
                                         reads=[B[5], B[2]], writes=[B[5]])
                                    S.op("act", lambda e, b=b: e.activation(out=g1[b][:], in_=sg[b][:], func=AF.Square,
                                                                            accum_out=sst[b][:, 0:1]),
                                         reads=[B[5], B[0]], writes=[B[0], B[6]])
                                    S.op("act", lambda e, b=b: e.activation(out=sst[b][:, 1:2], in_=sst[b][:, 0:1], func=AF.Sqrt,
                                                                            bias=EPS, scale=1.0 / 512), reads=[B[6]], writes=[B[6]])
                                    S.op("dve", lambda e, b=b: e.reciprocal(out=sst[b][:, 2:3], in_=sst[b][:, 1:2]),
                                         reads=[B[6]], writes=[B[6]])
                                    S.op("dve", lambda e, b=b: e.scalar_tensor_tensor(
                                        out=mb_[b][:], in0=sg[b][:], scalar=sst[b][:, 2:3], in1=gns_b[:], op0=ALU.mult, op1=ALU.mult),
                                        reads=[B[5], B[6], cst], writes=[B[7]])
                                    ptt, ptb_ = next_pt()
                                    for ft in range(4):
                                        S.op("pe", lambda e, ptt=ptt, b=b, ft=ft: e.transpose(
                                            out=ptt[:, ft * 128:(ft + 1) * 128], in_=mb_[b][:, ft * 128:(ft + 1) * 128],
                                            identity=ident_bf[:]), reads=[B[7], cst], writes=[ptb_])
                                    dst = mixT_s[:, :, sp * 1024:(sp + 1) * 1024].rearrange("p a (b i) -> p a i b", i=8)[:, :, i, :]
                                    S.op("act", lambda e, ptt=ptt, dst=dst: e.copy(
                                        out=dst, in_=ptt[:, 0:512].rearrange("p (a b) -> p a b", b=128)),
                                        reads=[ptb_], writes=[mixT_sb])
                            S.barrier()

                    chk('s2')
                    with ExitStack() as st3:
                        extra_bank(st3, False)
                        w_out = sb(st3, "w_out", [128, 8, D], BF16)
                        w_out_b = Buf()
                        S.dma("pool", w_out[:], wout_d, writes=[w_out_b])
                        score = [sb(st3, "score%d" % i, [128, SEQ], F32) for i in range(2)]
                        score_b = [Buf(), Buf()]
                        nm = [sb(st3, "nm%d" % i, [128, SEQ], BF16) for i in range(3)]
                        nm_b = [Buf() for _ in range(3)]
                        rbuf = [sb(st3, "rbuf%d" % i, [128, 8, 512], BF16) for i in range(2)]
                        rbuf_b = [Buf(), Buf()]
                        dg = [sb(st3, "dg%d" % i, [128, 8, 128], BF16) for i in range(2)]
                        dg_b = [Buf(), Buf()]
                        bs = [sb(st3, "bs%d" % i, [128, 16 + 3 * NITER], F32) for i in range(2)]
                        bs_b = [Buf(), Buf()]
                        PT = [sb(st3, "PT%d" % i, [128, 512], BF16) for i in range(3)]
                        PT_b = [Buf() for _ in range(3)]
                        yatt = sb(st3, "yatt", [128, 512], F32)
                        yatt_b = Buf()
                        rden = sb(st3, "rden", [128, 8], F32)
                        ajunk = sb(st3, "ajunk", [128, 512], BF16)
                        ast = sb(st3, "ast", [128, 4], F32)
                        mixa = sb(st3, "mixa", [128, 512], BF16)
                        mixa_b = Buf()
                        mixTa = sb(st3, "mixTa", [128, 4, 128], BF16)
                        mixTa_b = Buf()
                        xr = [sb(st3, "xr%d" % i, [128, D], F32) for i in range(2)]
                        xr_b = [Buf(), Buf()]
                        x1t = [sb(st3, "x1t%d" % i, [128, 512], F32) for i in range(2)]
                        x1t_b = [Buf(), Buf()]
                        prot = {"I": 0, "A": 0, "pt": 0}

                        def pwI():
                            i = prot["I"] % 3
                            prot["I"] += 1
                            if i == 2:
                                return xbank["t"], xbank["b"]
                            return pf[i], pfb[i]

                        def pwA():
                            i = 2 + prot["A"] % 2
                            prot["A"] += 1
                            return pf[i], pfb[i]

                        def gen_I(qt):
                            sl = qt % 2
                            nsl = qt % 3
                            nk = qt + 1
                            SK = nk * 128
                            qs = slice(qt * 128, (qt + 1) * 128)
                            sc, scb = score[sl], score_b[sl]
                            BS, BSb = bs[sl], bs_b[sl]
                            S.op("dve", lambda e: e.tensor_tensor(
                                out=dg[sl][:], in0=ident_bf[:].unsqueeze(1).to_broadcast([128, 8, 128]),
                                in1=wi[:, qt, :].unsqueeze(2).to_broadcast([128, 8, 128]), op=ALU.mult),
                                reads=[cst, wi_b], writes=[dg_b[sl]])
                            nch = (SK + 511) // 512
                            for c in range(nch):
                                cw = min(512, SK - c * 512)
                                ks = slice(c * 512, c * 512 + cw)
                                for h in range(8):
                                    j, half = h // 2, h % 2
                                    p, pb = pwI()
                                    S.op("pe", lambda e, p=p, j=j, half=half: e.matmul(
                                        p[:, 0:cw], qiT[:, j, qs], kiTz[:, half, ks], start=True, stop=True),
                                        reads=[qiT_b, kiT_b], writes=[pb])
                                    if False:
                                        S.op("act", lambda e, p=p, h=h: e.activation(
                                            out=rbuf[sl][:, h, 0:cw], in_=p[:, 0:cw], func=AF.Relu),
                                            reads=[pb], writes=[rbuf_b[sl]])
                                    else:
                                        S.op("dve", lambda e, p=p, h=h: e.tensor_scalar(
                                            out=rbuf[sl][:, h, 0:cw], in0=p[:, 0:cw], scalar1=0.0, scalar2=None, op0=ALU.max),
                                            reads=[pb], writes=[rbuf_b[sl]])
                                p, pb = pwI()
                                for h in range(8):
                                    S.op("pe", lambda e, p=p, h=h: e.matmul(
                                        p[:, 0:cw], dg[sl][:, h, :], rbuf[sl][:, h, 0:cw], start=(h == 0), stop=(h == 7)),
                                        reads=[dg_b[sl], rbuf_b[sl]], writes=[pb])
                                S.op("act", lambda e, p=p: e.copy(out=sc[:, ks], in_=p[:, 0:cw]),
                                     reads=[pb], writes=[scb])
                                S.op("dve", lambda e, c=c: e.tensor_reduce(
                                    out=BS[:, c:c + 1], in_=sc[:, ks], axis=AX.X, op=ALU.max, apply_absolute_value=True),
                                    reads=[scb], writes=[BSb])
                                yield
                            S.op("dve", lambda e: e.tensor_tensor(out=sc[:, qs], in0=sc[:, qs], in1=causal[:], op=ALU.add),
                                 reads=[scb, cst], writes=[scb])
                            if qt >= 2:
                                S.op("dve", lambda e: e.tensor_reduce(
                                    out=BS[:, 4:5], in_=BS[:, 0:nch], axis=AX.X, op=ALU.max), reads=[BSb], writes=[BSb])
                                S.op("dve", lambda e: e.tensor_scalar(
                                    out=BS[:, 8:8 + NITER + 1], in0=pow2[:, 0:NITER + 1], scalar1=BS[:, 4:5], scalar2=None,
                                    op0=ALU.mult), reads=[BSb, cst], writes=[BSb])
                                S.op("dve", lambda e: e.memset(BS[:, 5:6], 0.0), reads=[BSb], writes=[BSb])
                                thr = float(2 * TOPK - SK) - 0.5
                                S.op("dve", lambda e: e.memset(BS[:, 3:4], -thr), reads=[BSb], writes=[BSb])
                                S.op("dve", lambda e: e.tensor_scalar(
                                    out=BS[:, 9 + NITER:10 + 2 * NITER], in0=BS[:, 8:9 + NITER], scalar1=-1.0, scalar2=None,
                                    op0=ALU.mult), reads=[BSb], writes=[BSb])
                                for n in range(NITER):
                                    S.op("act", lambda e: e.activation(
                                        out=nm[nsl][:, 0:SK], in_=sc[:, 0:SK], func=AF.Sign, bias=BS[:, 5:6], scale=1.0,
                                        accum_out=BS[:, 6:7]), reads=[scb, BSb], writes=[nm_b[nsl], BSb])
                                    yield
                                    S.op("act", lambda e: e.activation(
                                        out=BS[:, 7:8], in_=BS[:, 6:7], func=AF.Sign, bias=BS[:, 3:4], scale=1.0),
                                        reads=[BSb], writes=[BSb])
                                    yield
                                    S.op("act", lambda e, n=n: e.activation(
                                        out=BS[:, 5:6], in_=BS[:, 7:8], func=AF.Identity, bias=BS[:, 5:6],
                                        scale=BS[:, 9 + NITER + 1 + n:10 + NITER + 1 + n]),
                                        reads=[BSb], writes=[BSb])
                                    yield
                                S.op("dve", lambda e: e.tensor_tensor(
                                    out=BS[:, 5:6], in0=BS[:, 5:6], in1=BS[:, 8 + NITER:9 + NITER], op=ALU.add),
                                    reads=[BSb], writes=[BSb])
                                ntau = BS[:, 5:6]
                            else:
                                ntau = taufix[:, 0:1]
                            S.op("dve", lambda e: e.tensor_scalar(
                                out=nm[nsl][:, 0:SK], in0=sc[:, 0:SK], scalar1=ntau, scalar2=0.0, op0=ALU.add, op1=ALU.is_lt),
                                reads=[scb, BSb, cst], writes=[nm_b[nsl]])
                            yield

                        def gen_A(qt):
                            nsl = qt % 3
                            nk = qt + 1
                            qs = slice(qt * 128, (qt + 1) * 128)
                            O = [pf[4], pf[5]]
                            Ob = [pfb[4], pfb[5]]
                            for kv in range(2):
                                S.op("pe", lambda e, kv=kv: e.matmul(O[kv][:, 0:260], zeros_bf[:, 0:128], zeros_bf[:, 0:260],
                                                                     start=True, stop=True), reads=[cst], writes=[Ob[kv]])
                            steps = [(kt, kv) for kt in range(nk) for kv in range(2)]

                            def emit_L(kt, kv):
                                ksl = slice(kt * 128, (kt + 1) * 128)
                                p, pb = pwA()
                                S.op("pe", lambda e, p=p: e.matmul(
                                    p[:], nm[nsl][:, ksl], negi4[:], start=True, stop=True),
                                    reads=[nm_b[nsl], cst], writes=[pb])
                                for hh in range(4):
                                    head = kv * 4 + hh
                                    j, half = head // 2, head % 2
                                    S.op("pe", lambda e, p=p, hh=hh, half=half, j=j: e.matmul(
                                        p[:, hh * 128:(hh + 1) * 128], kTz[:, kv, half, ksl], qT[:, j, qs],
                                        start=False, stop=True, skip_group_check=True),
                                        reads=[kT_b, qT_b], writes=[pb])
                                return p, pb

                            def emit_PV(kt, kv, p, pb):
                                pi3 = prot["pt"] % 3
                                prot["pt"] += 1
                                S.op("act", lambda e: e.activation(
                                    out=PT[pi3][:], in_=p[:], func=AF.Exp, bias=negM[:, 0:1], scale=1.0),
                                    reads=[pb, cst], writes=[PT_b[pi3]])
                                for hh in range(4):
                                    S.op("pe", lambda e, hh=hh: e.matmul(
                                        O[kv][:, hh * 65:(hh + 1) * 65], PT[pi3][:, hh * 128:(hh + 1) * 128], Vp[:, kt, kv, :],
                                        start=False, stop=True, skip_group_check=True),
                                        reads=[PT_b[pi3], Vp_b], writes=[Ob[kv]])

                            Lcur = emit_L(*steps[0])
                            for i, (kt, kv) in enumerate(steps):
                                Lnext = emit_L(*steps[i + 1]) if i + 1 < len(steps) else None
                                emit_PV(kt, kv, *Lcur)
                                Lcur = Lnext
                                yield
                            for kv in range(2):
                                ov = O[kv][:, 0:260].rearrange("p (h e) -> p h e", e=65)
                                S.op("dve", lambda e, kv=kv, ov=ov: e.reciprocal(out=rden[:, kv * 4:(kv + 1) * 4], in_=ov[:, :, 64]),
                                     reads=[Ob[kv]], writes=[yatt_b])
                                S.op("dve", lambda e, kv=kv, ov=ov: e.tensor_tensor(
                                    out=yatt[:, kv * 256:(kv + 1) * 256].rearrange("p (h d) -> p h d", d=64), in0=ov[:, :, 0:64],
                                    in1=rden[:, kv * 4:(kv + 1) * 4].unsqueeze(2).to_broadcast([128, 4, 64]), op=ALU.mult),
                                    reads=[Ob[kv], yatt_b], writes=[yatt_b])
                            S.op("act", lambda e: e.activation(out=ajunk[:], in_=yatt[:], func=AF.Square, accum_out=ast[:, 0:1]),
                                 reads=[yatt_b], writes=[yatt_b])
                            S.op("act", lambda e: e.activation(out=ast[:, 1:2], in_=ast[:, 0:1], func=AF.Sqrt, bias=EPS, scale=1.0 / 512),
                                 reads=[yatt_b], writes=[yatt_b])
                            S.op("dve", lambda e: e.reciprocal(out=ast[:, 2:3], in_=ast[:, 1:2]), reads=[yatt_b], writes=[yatt_b])
                            S.op("dve", lambda e: e.scalar_tensor_tensor(out=mixa[:], in0=yatt[:], scalar=ast[:, 2:3], in1=gna_b[:],
                                                                          op0=ALU.mult, op1=ALU.mult),
                                 reads=[yatt_b, cst], writes=[mixa_b])
                            ptt, ptb_ = next_pt()
                            for ft in range(4):
                                S.op("pe", lambda e, ptt=ptt, ft=ft: e.transpose(
                                    out=ptt[:, ft * 128:(ft + 1) * 128], in_=mixa[:, ft * 128:(ft + 1) * 128], identity=ident_bf[:]),
                                    reads=[mixa_b, cst], writes=[ptb_])
                            S.op("act", lambda e, ptt=ptt: e.copy(out=mixTa[:].rearrange("p a b -> p (a b)"), in_=ptt[:, 0:512]),
                                 reads=[ptb_], writes=[mixTa_b])
                            yield
                            xb = qt % 2
                            rows = slice(r0 + qt * 128, r0 + (qt + 1) * 128)
                            S.dma("sp", xr[xb][:], x_d[rows, :], writes=[xr_b[xb]])
                            for hf in range(2):
                                p, pb = pwA()
                                for k8 in range(8):
                                    lhs = mixT_s[:, k8, qs] if k8 < 4 else mixTa[:, k8 - 4, :]
                                    S.op("pe", lambda e, p=p, lhs=lhs, k8=k8, hf=hf: e.matmul(
                                        p[:], lhs, w_out[:, k8, hf * 512:(hf + 1) * 512], start=(k8 == 0), stop=(k8 == 7)),
                                        reads=[mixT_sb, mixTa_b, w_out_b], writes=[pb])
                                hs = slice(hf * 512, (hf + 1) * 512)
                                S.op("dve", lambda e, p=p, hf=hf, hs=hs: e.tensor_tensor(
                                    out=x1t[hf][:], in0=p[:], in1=tabsA[s][:, 2, hs], op=ALU.mult),
                                    reads=[pb, tabsA_b[s]], writes=[x1t_b[hf]])
                                S.op("dve", lambda e, xb=xb, hf=hf, hs=hs: e.tensor_tensor(
                                    out=xr[xb][:, hs], in0=x1t[hf][:], in1=xr[xb][:, hs], op=ALU.add),
                                    reads=[x1t_b[hf], xr_b[xb]], writes=[xr_b[xb]])
                            S.dma("sp", out_d[rows, :], xr[xb][:], reads=[xr_b[xb]])
                            yield

                        NQ = 16
                        actI = []
                        actA = None
                        nextI, nextA = 0, 0
                        doneI = set()
                        while nextA < NQ or actA is not None:
                            if actA is None and nextA in doneI:
                                actA = (nextA, gen_A(nextA))
                                nextA += 1
                            firstA = actA[0] if actA is not None else nextA
                            while (nextI < NQ and len(actI) < 2 and nextI - 3 < firstA
                                   and (nextI < 2 or (nextI - 2) in doneI)):
                                actI.append((nextI, gen_I(nextI)))
                                nextI += 1
                            for item in list(actI):
                                try:
                                    next(item[1])
                                except StopIteration:
                                    actI.remove(item)
                                    doneI.add(item[0])
                            if actA is not None:
                                try:
                                    next(actA[1])
                                except StopIteration:
                                    actA = None
                        S.barrier()

        chk('s3')
        NB = 256
        NTT = NB // 128
        with ExitStack() as stB:
            tabsB1 = sb(stB, "tabsB", [128, 3, D], F32)
            tabsB_b1 = Buf()
            extra_bank(stB, True)
            W1 = sb(stB, "W1", [128, 8, DFF], BF16)
            W2 = sb(stB, "W2", [128, 32, D], BF16)
            W_b = Buf()
            for kt in range(8):
                S.dma("pool", W1[:, kt, :], wff1_d[:, kt, :], writes=[W_b])
            for ft in range(0, 32, 4):
                S.dma("pool", W2[:, ft:ft + 4, :], wff2_d[:, ft:ft + 4, :], writes=[W_b])
            hidT = sb(stB, "hidT", [128, 32, NB], BF16)
            hid_b = [Buf() for _ in range(32)]
            rl = [sb(stB, "rl%d" % i, [128, NB], BF16) for i in range(2)]
            rl_b = [Buf(), Buf()]
            h2T = sb(stB, "h2T", [128, 8, NB], BF16)
            h2T_b = Buf()
            x1 = [sb(stB, "x1_%d" % i, [128, D], F32) for i in range(2)]
            x1_b = [Buf() for _ in range(2)]
            junkB = sb(stB, "junkB", [128, D], BF16)
            junkB_b = Buf()
            tB = sb(stB, "tB", [128, D], F32)
            tB_b = Buf()
            h2 = [sb(stB, "h2_%d" % i, [128, D], BF16) for i in range(2)]
            h2_b = [Buf(), Buf()]
            stb = [sb(stB, "stb%d" % i, [128, 4], F32) for i in range(2)]
            stb_b = [Buf(), Buf()]
            ot = [sb(stB, "ot%d" % i, [128, D], F32) for i in range(2)]
            ot_b = [Buf(), Buf()]
            oc = 0
            nblk = NSEQ * SEQ // NB
            for blk in range(nblk):
                s = (blk * NB) // SEQ
                if (blk * NB) % SEQ == 0:
                    S.dma("sp", tabsB1[:].rearrange("p a b -> p (a b)"), tabAll_d[NSEQ + s], reads=[tabAll_b], writes=[tabsB_b1])
                for tt in range(NTT):
                    rows = slice(blk * NB + tt * 128, blk * NB + (tt + 1) * 128)
                    b = tt % 2
                    S.dma("sp", x1[b][:], out_d[rows, :], writes=[x1_b[b]])
                    S.op("act", lambda e, b=b: e.activation(out=junkB[:], in_=x1[b][:], func=AF.Square,
                                                            accum_out=stb[b][:, 0:1]),
                         reads=[x1_b[b]], writes=[junkB_b, stb_b[b]])
                    S.op("act", lambda e, b=b: e.activation(out=stb[b][:, 1:2], in_=stb[b][:, 0:1], func=AF.Sqrt,
                                                            bias=EPS, scale=1.0 / D), reads=[stb_b[b]], writes=[stb_b[b]])
                    S.op("dve", lambda e, b=b: e.reciprocal(out=stb[b][:, 2:3], in_=stb[b][:, 1:2]),
                         reads=[stb_b[b]], writes=[stb_b[b]])
                    S.op("dve", lambda e, b=b: e.scalar_tensor_tensor(
                        out=tB[:], in0=x1[b][:], scalar=stb[b][:, 2:3], in1=tabsB1[:, 1, :], op0=ALU.mult, op1=ALU.mult),
                        reads=[x1_b[b], stb_b[b], tabsB_b1], writes=[tB_b])
                    S.op("dve", lambda e, b=b: e.tensor_tensor(out=h2[b][:], in0=tB[:], in1=tabsB1[:, 0, :], op=ALU.add),
                         reads=[tB_b, tabsB_b1], writes=[h2_b[b]])
                    ptt, ptb_ = next_pt()
                    for kt in range(8):
                        S.op("pe", lambda e, ptt=ptt, b=b, kt=kt: e.transpose(
                            out=ptt[:, kt * 128:(kt + 1) * 128], in_=h2[b][:, kt * 128:(kt + 1) * 128], identity=ident_bf[:]),
                            reads=[h2_b[b], cst], writes=[ptb_])
                    S.op("act", lambda e, ptt=ptt, tt=tt: e.copy(
                        out=h2T[:, :, tt * 128:(tt + 1) * 128], in_=ptt[:].rearrange("p (a b) -> p a b", b=128)),
                        reads=[ptb_], writes=[h2T_b])
                for ft in range(32):
                    p, pb = next_pw(2)
                    for kt in range(8):
                        S.op("pe", lambda e, p=p, kt=kt, ft=ft: e.matmul(
                            p[:, 0:NB], W1[:, kt, ft * 128:(ft + 1) * 128], h2T[:, kt, :], start=(kt == 0), stop=(kt == 7)),
                            reads=[W_b, h2T_b], writes=[pb])
                    b = ft % 2
                    S.op("act", lambda e, p=p, b=b: e.activation(out=rl[b][:], in_=p[:, 0:NB], func=AF.Relu),
                         reads=[pb], writes=[rl_b[b]])
                    S.op("pool", lambda e, b=b, ft=ft: e.tensor_tensor(out=hidT[:, ft, :], in0=rl[b][:], in1=rl[b][:], op=ALU.mult),
                         reads=[rl_b[b]], writes=[hid_b[ft]])
                for tt in range(NTT):
                    rows = slice(blk * NB + tt * 128, blk * NB + (tt + 1) * 128)
                    ob = oc % 2
                    oc += 1
                    S.dma("sp", ot[ob][:], out_d[rows, :], writes=[ot_b[ob]])
                    for hf in range(2):
                        bi = 2 + (2 * tt + hf) % 4
                        p, pb = pf[bi], pfb[bi]
                        for ft in range(32):
                            S.op("pe", lambda e, p=p, ft=ft, tt=tt, hf=hf: e.matmul(
                                p[:], hidT[:, ft, tt * 128:(tt + 1) * 128], W2[:, ft, hf * 512:(hf + 1) * 512],
                                start=(ft == 0), stop=(ft == 31)), reads=[hid_b[ft], W_b], writes=[pb])
                        hs = slice(hf * 512, (hf + 1) * 512)
                        S.op("dve", lambda e, p=p, hs=hs: e.tensor_tensor(
                            out=tB[:, hs], in0=p[:], in1=tabsB1[:, 2, hs], op=ALU.mult),
                            reads=[pb, tabsB_b1], writes=[tB_b])
                        S.op("dve", lambda e, ob=ob, hs=hs: e.tensor_tensor(
                            out=ot[ob][:, hs], in0=tB[:, hs], in1=ot[ob][:, hs], op=ALU.add),
                            reads=[tB_b, ot_b[ob]], writes=[ot_b[ob]])
                    S.dma("sp", out_d[rows, :], ot[ob][:], reads=[ot_b[ob]])
            S.barrier()
    except _Stop:
        pass
    return nc


_PROGRAM = None


def _prep_shared(inp):
    f = np.float32
    def kt_layout(w):
        K, N = w.shape
        return np.ascontiguousarray(w.reshape(K // 128, 128, N).transpose(1, 0, 2)).astype(f)
    def bc(v, n=128):
        return np.ascontiguousarray(np.broadcast_to(np.asarray(v, f).reshape(1, -1), (n, np.asarray(v).size)))
    def qlay(a):
        a = np.asarray(a, f)
        rest = a.shape[2:]
        return np.ascontiguousarray(a.reshape((16, 2, 64) + rest).transpose((1, 2, 0) + tuple(range(3, 3 + len(rest))))
                                    .reshape((128, 16) + rest))
    sh = {}
    sh["w_ada"] = kt_layout(inp["w_ada"][0])
    sh["b_ada_b"] = bc(inp["b_ada"][0])
    sh["w_in"] = kt_layout(inp["w_in"][0])
    sh["w_out"] = kt_layout(inp["w_out"][0])
    sh["w_glu"] = kt_layout(inp["w_glu"][0])
    sh["w_ff1"] = kt_layout(inp["w_ff1"][0])
    sh["w_ff2"] = kt_layout(inp["w_ff2"][0])
    sh["norm1_g_b"] = bc(inp["norm1_g"][0])
    sh["norm2_g_b"] = bc(inp["norm2_g"][0])
    sh["b_glu_b"] = bc(inp["b_glu"][0])
    sh["gn_ssm_b"] = bc(inp["gn_ssm"][0])
    sh["gn_attn_b"] = bc(inp["gn_attn"][0])
    sh["gq_b"] = bc(inp["q_gain"][0])
    sh["gk_b"] = bc(inp["k_gain"][0])
    sh["gq2"] = np.ascontiguousarray(np.tile(np.asarray(inp["q_gain"][0], f), 2).reshape(128, 1))
    sh["gk2"] = np.ascontiguousarray(np.tile(np.asarray(inp["k_gain"][0], f), 2).reshape(128, 1))
    sh["lamre_q"] = qlay(inp["lam_re"][0])
    sh["lamim_q"] = qlay(inp["lam_im"][0])
    sh["logdt_q"] = qlay(np.broadcast_to(np.asarray(inp["log_dt"][0], f)[:, None], (32, 64)))
    sh["bre_q"] = qlay(inp["ssm_b_re"][0])
    sh["bim_q"] = qlay(inp["ssm_b_im"][0])
    sh["cre_q"] = qlay(np.asarray(inp["ssm_c_re"][0]).transpose(0, 2, 1))
    sh["cim_q"] = qlay(np.asarray(inp["ssm_c_im"][0]).transpose(0, 2, 1))
    dsk = np.asarray(inp["d_skip"][0], f).reshape(32, 16)
    sh["dsk_b"] = np.ascontiguousarray(np.broadcast_to(np.tile(dsk, (1, 8))[None], (128, 32, 128))).astype(f)
    sh["ident"] = np.eye(128, dtype=f)
    r = np.arange(128)
    sh["causal"] = np.where(r[None, :] <= r[:, None], 0.0, -1.0e30).astype(f)
    sh["negi4"] = np.tile(-BIG * np.eye(128, dtype=f), (1, 4)).astype(f)
    ib, jb = r // 16, r // 16
    sh["bmask"] = (jb[None, :] >= ib[:, None]).astype(f)
    sh["dmask"] = (r[None, :] == r[:, None]).astype(f)
    sh["onesblk"] = ((r[None, :] // 64) == (r[:, None] // 64)).astype(f)
    sh["mtab"] = np.ascontiguousarray(np.broadcast_to(np.asarray(EXPS, f)[None, None, :], (128, 16, NE)))
    sh["pow2"] = np.ascontiguousarray(np.broadcast_to((2.0 ** -np.arange(NITER + 2)).astype(f)[None], (128, NITER + 2)))
    zm = np.zeros((128, 2), f)
    zm[:64, 0] = 1.0
    zm[64:, 1] = 1.0
    sh["zmask"] = zm
    return sh


def kernel(**inputs):
    global _PROGRAM
    inp = {k: np.asarray(v) for k, v in inputs.items()}
    if _PROGRAM is None:
        _PROGRAM = build_program()
    nc = _PROGRAM
    shared = _prep_shared(inp)
    x = np.asarray(inp["x"], np.float32)
    c = np.asarray(inp["c"], np.float32)
    in_maps = []
    for core in range(NCORES):
        m = dict(shared)
        m["x"] = np.ascontiguousarray(x[NSEQ * core: NSEQ * (core + 1)].reshape(NSEQ * SEQ, D))
        cc = c[NSEQ * core: NSEQ * (core + 1)]
        cT = cc.reshape(NSEQ, 8, 128).transpose(2, 1, 0)
        m["cT"] = np.ascontiguousarray(np.broadcast_to(cT[:, :, :, None], (128, 8, NSEQ, 128))).astype(np.float32)
        in_maps.append(m)
    res = run_bass_kernel_spmd(nc, in_maps, core_ids=list(range(NCORES)))
    outs = [np.asarray(r["out"], np.float32).reshape(NSEQ, SEQ, D) for r in res.results]
    return np.concatenate(outs, axis=0)
```

```python
import math
from contextlib import ExitStack

import numpy as np
import concourse.bass as bass
import concourse.mybir as mybir
from concourse.alu_op_type import AluOpType as ALU
from concourse.bass_utils import run_bass_kernel_spmd

F32 = mybir.dt.float32
BF16 = mybir.dt.bfloat16
I32 = mybir.dt.int32
AF = mybir.ActivationFunctionType
AX = mybir.AxisListType

NCORES = 8
D = 1024
SEQ = 2048
NSEQ = 2
DIN = 1864
DFF = 4096
EPS = 1e-6
IDX_SCALE = (64 ** -0.5) * (8 ** -0.5)
TOPK = 256
NITER = 16

DVE_BISECT = False
BIG = 30000.0
EXPS = list(range(-7, 9)) + [16, 32, 64, 128, 256, 512, 1024]
NE = len(EXPS)
EIDX = {m: i for i, m in enumerate(EXPS)}
TWO_PI = 2.0 * math.pi


class Buf:
    __slots__ = ("w", "r", "ex")

    def __init__(self, ex=False):
        self.w = None
        self.r = {}
        self.ex = ex


class Sched:
    NDS = 24

    def __init__(self, nc, st):
        self.nc = nc
        self.eng = {"pe": nc.tensor, "act": nc.scalar, "dve": nc.vector, "pool": nc.gpsimd, "sp": nc.sync}
        self.sem = {k: st.enter_context(nc.semaphore("s_" + k)) for k in self.eng}
        self.cnt = {k: 0 for k in self.eng}
        self.waited = {k: {} for k in self.eng}
        self.dsem = [st.enter_context(nc.semaphore("d%d" % i)) for i in range(self.NDS)]
        self.dcnt = [0] * self.NDS
        self.dpool = {"sp": list(range(0, 16)), "pool": list(range(16, 24))}
        self.dnext = {"sp": 0, "pool": 0}

    def _semof(self, key):
        return self.sem[key[1]] if key[0] == "e" else self.dsem[key[1]]

    def _wait(self, e, key, val):
        if key[0] == "e" and key[1] == e and e == "pe":
            return
        if self.waited[e].get(key, 0) >= val:
            return
        self.eng[e].wait_ge(self._semof(key), val)
        self.waited[e][key] = val

    def _deps(self, e, reads, writes):
        for b in reads:
            if b.w is not None:
                self._wait(e, b.w[0], b.w[1])
            if b.ex:
                for k, v in b.r.items():
                    if k != ("e", e):
                        self._wait(e, k, v)
        for b in writes:
            if b.w is not None:
                self._wait(e, b.w[0], b.w[1])
            for k, v in b.r.items():
                self._wait(e, k, v)

    def _mark(self, key, val, reads, writes):
        for b in reads:
            if b.r.get(key, 0) < val:
                b.r[key] = val
        for b in writes:
            b.w = (key, val)
            b.r = {}

    def op(self, e, fn, reads=(), writes=()):
        self._deps(e, reads, writes)
        ins = fn(self.eng[e])
        self.cnt[e] += 1
        ins.then_inc(self.sem[e], 1)
        self._mark(("e", e), self.cnt[e], reads, writes)

    def dma(self, e, out, in_, reads=(), writes=(), **kw):
        self._deps(e, reads, writes)
        pl = self.dpool[e]
        j = pl[self.dnext[e] % len(pl)]
        self.dnext[e] += 1
        if self.dcnt[j] > 0:
            self._wait(e, ("d", j), self.dcnt[j])
        ins = self.eng[e].dma_start(out=out, in_=in_, **kw)
        self.dcnt[j] += 16
        ins.then_inc(self.dsem[j], 16)
        self._mark(("d", j), self.dcnt[j], reads, writes)

    def barrier(self):
        for e in self.eng:
            for k in self.eng:
                if k != e and self.cnt[k] > 0:
                    self._wait(e, ("e", k), self.cnt[k])
            for j in range(self.NDS):
                if self.dcnt[j] > 0:
                    self._wait(e, ("d", j), self.dcnt[j])


class _Stop(Exception):
    pass


def build_program(stop=None):
    nc = bass.Bass("TRN2", target_bir_lowering=False)

    def din(name, shape, dt=F32):
        return nc.dram_tensor(name, list(shape), dt, kind="ExternalInput").ap()

    x_d = din("x", [NSEQ * SEQ, D])
    cT_d = din("cT", [128, 8, NSEQ, 128])
    wada_d = din("w_ada", [128, 8, 6 * D])
    bada_d = din("b_ada_b", [128, 6 * D])
    win_d = din("w_in", [128, 8, DIN])
    wout_d = din("w_out", [128, 8, D])
    wglu_d = din("w_glu", [128, 4, 512])
    wff1_d = din("w_ff1", [128, 8, DFF])
    wff2_d = din("w_ff2", [128, 32, D])
    n1g_d = din("norm1_g_b", [128, D])
    n2g_d = din("norm2_g_b", [128, D])
    bglu_d = din("b_glu_b", [128, 512])
    gns_d = din("gn_ssm_b", [128, 512])
    gna_d = din("gn_attn_b", [128, 512])
    gqb_d = din("gq_b", [128, 64])
    gkb_d = din("gk_b", [128, 64])
    gq2_d = din("gq2", [128, 1])
    gk2_d = din("gk2", [128, 1])
    lre_d = din("lamre_q", [128, 16])
    lim_d = din("lamim_q", [128, 16])
    ldt_d = din("logdt_q", [128, 16])
    bre_d = din("bre_q", [128, 16, 16])
    bim_d = din("bim_q", [128, 16, 16])
    cre_d = din("cre_q", [128, 16, 16])
    cim_d = din("cim_q", [128, 16, 16])
    dsk_d = din("dsk_b", [128, 32, 128])
    ident_d = din("ident", [128, 128])
    causal_d = din("causal", [128, 128])
    negi4_d = din("negi4", [128, 512])
    bmask_d = din("bmask", [128, 128])
    dmask_d = din("dmask", [128, 128])
    onesblk_d = din("onesblk", [128, 128])
    mtab_d = din("mtab", [128, 16, NE])
    pow2_d = din("pow2", [128, NITER + 2])
    zmask_d = din("zmask", [128, 2])
    out_d = nc.dram_tensor("out", [NSEQ * SEQ, D], F32, kind="ExternalOutput").ap()

    try:
      with ExitStack() as top:
        S = Sched(nc, top)

        def chk(name):
            if stop == name:
                S.barrier()
                raise _Stop()

        uid = [0]

        def sb(st, name, shape, dt):
            uid[0] += 1
            return st.enter_context(nc.sbuf_tensor("sb%d_%s" % (uid[0], name), list(shape), dt))

        def ps(st, name, shape, dt):
            uid[0] += 1
            return st.enter_context(nc.psum_tensor("ps%d_%s" % (uid[0], name), list(shape), dt))

        pt = [ps(top, "pt0", [128, 1024], BF16)]
        ptb = [Buf(ex=True)]
        xbank = {}

        def extra_bank(st, as_bf16):
            if as_bf16:
                t = ps(st, "ptx", [128, 1024], BF16)
                del pt[1:], ptb[1:]
                pt.append(t)
                ptb.append(Buf(ex=True))
                rot["pt"] = 0
            else:
                del pt[1:], ptb[1:]
                rot["pt"] = 0
                xbank["t"] = ps(st, "pfx", [128, 512], F32)
                xbank["b"] = Buf(ex=True)
        pf = [ps(top, "pf%d" % i, [128, 512], F32) for i in range(6)]
        pfb = [Buf(ex=True) for _ in range(6)]
        rot = {"pt": 0, "pw": 0}

        def next_pt():
            i = rot["pt"] % len(pt)
            rot["pt"] = i + 1
            return pt[i], ptb[i]

        def next_pw(n=4):
            i = rot["pw"] % n
            rot["pw"] += 1
            return pf[i], pfb[i]

        ident_bf = sb(top, "ident_bf", [128, 128], BF16)
        ident_f = sb(top, "ident_f", [128, 128], F32)
        causal = sb(top, "causal", [128, 128], F32)
        negi4 = sb(top, "negi4", [128, 512], BF16)
        onesblk = sb(top, "onesblk", [128, 128], BF16)
        zeros_bf = sb(top, "zeros_bf", [128, 260], BF16)
        bglu_b = sb(top, "bglu_b", [128, 512], F32)
        gns_b = sb(top, "gns_b", [128, 512], F32)
        gna_b = sb(top, "gna_b", [128, 512], F32)
        pow2 = sb(top, "pow2", [128, NITER + 2], F32)
        G2 = sb(top, "G2", [128, 1], F32)
        negM = sb(top, "negM", [128, 1], F32)
        taufix = sb(top, "taufix", [128, 1], F32)
        siluT = sb(top, "siluT", [128, 8, NSEQ, 128], BF16)
        cst = Buf()
        for t_, d_ in ((ident_bf, ident_d), (ident_f, ident_d), (causal, causal_d), (negi4, negi4_d),
                       (onesblk, onesblk_d), (bglu_b, bglu_d), (gns_b, gns_d), (gna_b, gna_d), (pow2, pow2_d)):
            S.dma("pool", t_[:], d_, writes=[cst])
        S.op("dve", lambda e: e.memset(zeros_bf[:], 0.0), writes=[cst])
        S.op("dve", lambda e: e.memset(taufix[:], 1.0e29), writes=[cst])

        with ExitStack() as st0:
            gqb = sb(st0, "gqb", [128, 64], F32)
            gkb = sb(st0, "gkb", [128, 64], F32)
            gq2 = sb(st0, "gq2", [128, 1], F32)
            gk2 = sb(st0, "gk2", [128, 1], F32)
            cTf = sb(st0, "cTf", [128, 8 * NSEQ * 128], F32)
            tb = Buf()
            S.dma("sp", gqb[:], gqb_d, writes=[tb])
            S.dma("sp", gkb[:], gkb_d, writes=[tb])
            S.dma("sp", gq2[:], gq2_d, writes=[tb])
            S.dma("sp", gk2[:], gk2_d, writes=[tb])
            S.dma("sp", cTf[:], cT_d.rearrange("p a b c -> p (a b c)"), writes=[tb])
            S.op("dve", lambda e: e.scalar_tensor_tensor(out=G2[:], in0=gq2[:], scalar=0.125, in1=gk2[:],
                                                          op0=ALU.mult, op1=ALU.mult), reads=[tb], writes=[cst])
            S.op("dve", lambda e: e.scalar_tensor_tensor(out=gqb[:], in0=gqb[:], scalar=0.125, in1=gkb[:],
                                                          op0=ALU.mult, op1=ALU.mult), reads=[tb], writes=[tb])
            S.op("dve", lambda e: e.tensor_reduce(out=negM[:], in_=gqb[:], axis=AX.X, op=ALU.max,
                                                  apply_absolute_value=True), reads=[tb], writes=[cst])
            S.op("dve", lambda e: e.tensor_scalar(out=negM[:], in0=negM[:], scalar1=-64.0, scalar2=None,
                                                  op0=ALU.mult), reads=[cst], writes=[cst])
            S.op("act", lambda e: e.activation(out=siluT[:].rearrange("p a b c -> p (a b c)"), in_=cTf[:],
                                               func=AF.Silu), reads=[tb], writes=[cst])
            S.barrier()

        chk('consts')
        def adaln_scratch(st):
            wst = [sb(st, "wst%d" % i, [128, 8, 512], BF16) for i in range(2)]
            wstb = [Buf(), Buf()]
            bst = [sb(st, "bst%d" % i, [128, 512], F32) for i in range(2)]
            gst = [sb(st, "gst%d" % i, [128, 512], F32) for i in range(2)]
            tmp = [sb(st, "adat%d" % i, [128, 512], F32) for i in range(2)]
            tmpb = [Buf(), Buf()]
            return wst, wstb, bst, gst, tmp, tmpb

        tabAll_d = nc.dram_tensor("tabAll_scr", [2 * NSEQ, 128, 3 * D], F32, kind="Internal").ap()
        tabAll_b = Buf()

        def adaln(tabs, tabs_b, seqs, first_j, ng_d, scr):
            wst, wstb, bst, gst, tmp, tmpb = scr
            it = 0
            for jj in range(3):
                for hc in range(2):
                    c0 = (first_j + jj) * D + hc * 512
                    b = it % 2
                    it += 1
                    S.dma("pool", wst[b][:], wada_d[:, :, c0:c0 + 512], writes=[wstb[b]])
                    S.dma("sp", bst[b][:], bada_d[:, c0:c0 + 512], writes=[wstb[b]])
                    if jj == 1:
                        S.dma("sp", gst[b][:], ng_d[:, hc * 512:(hc + 1) * 512], writes=[wstb[b]])
                    for n, s in enumerate(seqs):
                        p, pb = next_pw()
                        for kt in range(8):
                            S.op("pe", lambda e, p=p, b=b, kt=kt, s=s: e.matmul(
                                p[:], siluT[:, kt, s, :], wst[b][:, kt, :], start=(kt == 0), stop=(kt == 7)),
                                reads=[cst, wstb[b]], writes=[pb])
                        dst = tabs[n][:, jj, hc * 512:(hc + 1) * 512]
                        if jj == 1:
                            tb_ = tmpb[n]
                            S.op("dve", lambda e, p=p, b=b, n=n: e.tensor_tensor(
                                out=tmp[n][:], in0=p[:], in1=bst[b][:], op=ALU.add),
                                reads=[pb, wstb[b]], writes=[tb_])
                            S.op("dve", lambda e, b=b, n=n, dst=dst: e.scalar_tensor_tensor(
                                out=dst, in0=tmp[n][:], scalar=1.0, in1=gst[b][:], op0=ALU.add, op1=ALU.mult),
                                reads=[tb_, wstb[b]], writes=[tabs_b[n]])
                        else:
                            S.op("dve", lambda e, p=p, b=b, dst=dst: e.tensor_tensor(
                                out=dst, in0=p[:], in1=bst[b][:], op=ALU.add),
                                reads=[pb, wstb[b]], writes=[tabs_b[n]])

        with ExitStack() as stA:
            tabsA1 = sb(stA, "tabsA", [128, 3, D], F32)
            tabsA = [tabsA1, tabsA1]
            tabsA_b1 = Buf()
            tabsA_b = [tabsA_b1, tabsA_b1]

            ssmA_d = nc.dram_tensor("ssmA_scr", [128, 32, 128], BF16, kind="Internal").ap()
            ssmB_d = nc.dram_tensor("ssmB_scr", [128, 32, 128], BF16, kind="Internal").ap()
            ssmC_d = nc.dram_tensor("ssmC_scr", [128, 32, 2, 128], BF16, kind="Internal").ap()
            ssmd_b = Buf()
            ak = sb(stA, "ak", [128, 16, 8], F32)
            ck = sb(stA, "ck", [128, 16, 8], F32)
            nck = sb(stA, "nck", [128, 16, 8], F32)
            ssm_b = Buf()
            with ExitStack() as st1:
                extra_bank(st1, True)
                A_sb = sb(st1, "A_sb", [128, 32, 128], BF16)
                Bm_sb = sb(st1, "Bm_sb", [128, 32, 128], BF16)
                Cmz = sb(st1, "Cmz", [128, 32, 2, 128], BF16)

                def t3(name, n):
                    return sb(st1, name, [128, 16, n], F32)
                lre = t3("lre", 1); lim = t3("lim", 1); ldt = t3("ldt", 1)
                bre = t3("bre", 16); bim = t3("bim", 16); cre = t3("cre", 16); cim = t3("cim", 16)
                mtab = t3("mtab", NE)
                bmask = sb(st1, "bmask", [128, 128], F32)
                dmask = sb(st1, "dmask", [128, 128], F32)
                zmask = sb(st1, "zmask", [128, 2], F32)
                dsk = sb(st1, "dsk", [128, 32, 128], F32)
                ib = Buf()
                for t_, d_ in ((lre, lre_d), (lim, lim_d), (ldt, ldt_d)):
                    S.dma("sp", t_[:, :, 0], d_, writes=[ib])
                for t_, d_ in ((bre, bre_d), (bim, bim_d), (cre, cre_d), (cim, cim_d), (mtab, mtab_d),
                               (bmask, bmask_d), (dmask, dmask_d), (dsk, dsk_d), (zmask, zmask_d)):
                    S.dma("sp", t_[:], d_, writes=[ib])
                chk('ssm_a')
                dt_ = t3("dt_", 1); aa = t3("aa", 1); th = t3("th", 1)
                ang = t3("ang", NE); lmag = t3("lmag", NE); mag = t3("mag", NE)
                tq = t3("tq", NE); tqi = sb(st1, "tqi", [128, 16, NE], I32); tqf = t3("tqf", NE)
                wr = t3("wr", NE); sn = t3("sn", NE); cs = t3("cs", NE)
                pr_ = t3("pr_", NE); pi_ = t3("pi_", NE)
                wb = Buf()

                def V(fn, reads=(), writes=(wb,)):
                    S.op("dve", fn, reads=[ib, wb] + list(reads), writes=list(writes))

                def ACT(fn, reads=(), writes=(wb,)):
                    S.op("act", fn, reads=[ib, wb] + list(reads), writes=list(writes))

                ACT(lambda e: e.activation(out=dt_[:], in_=ldt[:], func=AF.Exp))
                V(lambda e: e.tensor_tensor(out=aa[:], in0=lre[:], in1=dt_[:], op=ALU.mult))
                V(lambda e: e.tensor_tensor(out=th[:], in0=lim[:], in1=dt_[:], op=ALU.mult))
                V(lambda e: e.tensor_tensor(out=lmag[:], in0=mtab[:], in1=aa[:].to_broadcast([128, 16, NE]), op=ALU.mult))
                V(lambda e: e.tensor_tensor(out=ang[:], in0=mtab[:], in1=th[:].to_broadcast([128, 16, NE]), op=ALU.mult))
                ACT(lambda e: e.activation(out=mag[:], in_=lmag[:], func=AF.Exp))
                V(lambda e: e.tensor_scalar(out=tq[:], in0=ang[:], scalar1=1.0 / TWO_PI, scalar2=None, op0=ALU.mult))
                V(lambda e: e.tensor_copy(out=tqi[:], in_=tq[:]))
                V(lambda e: e.tensor_copy(out=tqf[:], in_=tqi[:]))
                V(lambda e: e.scalar_tensor_tensor(out=wr[:], in0=tqf[:], scalar=-TWO_PI, in1=ang[:],
                                                   op0=ALU.mult, op1=ALU.add))
                wt_ = t3("wt_", NE)
                for t_, shift in ((sn, 0.0), (cs, math.pi / 2)):
                    V(lambda e, t_=t_, shift=shift: e.tensor_scalar(out=t_[:], in0=wr[:], scalar1=shift, scalar2=None, op0=ALU.add))
                    V(lambda e, t_=t_: e.tensor_scalar(out=wt_[:], in0=t_[:], scalar1=math.pi, scalar2=-TWO_PI,
                                                       op0=ALU.is_gt, op1=ALU.mult))
                    V(lambda e, t_=t_: e.tensor_scalar(out=tq[:], in0=t_[:], scalar1=-math.pi, scalar2=TWO_PI,
                                                       op0=ALU.is_lt, op1=ALU.mult))
                    V(lambda e, t_=t_: e.tensor_tensor(out=t_[:], in0=t_[:], in1=wt_[:], op=ALU.add))
                    V(lambda e, t_=t_: e.tensor_tensor(out=t_[:], in0=t_[:], in1=tq[:], op=ALU.add))
                for t_ in (sn, cs):
                    V(lambda e, t_=t_: e.tensor_scalar(out=t_[:], in0=t_[:], scalar1=3.14159, scalar2=-3.14159,
                                                       op0=ALU.min, op1=ALU.max))
                ACT(lambda e: e.activation(out=sn[:], in_=sn[:], func=AF.Sin))
                ACT(lambda e: e.activation(out=cs[:], in_=cs[:], func=AF.Sin))
                V(lambda e: e.tensor_tensor(out=pr_[:], in0=mag[:], in1=cs[:], op=ALU.mult))
                V(lambda e: e.tensor_tensor(out=pi_[:], in0=mag[:], in1=sn[:], op=ALU.mult))
                chk('ssm_b')
                i1 = EIDX[1]
                nr = t3("nr", 1); den = t3("den", 1); t1_ = t3("t1_", 1); gr = t3("gr", 1); gi = t3("gi", 1)
                V(lambda e: e.tensor_scalar(out=nr[:], in0=pr_[:, :, i1:i1 + 1], scalar1=-1.0, scalar2=None, op0=ALU.add))
                V(lambda e: e.tensor_tensor(out=den[:], in0=lre[:], in1=lre[:], op=ALU.mult))
                V(lambda e: e.tensor_tensor(out=t1_[:], in0=lim[:], in1=lim[:], op=ALU.mult))
                V(lambda e: e.tensor_tensor(out=den[:], in0=den[:], in1=t1_[:], op=ALU.add))
                V(lambda e: e.reciprocal(out=den[:], in_=den[:]))
                V(lambda e: e.tensor_tensor(out=gr[:], in0=nr[:], in1=lre[:], op=ALU.mult))
                V(lambda e: e.tensor_tensor(out=t1_[:], in0=pi_[:, :, i1:i1 + 1], in1=lim[:], op=ALU.mult))
                V(lambda e: e.tensor_tensor(out=gr[:], in0=gr[:], in1=t1_[:], op=ALU.add))
                V(lambda e: e.tensor_tensor(out=gr[:], in0=gr[:], in1=den[:], op=ALU.mult))
                V(lambda e: e.tensor_tensor(out=gi[:], in0=pi_[:, :, i1:i1 + 1], in1=lre[:], op=ALU.mult))
                V(lambda e: e.tensor_tensor(out=t1_[:], in0=nr[:], in1=lim[:], op=ALU.mult))
                V(lambda e: e.tensor_tensor(out=gi[:], in0=gi[:], in1=t1_[:], op=ALU.subtract))
                V(lambda e: e.tensor_tensor(out=gi[:], in0=gi[:], in1=den[:], op=ALU.mult))
                for k in range(8):
                    ii = EIDX[8 * (2 ** k)]
                    V(lambda e, k=k, ii=ii: e.tensor_copy(out=ak[:, :, k:k + 1], in_=pr_[:, :, ii:ii + 1]), writes=[wb, ssm_b])
                    V(lambda e, k=k, ii=ii: e.tensor_copy(out=ck[:, :, k:k + 1], in_=pi_[:, :, ii:ii + 1]), writes=[wb, ssm_b])
                    V(lambda e, k=k, ii=ii: e.tensor_scalar(out=nck[:, :, k:k + 1], in0=pi_[:, :, ii:ii + 1], scalar1=-1.0,
                                                            scalar2=None, op0=ALU.mult), writes=[wb, ssm_b])
                PBr = t3("PBr", 8); PBi = t3("PBi", 8); tt8 = t3("tt8", 8)
                e7 = EIDX[0]
                sl07 = slice(e7, e7 + 8)
                V(lambda e: e.tensor_tensor(out=PBr[:], in0=pr_[:, :, sl07], in1=gr[:].to_broadcast([128, 16, 8]), op=ALU.mult))
                V(lambda e: e.tensor_tensor(out=tt8[:], in0=pi_[:, :, sl07], in1=gi[:].to_broadcast([128, 16, 8]), op=ALU.mult))
                V(lambda e: e.tensor_tensor(out=PBr[:], in0=PBr[:], in1=tt8[:], op=ALU.subtract))
                V(lambda e: e.tensor_tensor(out=PBi[:], in0=pr_[:, :, sl07], in1=gi[:].to_broadcast([128, 16, 8]), op=ALU.mult))
                V(lambda e: e.tensor_tensor(out=tt8[:], in0=pi_[:, :, sl07], in1=gr[:].to_broadcast([128, 16, 8]), op=ALU.mult))
                V(lambda e: e.tensor_tensor(out=PBi[:], in0=PBi[:], in1=tt8[:], op=ALU.add))
                BmTr = sb(st1, "BmTr", [128, 16, 8, 16], F32)
                BmTi = sb(st1, "BmTi", [128, 16, 8, 16], F32)
                t816 = sb(st1, "t816", [128, 16, 8, 16], F32)
                for i in range(8):
                    m = 7 - i
                    def bc(t_, m=m):
                        return t_[:, :, m:m + 1].to_broadcast([128, 16, 16])
                    V(lambda e, i=i, bc=bc: e.tensor_tensor(out=BmTr[:, :, i, :], in0=bre[:], in1=bc(PBr), op=ALU.mult))
                    V(lambda e, i=i, bc=bc: e.tensor_tensor(out=t816[:, :, i, :], in0=bim[:], in1=bc(PBi), op=ALU.mult))
                    V(lambda e, i=i, bc=bc: e.tensor_tensor(out=BmTi[:, :, i, :], in0=bim[:], in1=bc(PBr), op=ALU.mult))
                V(lambda e: e.tensor_tensor(out=BmTr[:], in0=BmTr[:], in1=t816[:], op=ALU.subtract))
                for i in range(8):
                    m = 7 - i
                    V(lambda e, i=i, m=m: e.tensor_tensor(out=t816[:, :, i, :], in0=bre[:],
                                                          in1=PBi[:, :, m:m + 1].to_broadcast([128, 16, 16]), op=ALU.mult))
                V(lambda e: e.tensor_tensor(out=BmTi[:], in0=BmTi[:], in1=t816[:], op=ALU.add))
                Wcr = sb(st1, "Wcr", [128, 16, 8, 16], F32)
                Wci = sb(st1, "Wci", [128, 16, 8, 16], F32)
                Cmr = sb(st1, "Cmr", [128, 16, 8, 16], F32)
                Cmi = sb(st1, "Cmi", [128, 16, 8, 16], F32)
                for (dr, di, off) in ((Wcr, Wci, -7), (Cmr, Cmi, 1)):
                    for j in range(8):
                        ii = EIDX[j + off]
                        def bc2(t_, ii=ii):
                            return t_[:, :, ii:ii + 1].to_broadcast([128, 16, 16])
                        V(lambda e, j=j, bc2=bc2, dr=dr: e.tensor_tensor(out=dr[:, :, j, :], in0=cre[:], in1=bc2(pr_), op=ALU.mult))
                        V(lambda e, j=j, bc2=bc2: e.tensor_tensor(out=t816[:, :, j, :], in0=cim[:], in1=bc2(pi_), op=ALU.mult))
                        V(lambda e, j=j, bc2=bc2, di=di: e.tensor_tensor(out=di[:, :, j, :], in0=cre[:], in1=bc2(pi_), op=ALU.mult))
                    V(lambda e, dr=dr: e.tensor_tensor(out=dr[:], in0=dr[:], in1=t816[:], op=ALU.subtract))
                    for j in range(8):
                        ii = EIDX[j + off]
                        V(lambda e, j=j, ii=ii: e.tensor_tensor(out=t816[:, :, j, :], in0=cim[:],
                                                                in1=pr_[:, :, ii:ii + 1].to_broadcast([128, 16, 16]), op=ALU.mult))
                    V(lambda e, di=di: e.tensor_tensor(out=di[:], in0=di[:], in1=t816[:], op=ALU.add))
                    V(lambda e, di=di: e.tensor_scalar(out=di[:], in0=di[:], scalar1=-1.0, scalar2=None, op0=ALU.mult))
                chk('ssm_c')
                for pr in range(16):
                    for gp in range(2):
                        g = 2 * pr + gp
                        for ri, src in ((0, Cmr), (1, Cmi)):
                            V(lambda e, g=g, ri=ri, src=src, pr=pr, gp=gp: e.tensor_scalar(
                                out=Cmz[:, g, ri, :], in0=src[:, pr, :, :].rearrange("p a b -> p (a b)"),
                                scalar1=zmask[:, gp:gp + 1], scalar2=None, op0=ALU.mult), writes=[wb, ssm_b])
                chk('ssm_d')
                Bz = [sb(st1, "Bz%d" % i, [128, 2, 128], BF16) for i in range(2)]
                Wz = [sb(st1, "Wz%d" % i, [128, 2, 128], BF16) for i in range(2)]
                Bzb = [Buf(), Buf()]
                At = [sb(st1, "At%d" % i, [128, 128], F32) for i in range(2)]
                Atb = [Buf(), Buf()]
                for pr in range(16):
                    for gp in range(2):
                        g = 2 * pr + gp
                        b = g % 2
                        for ri, src in ((0, BmTr), (1, BmTi)):
                            V(lambda e, b=b, ri=ri, src=src, pr=pr, gp=gp: e.tensor_scalar(
                                out=Bz[b][:, ri, :], in0=src[:, pr, :, :].rearrange("p a b -> p (a b)"),
                                scalar1=zmask[:, gp:gp + 1], scalar2=None, op0=ALU.mult), writes=[wb, Bzb[b]])
                        for ri, src in ((0, Wcr), (1, Wci)):
                            V(lambda e, b=b, ri=ri, src=src, pr=pr, gp=gp: e.tensor_scalar(
                                out=Wz[b][:, ri, :], in0=src[:, pr, :, :].rearrange("p a b -> p (a b)"),
                                scalar1=zmask[:, gp:gp + 1], scalar2=None, op0=ALU.mult), writes=[wb, Bzb[b]])
                        ptt, ptb_ = next_pt()
                        for ri in range(2):
                            S.op("pe", lambda e, ptt=ptt, b=b, ri=ri: e.transpose(
                                out=ptt[:, ri * 128:(ri + 1) * 128], in_=Bz[b][:, ri, :], identity=ident_bf[:]),
                                reads=[Bzb[b], cst], writes=[ptb_])
                        for ri in range(2):
                            S.op("act", lambda e, ptt=ptt, g=g, ri=ri, gp=gp: e.copy(
                                out=Bm_sb[:, g, ri * 64:(ri + 1) * 64],
                                in_=ptt[:, ri * 128 + gp * 64: ri * 128 + gp * 64 + 64]),
                                reads=[ptb_], writes=[ssm_b])
                        p, pb = next_pw()
                        for ri in range(2):
                            S.op("pe", lambda e, p=p, b=b, ri=ri: e.matmul(
                                p[:, 0:128], Bz[b][:, ri, :], Wz[b][:, ri, :], start=(ri == 0), stop=(ri == 1)),
                                reads=[Bzb[b]], writes=[pb])
                        S.op("dve", lambda e, p=p, b=b: e.tensor_tensor(out=At[b][:], in0=p[:, 0:128], in1=bmask[:], op=ALU.mult),
                             reads=[pb, ib], writes=[Atb[b]])
                        S.op("dve", lambda e, b=b, g=g: e.tensor_tensor(out=dsk[:, g, :], in0=dsk[:, g, :], in1=dmask[:], op=ALU.mult),
                             reads=[ib], writes=[ib])
                        S.op("dve", lambda e, b=b, g=g: e.tensor_tensor(out=A_sb[:, g, :], in0=At[b][:], in1=dsk[:, g, :], op=ALU.add),
                             reads=[Atb[b], ib], writes=[ssm_b])
                chk('ssm_e')
                S.dma("sp", ssmA_d, A_sb[:], reads=[ssm_b], writes=[ssmd_b])
                S.dma("sp", ssmB_d, Bm_sb[:], reads=[ssm_b], writes=[ssmd_b])
                S.dma("sp", ssmC_d, Cmz[:], reads=[ssm_b], writes=[ssmd_b])
                scr = adaln_scratch(st1)
                tl = [tabsA1, sb(st1, "tabt1", [128, 3, D], F32)]
                tlb = [tabsA_b1, Buf()]
                for half, ngd in ((0, n1g_d), (1, n2g_d)):
                    adaln(tl, tlb, list(range(NSEQ)), 3 * half, ngd, scr)
                    for q in range(NSEQ):
                        S.dma("sp", tabAll_d[NSEQ * half + q], tl[q][:].rearrange("p a b -> p (a b)"),
                              reads=[tlb[q]], writes=[tabAll_b])
                S.barrier()

            chk('ssmsetup')
            for s in range(NSEQ):
                r0 = s * SEQ
                S.dma("sp", tabsA1[:].rearrange("p a b -> p (a b)"), tabAll_d[s], reads=[tabAll_b], writes=[tabsA_b1])
                chk('adaln')
                with ExitStack() as stS:
                    qT = sb(stS, "qT", [128, 4, SEQ], BF16)
                    kTz = sb(stS, "kTz", [128, 2, 2, SEQ], BF16)
                    qiT = sb(stS, "qiT", [128, 4, SEQ], BF16)
                    kiTz = sb(stS, "kiTz", [128, 2, SEQ], BF16)
                    Vp = sb(stS, "Vp", [128, 16, 2, 65], BF16)
                    wi = sb(stS, "wi", [128, 16, 8], F32)
                    mixT_s = sb(stS, "mixT_s", [128, 4, SEQ], BF16)
                    qT_b, kT_b, qiT_b, kiT_b, Vp_b, wi_b, mixT_sb = (Buf() for _ in range(7))
                    S.op("pool", lambda e: e.memset(kTz[:].rearrange("p a b c -> p (a b c)"), 0.0), writes=[kT_b])
                    S.op("pool", lambda e: e.memset(kiTz[:].rearrange("p a c -> p (a c)"), 0.0), writes=[kiT_b])
                    S.op("pool", lambda e: e.memset(Vp[:].rearrange("p a b c -> p (a b c)"), 1.0), writes=[Vp_b])

                    with ExitStack() as st12:
                        U8 = sb(st12, "U8", [128, 2, 32, 8, 16], BF16)
                        U8_b = Buf()
                        with ExitStack() as st1:
                            extra_bank(st1, True)
                            w_in = sb(st1, "w_in", [128, 8, DIN], BF16)
                            w_in_b = Buf()
                            for kt in range(8):
                                S.dma("pool", w_in[:, kt, :], win_d[:, kt, :], writes=[w_in_b])
                            hT = sb(st1, "hT", [128, 8, SEQ], BF16)
                            hT_b = Buf()
                            st1a = ExitStack()
                            xt = [sb(st1a, "xt%d" % i, [128, D], F32) for i in range(2)]
                            xt_b = [Buf(), Buf()]
                            junk = sb(st1a, "junk", [128, D], BF16)
                            junk_b = Buf()
                            t1 = sb(st1a, "t1", [128, D], F32)
                            hb = [sb(st1a, "hb%d" % i, [128, D], BF16) for i in range(2)]
                            hb_b = [Buf(), Buf()]
                            st_ = [sb(st1a, "st%d" % i, [128, 4], F32) for i in range(2)]
                            st_b = [Buf(), Buf()]
                            t1_b = Buf()
                            for t in range(16):
                                b = t % 2
                                S.dma("sp", xt[b][:], x_d[r0 + t * 128: r0 + (t + 1) * 128, :], writes=[xt_b[b]])
                                S.op("act", lambda e, b=b: e.activation(out=junk[:], in_=xt[b][:], func=AF.Square,
                                                                        accum_out=st_[b][:, 0:1]),
                                     reads=[xt_b[b]], writes=[junk_b, st_b[b]])
                                S.op("act", lambda e, b=b: e.activation(out=st_[b][:, 1:2], in_=st_[b][:, 0:1], func=AF.Sqrt,
                                                                        bias=EPS, scale=1.0 / D),
                                     reads=[st_b[b]], writes=[st_b[b]])
                                S.op("dve", lambda e, b=b: e.reciprocal(out=st_[b][:, 2:3], in_=st_[b][:, 1:2]),
                                     reads=[st_b[b]], writes=[st_b[b]])
                                S.op("dve", lambda e, b=b: e.scalar_tensor_tensor(
                                    out=t1[:], in0=xt[b][:], scalar=st_[b][:, 2:3], in1=tabsA[s][:, 1, :],
                                    op0=ALU.mult, op1=ALU.mult), reads=[xt_b[b], st_b[b], tabsA_b[s]], writes=[t1_b])
                                S.op("dve", lambda e, b=b: e.tensor_tensor(out=hb[b][:], in0=t1[:], in1=tabsA[s][:, 0, :], op=ALU.add),
                                     reads=[t1_b, tabsA_b[s]], writes=[hb_b[b]])
                                ptt, ptb_ = next_pt()
                                for kt in range(8):
                                    S.op("pe", lambda e, ptt=ptt, b=b, kt=kt: e.transpose(
                                        out=ptt[:, kt * 128:(kt + 1) * 128], in_=hb[b][:, kt * 128:(kt + 1) * 128],
                                        identity=ident_bf[:]), reads=[hb_b[b], cst], writes=[ptb_])
                                S.op("act", lambda e, ptt=ptt, t=t: e.copy(
                                    out=hT[:, :, t * 128:(t + 1) * 128], in_=ptt[:].rearrange("p (a b) -> p a b", b=128)),
                                    reads=[ptb_], writes=[hT_b])
                            S.barrier()
                            st1a.close()
                            sq = [sb(st1, "sq%d" % i, [128, 512], BF16) for i in range(2)]
                            sq_b = [Buf(), Buf()]
                            sd = [sb(st1, "sd%d" % i, [128, 512], F32) for i in range(2)]
                            sd_b = [Buf(), Buf()]
                            groups = []
                            for j in range(4):
                                groups.append(("q", j, [(512 + j * 128, 128, 0)]))
                            groups.append(("kA", 0, [(1024, 128, 0)]))
                            groups.append(("kB", 0, [(1088, 64, 0), (1024, 64, 64)]))
                            for j in range(4):
                                groups.append(("qi", j, [(1280 + j * 128, 128, 0)]))
                            groups.append(("ki", 0, [(1792, 64, 0), (1792, 64, 64)]))
                            it = 0
                            for kind, j, parts in groups:
                                for c in range(4):
                                    cs_ = slice(c * 512, (c + 1) * 512)
                                    p, pb = next_pw()
                                    for (c0, m, po) in parts:
                                        for kt in range(8):
                                            S.op("pe", lambda e, p=p, c0=c0, m=m, po=po, kt=kt, cs_=cs_: e.matmul(
                                                p[po:po + m, :], w_in[:, kt, c0:c0 + m], hT[:, kt, cs_],
                                                start=(kt == 0), stop=(kt == 7)),
                                                reads=[w_in_b, hT_b], writes=[pb])
                                    if kind == "qi":
                                        S.op("act", lambda e, p=p, j=j, cs_=cs_: e.copy(out=qiT[:, j, cs_], in_=p[:]),
                                             reads=[pb], writes=[qiT_b])
                                    elif kind == "ki":
                                        for half in range(2):
                                            rs = slice(half * 64, half * 64 + 64)
                                            S.op("act", lambda e, p=p, half=half, rs=rs, cs_=cs_: e.copy(
                                                out=kiTz[rs, half, cs_], in_=p[rs, :]), reads=[pb], writes=[kiT_b])
                                    else:
                                        b = it % 2
                                        it += 1
                                        S.op("act", lambda e, p=p, b=b: e.activation(out=sq[b][:], in_=p[:], func=AF.Square),
                                             reads=[pb], writes=[sq_b[b]])
                                        p2, p2b = next_pw()
                                        S.op("pe", lambda e, p2=p2, b=b: e.matmul(p2[:], onesblk[:], sq[b][:], start=True, stop=True),
                                             reads=[sq_b[b], cst], writes=[p2b])
                                        S.op("act", lambda e, p2=p2, b=b: e.activation(out=sd[b][:], in_=p2[:], func=AF.Sqrt,
                                                                                      bias=EPS, scale=1.0 / 64),
                                             reads=[p2b], writes=[sd_b[b]])
                                        S.op("dve", lambda e, b=b: e.reciprocal(out=sd[b][:], in_=sd[b][:]),
                                             reads=[sd_b[b]], writes=[sd_b[b]])
                                        if kind == "q":
                                            S.op("dve", lambda e, p=p, b=b, j=j, cs_=cs_: e.scalar_tensor_tensor(
                                                out=qT[:, j, cs_], in0=p[:], scalar=G2[:, 0:1], in1=sd[b][:],
                                                op0=ALU.mult, op1=ALU.mult), reads=[pb, sd_b[b], cst], writes=[qT_b])
                                        else:
                                            kvs = (0, 1) if kind == "kA" else (1, 0)
                                            for half in range(2):
                                                rs = slice(half * 64, half * 64 + 64)
                                                kv = kvs[half]
                                                S.op("dve", lambda e, p=p, b=b, rs=rs, kv=kv, half=half, cs_=cs_: e.tensor_tensor(
                                                    out=kTz[rs, kv, half, cs_], in0=p[rs, :], in1=sd[b][rs, :], op=ALU.mult),
                                                    reads=[pb, sd_b[b]], writes=[kT_b])
                            for t in range(16):
                                ts_ = slice(t * 128, (t + 1) * 128)
                                p, pb = next_pw()
                                for kt in range(8):
                                    S.op("pe", lambda e, p=p, kt=kt, ts_=ts_: e.matmul(
                                        p[:, 0:128], hT[:, kt, ts_], w_in[:, kt, 1152:1280], start=(kt == 0), stop=(kt == 7)),
                                        reads=[w_in_b, hT_b], writes=[pb])
                                for kt in range(8):
                                    S.op("pe", lambda e, p=p, kt=kt, ts_=ts_: e.matmul(
                                        p[:, 128:136], hT[:, kt, ts_], w_in[:, kt, 1856:1864], start=(kt == 0), stop=(kt == 7)),
                                        reads=[w_in_b, hT_b], writes=[pb])
                                S.op("act", lambda e, p=p, t=t: e.copy(
                                    out=Vp[:, t, :, 0:64], in_=p[:, 0:128].rearrange("p (a b) -> p a b", b=64)),
                                    reads=[pb], writes=[Vp_b])
                                S.op("act", lambda e, p=p, t=t: e.mul(out=wi[:, t, :], in_=p[:, 128:136], mul=IDX_SCALE),
                                     reads=[pb], writes=[wi_b])
                            for sp in range(2):
                                for i in range(8):
                                    p, pb = next_pw()
                                    for kt in range(8):
                                        lhs = hT[:, kt, sp * 1024:(sp + 1) * 1024].rearrange("p (b i) -> p i b", i=8)[:, i, :]
                                        S.op("pe", lambda e, p=p, lhs=lhs, kt=kt: e.matmul(
                                            p[:], lhs, w_in[:, kt, 0:512], start=(kt == 0), stop=(kt == 7)),
                                            reads=[w_in_b, hT_b], writes=[pb])
                                    S.op("act", lambda e, p=p, sp=sp, i=i: e.copy(
                                        out=U8[:, sp, :, i, :], in_=p[:].rearrange("p (g c) -> p g c", c=16)),
                                         reads=[pb], writes=[U8_b])
                            S.barrier()

                        chk('s1')
                        with ExitStack() as st2:
                            extra_bank(st2, True)
                            w_glu = sb(st2, "w_glu", [128, 4, 512], BF16)
                            w_glu_b = Buf()
                            S.dma("pool", w_glu[:], wglu_d, writes=[w_glu_b])
                            Ytok = sb(st2, "Ytok", [128, 2, 8, 512], F32)
                            Ytok_b = Buf()
                            U8T = [sb(st2, "U8T%d" % i, [128, 2, 256], BF16) for i in range(2)]
                            U8T_b = [Buf(), Buf()]
                            XA = [sb(st2, "XA%d" % i, [128, 2, 384], F32) for i in range(2)]
                            XB = [sb(st2, "XB%d" % i, [128, 2, 384], F32) for i in range(2)]
                            XA_b, XB_b = [Buf(), Buf()], [Buf(), Buf()]
                            TM = [sb(st2, "TM%d" % i, [128, 2, 256], F32) for i in range(2)]
                            TM_b = [Buf(), Buf()]
                            Xst = [sb(st2, "Xst%d" % i, [128, 2, 258], BF16) for i in range(2)]
                            Xst_b = [Buf(), Buf()]
                            Ysb = [sb(st2, "Ysb%d" % i, [128, 256], F32) for i in range(2)]
                            Ysb_b = [Buf(), Buf()]
                            for i in range(2):
                                S.op("pool", lambda e, i=i: e.memset(XA[i][:].rearrange("p a b -> p (a b)"), 0.0), writes=[XA_b[i]])
                                S.op("pool", lambda e, i=i: e.memset(XB[i][:].rearrange("p a b -> p (a b)"), 0.0), writes=[XB_b[i]])
                                S.op("pool", lambda e, i=i: e.memset(Xst[i][:].rearrange("p a b -> p (a b)"), 0.0), writes=[Xst_b[i]])
                            PAD = 128
                            A2 = [sb(st2, "A2_%d" % i, [128, 2, 128], BF16) for i in range(2)]
                            B2 = [sb(st2, "B2_%d" % i, [128, 2, 128], BF16) for i in range(2)]
                            C2 = [sb(st2, "C2_%d" % i, [128, 2, 2, 128], BF16) for i in range(2)]
                            M2_b = [Buf(), Buf()]

                            def gen_pair(pr):
                                ub = pr % 2
                                S.dma("sp", A2[ub][:], ssmA_d[:, 2 * pr:2 * pr + 2, :], reads=[ssmd_b], writes=[M2_b[ub]])
                                S.dma("sp", B2[ub][:], ssmB_d[:, 2 * pr:2 * pr + 2, :], reads=[ssmd_b], writes=[M2_b[ub]])
                                S.dma("sp", C2[ub][:], ssmC_d[:, 2 * pr:2 * pr + 2, :, :], reads=[ssmd_b], writes=[M2_b[ub]])
                                ptt, ptb_ = next_pt()
                                for gp in range(2):
                                    g = 2 * pr + gp
                                    for sp in range(2):
                                        S.op("pe", lambda e, ptt=ptt, gp=gp, sp=sp, g=g: e.transpose(
                                            out=ptt[:, (gp * 2 + sp) * 128:(gp * 2 + sp + 1) * 128],
                                            in_=U8[:, sp, g, :, :].rearrange("p a b -> p (a b)"), identity=ident_bf[:]),
                                            reads=[U8_b, cst], writes=[ptb_])
                                S.op("act", lambda e, ptt=ptt, ub=ub: e.copy(
                                    out=U8T[ub][:].rearrange("p a b -> p (a b)"), in_=ptt[:, 0:512]),
                                    reads=[ptb_], writes=[U8T_b[ub]])
                                p, pb = next_pw()
                                for gp in range(2):
                                    g = 2 * pr + gp
                                    for ri in range(2):
                                        S.op("pe", lambda e, p=p, gp=gp, g=g, ri=ri, ub=ub: e.matmul(
                                            p[gp * 64:(gp + 1) * 64, ri * 256:(ri + 1) * 256],
                                            B2[ub][:, gp, ri * 64:(ri + 1) * 64], U8T[ub][:, gp, :], start=True, stop=True),
                                            reads=[M2_b[ub], U8T_b[ub]], writes=[pb])
                                S.op("act", lambda e, p=p: e.copy(out=XA[ub][:, :, PAD:PAD + 256],
                                                                  in_=p[:].rearrange("p (a b) -> p a b", b=256)),
                                     reads=[pb], writes=[XA_b[ub]])
                                yield
                                cur, curb, nxt, nxtb = XA[ub], XA_b[ub], XB[ub], XB_b[ub]
                                xs = Xst[ub]
                                tm, tmb = TM[ub], TM_b[ub]
                                for k in range(8):
                                    sft = 2 ** k
                                    last = (k == 7)
                                    a_ = ak[:, pr, k:k + 1]
                                    c_ = ck[:, pr, k:k + 1]
                                    nc_ = nck[:, pr, k:k + 1]
                                    sh = slice(PAD - sft, PAD - sft + 256)
                                    ce = slice(PAD, PAD + 256)
                                    outr = xs[:, 0, 1:257] if last else nxt[:, 0, ce]
                                    outi = xs[:, 1, 1:257] if last else nxt[:, 1, ce]
                                    ob = Xst_b[ub] if last else nxtb
                                    S.op("dve", lambda e, cur=cur, a_=a_, sh=sh, ce=ce: e.scalar_tensor_tensor(
                                        out=tm[:, 0, :], in0=cur[:, 0, sh], scalar=a_, in1=cur[:, 0, ce], op0=ALU.mult, op1=ALU.add),
                                        reads=[curb, ssm_b], writes=[tmb])
                                    S.op("dve", lambda e, cur=cur, a_=a_, sh=sh, ce=ce: e.scalar_tensor_tensor(
                                        out=tm[:, 1, :], in0=cur[:, 1, sh], scalar=a_, in1=cur[:, 1, ce], op0=ALU.mult, op1=ALU.add),
                                        reads=[curb, ssm_b], writes=[tmb])
                                    yield
                                    S.op("dve", lambda e, cur=cur, nc_=nc_, sh=sh, outr=outr: e.scalar_tensor_tensor(
                                        out=outr, in0=cur[:, 1, sh], scalar=nc_, in1=tm[:, 0, :], op0=ALU.mult, op1=ALU.add),
                                        reads=[curb, tmb, ssm_b], writes=[ob])
                                    S.op("dve", lambda e, cur=cur, c_=c_, sh=sh, outi=outi: e.scalar_tensor_tensor(
                                        out=outi, in0=cur[:, 0, sh], scalar=c_, in1=tm[:, 1, :], op0=ALU.mult, op1=ALU.add),
                                        reads=[curb, tmb, ssm_b], writes=[ob])
                                    yield
                                    cur, curb, nxt, nxtb = nxt, nxtb, cur, curb
                                for gp in range(2):
                                    g = 2 * pr + gp
                                    yb = g % 2
                                    p, pb = next_pw()
                                    S.op("pe", lambda e, p=p, g=g, gp=gp, ub=ub: e.matmul(
                                        p[:, 0:256], A2[ub][:, gp, :], U8T[ub][:, gp, :], start=True, stop=False),
                                        reads=[M2_b[ub], U8T_b[ub]], writes=[pb])
                                    for ri in range(2):
                                        S.op("pe", lambda e, p=p, g=g, ri=ri, xs=xs: e.matmul(
                                            p[:, 0:256], C2[ub][:, gp, ri, :], xs[:, ri, 0:256], start=False, stop=(ri == 1)),
                                            reads=[M2_b[ub], Xst_b[ub]], writes=[pb])
                                    S.op("act", lambda e, p=p, yb=yb: e.copy(out=Ysb[yb][:], in_=p[:, 0:256]),
                                         reads=[pb], writes=[Ysb_b[yb]])
                                    p2, p2b = next_pw()
                                    for sp in range(2):
                                        S.op("pe", lambda e, p2=p2, sp=sp, yb=yb: e.transpose(
                                            out=p2[:, sp * 128:(sp + 1) * 128], in_=Ysb[yb][:, sp * 128:(sp + 1) * 128],
                                            identity=ident_f[:]), reads=[Ysb_b[yb], cst], writes=[p2b])
                                    for sp in range(2):
                                        S.op("act", lambda e, p2=p2, sp=sp, g=g: e.copy(
                                            out=Ytok[:, sp, :, g * 16:(g + 1) * 16],
                                            in_=p2[:, sp * 128:(sp + 1) * 128].rearrange("p (a b) -> p a b", b=16)),
                                            reads=[p2b], writes=[Ytok_b])
                                    yield

                            act_p = []
                            nxt_p = 0
                            steps = 0
                            while nxt_p < 16 or act_p:
                                if nxt_p < 16 and len(act_p) < 2 and (nxt_p == 0 or steps >= 6):
                                    if not act_p or act_p[-1][0] % 2 != nxt_p % 2:
                                        act_p.append((nxt_p, gen_pair(nxt_p)))
                                        nxt_p += 1
                                for item in list(act_p):
                                    try:
                                        next(item[1])
                                    except StopIteration:
                                        act_p.remove(item)
                                steps += 1
                            g1 = [sb(st2, "g1_%d" % i, [128, 512], F32) for i in range(2)]
                            g2_ = [sb(st2, "g2_%d" % i, [128, 512], F32) for i in range(2)]
                            zf = [sb(st2, "zf%d" % i, [128, 512], F32) for i in range(2)]
                            zb = [sb(st2, "zb%d" % i, [128, 512], BF16) for i in range(2)]
                            zT = [sb(st2, "zT%d" % i, [128, 4, 128], BF16) for i in range(2)]
                            sg = [sb(st2, "sg%d" % i, [128, 512], F32) for i in range(2)]
                            mb_ = [sb(st2, "mb%d" % i, [128, 512], BF16) for i in range(2)]
                            sst = [sb(st2, "sst%d" % i, [128, 4], F32) for i in range(2)]
                            gb = [[Buf() for _ in range(8)] for _ in range(2)]
                            KG = 2.0 * math.sqrt(2.0 / math.pi)
                            for sp in range(2):
                                for i in range(8):
                                    b = i % 2
                                    B = gb[b]
                                    y = Ytok[:, sp, i, :]
                                    S.op("act", lambda e, b=b, y=y: e.activation(out=g1[b][:], in_=y, func=AF.Square),
                                         reads=[Ytok_b], writes=[B[0]])
                                    S.op("dve", lambda e, b=b: e.tensor_scalar(out=g1[b][:], in0=g1[b][:], scalar1=0.044715,
                                                                               scalar2=1.0, op0=ALU.mult, op1=ALU.add),
                                         reads=[B[0]], writes=[B[0]])
                                    S.op("dve", lambda e, b=b, y=y: e.tensor_tensor(out=g2_[b][:], in0=g1[b][:], in1=y, op=ALU.mult),
                                         reads=[B[0], Ytok_b], writes=[B[1]])
                                    S.op("act", lambda e, b=b: e.activation(out=g2_[b][:], in_=g2_[b][:], func=AF.Sigmoid, scale=KG),
                                         reads=[B[1]], writes=[B[1]])
                                    S.op("dve", lambda e, b=b, y=y: e.tensor_tensor(out=zf[b][:], in0=g2_[b][:], in1=y, op=ALU.mult),
                                         reads=[B[1], Ytok_b], writes=[B[2]])
                                    S.op("act", lambda e, b=b: e.copy(out=zb[b][:], in_=zf[b][:]), reads=[B[2]], writes=[B[3]])
                                    ptt, ptb_ = next_pt()
                                    for ft in range(4):
                                        S.op("pe", lambda e, ptt=ptt, b=b, ft=ft: e.transpose(
                                            out=ptt[:, ft * 128:(ft + 1) * 128], in_=zb[b][:, ft * 128:(ft + 1) * 128],
                                            identity=ident_bf[:]), reads=[B[3], cst], writes=[ptb_])
                                    S.op("act", lambda e, ptt=ptt, b=b: e.copy(out=zT[b][:].rearrange("p a b -> p (a b)"),
                                                                               in_=ptt[:, 0:512]), reads=[ptb_], writes=[B[4]])
                                    p, pb = next_pw()
                                    for ft in range(4):
                                        S.op("pe", lambda e, p=p, b=b, ft=ft: e.matmul(
                                            p[:], zT[b][:, ft, :], w_glu[:, ft, :], start=(ft == 0), stop=(ft == 3)),
                                            reads=[B[4], w_glu_b], writes=[pb])
                                    S.op("dve", lambda e, p=p, b=b: e.tensor_tensor(out=sg[b][:], in0=p[:], in1=bglu_b[:], op=ALU.add),
                                         reads=[pb, cst], writes=[B[5]])
                                    S.op("act", lambda e, b=b: e.activation(out=sg[b][:], in_=sg[b][:], func=AF.Sigmoid),
                                         reads=[B[5]], writes=[B[5]])
                                    S.op("dve", lambda e, b=b: e.tensor_tensor(out=sg[b][:], in0=sg[b][:], in1=zf[b][:], op=ALU.mult),
                                         reads=[B[5], B[2]], writes=[B[5]])
                                    S.op("act", lambda e, b=b: e.activation(out=g1[b][:], in_=sg[b][:], func=AF.Square,
                                                                            accum_out=sst[b][:, 0:1]),
                                         reads=[B[5], B[0]], writes=[B[0], B[6]])
                                    S.op("act", lambda e, b=b: e.activation(out=sst[b][:, 1:2], in_=sst[b][:, 0:1], func=AF.Sqrt,
                                                                            bias=EPS, scale=1.0 / 512), reads=[B[6]], writes=[B[6]])
                                    S.op("dve", lambda e, b=b: e.reciprocal(out=sst[b][:, 2:3], in_=sst[b][:, 1:2]),
                                         reads=[B[6]], writes=[B[6]])
                                    S.op("dve", lambda e, b=b: e.scalar_tensor_tensor(
                                        out=mb_[b][:], in0=sg[b][:], scalar=sst[b][:, 2:3], in1=gns_b[:], op0=ALU.mult, op1=ALU.mult),
                                        reads=[B[5], B[6], cst], writes=[B[7]])
                                    ptt, ptb_ = next_pt()
                                    for ft in range(4):
                                        S.op("pe", lambda e, ptt=ptt, b=b, ft=ft: e.transpose(
                                            out=ptt[:, ft * 128:(ft + 1) * 128], in_=mb_[b][:, ft * 128:(ft + 1) * 128],
                                            identity=ident_bf[:]), reads=[B[7], cst], writes=[ptb_])
                                    dst = mixT_s[:, :, sp * 1024:(sp + 1) * 1024].rearrange("p a (b i) -> p a i b", i=8)[:, :, i, :]
                                    S.op("act", lambda e, ptt=ptt, dst=dst: e.copy(
                                        out=dst, in_=ptt[:, 0:512].rearrange("p (a b) -> p a b", b=128)),
                                        reads=[ptb_], writes=[mixT_sb])
                            S.barrier()

                    chk('s2')
                    with ExitStack() as st3:
                        extra_bank(st3, False)
                        w_out = sb(st3, "w_out", [128, 8, D], BF16)
                        w_out_b = Buf()
                        S.dma("pool", w_out[:], wout_d, writes=[w_out_b])
                        score = [sb(st3, "score%d" % i, [128, SEQ], F32) for i in range(2)]
                        score_b = [Buf(), Buf()]
                        nm = [sb(st3, "nm%d" % i, [128, SEQ], BF16) for i in range(3)]
                        nm_b = [Buf() for _ in range(3)]
                        rbuf = [sb(st3, "rbuf%d" % i, [128, 8, 512], BF16) for i in range(2)]
                        rbuf_b = [Buf(), Buf()]
                        dg = [sb(st3, "dg%d" % i, [128, 8, 128], BF16) for i in range(2)]
                        dg_b = [Buf(), Buf()]
                        bs = [sb(st3, "bs%d" % i, [128, 16 + 3 * NITER], F32) for i in range(2)]
                        bs_b = [Buf(), Buf()]
                        PT = [sb(st3, "PT%d" % i, [128, 512], BF16) for i in range(3)]
                        PT_b = [Buf() for _ in range(3)]
                        yatt = sb(st3, "yatt", [128, 512], F32)
                        yatt_b = Buf()
                        rden = sb(st3, "rden", [128, 8], F32)
                        ajunk = sb(st3, "ajunk", [128, 512], BF16)
                        ast = sb(st3, "ast", [128, 4], F32)
                        mixa = sb(st3, "mixa", [128, 512], BF16)
                        mixa_b = Buf()
                        mixTa = sb(st3, "mixTa", [128, 4, 128], BF16)
                        mixTa_b = Buf()
                        xr = [sb(st3, "xr%d" % i, [128, D], F32) for i in range(2)]
                        xr_b = [Buf(), Buf()]
                        x1t = [sb(st3, "x1t%d" % i, [128, 512], F32) for i in range(2)]
                        x1t_b = [Buf(), Buf()]
                        prot = {"I": 0, "A": 0, "pt": 0}

                        def pwI():
                            i = prot["I"] % 3
                            prot["I"] += 1
                            if i == 2:
                                return xbank["t"], xbank["b"]
                            return pf[i], pfb[i]

                        def pwA():
                            i = 2 + prot["A"] % 2
                            prot["A"] += 1
                            return pf[i], pfb[i]

                        def gen_I(qt):
                            sl = qt % 2
                            nsl = qt % 3
                            nk = qt + 1
                            SK = nk * 128
                            qs = slice(qt * 128, (qt + 1) * 128)
                            sc, scb = score[sl], score_b[sl]
                            BS, BSb = bs[sl], bs_b[sl]
                            S.op("dve", lambda e: e.tensor_tensor(
                                out=dg[sl][:], in0=ident_bf[:].unsqueeze(1).to_broadcast([128, 8, 128]),
                                in1=wi[:, qt, :].unsqueeze(2).to_broadcast([128, 8, 128]), op=ALU.mult),
                                reads=[cst, wi_b], writes=[dg_b[sl]])
                            nch = (SK + 511) // 512
                            for c in range(nch):
                                cw = min(512, SK - c * 512)
                                ks = slice(c * 512, c * 512 + cw)
                                for h in range(8):
                                    j, half = h // 2, h % 2
                                    p, pb = pwI()
                                    S.op("pe", lambda e, p=p, j=j, half=half: e.matmul(
                                        p[:, 0:cw], qiT[:, j, qs], kiTz[:, half, ks], start=True, stop=True),
                                        reads=[qiT_b, kiT_b], writes=[pb])
                                    if False:
                                        S.op("act", lambda e, p=p, h=h: e.activation(
                                            out=rbuf[sl][:, h, 0:cw], in_=p[:, 0:cw], func=AF.Relu),
                                            reads=[pb], writes=[rbuf_b[sl]])
                                    else:
                                        S.op("dve", lambda e, p=p, h=h: e.tensor_scalar(
                                            out=rbuf[sl][:, h, 0:cw], in0=p[:, 0:cw], scalar1=0.0, scalar2=None, op0=ALU.max),
                                            reads=[pb], writes=[rbuf_b[sl]])
                                p, pb = pwI()
                                for h in range(8):
                                    S.op("pe", lambda e, p=p, h=h: e.matmul(
                                        p[:, 0:cw], dg[sl][:, h, :], rbuf[sl][:, h, 0:cw], start=(h == 0), stop=(h == 7)),
                                        reads=[dg_b[sl], rbuf_b[sl]], writes=[pb])
                                S.op("act", lambda e, p=p: e.copy(out=sc[:, ks], in_=p[:, 0:cw]),
                                     reads=[pb], writes=[scb])
                                S.op("dve", lambda e, c=c: e.tensor_reduce(
                                    out=BS[:, c:c + 1], in_=sc[:, ks], axis=AX.X, op=ALU.max, apply_absolute_value=True),
                                    reads=[scb], writes=[BSb])
                                yield
                            S.op("dve", lambda e: e.tensor_tensor(out=sc[:, qs], in0=sc[:, qs], in1=causal[:], op=ALU.add),
                                 reads=[scb, cst], writes=[scb])
                            if DVE_BISECT and qt >= 2 and qt % 2 == 1:
                                S.op("dve", lambda e: e.tensor_reduce(
                                    out=BS[:, 4:5], in_=BS[:, 0:nch], axis=AX.X, op=ALU.max), reads=[BSb], writes=[BSb])
                                S.op("dve", lambda e: e.tensor_scalar(
                                    out=BS[:, 8:8 + NITER + 1], in0=pow2[:, 0:NITER + 1], scalar1=BS[:, 4:5], scalar2=None,
                                    op0=ALU.mult), reads=[BSb, cst], writes=[BSb])
                                S.op("dve", lambda e: e.memset(BS[:, 5:6], 0.0), reads=[BSb], writes=[BSb])
                                for n in range(NITER):
                                    S.op("dve", lambda e: e.tensor_scalar(
                                        out=nm[nsl][:, 0:SK], in0=sc[:, 0:SK], scalar1=BS[:, 5:6], scalar2=None,
                                        op0=ALU.is_ge, op1=ALU.add, accum_out=BS[:, 6:7]),
                                        reads=[scb, BSb], writes=[nm_b[nsl], BSb])
                                    yield
                                    S.op("dve", lambda e, n=n: e.tensor_scalar(
                                        out=BS[:, 7:8], in0=BS[:, 6:7], scalar1=float(TOPK) - 0.5, scalar2=BS[:, 8 + n:9 + n],
                                        op0=ALU.is_ge, op1=ALU.mult), reads=[BSb], writes=[BSb])
                                    yield
                                    S.op("dve", lambda e, n=n: e.scalar_tensor_tensor(
                                        out=BS[:, 5:6], in0=BS[:, 7:8], scalar=BS[:, 9 + n:10 + n], in1=BS[:, 5:6],
                                        op0=ALU.subtract, op1=ALU.add), reads=[BSb], writes=[BSb])
                                    yield
                                S.op("dve", lambda e: e.tensor_tensor(
                                    out=BS[:, 5:6], in0=BS[:, 5:6], in1=BS[:, 8 + NITER:9 + NITER], op=ALU.subtract),
                                    reads=[BSb], writes=[BSb])
                                S.op("dve", lambda e: e.tensor_scalar(
                                    out=nm[nsl][:, 0:SK], in0=sc[:, 0:SK], scalar1=BS[:, 5:6], scalar2=None, op0=ALU.is_lt),
                                    reads=[scb, BSb], writes=[nm_b[nsl]])
                                yield
                                return
                            if qt >= 2:
                                S.op("dve", lambda e: e.tensor_reduce(
                                    out=BS[:, 4:5], in_=BS[:, 0:nch], axis=AX.X, op=ALU.max), reads=[BSb], writes=[BSb])
                                S.op("dve", lambda e: e.tensor_scalar(
                                    out=BS[:, 8:8 + NITER + 1], in0=pow2[:, 0:NITER + 1], scalar1=BS[:, 4:5], scalar2=None,
                                    op0=ALU.mult), reads=[BSb, cst], writes=[BSb])
                                S.op("dve", lambda e: e.memset(BS[:, 5:6], 0.0), reads=[BSb], writes=[BSb])
                                thr = float(2 * TOPK - SK) - 0.5
                                S.op("dve", lambda e: e.memset(BS[:, 3:4], -thr), reads=[BSb], writes=[BSb])
                                S.op("dve", lambda e: e.tensor_scalar(
                                    out=BS[:, 9 + NITER:10 + 2 * NITER], in0=BS[:, 8:9 + NITER], scalar1=-1.0, scalar2=None,
                                    op0=ALU.mult), reads=[BSb], writes=[BSb])
                                for n in range(NITER):
                                    S.op("act", lambda e: e.activation(
                                        out=nm[nsl][:, 0:SK], in_=sc[:, 0:SK], func=AF.Sign, bias=BS[:, 5:6], scale=1.0,
                                        accum_out=BS[:, 6:7]), reads=[scb, BSb], writes=[nm_b[nsl], BSb])
                                    yield
                                    S.op("act", lambda e: e.activation(
                                        out=BS[:, 7:8], in_=BS[:, 6:7], func=AF.Sign, bias=BS[:, 3:4], scale=1.0),
                                        reads=[BSb], writes=[BSb])
                                    yield
                                    S.op("act", lambda e, n=n: e.activation(
                                        out=BS[:, 5:6], in_=BS[:, 7:8], func=AF.Identity, bias=BS[:, 5:6],
                                        scale=BS[:, 9 + NITER + 1 + n:10 + NITER + 1 + n]),
                                        reads=[BSb], writes=[BSb])
                                    yield
                                S.op("dve", lambda e: e.tensor_tensor(
                                    out=BS[:, 5:6], in0=BS[:, 5:6], in1=BS[:, 8 + NITER:9 + NITER], op=ALU.add),
                                    reads=[BSb], writes=[BSb])
                                ntau = BS[:, 5:6]
                            else:
                                ntau = taufix[:, 0:1]
                            S.op("dve", lambda e: e.tensor_scalar(
                                out=nm[nsl][:, 0:SK], in0=sc[:, 0:SK], scalar1=ntau, scalar2=0.0, op0=ALU.add, op1=ALU.is_lt),
                                reads=[scb, BSb, cst], writes=[nm_b[nsl]])
                            yield

                        def gen_A(qt):
                            nsl = qt % 3
                            nk = qt + 1
                            qs = slice(qt * 128, (qt + 1) * 128)
                            O = [pf[4], pf[5]]
                            Ob = [pfb[4], pfb[5]]
                            for kv in range(2):
                                S.op("pe", lambda e, kv=kv: e.matmul(O[kv][:, 0:260], zeros_bf[:, 0:128], zeros_bf[:, 0:260],
                                                                     start=True, stop=True), reads=[cst], writes=[Ob[kv]])
                            steps = [(kt, kv) for kt in range(nk) for kv in range(2)]

                            def emit_L(kt, kv):
                                ksl = slice(kt * 128, (kt + 1) * 128)
                                p, pb = pwA()
                                S.op("pe", lambda e, p=p: e.matmul(
                                    p[:], nm[nsl][:, ksl], negi4[:], start=True, stop=True),
                                    reads=[nm_b[nsl], cst], writes=[pb])
                                for hh in range(4):
                                    head = kv * 4 + hh
                                    j, half = head // 2, head % 2
                                    S.op("pe", lambda e, p=p, hh=hh, half=half, j=j: e.matmul(
                                        p[:, hh * 128:(hh + 1) * 128], kTz[:, kv, half, ksl], qT[:, j, qs],
                                        start=False, stop=True, skip_group_check=True),
                                        reads=[kT_b, qT_b], writes=[pb])
                                return p, pb

                            def emit_PV(kt, kv, p, pb):
                                pi3 = prot["pt"] % 3
                                prot["pt"] += 1
                                S.op("act", lambda e: e.activation(
                                    out=PT[pi3][:], in_=p[:], func=AF.Exp, bias=negM[:, 0:1], scale=1.0),
                                    reads=[pb, cst], writes=[PT_b[pi3]])
                                for hh in range(4):
                                    S.op("pe", lambda e, hh=hh: e.matmul(
                                        O[kv][:, hh * 65:(hh + 1) * 65], PT[pi3][:, hh * 128:(hh + 1) * 128], Vp[:, kt, kv, :],
                                        start=False, stop=True, skip_group_check=True),
                                        reads=[PT_b[pi3], Vp_b], writes=[Ob[kv]])

                            Lcur = emit_L(*steps[0])
                            for i, (kt, kv) in enumerate(steps):
                                Lnext = emit_L(*steps[i + 1]) if i + 1 < len(steps) else None
                                emit_PV(kt, kv, *Lcur)
                                Lcur = Lnext
                                yield
                            for kv in range(2):
                                ov = O[kv][:, 0:260].rearrange("p (h e) -> p h e", e=65)
                                S.op("dve", lambda e, kv=kv, ov=ov: e.reciprocal(out=rden[:, kv * 4:(kv + 1) * 4], in_=ov[:, :, 64]),
                                     reads=[Ob[kv]], writes=[yatt_b])
                                S.op("dve", lambda e, kv=kv, ov=ov: e.tensor_tensor(
                                    out=yatt[:, kv * 256:(kv + 1) * 256].rearrange("p (h d) -> p h d", d=64), in0=ov[:, :, 0:64],
                                    in1=rden[:, kv * 4:(kv + 1) * 4].unsqueeze(2).to_broadcast([128, 4, 64]), op=ALU.mult),
                                    reads=[Ob[kv], yatt_b], writes=[yatt_b])
                            S.op("act", lambda e: e.activation(out=ajunk[:], in_=yatt[:], func=AF.Square, accum_out=ast[:, 0:1]),
                                 reads=[yatt_b], writes=[yatt_b])
                            S.op("act", lambda e: e.activation(out=ast[:, 1:2], in_=ast[:, 0:1], func=AF.Sqrt, bias=EPS, scale=1.0 / 512),
                                 reads=[yatt_b], writes=[yatt_b])
                            S.op("dve", lambda e: e.reciprocal(out=ast[:, 2:3], in_=ast[:, 1:2]), reads=[yatt_b], writes=[yatt_b])
                            S.op("dve", lambda e: e.scalar_tensor_tensor(out=mixa[:], in0=yatt[:], scalar=ast[:, 2:3], in1=gna_b[:],
                                                                          op0=ALU.mult, op1=ALU.mult),
                                 reads=[yatt_b, cst], writes=[mixa_b])
                            ptt, ptb_ = next_pt()
                            for ft in range(4):
                                S.op("pe", lambda e, ptt=ptt, ft=ft: e.transpose(
                                    out=ptt[:, ft * 128:(ft + 1) * 128], in_=mixa[:, ft * 128:(ft + 1) * 128], identity=ident_bf[:]),
                                    reads=[mixa_b, cst], writes=[ptb_])
                            S.op("act", lambda e, ptt=ptt: e.copy(out=mixTa[:].rearrange("p a b -> p (a b)"), in_=ptt[:, 0:512]),
                                 reads=[ptb_], writes=[mixTa_b])
                            yield
                            xb = qt % 2
                            rows = slice(r0 + qt * 128, r0 + (qt + 1) * 128)
                            S.dma("sp", xr[xb][:], x_d[rows, :], writes=[xr_b[xb]])
                            for hf in range(2):
                                p, pb = pwA()
                                for k8 in range(8):
                                    lhs = mixT_s[:, k8, qs] if k8 < 4 else mixTa[:, k8 - 4, :]
                                    S.op("pe", lambda e, p=p, lhs=lhs, k8=k8, hf=hf: e.matmul(
                                        p[:], lhs, w_out[:, k8, hf * 512:(hf + 1) * 512], start=(k8 == 0), stop=(k8 == 7)),
                                        reads=[mixT_sb, mixTa_b, w_out_b], writes=[pb])
                                hs = slice(hf * 512, (hf + 1) * 512)
                                S.op("dve", lambda e, p=p, hf=hf, hs=hs: e.tensor_tensor(
                                    out=x1t[hf][:], in0=p[:], in1=tabsA[s][:, 2, hs], op=ALU.mult),
                                    reads=[pb, tabsA_b[s]], writes=[x1t_b[hf]])
                                S.op("dve", lambda e, xb=xb, hf=hf, hs=hs: e.tensor_tensor(
                                    out=xr[xb][:, hs], in0=x1t[hf][:], in1=xr[xb][:, hs], op=ALU.add),
                                    reads=[x1t_b[hf], xr_b[xb]], writes=[xr_b[xb]])
                            S.dma("sp", out_d[rows, :], xr[xb][:], reads=[xr_b[xb]])
                            yield

                        NQ = 16
                        actI = []
                        actA = None
                        nextI, nextA = 0, 0
                        doneI = set()
                        while nextA < NQ or actA is not None:
                            if actA is None and nextA in doneI:
                                actA = (nextA, gen_A(nextA))
                                nextA += 1
                            firstA = actA[0] if actA is not None else nextA
                            while (nextI < NQ and len(actI) < 2 and nextI - 3 < firstA
                                   and (nextI < 2 or (nextI - 2) in doneI)):
                                actI.append((nextI, gen_I(nextI)))
                                nextI += 1
                            for item in list(actI):
                                try:
                                    next(item[1])
                                except StopIteration:
                                    actI.remove(item)
                                    doneI.add(item[0])
                            if actA is not None:
                                try:
                                    next(actA[1])
                                except StopIteration:
                                    actA = None
                        S.barrier()

        chk('s3')
        NB = 256
        NTT = NB // 128
        with ExitStack() as stB:
            tabsB1 = sb(stB, "tabsB", [128, 3, D], F32)
            tabsB_b1 = Buf()
            extra_bank(stB, True)
            W1 = sb(stB, "W1", [128, 8, DFF], BF16)
            W2 = sb(stB, "W2", [128, 32, D], BF16)
            W_b = Buf()
            with ExitStack() as stW:
                NSTG = 4
                stg = [sb(stW, "stg%d" % i, [128, 2048], F32) for i in range(NSTG)]
                stg_b = [Buf() for _ in range(NSTG)]
                ci = 0
                chunks = []
                for kt in range(8):
                    for hc in range(2):
                        chunks.append((W1[:, kt, hc * 2048:(hc + 1) * 2048], wff1_d[:, kt, hc * 2048:(hc + 1) * 2048]))
                for ft in range(0, 32, 2):
                    chunks.append((W2[:, ft:ft + 2, :].rearrange("p a b -> p (a b)"),
                                   wff2_d[:, ft:ft + 2, :].rearrange("p a b -> p (a b)")))
                for dst, src in chunks:
                    b = ci % NSTG
                    S.dma("sp", stg[b][:], src, writes=[stg_b[b]])
                    eng = ("act", "pool", "dve")[ci % 3]
                    if eng == "act":
                        S.op("act", lambda e, dst=dst, b=b: e.copy(out=dst, in_=stg[b][:]), reads=[stg_b[b]], writes=[W_b])
                    else:
                        S.op(eng, lambda e, dst=dst, b=b: e.tensor_copy(out=dst, in_=stg[b][:]), reads=[stg_b[b]], writes=[W_b])
                    ci += 1
                S.barrier()
            hidT = sb(stB, "hidT", [128, 32, NB], BF16)
            hid_b = [Buf() for _ in range(32)]
            rl = [sb(stB, "rl%d" % i, [128, NB], BF16) for i in range(2)]
            rl_b = [Buf(), Buf()]
            h2T = sb(stB, "h2T", [128, 8, NB], BF16)
            h2T_b = Buf()
            x1 = [sb(stB, "x1_%d" % i, [128, D], F32) for i in range(2)]
            x1_b = [Buf() for _ in range(2)]
            junkB = sb(stB, "junkB", [128, D], BF16)
            junkB_b = Buf()
            tB = sb(stB, "tB", [128, D], F32)
            tB_b = Buf()
            h2 = [sb(stB, "h2_%d" % i, [128, D], BF16) for i in range(2)]
            h2_b = [Buf(), Buf()]
            stb = [sb(stB, "stb%d" % i, [128, 4], F32) for i in range(2)]
            stb_b = [Buf(), Buf()]
            ot = [sb(stB, "ot%d" % i, [128, D], F32) for i in range(2)]
            ot_b = [Buf(), Buf()]
            oc = 0
            nblk = NSEQ * SEQ // NB
            for blk in range(nblk):
                s = (blk * NB) // SEQ
                if (blk * NB) % SEQ == 0:
                    S.dma("sp", tabsB1[:].rearrange("p a b -> p (a b)"), tabAll_d[NSEQ + s], reads=[tabAll_b], writes=[tabsB_b1])
                for tt in range(NTT):
                    rows = slice(blk * NB + tt * 128, blk * NB + (tt + 1) * 128)
                    b = tt % 2
                    S.dma("sp", x1[b][:], out_d[rows, :], writes=[x1_b[b]])
                    S.op("act", lambda e, b=b: e.activation(out=junkB[:], in_=x1[b][:], func=AF.Square,
                                                            accum_out=stb[b][:, 0:1]),
                         reads=[x1_b[b]], writes=[junkB_b, stb_b[b]])
                    S.op("act", lambda e, b=b: e.activation(out=stb[b][:, 1:2], in_=stb[b][:, 0:1], func=AF.Sqrt,
                                                            bias=EPS, scale=1.0 / D), reads=[stb_b[b]], writes=[stb_b[b]])
                    S.op("dve", lambda e, b=b: e.reciprocal(out=stb[b][:, 2:3], in_=stb[b][:, 1:2]),
                         reads=[stb_b[b]], writes=[stb_b[b]])
                    S.op("dve", lambda e, b=b: e.scalar_tensor_tensor(
                        out=tB[:], in0=x1[b][:], scalar=stb[b][:, 2:3], in1=tabsB1[:, 1, :], op0=ALU.mult, op1=ALU.mult),
                        reads=[x1_b[b], stb_b[b], tabsB_b1], writes=[tB_b])
                    S.op("dve", lambda e, b=b: e.tensor_tensor(out=h2[b][:], in0=tB[:], in1=tabsB1[:, 0, :], op=ALU.add),
                         reads=[tB_b, tabsB_b1], writes=[h2_b[b]])
                    ptt, ptb_ = next_pt()
                    for kt in range(8):
                        S.op("pe", lambda e, ptt=ptt, b=b, kt=kt: e.transpose(
                            out=ptt[:, kt * 128:(kt + 1) * 128], in_=h2[b][:, kt * 128:(kt + 1) * 128], identity=ident_bf[:]),
                            reads=[h2_b[b], cst], writes=[ptb_])
                    S.op("act", lambda e, ptt=ptt, tt=tt: e.copy(
                        out=h2T[:, :, tt * 128:(tt + 1) * 128], in_=ptt[:].rearrange("p (a b) -> p a b", b=128)),
                        reads=[ptb_], writes=[h2T_b])
                for ft in range(32):
                    p, pb = next_pw(2)
                    for kt in range(8):
                        S.op("pe", lambda e, p=p, kt=kt, ft=ft: e.matmul(
                            p[:, 0:NB], W1[:, kt, ft * 128:(ft + 1) * 128], h2T[:, kt, :], start=(kt == 0), stop=(kt == 7)),
                            reads=[W_b, h2T_b], writes=[pb])
                    b = ft % 2
                    S.op("act", lambda e, p=p, b=b: e.activation(out=rl[b][:], in_=p[:, 0:NB], func=AF.Relu),
                         reads=[pb], writes=[rl_b[b]])
                    S.op("pool", lambda e, b=b, ft=ft: e.tensor_tensor(out=hidT[:, ft, :], in0=rl[b][:], in1=rl[b][:], op=ALU.mult),
                         reads=[rl_b[b]], writes=[hid_b[ft]])
                for tt in range(NTT):
                    rows = slice(blk * NB + tt * 128, blk * NB + (tt + 1) * 128)
                    ob = oc % 2
                    oc += 1
                    S.dma("sp", ot[ob][:], out_d[rows, :], writes=[ot_b[ob]])
                    for hf in range(2):
                        bi = 2 + (2 * tt + hf) % 4
                        p, pb = pf[bi], pfb[bi]
                        for ft in range(32):
                            S.op("pe", lambda e, p=p, ft=ft, tt=tt, hf=hf: e.matmul(
                                p[:], hidT[:, ft, tt * 128:(tt + 1) * 128], W2[:, ft, hf * 512:(hf + 1) * 512],
                                start=(ft == 0), stop=(ft == 31)), reads=[hid_b[ft], W_b], writes=[pb])
                        hs = slice(hf * 512, (hf + 1) * 512)
                        S.op("dve", lambda e, p=p, hs=hs: e.tensor_tensor(
                            out=tB[:, hs], in0=p[:], in1=tabsB1[:, 2, hs], op=ALU.mult),
                            reads=[pb, tabsB_b1], writes=[tB_b])
                        S.op("dve", lambda e, ob=ob, hs=hs: e.tensor_tensor(
                            out=ot[ob][:, hs], in0=tB[:, hs], in1=ot[ob][:, hs], op=ALU.add),
                            reads=[tB_b, ot_b[ob]], writes=[ot_b[ob]])
                    S.dma("sp", out_d[rows, :], ot[ob][:], reads=[ot_b[ob]])
            S.barrier()
    except _Stop:
        pass
    return nc


_PROGRAM = None


def _prep_shared(inp):
    f = np.float32
    def kt_layout(w):
        K, N = w.shape
        return np.ascontiguousarray(w.reshape(K // 128, 128, N).transpose(1, 0, 2)).astype(f)
    def bc(v, n=128):
        return np.ascontiguousarray(np.broadcast_to(np.asarray(v, f).reshape(1, -1), (n, np.asarray(v).size)))
    def qlay(a):
        a = np.asarray(a, f)
        rest = a.shape[2:]
        return np.ascontiguousarray(a.reshape((16, 2, 64) + rest).transpose((1, 2, 0) + tuple(range(3, 3 + len(rest))))
                                    .reshape((128, 16) + rest))
    sh = {}
    sh["w_ada"] = kt_layout(inp["w_ada"][0])
    sh["b_ada_b"] = bc(inp["b_ada"][0])
    sh["w_in"] = kt_layout(inp["w_in"][0])
    sh["w_out"] = kt_layout(inp["w_out"][0])
    sh["w_glu"] = kt_layout(inp["w_glu"][0])
    sh["w_ff1"] = kt_layout(inp["w_ff1"][0])
    sh["w_ff2"] = kt_layout(inp["w_ff2"][0])
    sh["norm1_g_b"] = bc(inp["norm1_g"][0])
    sh["norm2_g_b"] = bc(inp["norm2_g"][0])
    sh["b_glu_b"] = bc(inp["b_glu"][0])
    sh["gn_ssm_b"] = bc(inp["gn_ssm"][0])
    sh["gn_attn_b"] = bc(inp["gn_attn"][0])
    sh["gq_b"] = bc(inp["q_gain"][0])
    sh["gk_b"] = bc(inp["k_gain"][0])
    sh["gq2"] = np.ascontiguousarray(np.tile(np.asarray(inp["q_gain"][0], f), 2).reshape(128, 1))
    sh["gk2"] = np.ascontiguousarray(np.tile(np.asarray(inp["k_gain"][0], f), 2).reshape(128, 1))
    sh["lamre_q"] = qlay(inp["lam_re"][0])
    sh["lamim_q"] = qlay(inp["lam_im"][0])
    sh["logdt_q"] = qlay(np.broadcast_to(np.asarray(inp["log_dt"][0], f)[:, None], (32, 64)))
    sh["bre_q"] = qlay(inp["ssm_b_re"][0])
    sh["bim_q"] = qlay(inp["ssm_b_im"][0])
    sh["cre_q"] = qlay(np.asarray(inp["ssm_c_re"][0]).transpose(0, 2, 1))
    sh["cim_q"] = qlay(np.asarray(inp["ssm_c_im"][0]).transpose(0, 2, 1))
    dsk = np.asarray(inp["d_skip"][0], f).reshape(32, 16)
    sh["dsk_b"] = np.ascontiguousarray(np.broadcast_to(np.tile(dsk, (1, 8))[None], (128, 32, 128))).astype(f)
    sh["ident"] = np.eye(128, dtype=f)
    r = np.arange(128)
    sh["causal"] = np.where(r[None, :] <= r[:, None], 0.0, -1.0e30).astype(f)
    sh["negi4"] = np.tile(-BIG * np.eye(128, dtype=f), (1, 4)).astype(f)
    ib, jb = r // 16, r // 16
    sh["bmask"] = (jb[None, :] >= ib[:, None]).astype(f)
    sh["dmask"] = (r[None, :] == r[:, None]).astype(f)
    sh["onesblk"] = ((r[None, :] // 64) == (r[:, None] // 64)).astype(f)
    sh["mtab"] = np.ascontiguousarray(np.broadcast_to(np.asarray(EXPS, f)[None, None, :], (128, 16, NE)))
    sh["pow2"] = np.ascontiguousarray(np.broadcast_to((2.0 ** -np.arange(NITER + 2)).astype(f)[None], (128, NITER + 2)))
    zm = np.zeros((128, 2), f)
    zm[:64, 0] = 1.0
    zm[64:, 1] = 1.0
    sh["zmask"] = zm
    return sh


def kernel(**inputs):
    global _PROGRAM
    inp = {k: np.asarray(v) for k, v in inputs.items()}
    if _PROGRAM is None:
        _PROGRAM = build_program()
    nc = _PROGRAM
    shared = _prep_shared(inp)
    x = np.asarray(inp["x"], np.float32)
    c = np.asarray(inp["c"], np.float32)
    in_maps = []
    for core in range(NCORES):
        m = dict(shared)
        m["x"] = np.ascontiguousarray(x[NSEQ * core: NSEQ * (core + 1)].reshape(NSEQ * SEQ, D))
        cc = c[NSEQ * core: NSEQ * (core + 1)]
        cT = cc.reshape(NSEQ, 8, 128).transpose(2, 1, 0)
        m["cT"] = np.ascontiguousarray(np.broadcast_to(cT[:, :, :, None], (128, 8, NSEQ, 128))).astype(np.float32)
        in_maps.append(m)
    res = run_bass_kernel_spmd(nc, in_maps, core_ids=list(range(NCORES)))
    outs = [np.asarray(r["out"], np.float32).reshape(NSEQ, SEQ, D) for r in res.results]
    return np.concatenate(outs, axis=0)
```

```python
import math
from contextlib import ExitStack

import numpy as np
import concourse.bass as bass
import concourse.mybir as mybir
from concourse.alu_op_type import AluOpType as ALU
from concourse.bass_utils import run_bass_kernel_spmd

F32 = mybir.dt.float32
BF16 = mybir.dt.bfloat16
I32 = mybir.dt.int32
AF = mybir.ActivationFunctionType
AX = mybir.AxisListType

NCORES = 8
D = 1024
SEQ = 2048
NSEQ = 2
DIN = 1864
DFF = 4096
EPS = 1e-6
IDX_SCALE = (64 ** -0.5) * (8 ** -0.5)
TOPK = 256
NITER = 14

DVE_BISECT = False
BIG = 30000.0
EXPS = list(range(-7, 9)) + [16, 32, 64, 128, 256, 512, 1024]
NE = len(EXPS)
EIDX = {m: i for i, m in enumerate(EXPS)}
TWO_PI = 2.0 * math.pi


class Buf:
    __slots__ = ("w", "r", "ex")

    def __init__(self, ex=False):
        self.w = None
        self.r = {}
        self.ex = ex


class Sched:
    NDS = 24

    def __init__(self, nc, st):
        self.nc = nc
        self.eng = {"pe": nc.tensor, "act": nc.scalar, "dve": nc.vector, "pool": nc.gpsimd, "sp": nc.sync}
        self.sem = {k: st.enter_context(nc.semaphore("s_" + k)) for k in self.eng}
        self.cnt = {k: 0 for k in self.eng}
        self.waited = {k: {} for k in self.eng}
        self.dsem = [st.enter_context(nc.semaphore("d%d" % i)) for i in range(self.NDS)]
        self.dcnt = [0] * self.NDS
        self.dpool = {"sp": list(range(0, 16)), "pool": list(range(16, 24))}
        self.dnext = {"sp": 0, "pool": 0}

    def _semof(self, key):
        return self.sem[key[1]] if key[0] == "e" else self.dsem[key[1]]

    def _wait(self, e, key, val):
        if key[0] == "e" and key[1] == e and e == "pe":
            return
        if self.waited[e].get(key, 0) >= val:
            return
        self.eng[e].wait_ge(self._semof(key), val)
        self.waited[e][key] = val

    def _deps(self, e, reads, writes):
        for b in reads:
            if b.w is not None:
                self._wait(e, b.w[0], b.w[1])
            if b.ex:
                for k, v in b.r.items():
                    if k != ("e", e):
                        self._wait(e, k, v)
        for b in writes:
            if b.w is not None:
                self._wait(e, b.w[0], b.w[1])
            for k, v in b.r.items():
                self._wait(e, k, v)

    def _mark(self, key, val, reads, writes):
        for b in reads:
            if b.r.get(key, 0) < val:
                b.r[key] = val
        for b in writes:
            b.w = (key, val)
            b.r = {}

    def op(self, e, fn, reads=(), writes=()):
        self._deps(e, reads, writes)
        ins = fn(self.eng[e])
        self.cnt[e] += 1
        ins.then_inc(self.sem[e], 1)
        self._mark(("e", e), self.cnt[e], reads, writes)

    def dma(self, e, out, in_, reads=(), writes=(), **kw):
        self._deps(e, reads, writes)
        pl = self.dpool[e]
        j = pl[self.dnext[e] % len(pl)]
        self.dnext[e] += 1
        if self.dcnt[j] > 0:
            self._wait(e, ("d", j), self.dcnt[j])
        ins = self.eng[e].dma_start(out=out, in_=in_, **kw)
        self.dcnt[j] += 16
        ins.then_inc(self.dsem[j], 16)
        self._mark(("d", j), self.dcnt[j], reads, writes)

    def barrier(self):
        for e in self.eng:
            for k in self.eng:
                if k != e and self.cnt[k] > 0:
                    self._wait(e, ("e", k), self.cnt[k])
            for j in range(self.NDS):
                if self.dcnt[j] > 0:
                    self._wait(e, ("d", j), self.dcnt[j])


class _Stop(Exception):
    pass


def build_program(stop=None):
    nc = bass.Bass("TRN2", target_bir_lowering=False)

    def din(name, shape, dt=F32):
        return nc.dram_tensor(name, list(shape), dt, kind="ExternalInput").ap()

    x_d = din("x", [NSEQ * SEQ, D])
    cT_d = din("cT", [128, 8, NSEQ, 128])
    wada_d = din("w_ada", [128, 8, 6 * D])
    bada_d = din("b_ada_b", [128, 6 * D])
    win_d = din("w_in", [128, 8, DIN])
    wout_d = din("w_out", [128, 8, D])
    wglu_d = din("w_glu", [128, 4, 512])
    wff1_d = din("w_ff1", [128, 8, DFF])
    wff2_d = din("w_ff2", [128, 32, D])
    n1g_d = din("norm1_g_b", [128, D])
    n2g_d = din("norm2_g_b", [128, D])
    bglu_d = din("b_glu_b", [128, 512])
    gns_d = din("gn_ssm_b", [128, 512])
    gna_d = din("gn_attn_b", [128, 512])
    gqb_d = din("gq_b", [128, 64])
    gkb_d = din("gk_b", [128, 64])
    gq2_d = din("gq2", [128, 1])
    gk2_d = din("gk2", [128, 1])
    lre_d = din("lamre_q", [128, 16])
    lim_d = din("lamim_q", [128, 16])
    ldt_d = din("logdt_q", [128, 16])
    bre_d = din("bre_q", [128, 16, 16])
    bim_d = din("bim_q", [128, 16, 16])
    cre_d = din("cre_q", [128, 16, 16])
    cim_d = din("cim_q", [128, 16, 16])
    dsk_d = din("dsk_b", [128, 32, 128])
    ident_d = din("ident", [128, 128])
    causal_d = din("causal", [128, 128])
    negi4_d = din("negi4", [128, 512])
    bmask_d = din("bmask", [128, 128])
    dmask_d = din("dmask", [128, 128])
    onesblk_d = din("onesblk", [128, 128])
    mtab_d = din("mtab", [128, 16, NE])
    pow2_d = din("pow2", [128, NITER + 2])
    zmask_d = din("zmask", [128, 2])
    out_d = nc.dram_tensor("out", [NSEQ * SEQ, D], F32, kind="ExternalOutput").ap()

    try:
      with ExitStack() as top:
        S = Sched(nc, top)

        def chk(name):
            if stop == name:
                S.barrier()
                raise _Stop()

        uid = [0]

        def sb(st, name, shape, dt):
            uid[0] += 1
            return st.enter_context(nc.sbuf_tensor("sb%d_%s" % (uid[0], name), list(shape), dt))

        def ps(st, name, shape, dt):
            uid[0] += 1
            return st.enter_context(nc.psum_tensor("ps%d_%s" % (uid[0], name), list(shape), dt))

        pt = [ps(top, "pt0", [128, 1024], BF16)]
        ptb = [Buf(ex=True)]
        xbank = {}

        def extra_bank(st, as_bf16):
            if as_bf16:
                t = ps(st, "ptx", [128, 1024], BF16)
                del pt[1:], ptb[1:]
                pt.append(t)
                ptb.append(Buf(ex=True))
                rot["pt"] = 0
            else:
                del pt[1:], ptb[1:]
                rot["pt"] = 0
                xbank["t"] = ps(st, "pfx", [128, 512], F32)
                xbank["b"] = Buf(ex=True)
        pf = [ps(top, "pf%d" % i, [128, 512], F32) for i in range(6)]
        pfb = [Buf(ex=True) for _ in range(6)]
        rot = {"pt": 0, "pw": 0}

        def next_pt():
            i = rot["pt"] % len(pt)
            rot["pt"] = i + 1
            return pt[i], ptb[i]

        def next_pw(n=4):
            i = rot["pw"] % n
            rot["pw"] += 1
            return pf[i], pfb[i]

        ident_bf = sb(top, "ident_bf", [128, 128], BF16)
        ident_f = sb(top, "ident_f", [128, 128], F32)
        causal = sb(top, "causal", [128, 128], F32)
        negi4 = sb(top, "negi4", [128, 512], BF16)
        onesblk = sb(top, "onesblk", [128, 128], BF16)
        zeros_bf = sb(top, "zeros_bf", [128, 260], BF16)
        bglu_b = sb(top, "bglu_b", [128, 512], F32)
        gns_b = sb(top, "gns_b", [128, 512], F32)
        gna_b = sb(top, "gna_b", [128, 512], F32)
        pow2 = sb(top, "pow2", [128, NITER + 2], F32)
        G2 = sb(top, "G2", [128, 1], F32)
        negM = sb(top, "negM", [128, 1], F32)
        taufix = sb(top, "taufix", [128, 1], F32)
        neghalf = sb(top, "neghalf", [128, 1], F32)
        siluT = sb(top, "siluT", [128, 8, NSEQ, 128], BF16)
        cst = Buf()
        for t_, d_ in ((ident_bf, ident_d), (ident_f, ident_d), (causal, causal_d), (negi4, negi4_d),
                       (onesblk, onesblk_d), (bglu_b, bglu_d), (gns_b, gns_d), (gna_b, gna_d), (pow2, pow2_d)):
            S.dma("pool", t_[:], d_, writes=[cst])
        S.op("dve", lambda e: e.memset(zeros_bf[:], 0.0), writes=[cst])
        S.op("dve", lambda e: e.memset(taufix[:], 1.0e29), writes=[cst])
        S.op("dve", lambda e: e.memset(neghalf[:], -0.5), writes=[cst])

        with ExitStack() as st0:
            gqb = sb(st0, "gqb", [128, 64], F32)
            gkb = sb(st0, "gkb", [128, 64], F32)
            gq2 = sb(st0, "gq2", [128, 1], F32)
            gk2 = sb(st0, "gk2", [128, 1], F32)
            cTf = sb(st0, "cTf", [128, 8 * NSEQ * 128], F32)
            tb = Buf()
            S.dma("sp", gqb[:], gqb_d, writes=[tb])
            S.dma("sp", gkb[:], gkb_d, writes=[tb])
            S.dma("sp", gq2[:], gq2_d, writes=[tb])
            S.dma("sp", gk2[:], gk2_d, writes=[tb])
            S.dma("sp", cTf[:], cT_d.rearrange("p a b c -> p (a b c)"), writes=[tb])
            S.op("dve", lambda e: e.scalar_tensor_tensor(out=G2[:], in0=gq2[:], scalar=0.125, in1=gk2[:],
                                                          op0=ALU.mult, op1=ALU.mult), reads=[tb], writes=[cst])
            S.op("dve", lambda e: e.scalar_tensor_tensor(out=gqb[:], in0=gqb[:], scalar=0.125, in1=gkb[:],
                                                          op0=ALU.mult, op1=ALU.mult), reads=[tb], writes=[tb])
            S.op("dve", lambda e: e.tensor_reduce(out=negM[:], in_=gqb[:], axis=AX.X, op=ALU.max,
                                                  apply_absolute_value=True), reads=[tb], writes=[cst])
            S.op("dve", lambda e: e.tensor_scalar(out=negM[:], in0=negM[:], scalar1=-64.0, scalar2=None,
                                                  op0=ALU.mult), reads=[cst], writes=[cst])
            S.op("act", lambda e: e.activation(out=siluT[:].rearrange("p a b c -> p (a b c)"), in_=cTf[:],
                                               func=AF.Silu), reads=[tb], writes=[cst])
            S.barrier()

        chk('consts')
        def adaln_scratch(st):
            wst = [sb(st, "wst%d" % i, [128, 8, 512], BF16) for i in range(2)]
            wstb = [Buf(), Buf()]
            bst = [sb(st, "bst%d" % i, [128, 512], F32) for i in range(2)]
            gst = [sb(st, "gst%d" % i, [128, 512], F32) for i in range(2)]
            tmp = [sb(st, "adat%d" % i, [128, 512], F32) for i in range(2)]
            tmpb = [Buf(), Buf()]
            return wst, wstb, bst, gst, tmp, tmpb

        tabAll_d = nc.dram_tensor("tabAll_scr", [2 * NSEQ, 128, 3 * D], F32, kind="Internal").ap()
        tabAll_b = Buf()

        def adaln(tabs, tabs_b, seqs, first_j, ng_d, scr):
            wst, wstb, bst, gst, tmp, tmpb = scr
            it = 0
            for jj in range(3):
                for hc in range(2):
                    c0 = (first_j + jj) * D + hc * 512
                    b = it % 2
                    it += 1
                    S.dma("pool", wst[b][:], wada_d[:, :, c0:c0 + 512], writes=[wstb[b]])
                    S.dma("sp", bst[b][:], bada_d[:, c0:c0 + 512], writes=[wstb[b]])
                    if jj == 1:
                        S.dma("sp", gst[b][:], ng_d[:, hc * 512:(hc + 1) * 512], writes=[wstb[b]])
                    for n, s in enumerate(seqs):
                        p, pb = next_pw()
                        for kt in range(8):
                            S.op("pe", lambda e, p=p, b=b, kt=kt, s=s: e.matmul(
                                p[:], siluT[:, kt, s, :], wst[b][:, kt, :], start=(kt == 0), stop=(kt == 7)),
                                reads=[cst, wstb[b]], writes=[pb])
                        dst = tabs[n][:, jj, hc * 512:(hc + 1) * 512]
                        if jj == 1:
                            tb_ = tmpb[n]
                            S.op("dve", lambda e, p=p, b=b, n=n: e.tensor_tensor(
                                out=tmp[n][:], in0=p[:], in1=bst[b][:], op=ALU.add),
                                reads=[pb, wstb[b]], writes=[tb_])
                            S.op("dve", lambda e, b=b, n=n, dst=dst: e.scalar_tensor_tensor(
                                out=dst, in0=tmp[n][:], scalar=1.0, in1=gst[b][:], op0=ALU.add, op1=ALU.mult),
                                reads=[tb_, wstb[b]], writes=[tabs_b[n]])
                        else:
                            S.op("dve", lambda e, p=p, b=b, dst=dst: e.tensor_tensor(
                                out=dst, in0=p[:], in1=bst[b][:], op=ALU.add),
                                reads=[pb, wstb[b]], writes=[tabs_b[n]])

        with ExitStack() as stA:
            tabsA1 = sb(stA, "tabsA", [128, 3, D], F32)
            tabsA = [tabsA1, tabsA1]
            tabsA_b1 = Buf()
            tabsA_b = [tabsA_b1, tabsA_b1]

            ssmA_d = nc.dram_tensor("ssmA_scr", [128, 32, 128], BF16, kind="Internal").ap()
            ssmB_d = nc.dram_tensor("ssmB_scr", [128, 32, 128], BF16, kind="Internal").ap()
            ssmC_d = nc.dram_tensor("ssmC_scr", [128, 32, 2, 128], BF16, kind="Internal").ap()
            ssmd_b = Buf()
            ak = sb(stA, "ak", [128, 16, 8], F32)
            ck = sb(stA, "ck", [128, 16, 8], F32)
            nck = sb(stA, "nck", [128, 16, 8], F32)
            ssm_b = Buf()
            with ExitStack() as st1:
                extra_bank(st1, True)
                A_sb = sb(st1, "A_sb", [128, 32, 128], BF16)
                Bm_sb = sb(st1, "Bm_sb", [128, 32, 128], BF16)
                Cmz = sb(st1, "Cmz", [128, 32, 2, 128], BF16)

                def t3(name, n):
                    return sb(st1, name, [128, 16, n], F32)
                lre = t3("lre", 1); lim = t3("lim", 1); ldt = t3("ldt", 1)
                bre = t3("bre", 16); bim = t3("bim", 16); cre = t3("cre", 16); cim = t3("cim", 16)
                mtab = t3("mtab", NE)
                bmask = sb(st1, "bmask", [128, 128], F32)
                dmask = sb(st1, "dmask", [128, 128], F32)
                zmask = sb(st1, "zmask", [128, 2], F32)
                dsk = sb(st1, "dsk", [128, 32, 128], F32)
                ib = Buf()
                for t_, d_ in ((lre, lre_d), (lim, lim_d), (ldt, ldt_d)):
                    S.dma("sp", t_[:, :, 0], d_, writes=[ib])
                for t_, d_ in ((bre, bre_d), (bim, bim_d), (cre, cre_d), (cim, cim_d), (mtab, mtab_d),
                               (bmask, bmask_d), (dmask, dmask_d), (dsk, dsk_d), (zmask, zmask_d)):
                    S.dma("sp", t_[:], d_, writes=[ib])
                chk('ssm_a')
                dt_ = t3("dt_", 1); aa = t3("aa", 1); th = t3("th", 1)
                ang = t3("ang", NE); lmag = t3("lmag", NE); mag = t3("mag", NE)
                tq = t3("tq", NE); tqi = sb(st1, "tqi", [128, 16, NE], I32); tqf = t3("tqf", NE)
                wr = t3("wr", NE); sn = t3("sn", NE); cs = t3("cs", NE)
                pr_ = t3("pr_", NE); pi_ = t3("pi_", NE)
                wb = Buf()

                def V(fn, reads=(), writes=(wb,)):
                    S.op("dve", fn, reads=[ib, wb] + list(reads), writes=list(writes))

                def ACT(fn, reads=(), writes=(wb,)):
                    S.op("act", fn, reads=[ib, wb] + list(reads), writes=list(writes))

                ACT(lambda e: e.activation(out=dt_[:], in_=ldt[:], func=AF.Exp))
                V(lambda e: e.tensor_tensor(out=aa[:], in0=lre[:], in1=dt_[:], op=ALU.mult))
                V(lambda e: e.tensor_tensor(out=th[:], in0=lim[:], in1=dt_[:], op=ALU.mult))
                V(lambda e: e.tensor_tensor(out=lmag[:], in0=mtab[:], in1=aa[:].to_broadcast([128, 16, NE]), op=ALU.mult))
                V(lambda e: e.tensor_tensor(out=ang[:], in0=mtab[:], in1=th[:].to_broadcast([128, 16, NE]), op=ALU.mult))
                ACT(lambda e: e.activation(out=mag[:], in_=lmag[:], func=AF.Exp))
                V(lambda e: e.tensor_scalar(out=tq[:], in0=ang[:], scalar1=1.0 / TWO_PI, scalar2=None, op0=ALU.mult))
                V(lambda e: e.tensor_copy(out=tqi[:], in_=tq[:]))
                V(lambda e: e.tensor_copy(out=tqf[:], in_=tqi[:]))
                V(lambda e: e.scalar_tensor_tensor(out=wr[:], in0=tqf[:], scalar=-TWO_PI, in1=ang[:],
                                                   op0=ALU.mult, op1=ALU.add))
                wt_ = t3("wt_", NE)
                for t_, shift in ((sn, 0.0), (cs, math.pi / 2)):
                    V(lambda e, t_=t_, shift=shift: e.tensor_scalar(out=t_[:], in0=wr[:], scalar1=shift, scalar2=None, op0=ALU.add))
                    V(lambda e, t_=t_: e.tensor_scalar(out=wt_[:], in0=t_[:], scalar1=math.pi, scalar2=-TWO_PI,
                                                       op0=ALU.is_gt, op1=ALU.mult))
                    V(lambda e, t_=t_: e.tensor_scalar(out=tq[:], in0=t_[:], scalar1=-math.pi, scalar2=TWO_PI,
                                                       op0=ALU.is_lt, op1=ALU.mult))
                    V(lambda e, t_=t_: e.tensor_tensor(out=t_[:], in0=t_[:], in1=wt_[:], op=ALU.add))
                    V(lambda e, t_=t_: e.tensor_tensor(out=t_[:], in0=t_[:], in1=tq[:], op=ALU.add))
                for t_ in (sn, cs):
                    V(lambda e, t_=t_: e.tensor_scalar(out=t_[:], in0=t_[:], scalar1=3.14159, scalar2=-3.14159,
                                                       op0=ALU.min, op1=ALU.max))
                ACT(lambda e: e.activation(out=sn[:], in_=sn[:], func=AF.Sin))
                ACT(lambda e: e.activation(out=cs[:], in_=cs[:], func=AF.Sin))
                V(lambda e: e.tensor_tensor(out=pr_[:], in0=mag[:], in1=cs[:], op=ALU.mult))
                V(lambda e: e.tensor_tensor(out=pi_[:], in0=mag[:], in1=sn[:], op=ALU.mult))
                chk('ssm_b')
                i1 = EIDX[1]
                nr = t3("nr", 1); den = t3("den", 1); t1_ = t3("t1_", 1); gr = t3("gr", 1); gi = t3("gi", 1)
                V(lambda e: e.tensor_scalar(out=nr[:], in0=pr_[:, :, i1:i1 + 1], scalar1=-1.0, scalar2=None, op0=ALU.add))
                V(lambda e: e.tensor_tensor(out=den[:], in0=lre[:], in1=lre[:], op=ALU.mult))
                V(lambda e: e.tensor_tensor(out=t1_[:], in0=lim[:], in1=lim[:], op=ALU.mult))
                V(lambda e: e.tensor_tensor(out=den[:], in0=den[:], in1=t1_[:], op=ALU.add))
                V(lambda e: e.reciprocal(out=den[:], in_=den[:]))
                V(lambda e: e.tensor_tensor(out=gr[:], in0=nr[:], in1=lre[:], op=ALU.mult))
                V(lambda e: e.tensor_tensor(out=t1_[:], in0=pi_[:, :, i1:i1 + 1], in1=lim[:], op=ALU.mult))
                V(lambda e: e.tensor_tensor(out=gr[:], in0=gr[:], in1=t1_[:], op=ALU.add))
                V(lambda e: e.tensor_tensor(out=gr[:], in0=gr[:], in1=den[:], op=ALU.mult))
                V(lambda e: e.tensor_tensor(out=gi[:], in0=pi_[:, :, i1:i1 + 1], in1=lre[:], op=ALU.mult))
                V(lambda e: e.tensor_tensor(out=t1_[:], in0=nr[:], in1=lim[:], op=ALU.mult))
                V(lambda e: e.tensor_tensor(out=gi[:], in0=gi[:], in1=t1_[:], op=ALU.subtract))
                V(lambda e: e.tensor_tensor(out=gi[:], in0=gi[:], in1=den[:], op=ALU.mult))
                for k in range(8):
                    ii = EIDX[8 * (2 ** k)]
                    V(lambda e, k=k, ii=ii: e.tensor_copy(out=ak[:, :, k:k + 1], in_=pr_[:, :, ii:ii + 1]), writes=[wb, ssm_b])
                    V(lambda e, k=k, ii=ii: e.tensor_copy(out=ck[:, :, k:k + 1], in_=pi_[:, :, ii:ii + 1]), writes=[wb, ssm_b])
                    V(lambda e, k=k, ii=ii: e.tensor_scalar(out=nck[:, :, k:k + 1], in0=pi_[:, :, ii:ii + 1], scalar1=-1.0,
                                                            scalar2=None, op0=ALU.mult), writes=[wb, ssm_b])
                PBr = t3("PBr", 8); PBi = t3("PBi", 8); tt8 = t3("tt8", 8)
                e7 = EIDX[0]
                sl07 = slice(e7, e7 + 8)
                V(lambda e: e.tensor_tensor(out=PBr[:], in0=pr_[:, :, sl07], in1=gr[:].to_broadcast([128, 16, 8]), op=ALU.mult))
                V(lambda e: e.tensor_tensor(out=tt8[:], in0=pi_[:, :, sl07], in1=gi[:].to_broadcast([128, 16, 8]), op=ALU.mult))
                V(lambda e: e.tensor_tensor(out=PBr[:], in0=PBr[:], in1=tt8[:], op=ALU.subtract))
                V(lambda e: e.tensor_tensor(out=PBi[:], in0=pr_[:, :, sl07], in1=gi[:].to_broadcast([128, 16, 8]), op=ALU.mult))
                V(lambda e: e.tensor_tensor(out=tt8[:], in0=pi_[:, :, sl07], in1=gr[:].to_broadcast([128, 16, 8]), op=ALU.mult))
                V(lambda e: e.tensor_tensor(out=PBi[:], in0=PBi[:], in1=tt8[:], op=ALU.add))
                BmTr = sb(st1, "BmTr", [128, 16, 8, 16], F32)
                BmTi = sb(st1, "BmTi", [128, 16, 8, 16], F32)
                t816 = sb(st1, "t816", [128, 16, 8, 16], F32)
                for i in range(8):
                    m = 7 - i
                    def bc(t_, m=m):
                        return t_[:, :, m:m + 1].to_broadcast([128, 16, 16])
                    V(lambda e, i=i, bc=bc: e.tensor_tensor(out=BmTr[:, :, i, :], in0=bre[:], in1=bc(PBr), op=ALU.mult))
                    V(lambda e, i=i, bc=bc: e.tensor_tensor(out=t816[:, :, i, :], in0=bim[:], in1=bc(PBi), op=ALU.mult))
                    V(lambda e, i=i, bc=bc: e.tensor_tensor(out=BmTi[:, :, i, :], in0=bim[:], in1=bc(PBr), op=ALU.mult))
                V(lambda e: e.tensor_tensor(out=BmTr[:], in0=BmTr[:], in1=t816[:], op=ALU.subtract))
                for i in range(8):
                    m = 7 - i
                    V(lambda e, i=i, m=m: e.tensor_tensor(out=t816[:, :, i, :], in0=bre[:],
                                                          in1=PBi[:, :, m:m + 1].to_broadcast([128, 16, 16]), op=ALU.mult))
                V(lambda e: e.tensor_tensor(out=BmTi[:], in0=BmTi[:], in1=t816[:], op=ALU.add))
                Wcr = sb(st1, "Wcr", [128, 16, 8, 16], F32)
                Wci = sb(st1, "Wci", [128, 16, 8, 16], F32)
                Cmr = sb(st1, "Cmr", [128, 16, 8, 16], F32)
                Cmi = sb(st1, "Cmi", [128, 16, 8, 16], F32)
                for (dr, di, off) in ((Wcr, Wci, -7), (Cmr, Cmi, 1)):
                    for j in range(8):
                        ii = EIDX[j + off]
                        def bc2(t_, ii=ii):
                            return t_[:, :, ii:ii + 1].to_broadcast([128, 16, 16])
                        V(lambda e, j=j, bc2=bc2, dr=dr: e.tensor_tensor(out=dr[:, :, j, :], in0=cre[:], in1=bc2(pr_), op=ALU.mult))
                        V(lambda e, j=j, bc2=bc2: e.tensor_tensor(out=t816[:, :, j, :], in0=cim[:], in1=bc2(pi_), op=ALU.mult))
                        V(lambda e, j=j, bc2=bc2, di=di: e.tensor_tensor(out=di[:, :, j, :], in0=cre[:], in1=bc2(pi_), op=ALU.mult))
                    V(lambda e, dr=dr: e.tensor_tensor(out=dr[:], in0=dr[:], in1=t816[:], op=ALU.subtract))
                    for j in range(8):
                        ii = EIDX[j + off]
                        V(lambda e, j=j, ii=ii: e.tensor_tensor(out=t816[:, :, j, :], in0=cim[:],
                                                                in1=pr_[:, :, ii:ii + 1].to_broadcast([128, 16, 16]), op=ALU.mult))
                    V(lambda e, di=di: e.tensor_tensor(out=di[:], in0=di[:], in1=t816[:], op=ALU.add))
                    V(lambda e, di=di: e.tensor_scalar(out=di[:], in0=di[:], scalar1=-1.0, scalar2=None, op0=ALU.mult))
                chk('ssm_c')
                for pr in range(16):
                    for gp in range(2):
                        g = 2 * pr + gp
                        for ri, src in ((0, Cmr), (1, Cmi)):
                            V(lambda e, g=g, ri=ri, src=src, pr=pr, gp=gp: e.tensor_scalar(
                                out=Cmz[:, g, ri, :], in0=src[:, pr, :, :].rearrange("p a b -> p (a b)"),
                                scalar1=zmask[:, gp:gp + 1], scalar2=None, op0=ALU.mult), writes=[wb, ssm_b])
                chk('ssm_d')
                Bz = [sb(st1, "Bz%d" % i, [128, 2, 128], BF16) for i in range(2)]
                Wz = [sb(st1, "Wz%d" % i, [128, 2, 128], BF16) for i in range(2)]
                Bzb = [Buf(), Buf()]
                At = [sb(st1, "At%d" % i, [128, 128], F32) for i in range(2)]
                Atb = [Buf(), Buf()]
                for pr in range(16):
                    for gp in range(2):
                        g = 2 * pr + gp
                        b = g % 2
                        for ri, src in ((0, BmTr), (1, BmTi)):
                            V(lambda e, b=b, ri=ri, src=src, pr=pr, gp=gp: e.tensor_scalar(
                                out=Bz[b][:, ri, :], in0=src[:, pr, :, :].rearrange("p a b -> p (a b)"),
                                scalar1=zmask[:, gp:gp + 1], scalar2=None, op0=ALU.mult), writes=[wb, Bzb[b]])
                        for ri, src in ((0, Wcr), (1, Wci)):
                            V(lambda e, b=b, ri=ri, src=src, pr=pr, gp=gp: e.tensor_scalar(
                                out=Wz[b][:, ri, :], in0=src[:, pr, :, :].rearrange("p a b -> p (a b)"),
                                scalar1=zmask[:, gp:gp + 1], scalar2=None, op0=ALU.mult), writes=[wb, Bzb[b]])
                        ptt, ptb_ = next_pt()
                        for ri in range(2):
                            S.op("pe", lambda e, ptt=ptt, b=b, ri=ri: e.transpose(
                                out=ptt[:, ri * 128:(ri + 1) * 128], in_=Bz[b][:, ri, :], identity=ident_bf[:]),
                                reads=[Bzb[b], cst], writes=[ptb_])
                        for ri in range(2):
                            S.op("act", lambda e, ptt=ptt, g=g, ri=ri, gp=gp: e.copy(
                                out=Bm_sb[:, g, ri * 64:(ri + 1) * 64],
                                in_=ptt[:, ri * 128 + gp * 64: ri * 128 + gp * 64 + 64]),
                                reads=[ptb_], writes=[ssm_b])
                        p, pb = next_pw()
                        for ri in range(2):
                            S.op("pe", lambda e, p=p, b=b, ri=ri: e.matmul(
                                p[:, 0:128], Bz[b][:, ri, :], Wz[b][:, ri, :], start=(ri == 0), stop=(ri == 1)),
                                reads=[Bzb[b]], writes=[pb])
                        S.op("dve", lambda e, p=p, b=b: e.tensor_tensor(out=At[b][:], in0=p[:, 0:128], in1=bmask[:], op=ALU.mult),
                             reads=[pb, ib], writes=[Atb[b]])
                        S.op("dve", lambda e, b=b, g=g: e.tensor_tensor(out=dsk[:, g, :], in0=dsk[:, g, :], in1=dmask[:], op=ALU.mult),
                             reads=[ib], writes=[ib])
                        S.op("dve", lambda e, b=b, g=g: e.tensor_tensor(out=A_sb[:, g, :], in0=At[b][:], in1=dsk[:, g, :], op=ALU.add),
                             reads=[Atb[b], ib], writes=[ssm_b])
                chk('ssm_e')
                S.dma("sp", ssmA_d, A_sb[:], reads=[ssm_b], writes=[ssmd_b])
                S.dma("sp", ssmB_d, Bm_sb[:], reads=[ssm_b], writes=[ssmd_b])
                S.dma("sp", ssmC_d, Cmz[:], reads=[ssm_b], writes=[ssmd_b])
                scr = adaln_scratch(st1)
                tl = [tabsA1, sb(st1, "tabt1", [128, 3, D], F32)]
                tlb = [tabsA_b1, Buf()]
                for half, ngd in ((0, n1g_d), (1, n2g_d)):
                    adaln(tl, tlb, list(range(NSEQ)), 3 * half, ngd, scr)
                    for q in range(NSEQ):
                        S.dma("sp", tabAll_d[NSEQ * half + q], tl[q][:].rearrange("p a b -> p (a b)"),
                              reads=[tlb[q]], writes=[tabAll_b])
                S.barrier()

            chk('ssmsetup')
            for s in range(NSEQ):
                r0 = s * SEQ
                S.dma("sp", tabsA1[:].rearrange("p a b -> p (a b)"), tabAll_d[s], reads=[tabAll_b], writes=[tabsA_b1])
                chk('adaln')
                with ExitStack() as stS:
                    qT = sb(stS, "qT", [128, 4, SEQ], BF16)
                    kTz = sb(stS, "kTz", [128, 2, 2, SEQ], BF16)
                    qiT = sb(stS, "qiT", [128, 4, SEQ], BF16)
                    kiTz = sb(stS, "kiTz", [128, 2, SEQ], BF16)
                    Vp = sb(stS, "Vp", [128, 16, 2, 65], BF16)
                    wi = sb(stS, "wi", [128, 16, 8], F32)
                    mixT_s = sb(stS, "mixT_s", [128, 4, SEQ], BF16)
                    qT_b, kT_b, qiT_b, kiT_b, Vp_b, wi_b, mixT_sb = (Buf() for _ in range(7))
                    S.op("pool", lambda e: e.memset(kTz[:].rearrange("p a b c -> p (a b c)"), 0.0), writes=[kT_b])
                    S.op("pool", lambda e: e.memset(kiTz[:].rearrange("p a c -> p (a c)"), 0.0), writes=[kiT_b])
                    S.op("pool", lambda e: e.memset(Vp[:].rearrange("p a b c -> p (a b c)"), 1.0), writes=[Vp_b])

                    with ExitStack() as st12:
                        U8 = sb(st12, "U8", [128, 2, 32, 8, 16], BF16)
                        U8_b = Buf()
                        with ExitStack() as st1:
                            extra_bank(st1, True)
                            w_in = sb(st1, "w_in", [128, 8, DIN], BF16)
                            w_in_b = Buf()
                            for kt in range(8):
                                S.dma("pool", w_in[:, kt, :], win_d[:, kt, :], writes=[w_in_b])
                            hT = sb(st1, "hT", [128, 8, SEQ], BF16)
                            hT_b = Buf()
                            st1a = ExitStack()
                            xt = [sb(st1a, "xt%d" % i, [128, D], F32) for i in range(2)]
                            xt_b = [Buf(), Buf()]
                            junk = sb(st1a, "junk", [128, D], BF16)
                            junk_b = Buf()
                            t1 = sb(st1a, "t1", [128, D], F32)
                            hb = [sb(st1a, "hb%d" % i, [128, D], BF16) for i in range(2)]
                            hb_b = [Buf(), Buf()]
                            st_ = [sb(st1a, "st%d" % i, [128, 4], F32) for i in range(2)]
                            st_b = [Buf(), Buf()]
                            t1_b = Buf()
                            for t in range(16):
                                b = t % 2
                                S.dma("sp", xt[b][:], x_d[r0 + t * 128: r0 + (t + 1) * 128, :], writes=[xt_b[b]])
                                S.op("act", lambda e, b=b: e.activation(out=junk[:], in_=xt[b][:], func=AF.Square,
                                                                        accum_out=st_[b][:, 0:1]),
                                     reads=[xt_b[b]], writes=[junk_b, st_b[b]])
                                S.op("act", lambda e, b=b: e.activation(out=st_[b][:, 1:2], in_=st_[b][:, 0:1], func=AF.Sqrt,
                                                                        bias=EPS, scale=1.0 / D),
                                     reads=[st_b[b]], writes=[st_b[b]])
                                S.op("dve", lambda e, b=b: e.reciprocal(out=st_[b][:, 2:3], in_=st_[b][:, 1:2]),
                                     reads=[st_b[b]], writes=[st_b[b]])
                                S.op("dve", lambda e, b=b: e.scalar_tensor_tensor(
                                    out=t1[:], in0=xt[b][:], scalar=st_[b][:, 2:3], in1=tabsA[s][:, 1, :],
                                    op0=ALU.mult, op1=ALU.mult), reads=[xt_b[b], st_b[b], tabsA_b[s]], writes=[t1_b])
                                S.op("dve", lambda e, b=b: e.tensor_tensor(out=hb[b][:], in0=t1[:], in1=tabsA[s][:, 0, :], op=ALU.add),
                                     reads=[t1_b, tabsA_b[s]], writes=[hb_b[b]])
                                ptt, ptb_ = next_pt()
                                for kt in range(8):
                                    S.op("pe", lambda e, ptt=ptt, b=b, kt=kt: e.transpose(
                                        out=ptt[:, kt * 128:(kt + 1) * 128], in_=hb[b][:, kt * 128:(kt + 1) * 128],
                                        identity=ident_bf[:]), reads=[hb_b[b], cst], writes=[ptb_])
                                S.op("act", lambda e, ptt=ptt, t=t: e.copy(
                                    out=hT[:, :, t * 128:(t + 1) * 128], in_=ptt[:].rearrange("p (a b) -> p a b", b=128)),
                                    reads=[ptb_], writes=[hT_b])
                            S.barrier()
                            st1a.close()
                            sq = [sb(st1, "sq%d" % i, [128, 512], BF16) for i in range(2)]
                            sq_b = [Buf(), Buf()]
                            sd = [sb(st1, "sd%d" % i, [128, 512], F32) for i in range(2)]
                            sd_b = [Buf(), Buf()]
                            groups = []
                            for j in range(4):
                                groups.append(("q", j, [(512 + j * 128, 128, 0)]))
                            groups.append(("kA", 0, [(1024, 128, 0)]))
                            groups.append(("kB", 0, [(1088, 64, 0), (1024, 64, 64)]))
                            for j in range(4):
                                groups.append(("qi", j, [(1280 + j * 128, 128, 0)]))
                            groups.append(("ki", 0, [(1792, 64, 0), (1792, 64, 64)]))
                            it = 0
                            for kind, j, parts in groups:
                                for c in range(4):
                                    cs_ = slice(c * 512, (c + 1) * 512)
                                    p, pb = next_pw()
                                    for (c0, m, po) in parts:
                                        for kt in range(8):
                                            S.op("pe", lambda e, p=p, c0=c0, m=m, po=po, kt=kt, cs_=cs_: e.matmul(
                                                p[po:po + m, :], w_in[:, kt, c0:c0 + m], hT[:, kt, cs_],
                                                start=(kt == 0), stop=(kt == 7)),
                                                reads=[w_in_b, hT_b], writes=[pb])
                                    if kind == "qi":
                                        S.op("act", lambda e, p=p, j=j, cs_=cs_: e.copy(out=qiT[:, j, cs_], in_=p[:]),
                                             reads=[pb], writes=[qiT_b])
                                    elif kind == "ki":
                                        for half in range(2):
                                            rs = slice(half * 64, half * 64 + 64)
                                            S.op("act", lambda e, p=p, half=half, rs=rs, cs_=cs_: e.copy(
                                                out=kiTz[rs, half, cs_], in_=p[rs, :]), reads=[pb], writes=[kiT_b])
                                    else:
                                        b = it % 2
                                        it += 1
                                        S.op("act", lambda e, p=p, b=b: e.activation(out=sq[b][:], in_=p[:], func=AF.Square),
                                             reads=[pb], writes=[sq_b[b]])
                                        p2, p2b = next_pw()
                                        S.op("pe", lambda e, p2=p2, b=b: e.matmul(p2[:], onesblk[:], sq[b][:], start=True, stop=True),
                                             reads=[sq_b[b], cst], writes=[p2b])
                                        S.op("act", lambda e, p2=p2, b=b: e.activation(out=sd[b][:], in_=p2[:], func=AF.Sqrt,
                                                                                      bias=EPS, scale=1.0 / 64),
                                             reads=[p2b], writes=[sd_b[b]])
                                        S.op("dve", lambda e, b=b: e.reciprocal(out=sd[b][:], in_=sd[b][:]),
                                             reads=[sd_b[b]], writes=[sd_b[b]])
                                        if kind == "q":
                                            S.op("dve", lambda e, p=p, b=b, j=j, cs_=cs_: e.scalar_tensor_tensor(
                                                out=qT[:, j, cs_], in0=p[:], scalar=G2[:, 0:1], in1=sd[b][:],
                                                op0=ALU.mult, op1=ALU.mult), reads=[pb, sd_b[b], cst], writes=[qT_b])
                                        else:
                                            kvs = (0, 1) if kind == "kA" else (1, 0)
                                            for half in range(2):
                                                rs = slice(half * 64, half * 64 + 64)
                                                kv = kvs[half]
                                                S.op("dve", lambda e, p=p, b=b, rs=rs, kv=kv, half=half, cs_=cs_: e.tensor_tensor(
                                                    out=kTz[rs, kv, half, cs_], in0=p[rs, :], in1=sd[b][rs, :], op=ALU.mult),
                                                    reads=[pb, sd_b[b]], writes=[kT_b])
                            for t in range(16):
                                ts_ = slice(t * 128, (t + 1) * 128)
                                p, pb = next_pw()
                                for kt in range(8):
                                    S.op("pe", lambda e, p=p, kt=kt, ts_=ts_: e.matmul(
                                        p[:, 0:128], hT[:, kt, ts_], w_in[:, kt, 1152:1280], start=(kt == 0), stop=(kt == 7)),
                                        reads=[w_in_b, hT_b], writes=[pb])
                                for kt in range(8):
                                    S.op("pe", lambda e, p=p, kt=kt, ts_=ts_: e.matmul(
                                        p[:, 128:136], hT[:, kt, ts_], w_in[:, kt, 1856:1864], start=(kt == 0), stop=(kt == 7)),
                                        reads=[w_in_b, hT_b], writes=[pb])
                                S.op("act", lambda e, p=p, t=t: e.copy(
                                    out=Vp[:, t, :, 0:64], in_=p[:, 0:128].rearrange("p (a b) -> p a b", b=64)),
                                    reads=[pb], writes=[Vp_b])
                                S.op("act", lambda e, p=p, t=t: e.mul(out=wi[:, t, :], in_=p[:, 128:136], mul=IDX_SCALE),
                                     reads=[pb], writes=[wi_b])
                            for sp in range(2):
                                for i in range(8):
                                    p, pb = next_pw()
                                    for kt in range(8):
                                        lhs = hT[:, kt, sp * 1024:(sp + 1) * 1024].rearrange("p (b i) -> p i b", i=8)[:, i, :]
                                        S.op("pe", lambda e, p=p, lhs=lhs, kt=kt: e.matmul(
                                            p[:], lhs, w_in[:, kt, 0:512], start=(kt == 0), stop=(kt == 7)),
                                            reads=[w_in_b, hT_b], writes=[pb])
                                    S.op("act", lambda e, p=p, sp=sp, i=i: e.copy(
                                        out=U8[:, sp, :, i, :], in_=p[:].rearrange("p (g c) -> p g c", c=16)),
                                         reads=[pb], writes=[U8_b])
                            S.barrier()

                        chk('s1')
                        with ExitStack() as st2:
                            extra_bank(st2, True)
                            w_glu = sb(st2, "w_glu", [128, 4, 512], BF16)
                            w_glu_b = Buf()
                            S.dma("pool", w_glu[:], wglu_d, writes=[w_glu_b])
                            Ytok = sb(st2, "Ytok", [128, 2, 8, 512], F32)
                            Ytok_b = Buf()
                            U8T = [sb(st2, "U8T%d" % i, [128, 2, 256], BF16) for i in range(2)]
                            U8T_b = [Buf(), Buf()]
                            XA = [sb(st2, "XA%d" % i, [128, 2, 384], F32) for i in range(2)]
                            XB = [sb(st2, "XB%d" % i, [128, 2, 384], F32) for i in range(2)]
                            XA_b, XB_b = [Buf(), Buf()], [Buf(), Buf()]
                            TM = [sb(st2, "TM%d" % i, [128, 2, 256], F32) for i in range(2)]
                            TM_b = [Buf(), Buf()]
                            Xst = [sb(st2, "Xst%d" % i, [128, 2, 258], BF16) for i in range(2)]
                            Xst_b = [Buf(), Buf()]
                            Ysb = [sb(st2, "Ysb%d" % i, [128, 256], F32) for i in range(2)]
                            Ysb_b = [Buf(), Buf()]
                            for i in range(2):
                                S.op("pool", lambda e, i=i: e.memset(XA[i][:].rearrange("p a b -> p (a b)"), 0.0), writes=[XA_b[i]])
                                S.op("pool", lambda e, i=i: e.memset(XB[i][:].rearrange("p a b -> p (a b)"), 0.0), writes=[XB_b[i]])
                                S.op("pool", lambda e, i=i: e.memset(Xst[i][:].rearrange("p a b -> p (a b)"), 0.0), writes=[Xst_b[i]])
                            PAD = 128
                            A2 = [sb(st2, "A2_%d" % i, [128, 2, 128], BF16) for i in range(2)]
                            B2 = [sb(st2, "B2_%d" % i, [128, 2, 128], BF16) for i in range(2)]
                            C2 = [sb(st2, "C2_%d" % i, [128, 2, 2, 128], BF16) for i in range(2)]
                            M2_b = [Buf(), Buf()]

                            def gen_pair(pr):
                                ub = pr % 2
                                S.dma("sp", A2[ub][:], ssmA_d[:, 2 * pr:2 * pr + 2, :], reads=[ssmd_b], writes=[M2_b[ub]])
                                S.dma("sp", B2[ub][:], ssmB_d[:, 2 * pr:2 * pr + 2, :], reads=[ssmd_b], writes=[M2_b[ub]])
                                S.dma("sp", C2[ub][:], ssmC_d[:, 2 * pr:2 * pr + 2, :, :], reads=[ssmd_b], writes=[M2_b[ub]])
                                ptt, ptb_ = next_pt()
                                for gp in range(2):
                                    g = 2 * pr + gp
                                    for sp in range(2):
                                        S.op("pe", lambda e, ptt=ptt, gp=gp, sp=sp, g=g: e.transpose(
                                            out=ptt[:, (gp * 2 + sp) * 128:(gp * 2 + sp + 1) * 128],
                                            in_=U8[:, sp, g, :, :].rearrange("p a b -> p (a b)"), identity=ident_bf[:]),
                                            reads=[U8_b, cst], writes=[ptb_])
                                S.op("act", lambda e, ptt=ptt, ub=ub: e.copy(
                                    out=U8T[ub][:].rearrange("p a b -> p (a b)"), in_=ptt[:, 0:512]),
                                    reads=[ptb_], writes=[U8T_b[ub]])
                                p, pb = next_pw()
                                for gp in range(2):
                                    g = 2 * pr + gp
                                    for ri in range(2):
                                        S.op("pe", lambda e, p=p, gp=gp, g=g, ri=ri, ub=ub: e.matmul(
                                            p[gp * 64:(gp + 1) * 64, ri * 256:(ri + 1) * 256],
                                            B2[ub][:, gp, ri * 64:(ri + 1) * 64], U8T[ub][:, gp, :], start=True, stop=True),
                                            reads=[M2_b[ub], U8T_b[ub]], writes=[pb])
                                S.op("act", lambda e, p=p: e.copy(out=XA[ub][:, :, PAD:PAD + 256],
                                                                  in_=p[:].rearrange("p (a b) -> p a b", b=256)),
                                     reads=[pb], writes=[XA_b[ub]])
                                yield
                                cur, curb, nxt, nxtb = XA[ub], XA_b[ub], XB[ub], XB_b[ub]
                                xs = Xst[ub]
                                tm, tmb = TM[ub], TM_b[ub]
                                for k in range(8):
                                    sft = 2 ** k
                                    last = (k == 7)
                                    a_ = ak[:, pr, k:k + 1]
                                    c_ = ck[:, pr, k:k + 1]
                                    nc_ = nck[:, pr, k:k + 1]
                                    sh = slice(PAD - sft, PAD - sft + 256)
                                    ce = slice(PAD, PAD + 256)
                                    outr = xs[:, 0, 1:257] if last else nxt[:, 0, ce]
                                    outi = xs[:, 1, 1:257] if last else nxt[:, 1, ce]
                                    ob = Xst_b[ub] if last else nxtb
                                    S.op("dve", lambda e, cur=cur, a_=a_, sh=sh, ce=ce: e.scalar_tensor_tensor(
                                        out=tm[:, 0, :], in0=cur[:, 0, sh], scalar=a_, in1=cur[:, 0, ce], op0=ALU.mult, op1=ALU.add),
                                        reads=[curb, ssm_b], writes=[tmb])
                                    S.op("dve", lambda e, cur=cur, a_=a_, sh=sh, ce=ce: e.scalar_tensor_tensor(
                                        out=tm[:, 1, :], in0=cur[:, 1, sh], scalar=a_, in1=cur[:, 1, ce], op0=ALU.mult, op1=ALU.add),
                                        reads=[curb, ssm_b], writes=[tmb])
                                    yield
                                    S.op("dve", lambda e, cur=cur, nc_=nc_, sh=sh, outr=outr: e.scalar_tensor_tensor(
                                        out=outr, in0=cur[:, 1, sh], scalar=nc_, in1=tm[:, 0, :], op0=ALU.mult, op1=ALU.add),
                                        reads=[curb, tmb, ssm_b], writes=[ob])
                                    S.op("dve", lambda e, cur=cur, c_=c_, sh=sh, outi=outi: e.scalar_tensor_tensor(
                                        out=outi, in0=cur[:, 0, sh], scalar=c_, in1=tm[:, 1, :], op0=ALU.mult, op1=ALU.add),
                                        reads=[curb, tmb, ssm_b], writes=[ob])
                                    yield
                                    cur, curb, nxt, nxtb = nxt, nxtb, cur, curb
                                for gp in range(2):
                                    g = 2 * pr + gp
                                    yb = g % 2
                                    p, pb = next_pw()
                                    S.op("pe", lambda e, p=p, g=g, gp=gp, ub=ub: e.matmul(
                                        p[:, 0:256], A2[ub][:, gp, :], U8T[ub][:, gp, :], start=True, stop=False),
                                        reads=[M2_b[ub], U8T_b[ub]], writes=[pb])
                                    for ri in range(2):
                                        S.op("pe", lambda e, p=p, g=g, ri=ri, xs=xs: e.matmul(
                                            p[:, 0:256], C2[ub][:, gp, ri, :], xs[:, ri, 0:256], start=False, stop=(ri == 1)),
                                            reads=[M2_b[ub], Xst_b[ub]], writes=[pb])
                                    S.op("act", lambda e, p=p, yb=yb: e.copy(out=Ysb[yb][:], in_=p[:, 0:256]),
                                         reads=[pb], writes=[Ysb_b[yb]])
                                    p2, p2b = next_pw()
                                    for sp in range(2):
                                        S.op("pe", lambda e, p2=p2, sp=sp, yb=yb: e.transpose(
                                            out=p2[:, sp * 128:(sp + 1) * 128], in_=Ysb[yb][:, sp * 128:(sp + 1) * 128],
                                            identity=ident_f[:]), reads=[Ysb_b[yb], cst], writes=[p2b])
                                    for sp in range(2):
                                        S.op("act", lambda e, p2=p2, sp=sp, g=g: e.copy(
                                            out=Ytok[:, sp, :, g * 16:(g + 1) * 16],
                                            in_=p2[:, sp * 128:(sp + 1) * 128].rearrange("p (a b) -> p a b", b=16)),
                                            reads=[p2b], writes=[Ytok_b])
                                    yield

                            act_p = []
                            nxt_p = 0
                            steps = 0
                            while nxt_p < 16 or act_p:
                                if nxt_p < 16 and len(act_p) < 2 and (nxt_p == 0 or steps >= 6):
                                    if not act_p or act_p[-1][0] % 2 != nxt_p % 2:
                                        act_p.append((nxt_p, gen_pair(nxt_p)))
                                        nxt_p += 1
                                for item in list(act_p):
                                    try:
                                        next(item[1])
                                    except StopIteration:
                                        act_p.remove(item)
                                steps += 1
                            g1 = [sb(st2, "g1_%d" % i, [128, 512], F32) for i in range(2)]
                            g2_ = [sb(st2, "g2_%d" % i, [128, 512], F32) for i in range(2)]
                            zf = [sb(st2, "zf%d" % i, [128, 512], F32) for i in range(2)]
                            zb = [sb(st2, "zb%d" % i, [128, 512], BF16) for i in range(2)]
                            zT = [sb(st2, "zT%d" % i, [128, 4, 128], BF16) for i in range(2)]
                            sg = [sb(st2, "sg%d" % i, [128, 512], F32) for i in range(2)]
                            mb_ = [sb(st2, "mb%d" % i, [128, 512], BF16) for i in range(2)]
                            sst = [sb(st2, "sst%d" % i, [128, 4], F32) for i in range(2)]
                            gb = [[Buf() for _ in range(8)] for _ in range(2)]
                            KG = 2.0 * math.sqrt(2.0 / math.pi)
                            for sp in range(2):
                                for i in range(8):
                                    b = i % 2
                                    B = gb[b]
                                    y = Ytok[:, sp, i, :]
                                    S.op("act", lambda e, b=b, y=y: e.activation(out=g1[b][:], in_=y, func=AF.Square),
                                         reads=[Ytok_b], writes=[B[0]])
                                    S.op("dve", lambda e, b=b: e.tensor_scalar(out=g1[b][:], in0=g1[b][:], scalar1=0.044715,
                                                                               scalar2=1.0, op0=ALU.mult, op1=ALU.add),
                                         reads=[B[0]], writes=[B[0]])
                                    S.op("dve", lambda e, b=b, y=y: e.tensor_tensor(out=g2_[b][:], in0=g1[b][:], in1=y, op=ALU.mult),
                                         reads=[B[0], Ytok_b], writes=[B[1]])
                                    S.op("act", lambda e, b=b: e.activation(out=g2_[b][:], in_=g2_[b][:], func=AF.Sigmoid, scale=KG),
                                         reads=[B[1]], writes=[B[1]])
                                    S.op("dve", lambda e, b=b, y=y: e.tensor_tensor(out=zf[b][:], in0=g2_[b][:], in1=y, op=ALU.mult),
                                         reads=[B[1], Ytok_b], writes=[B[2]])
                                    S.op("act", lambda e, b=b: e.copy(out=zb[b][:], in_=zf[b][:]), reads=[B[2]], writes=[B[3]])
                                    ptt, ptb_ = next_pt()
                                    for ft in range(4):
                                        S.op("pe", lambda e, ptt=ptt, b=b, ft=ft: e.transpose(
                                            out=ptt[:, ft * 128:(ft + 1) * 128], in_=zb[b][:, ft * 128:(ft + 1) * 128],
                                            identity=ident_bf[:]), reads=[B[3], cst], writes=[ptb_])
                                    S.op("act", lambda e, ptt=ptt, b=b: e.copy(out=zT[b][:].rearrange("p a b -> p (a b)"),
                                                                               in_=ptt[:, 0:512]), reads=[ptb_], writes=[B[4]])
                                    p, pb = next_pw()
                                    for ft in range(4):
                                        S.op("pe", lambda e, p=p, b=b, ft=ft: e.matmul(
                                            p[:], zT[b][:, ft, :], w_glu[:, ft, :], start=(ft == 0), stop=(ft == 3)),
                                            reads=[B[4], w_glu_b], writes=[pb])
                                    S.op("dve", lambda e, p=p, b=b: e.tensor_tensor(out=sg[b][:], in0=p[:], in1=bglu_b[:], op=ALU.add),
                                         reads=[pb, cst], writes=[B[5]])
                                    S.op("act", lambda e, b=b: e.activation(out=sg[b][:], in_=sg[b][:], func=AF.Sigmoid),
                                         reads=[B[5]], writes=[B[5]])
                                    S.op("dve", lambda e, b=b: e.tensor_tensor(out=sg[b][:], in0=sg[b][:], in1=zf[b][:], op=ALU.mult),
                                         reads=[B[5], B[2]], writes=[B[5]])
                                    S.op("act", lambda e, b=b: e.activation(out=g1[b][:], in_=sg[b][:], func=AF.Square,
                                                                            accum_out=sst[b][:, 0:1]),
                                         reads=[B[5], B[0]], writes=[B[0], B[6]])
                                    S.op("dve", lambda e, b=b: e.tensor_scalar(out=sst[b][:, 1:2], in0=sst[b][:, 0:1], scalar1=1.0 / 512,
                                                                               scalar2=EPS, op0=ALU.mult, op1=ALU.add),
                                         reads=[B[6]], writes=[B[6]])
                                    S.op("pool", lambda e, b=b: e.tensor_tensor(out=sst[b][:, 2:3], in0=sst[b][:, 1:2], in1=neghalf[:, 0:1],
                                                                                op=ALU.pow), reads=[B[6], cst], writes=[B[6]])
                                    S.op("dve", lambda e, b=b: e.scalar_tensor_tensor(
                                        out=mb_[b][:], in0=sg[b][:], scalar=sst[b][:, 2:3], in1=gns_b[:], op0=ALU.mult, op1=ALU.mult),
                                        reads=[B[5], B[6], cst], writes=[B[7]])
                                    ptt, ptb_ = next_pt()
                                    for ft in range(4):
                                        S.op("pe", lambda e, ptt=ptt, b=b, ft=ft: e.transpose(
                                            out=ptt[:, ft * 128:(ft + 1) * 128], in_=mb_[b][:, ft * 128:(ft + 1) * 128],
                                            identity=ident_bf[:]), reads=[B[7], cst], writes=[ptb_])
                                    dst = mixT_s[:, :, sp * 1024:(sp + 1) * 1024].rearrange("p a (b i) -> p a i b", i=8)[:, :, i, :]
                                    S.op("act", lambda e, ptt=ptt, dst=dst: e.copy(
                                        out=dst, in_=ptt[:, 0:512].rearrange("p (a b) -> p a b", b=128)),
                                        reads=[ptb_], writes=[mixT_sb])
                            S.barrier()

                    chk('s2')
                    with ExitStack() as st3:
                        extra_bank(st3, False)
                        w_out = sb(st3, "w_out", [128, 8, D], BF16)
                        w_out_b = Buf()
                        S.dma("pool", w_out[:], wout_d, writes=[w_out_b])
                        score = [sb(st3, "score%d" % i, [128, SEQ], F32) for i in range(2)]
                        score_b = [Buf(), Buf()]
                        nm = [sb(st3, "nm%d" % i, [128, SEQ], BF16) for i in range(3)]
                        nm_b = [Buf() for _ in range(3)]
                        rbuf = [sb(st3, "rbuf%d" % i, [128, 8, 512], BF16) for i in range(2)]
                        rbuf_b = [Buf(), Buf()]
                        dg = [sb(st3, "dg%d" % i, [128, 8, 128], BF16) for i in range(2)]
                        dg_b = [Buf(), Buf()]
                        bs = [sb(st3, "bs%d" % i, [128, 16 + 3 * NITER], F32) for i in range(2)]
                        bs_b = [Buf(), Buf()]
                        PT = [sb(st3, "PT%d" % i, [128, 512], BF16) for i in range(3)]
                        PT_b = [Buf() for _ in range(3)]
                        yatt = sb(st3, "yatt", [128, 512], F32)
                        yatt_b = Buf()
                        rden = sb(st3, "rden", [128, 8], F32)
                        ajunk = sb(st3, "ajunk", [128, 512], BF16)
                        ast = sb(st3, "ast", [128, 4], F32)
                        mixa = sb(st3, "mixa", [128, 512], BF16)
                        mixa_b = Buf()
                        mixTa = sb(st3, "mixTa", [128, 4, 128], BF16)
                        mixTa_b = Buf()
                        xr = [sb(st3, "xr%d" % i, [128, D], F32) for i in range(2)]
                        xr_b = [Buf(), Buf()]
                        x1t = [sb(st3, "x1t%d" % i, [128, 512], F32) for i in range(2)]
                        x1t_b = [Buf(), Buf()]
                        prot = {"I": 0, "A": 0, "pt": 0}

                        def pwI():
                            i = prot["I"] % 3
                            prot["I"] += 1
                            if i == 2:
                                return xbank["t"], xbank["b"]
                            return pf[i], pfb[i]

                        def pwA():
                            i = 2 + prot["A"] % 2
                            prot["A"] += 1
                            return pf[i], pfb[i]

                        def gen_I(qt):
                            sl = qt % 2
                            nsl = qt % 3
                            nk = qt + 1
                            SK = nk * 128
                            qs = slice(qt * 128, (qt + 1) * 128)
                            sc, scb = score[sl], score_b[sl]
                            BS, BSb = bs[sl], bs_b[sl]
                            S.op("dve", lambda e: e.tensor_tensor(
                                out=dg[sl][:], in0=ident_bf[:].unsqueeze(1).to_broadcast([128, 8, 128]),
                                in1=wi[:, qt, :].unsqueeze(2).to_broadcast([128, 8, 128]), op=ALU.mult),
                                reads=[cst, wi_b], writes=[dg_b[sl]])
                            nch = (SK + 511) // 512
                            for c in range(nch):
                                cw = min(512, SK - c * 512)
                                ks = slice(c * 512, c * 512 + cw)
                                for h in range(8):
                                    j, half = h // 2, h % 2
                                    p, pb = pwI()
                                    S.op("pe", lambda e, p=p, j=j, half=half: e.matmul(
                                        p[:, 0:cw], qiT[:, j, qs], kiTz[:, half, ks], start=True, stop=True),
                                        reads=[qiT_b, kiT_b], writes=[pb])
                                    if False:
                                        S.op("act", lambda e, p=p, h=h: e.activation(
                                            out=rbuf[sl][:, h, 0:cw], in_=p[:, 0:cw], func=AF.Relu),
                                            reads=[pb], writes=[rbuf_b[sl]])
                                    else:
                                        S.op("dve", lambda e, p=p, h=h: e.tensor_scalar(
                                            out=rbuf[sl][:, h, 0:cw], in0=p[:, 0:cw], scalar1=0.0, scalar2=None, op0=ALU.max),
                                            reads=[pb], writes=[rbuf_b[sl]])
                                p, pb = pwI()
                                for h in range(8):
                                    S.op("pe", lambda e, p=p, h=h: e.matmul(
                                        p[:, 0:cw], dg[sl][:, h, :], rbuf[sl][:, h, 0:cw], start=(h == 0), stop=(h == 7)),
                                        reads=[dg_b[sl], rbuf_b[sl]], writes=[pb])
                                S.op("act", lambda e, p=p: e.copy(out=sc[:, ks], in_=p[:, 0:cw]),
                                     reads=[pb], writes=[scb])
                                S.op("dve", lambda e, c=c: e.tensor_reduce(
                                    out=BS[:, c:c + 1], in_=sc[:, ks], axis=AX.X, op=ALU.max, apply_absolute_value=True),
                                    reads=[scb], writes=[BSb])
                                yield
                            S.op("dve", lambda e: e.tensor_tensor(out=sc[:, qs], in0=sc[:, qs], in1=causal[:], op=ALU.add),
                                 reads=[scb, cst], writes=[scb])
                            if DVE_BISECT and qt >= 2 and qt % 2 == 1:
                                S.op("dve", lambda e: e.tensor_reduce(
                                    out=BS[:, 4:5], in_=BS[:, 0:nch], axis=AX.X, op=ALU.max), reads=[BSb], writes=[BSb])
                                S.op("dve", lambda e: e.tensor_scalar(
                                    out=BS[:, 8:8 + NITER + 1], in0=pow2[:, 0:NITER + 1], scalar1=BS[:, 4:5], scalar2=None,
                                    op0=ALU.mult), reads=[BSb, cst], writes=[BSb])
                                S.op("dve", lambda e: e.memset(BS[:, 5:6], 0.0), reads=[BSb], writes=[BSb])
                                for n in range(NITER):
                                    S.op("dve", lambda e: e.tensor_scalar(
                                        out=nm[nsl][:, 0:SK], in0=sc[:, 0:SK], scalar1=BS[:, 5:6], scalar2=None,
                                        op0=ALU.is_ge, op1=ALU.add, accum_out=BS[:, 6:7]),
                                        reads=[scb, BSb], writes=[nm_b[nsl], BSb])
                                    yield
                                    S.op("dve", lambda e, n=n: e.tensor_scalar(
                                        out=BS[:, 7:8], in0=BS[:, 6:7], scalar1=float(TOPK) - 0.5, scalar2=BS[:, 8 + n:9 + n],
                                        op0=ALU.is_ge, op1=ALU.mult), reads=[BSb], writes=[BSb])
                                    yield
                                    S.op("dve", lambda e, n=n: e.scalar_tensor_tensor(
                                        out=BS[:, 5:6], in0=BS[:, 7:8], scalar=BS[:, 9 + n:10 + n], in1=BS[:, 5:6],
                                        op0=ALU.subtract, op1=ALU.add), reads=[BSb], writes=[BSb])
                                    yield
                                S.op("dve", lambda e: e.tensor_tensor(
                                    out=BS[:, 5:6], in0=BS[:, 5:6], in1=BS[:, 8 + NITER:9 + NITER], op=ALU.subtract),
                                    reads=[BSb], writes=[BSb])
                                S.op("dve", lambda e: e.tensor_scalar(
                                    out=nm[nsl][:, 0:SK], in0=sc[:, 0:SK], scalar1=BS[:, 5:6], scalar2=None, op0=ALU.is_lt),
                                    reads=[scb, BSb], writes=[nm_b[nsl]])
                                yield
                                return
                            if qt >= 2:
                                S.op("dve", lambda e: e.tensor_reduce(
                                    out=BS[:, 4:5], in_=BS[:, 0:nch], axis=AX.X, op=ALU.max), reads=[BSb], writes=[BSb])
                                S.op("dve", lambda e: e.tensor_scalar(
                                    out=BS[:, 8:8 + NITER + 1], in0=pow2[:, 0:NITER + 1], scalar1=BS[:, 4:5], scalar2=None,
                                    op0=ALU.mult), reads=[BSb, cst], writes=[BSb])
                                S.op("dve", lambda e: e.memset(BS[:, 5:6], 0.0), reads=[BSb], writes=[BSb])
                                thr = float(2 * TOPK - SK) - 0.5
                                S.op("dve", lambda e: e.memset(BS[:, 3:4], -thr), reads=[BSb], writes=[BSb])
                                S.op("dve", lambda e: e.tensor_scalar(
                                    out=BS[:, 9 + NITER:10 + 2 * NITER], in0=BS[:, 8:9 + NITER], scalar1=-1.0, scalar2=None,
                                    op0=ALU.mult), reads=[BSb], writes=[BSb])
                                for n in range(NITER):
                                    S.op("act", lambda e: e.activation(
                                        out=nm[nsl][:, 0:SK], in_=sc[:, 0:SK], func=AF.Sign, bias=BS[:, 5:6], scale=1.0,
                                        accum_out=BS[:, 6:7]), reads=[scb, BSb], writes=[nm_b[nsl], BSb])
                                    yield
                                    S.op("act", lambda e: e.activation(
                                        out=BS[:, 7:8], in_=BS[:, 6:7], func=AF.Sign, bias=BS[:, 3:4], scale=1.0),
                                        reads=[BSb], writes=[BSb])
                                    yield
                                    S.op("act", lambda e, n=n: e.activation(
                                        out=BS[:, 5:6], in_=BS[:, 7:8], func=AF.Identity, bias=BS[:, 5:6],
                                        scale=BS[:, 9 + NITER + 1 + n:10 + NITER + 1 + n]),
                                        reads=[BSb], writes=[BSb])
                                    yield
                                S.op("dve", lambda e: e.tensor_tensor(
                                    out=BS[:, 5:6], in0=BS[:, 5:6], in1=BS[:, 8 + NITER:9 + NITER], op=ALU.add),
                                    reads=[BSb], writes=[BSb])
                                ntau = BS[:, 5:6]
                            else:
                                ntau = taufix[:, 0:1]
                            S.op("dve", lambda e: e.tensor_scalar(
                                out=nm[nsl][:, 0:SK], in0=sc[:, 0:SK], scalar1=ntau, scalar2=0.0, op0=ALU.add, op1=ALU.is_lt),
                                reads=[scb, BSb, cst], writes=[nm_b[nsl]])
                            yield

                        def gen_A(qt):
                            nsl = qt % 3
                            nk = qt + 1
                            qs = slice(qt * 128, (qt + 1) * 128)
                            O = [pf[4], pf[5]]
                            Ob = [pfb[4], pfb[5]]
                            for kv in range(2):
                                S.op("pe", lambda e, kv=kv: e.matmul(O[kv][:, 0:260], zeros_bf[:, 0:128], zeros_bf[:, 0:260],
                                                                     start=True, stop=True), reads=[cst], writes=[Ob[kv]])
                            steps = [(kt, kv) for kt in range(nk) for kv in range(2)]

                            def emit_L(kt, kv):
                                ksl = slice(kt * 128, (kt + 1) * 128)
                                p, pb = pwA()
                                S.op("pe", lambda e, p=p: e.matmul(
                                    p[:], nm[nsl][:, ksl], negi4[:], start=True, stop=True),
                                    reads=[nm_b[nsl], cst], writes=[pb])
                                for hh in range(4):
                                    head = kv * 4 + hh
                                    j, half = head // 2, head % 2
                                    S.op("pe", lambda e, p=p, hh=hh, half=half, j=j: e.matmul(
                                        p[:, hh * 128:(hh + 1) * 128], kTz[:, kv, half, ksl], qT[:, j, qs],
                                        start=False, stop=True, skip_group_check=True),
                                        reads=[kT_b, qT_b], writes=[pb])
                                return p, pb

                            def emit_PV(kt, kv, p, pb):
                                pi3 = prot["pt"] % 3
                                prot["pt"] += 1
                                S.op("act", lambda e: e.activation(
                                    out=PT[pi3][:], in_=p[:], func=AF.Exp, bias=negM[:, 0:1], scale=1.0),
                                    reads=[pb, cst], writes=[PT_b[pi3]])
                                for hh in range(4):
                                    S.op("pe", lambda e, hh=hh: e.matmul(
                                        O[kv][:, hh * 65:(hh + 1) * 65], PT[pi3][:, hh * 128:(hh + 1) * 128], Vp[:, kt, kv, :],
                                        start=False, stop=True, skip_group_check=True),
                                        reads=[PT_b[pi3], Vp_b], writes=[Ob[kv]])

                            Lcur = emit_L(*steps[0])
                            for i, (kt, kv) in enumerate(steps):
                                Lnext = emit_L(*steps[i + 1]) if i + 1 < len(steps) else None
                                emit_PV(kt, kv, *Lcur)
                                Lcur = Lnext
                                yield
                            for kv in range(2):
                                ov = O[kv][:, 0:260].rearrange("p (h e) -> p h e", e=65)
                                S.op("dve", lambda e, kv=kv, ov=ov: e.reciprocal(out=rden[:, kv * 4:(kv + 1) * 4], in_=ov[:, :, 64]),
                                     reads=[Ob[kv]], writes=[yatt_b])
                                S.op("dve", lambda e, kv=kv, ov=ov: e.tensor_tensor(
                                    out=yatt[:, kv * 256:(kv + 1) * 256].rearrange("p (h d) -> p h d", d=64), in0=ov[:, :, 0:64],
                                    in1=rden[:, kv * 4:(kv + 1) * 4].unsqueeze(2).to_broadcast([128, 4, 64]), op=ALU.mult),
                                    reads=[Ob[kv], yatt_b], writes=[yatt_b])
                            S.op("act", lambda e: e.activation(out=ajunk[:], in_=yatt[:], func=AF.Square, accum_out=ast[:, 0:1]),
                                 reads=[yatt_b], writes=[yatt_b])
                            S.op("dve", lambda e: e.tensor_scalar(out=ast[:, 1:2], in0=ast[:, 0:1], scalar1=1.0 / 512, scalar2=EPS,
                                                                  op0=ALU.mult, op1=ALU.add), reads=[yatt_b], writes=[yatt_b])
                            S.op("pool", lambda e: e.tensor_tensor(out=ast[:, 2:3], in0=ast[:, 1:2], in1=neghalf[:, 0:1], op=ALU.pow),
                                 reads=[yatt_b, cst], writes=[yatt_b])
                            S.op("dve", lambda e: e.scalar_tensor_tensor(out=mixa[:], in0=yatt[:], scalar=ast[:, 2:3], in1=gna_b[:],
                                                                          op0=ALU.mult, op1=ALU.mult),
                                 reads=[yatt_b, cst], writes=[mixa_b])
                            ptt, ptb_ = next_pt()
                            for ft in range(4):
                                S.op("pe", lambda e, ptt=ptt, ft=ft: e.transpose(
                                    out=ptt[:, ft * 128:(ft + 1) * 128], in_=mixa[:, ft * 128:(ft + 1) * 128], identity=ident_bf[:]),
                                    reads=[mixa_b, cst], writes=[ptb_])
                            S.op("act", lambda e, ptt=ptt: e.copy(out=mixTa[:].rearrange("p a b -> p (a b)"), in_=ptt[:, 0:512]),
                                 reads=[ptb_], writes=[mixTa_b])
                            yield
                            xb = qt % 2
                            rows = slice(r0 + qt * 128, r0 + (qt + 1) * 128)
                            S.dma("sp", xr[xb][:], x_d[rows, :], writes=[xr_b[xb]])
                            for hf in range(2):
                                p, pb = pwA()
                                for k8 in range(8):
                                    lhs = mixT_s[:, k8, qs] if k8 < 4 else mixTa[:, k8 - 4, :]
                                    S.op("pe", lambda e, p=p, lhs=lhs, k8=k8, hf=hf: e.matmul(
                                        p[:], lhs, w_out[:, k8, hf * 512:(hf + 1) * 512], start=(k8 == 0), stop=(k8 == 7)),
                                        reads=[mixT_sb, mixTa_b, w_out_b], writes=[pb])
                                hs = slice(hf * 512, (hf + 1) * 512)
                                S.op("dve", lambda e, p=p, hf=hf, hs=hs: e.tensor_tensor(
                                    out=x1t[hf][:], in0=p[:], in1=tabsA[s][:, 2, hs], op=ALU.mult),
                                    reads=[pb, tabsA_b[s]], writes=[x1t_b[hf]])
                                S.op("dve", lambda e, xb=xb, hf=hf, hs=hs: e.tensor_tensor(
                                    out=xr[xb][:, hs], in0=x1t[hf][:], in1=xr[xb][:, hs], op=ALU.add),
                                    reads=[x1t_b[hf], xr_b[xb]], writes=[xr_b[xb]])
                            S.dma("sp", out_d[rows, :], xr[xb][:], reads=[xr_b[xb]])
                            yield

                        NQ = 16
                        actI = []
                        actA = None
                        nextI, nextA = 0, 0
                        doneI = set()
                        while nextA < NQ or actA is not None:
                            if actA is None and nextA in doneI:
                                actA = (nextA, gen_A(nextA))
                                nextA += 1
                            firstA = actA[0] if actA is not None else nextA
                            while (nextI < NQ and len(actI) < 2 and nextI - 3 < firstA
                                   and (nextI < 2 or (nextI - 2) in doneI)):
                                actI.append((nextI, gen_I(nextI)))
                                nextI += 1
                            for item in list(actI):
                                try:
                                    next(item[1])
                                except StopIteration:
                                    actI.remove(item)
                                    doneI.add(item[0])
                            if actA is not None:
                                try:
                                    next(actA[1])
                                except StopIteration:
                                    actA = None
                        S.barrier()

        chk('s3')
        NB = 256
        NTT = NB // 128
        with ExitStack() as stB:
            tabsB1 = sb(stB, "tabsB", [128, 3, D], F32)
            tabsB_b1 = Buf()
            extra_bank(stB, True)
            W1 = sb(stB, "W1", [128, 8, DFF], BF16)
            W2 = sb(stB, "W2", [128, 32, D], BF16)
            W_b = Buf()
            with ExitStack() as stW:
                NSTG = 4
                stg = [sb(stW, "stg%d" % i, [128, 2048], F32) for i in range(NSTG)]
                stg_b = [Buf() for _ in range(NSTG)]
                ci = 0
                chunks = []
                for kt in range(8):
                    for hc in range(2):
                        chunks.append((W1[:, kt, hc * 2048:(hc + 1) * 2048], wff1_d[:, kt, hc * 2048:(hc + 1) * 2048]))
                for ft in range(0, 32, 2):
                    chunks.append((W2[:, ft:ft + 2, :].rearrange("p a b -> p (a b)"),
                                   wff2_d[:, ft:ft + 2, :].rearrange("p a b -> p (a b)")))
                for dst, src in chunks:
                    b = ci % NSTG
                    S.dma("sp", stg[b][:], src, writes=[stg_b[b]])
                    eng = ("act", "pool", "dve")[ci % 3]
                    if eng == "act":
                        S.op("act", lambda e, dst=dst, b=b: e.copy(out=dst, in_=stg[b][:]), reads=[stg_b[b]], writes=[W_b])
                    else:
                        S.op(eng, lambda e, dst=dst, b=b: e.tensor_copy(out=dst, in_=stg[b][:]), reads=[stg_b[b]], writes=[W_b])
                    ci += 1
                S.barrier()
            hidT = sb(stB, "hidT", [128, 32, NB], BF16)
            hid_b = [Buf() for _ in range(32)]
            rl = [sb(stB, "rl%d" % i, [128, NB], BF16) for i in range(2)]
            rl_b = [Buf(), Buf()]
            h2T = sb(stB, "h2T", [128, 8, NB], BF16)
            h2T_b = Buf()
            x1 = [sb(stB, "x1_%d" % i, [128, D], F32) for i in range(2)]
            x1_b = [Buf() for _ in range(2)]
            junkB = sb(stB, "junkB", [128, D], BF16)
            junkB_b = Buf()
            tB = sb(stB, "tB", [128, D], F32)
            tB_b = Buf()
            h2 = [sb(stB, "h2_%d" % i, [128, D], BF16) for i in range(2)]
            h2_b = [Buf(), Buf()]
            stb = [sb(stB, "stb%d" % i, [128, 4], F32) for i in range(2)]
            stb_b = [Buf(), Buf()]
            ot = [sb(stB, "ot%d" % i, [128, D], F32) for i in range(2)]
            ot_b = [Buf(), Buf()]
            oc = 0
            nblk = NSEQ * SEQ // NB
            for blk in range(nblk):
                s = (blk * NB) // SEQ
                if (blk * NB) % SEQ == 0:
                    S.dma("sp", tabsB1[:].rearrange("p a b -> p (a b)"), tabAll_d[NSEQ + s], reads=[tabAll_b], writes=[tabsB_b1])
                for tt in range(NTT):
                    rows = slice(blk * NB + tt * 128, blk * NB + (tt + 1) * 128)
                    b = tt % 2
                    S.dma("sp", x1[b][:], out_d[rows, :], writes=[x1_b[b]])
                    S.op("act", lambda e, b=b: e.activation(out=junkB[:], in_=x1[b][:], func=AF.Square,
                                                            accum_out=stb[b][:, 0:1]),
                         reads=[x1_b[b]], writes=[junkB_b, stb_b[b]])
                    S.op("act", lambda e, b=b: e.activation(out=stb[b][:, 1:2], in_=stb[b][:, 0:1], func=AF.Sqrt,
                                                            bias=EPS, scale=1.0 / D), reads=[stb_b[b]], writes=[stb_b[b]])
                    S.op("dve", lambda e, b=b: e.reciprocal(out=stb[b][:, 2:3], in_=stb[b][:, 1:2]),
                         reads=[stb_b[b]], writes=[stb_b[b]])
                    S.op("dve", lambda e, b=b: e.scalar_tensor_tensor(
                        out=tB[:], in0=x1[b][:], scalar=stb[b][:, 2:3], in1=tabsB1[:, 1, :], op0=ALU.mult, op1=ALU.mult),
                        reads=[x1_b[b], stb_b[b], tabsB_b1], writes=[tB_b])
                    S.op("dve", lambda e, b=b: e.tensor_tensor(out=h2[b][:], in0=tB[:], in1=tabsB1[:, 0, :], op=ALU.add),
                         reads=[tB_b, tabsB_b1], writes=[h2_b[b]])
                    ptt, ptb_ = next_pt()
                    for kt in range(8):
                        S.op("pe", lambda e, ptt=ptt, b=b, kt=kt: e.transpose(
                            out=ptt[:, kt * 128:(kt + 1) * 128], in_=h2[b][:, kt * 128:(kt + 1) * 128], identity=ident_bf[:]),
                            reads=[h2_b[b], cst], writes=[ptb_])
                    S.op("act", lambda e, ptt=ptt, tt=tt: e.copy(
                        out=h2T[:, :, tt * 128:(tt + 1) * 128], in_=ptt[:].rearrange("p (a b) -> p a b", b=128)),
                        reads=[ptb_], writes=[h2T_b])
                for ft in range(32):
                    p, pb = next_pw(2)
                    for kt in range(8):
                        S.op("pe", lambda e, p=p, kt=kt, ft=ft: e.matmul(
                            p[:, 0:NB], W1[:, kt, ft * 128:(ft + 1) * 128], h2T[:, kt, :], start=(kt == 0), stop=(kt == 7)),
                            reads=[W_b, h2T_b], writes=[pb])
                    b = ft % 2
                    S.op("act", lambda e, p=p, b=b: e.activation(out=rl[b][:], in_=p[:, 0:NB], func=AF.Relu),
                         reads=[pb], writes=[rl_b[b]])
                    S.op("pool", lambda e, b=b, ft=ft: e.tensor_tensor(out=hidT[:, ft, :], in0=rl[b][:], in1=rl[b][:], op=ALU.mult),
                         reads=[rl_b[b]], writes=[hid_b[ft]])
                for tt in range(NTT):
                    rows = slice(blk * NB + tt * 128, blk * NB + (tt + 1) * 128)
                    ob = oc % 2
                    oc += 1
                    S.dma("sp", ot[ob][:], out_d[rows, :], writes=[ot_b[ob]])
                    for hf in range(2):
                        bi = 2 + (2 * tt + hf) % 4
                        p, pb = pf[bi], pfb[bi]
                        for ft in range(32):
                            S.op("pe", lambda e, p=p, ft=ft, tt=tt, hf=hf: e.matmul(
                                p[:], hidT[:, ft, tt * 128:(tt + 1) * 128], W2[:, ft, hf * 512:(hf + 1) * 512],
                                start=(ft == 0), stop=(ft == 31)), reads=[hid_b[ft], W_b], writes=[pb])
                        hs = slice(hf * 512, (hf + 1) * 512)
                        S.op("dve", lambda e, p=p, hs=hs: e.tensor_tensor(
                            out=tB[:, hs], in0=p[:], in1=tabsB1[:, 2, hs], op=ALU.mult),
                            reads=[pb, tabsB_b1], writes=[tB_b])
                        S.op("dve", lambda e, ob=ob, hs=hs: e.tensor_tensor(
                            out=ot[ob][:, hs], in0=tB[:, hs], in1=ot[ob][:, hs], op=ALU.add),
                            reads=[tB_b, ot_b[ob]], writes=[ot_b[ob]])
                    S.dma("sp", out_d[rows, :], ot[ob][:], reads=[ot_b[ob]])
            S.barrier()
    except _Stop:
        pass
    return nc


_PROGRAM = None


def _prep_shared(inp):
    f = np.float32
    def kt_layout(w):
        K, N = w.shape
        return np.ascontiguousarray(w.reshape(K // 128, 128, N).transpose(1, 0, 2)).astype(f)
    def bc(v, n=128):
        return np.ascontiguousarray(np.broadcast_to(np.asarray(v, f).reshape(1, -1), (n, np.asarray(v).size)))
    def qlay(a):
        a = np.asarray(a, f)
        rest = a.shape[2:]
        return np.ascontiguousarray(a.reshape((16, 2, 64) + rest).transpose((1, 2, 0) + tuple(range(3, 3 + len(rest))))
                                    .reshape((128, 16) + rest))
    sh = {}
    sh["w_ada"] = kt_layout(inp["w_ada"][0])
    sh["b_ada_b"] = bc(inp["b_ada"][0])
    sh["w_in"] = kt_layout(inp["w_in"][0])
    sh["w_out"] = kt_layout(inp["w_out"][0])
    sh["w_glu"] = kt_layout(inp["w_glu"][0])
    sh["w_ff1"] = kt_layout(inp["w_ff1"][0])
    sh["w_ff2"] = kt_layout(inp["w_ff2"][0])
    sh["norm1_g_b"] = bc(inp["norm1_g"][0])
    sh["norm2_g_b"] = bc(inp["norm2_g"][0])
    sh["b_glu_b"] = bc(inp["b_glu"][0])
    sh["gn_ssm_b"] = bc(inp["gn_ssm"][0])
    sh["gn_attn_b"] = bc(inp["gn_attn"][0])
    sh["gq_b"] = bc(inp["q_gain"][0])
    sh["gk_b"] = bc(inp["k_gain"][0])
    sh["gq2"] = np.ascontiguousarray(np.tile(np.asarray(inp["q_gain"][0], f), 2).reshape(128, 1))
    sh["gk2"] = np.ascontiguousarray(np.tile(np.asarray(inp["k_gain"][0], f), 2).reshape(128, 1))
    sh["lamre_q"] = qlay(inp["lam_re"][0])
    sh["lamim_q"] = qlay(inp["lam_im"][0])
    sh["logdt_q"] = qlay(np.broadcast_to(np.asarray(inp["log_dt"][0], f)[:, None], (32, 64)))
    sh["bre_q"] = qlay(inp["ssm_b_re"][0])
    sh["bim_q"] = qlay(inp["ssm_b_im"][0])
    sh["cre_q"] = qlay(np.asarray(inp["ssm_c_re"][0]).transpose(0, 2, 1))
    sh["cim_q"] = qlay(np.asarray(inp["ssm_c_im"][0]).transpose(0, 2, 1))
    dsk = np.asarray(inp["d_skip"][0], f).reshape(32, 16)
    sh["dsk_b"] = np.ascontiguousarray(np.broadcast_to(np.tile(dsk, (1, 8))[None], (128, 32, 128))).astype(f)
    sh["ident"] = np.eye(128, dtype=f)
    r = np.arange(128)
    sh["causal"] = np.where(r[None, :] <= r[:, None], 0.0, -1.0e30).astype(f)
    sh["negi4"] = np.tile(-BIG * np.eye(128, dtype=f), (1, 4)).astype(f)
    ib, jb = r // 16, r // 16
    sh["bmask"] = (jb[None, :] >= ib[:, None]).astype(f)
    sh["dmask"] = (r[None, :] == r[:, None]).astype(f)
    sh["onesblk"] = ((r[None, :] // 64) == (r[:, None] // 64)).astype(f)
    sh["mtab"] = np.ascontiguousarray(np.broadcast_to(np.asarray(EXPS, f)[None, None, :], (128, 16, NE)))
    sh["pow2"] = np.ascontiguousarray(np.broadcast_to((2.0 ** -np.arange(NITER + 2)).astype(f)[None], (128, NITER + 2)))
    zm = np.zeros((128, 2), f)
    zm[:64, 0] = 1.0
    zm[64:, 1] = 1.0
    sh["zmask"] = zm
    return sh


def kernel(**inputs):
    global _PROGRAM
    inp = {k: np.asarray(v) for k, v in inputs.items()}
    if _PROGRAM is None:
        _PROGRAM = build_program()
    nc = _PROGRAM
    shared = _prep_shared(inp)
    x = np.asarray(inp["x"], np.float32)
    c = np.asarray(inp["c"], np.float32)
    in_maps = []
    for core in range(NCORES):
        m = dict(shared)
        m["x"] = np.ascontiguousarray(x[NSEQ * core: NSEQ * (core + 1)].reshape(NSEQ * SEQ, D))
        cc = c[NSEQ * core: NSEQ * (core + 1)]
        cT = cc.reshape(NSEQ, 8, 128).transpose(2, 1, 0)
        m["cT"] = np.ascontiguousarray(np.broadcast_to(cT[:, :, :, None], (128, 8, NSEQ, 128))).astype(np.float32)
        in_maps.append(m)
    res = run_bass_kernel_spmd(nc, in_maps, core_ids=list(range(NCORES)))
    outs = [np.asarray(r["out"], np.float32).reshape(NSEQ, SEQ, D) for r in res.results]
    return np.concatenate(outs, axis=0)
```

```python
import math
from contextlib import ExitStack

import numpy as np
import concourse.bass as bass
import concourse.mybir as mybir
from concourse.alu_op_type import AluOpType as ALU
from concourse.bass_utils import run_bass_kernel_spmd

F32 = mybir.dt.float32
BF16 = mybir.dt.bfloat16
I32 = mybir.dt.int32
AF = mybir.ActivationFunctionType
AX = mybir.AxisListType

NCORES = 8
D = 1024
SEQ = 2048
NSEQ = 2
DIN = 1864
DFF = 4096
EPS = 1e-6
IDX_SCALE = (64 ** -0.5) * (8 ** -0.5)
TOPK = 256
NITER = 14

DVE_BISECT = False
BIG = 30000.0
EXPS = list(range(-7, 9)) + [16, 32, 64, 128, 256, 512, 1024]
NE = len(EXPS)
EIDX = {m: i for i, m in enumerate(EXPS)}
TWO_PI = 2.0 * math.pi


class Buf:
    __slots__ = ("w", "r", "ex")

    def __init__(self, ex=False):
        self.w = None
        self.r = {}
        self.ex = ex


class Sched:
    NDS = 24

    def __init__(self, nc, st):
        self.nc = nc
        self.eng = {"pe": nc.tensor, "act": nc.scalar, "dve": nc.vector, "pool": nc.gpsimd, "sp": nc.sync}
        self.sem = {k: st.enter_context(nc.semaphore("s_" + k)) for k in self.eng}
        self.cnt = {k: 0 for k in self.eng}
        self.waited = {k: {} for k in self.eng}
        self.dsem = [st.enter_context(nc.semaphore("d%d" % i)) for i in range(self.NDS)]
        self.dcnt = [0] * self.NDS
        self.dpool = {"sp": list(range(0, 16)), "pool": list(range(16, 24))}
        self.dnext = {"sp": 0, "pool": 0}

    def _semof(self, key):
        return self.sem[key[1]] if key[0] == "e" else self.dsem[key[1]]

    def _wait(self, e, key, val):
        if key[0] == "e" and key[1] == e and e == "pe":
            return
        if self.waited[e].get(key, 0) >= val:
            return
        self.eng[e].wait_ge(self._semof(key), val)
        self.waited[e][key] = val

    def _deps(self, e, reads, writes):
        for b in reads:
            if b.w is not None:
                self._wait(e, b.w[0], b.w[1])
            if b.ex:
                for k, v in b.r.items():
                    if k != ("e", e):
                        self._wait(e, k, v)
        for b in writes:
            if b.w is not None:
                self._wait(e, b.w[0], b.w[1])
            for k, v in b.r.items():
                self._wait(e, k, v)

    def _mark(self, key, val, reads, writes):
        for b in reads:
            if b.r.get(key, 0) < val:
                b.r[key] = val
        for b in writes:
            b.w = (key, val)
            b.r = {}

    def op(self, e, fn, reads=(), writes=()):
        self._deps(e, reads, writes)
        ins = fn(self.eng[e])
        self.cnt[e] += 1
        ins.then_inc(self.sem[e], 1)
        self._mark(("e", e), self.cnt[e], reads, writes)

    def dma(self, e, out, in_, reads=(), writes=(), **kw):
        self._deps(e, reads, writes)
        pl = self.dpool[e]
        j = pl[self.dnext[e] % len(pl)]
        self.dnext[e] += 1
        if self.dcnt[j] > 0:
            self._wait(e, ("d", j), self.dcnt[j])
        ins = self.eng[e].dma_start(out=out, in_=in_, **kw)
        self.dcnt[j] += 16
        ins.then_inc(self.dsem[j], 16)
        self._mark(("d", j), self.dcnt[j], reads, writes)

    def barrier(self):
        for e in self.eng:
            for k in self.eng:
                if k != e and self.cnt[k] > 0:
                    self._wait(e, ("e", k), self.cnt[k])
            for j in range(self.NDS):
                if self.dcnt[j] > 0:
                    self._wait(e, ("d", j), self.dcnt[j])


class _Stop(Exception):
    pass


def build_program(stop=None):
    nc = bass.Bass("TRN2", target_bir_lowering=False)

    def din(name, shape, dt=F32):
        return nc.dram_tensor(name, list(shape), dt, kind="ExternalInput").ap()

    x_d = din("x", [NSEQ * SEQ, D])
    cT_d = din("cT", [128, 8, NSEQ, 128])
    wada_d = din("w_ada", [128, 8, 6 * D])
    bada_d = din("b_ada_b", [128, 6 * D])
    win_d = din("w_in", [128, 8, DIN])
    wout_d = din("w_out", [128, 8, D])
    wglu_d = din("w_glu", [128, 4, 512])
    wff1_d = din("w_ff1", [128, 8, DFF])
    wff2_d = din("w_ff2", [128, 32, D])
    n1g_d = din("norm1_g_b", [128, D])
    n2g_d = din("norm2_g_b", [128, D])
    bglu_d = din("b_glu_b", [128, 512])
    gns_d = din("gn_ssm_b", [128, 512])
    gna_d = din("gn_attn_b", [128, 512])
    gqb_d = din("gq_b", [128, 64])
    gkb_d = din("gk_b", [128, 64])
    gq2_d = din("gq2", [128, 1])
    gk2_d = din("gk2", [128, 1])
    lre_d = din("lamre_q", [128, 16])
    lim_d = din("lamim_q", [128, 16])
    ldt_d = din("logdt_q", [128, 16])
    bre_d = din("bre_q", [128, 16, 16])
    bim_d = din("bim_q", [128, 16, 16])
    cre_d = din("cre_q", [128, 16, 16])
    cim_d = din("cim_q", [128, 16, 16])
    dsk_d = din("dsk_b", [128, 32, 128])
    ident_d = din("ident", [128, 128])
    causal_d = din("causal", [128, 128])
    negi4_d = din("negi4", [128, 512])
    bmask_d = din("bmask", [128, 128])
    dmask_d = din("dmask", [128, 128])
    onesblk_d = din("onesblk", [128, 128])
    mtab_d = din("mtab", [128, 16, NE])
    pow2_d = din("pow2", [128, NITER + 2])
    zmask_d = din("zmask", [128, 2])
    out_d = nc.dram_tensor("out", [NSEQ * SEQ, D], F32, kind="ExternalOutput").ap()

    try:
      with ExitStack() as top:
        S = Sched(nc, top)

        def chk(name):
            if stop == name:
                S.barrier()
                raise _Stop()

        uid = [0]

        def sb(st, name, shape, dt):
            uid[0] += 1
            return st.enter_context(nc.sbuf_tensor("sb%d_%s" % (uid[0], name), list(shape), dt))

        def ps(st, name, shape, dt):
            uid[0] += 1
            return st.enter_context(nc.psum_tensor("ps%d_%s" % (uid[0], name), list(shape), dt))

        pt = [ps(top, "pt0", [128, 1024], BF16)]
        ptb = [Buf(ex=True)]
        xbank = {}

        def extra_bank(st, as_bf16):
            if as_bf16:
                t = ps(st, "ptx", [128, 1024], BF16)
                del pt[1:], ptb[1:]
                pt.append(t)
                ptb.append(Buf(ex=True))
                rot["pt"] = 0
            else:
                del pt[1:], ptb[1:]
                rot["pt"] = 0
                xbank["t"] = ps(st, "pfx", [128, 512], F32)
                xbank["b"] = Buf(ex=True)
        pf = [ps(top, "pf%d" % i, [128, 512], F32) for i in range(6)]
        pfb = [Buf(ex=True) for _ in range(6)]
        rot = {"pt": 0, "pw": 0}

        def next_pt():
            i = rot["pt"] % len(pt)
            rot["pt"] = i + 1
            return pt[i], ptb[i]

        def next_pw(n=4):
            i = rot["pw"] % n
            rot["pw"] += 1
            return pf[i], pfb[i]

        ident_bf = sb(top, "ident_bf", [128, 128], BF16)
        stC = ExitStack()
        top.enter_context(stC)
        ident_f = sb(stC, "ident_f", [128, 128], F32)
        causal = sb(stC, "causal", [128, 128], F32)
        negi4 = sb(stC, "negi4", [128, 512], BF16)
        onesblk = sb(stC, "onesblk", [128, 128], BF16)
        zeros_bf = sb(stC, "zeros_bf", [128, 260], BF16)
        bglu_b = sb(stC, "bglu_b", [128, 512], F32)
        gns_b = sb(stC, "gns_b", [128, 512], F32)
        gna_b = sb(stC, "gna_b", [128, 512], F32)
        pow2 = sb(stC, "pow2", [128, NITER + 2], F32)
        G2 = sb(stC, "G2", [128, 1], F32)
        negM = sb(stC, "negM", [128, 1], F32)
        taufix = sb(stC, "taufix", [128, 1], F32)
        neghalf = sb(stC, "neghalf", [128, 1], F32)
        siluT = sb(stC, "siluT", [128, 8, NSEQ, 128], BF16)
        cst = Buf()
        for t_, d_ in ((ident_bf, ident_d), (ident_f, ident_d), (causal, causal_d), (negi4, negi4_d),
                       (onesblk, onesblk_d), (bglu_b, bglu_d), (gns_b, gns_d), (gna_b, gna_d), (pow2, pow2_d)):
            S.dma("pool", t_[:], d_, writes=[cst])
        S.op("dve", lambda e: e.memset(zeros_bf[:], 0.0), writes=[cst])
        S.op("dve", lambda e: e.memset(taufix[:], 1.0e29), writes=[cst])
        S.op("dve", lambda e: e.memset(neghalf[:], -0.5), writes=[cst])

        with ExitStack() as st0:
            gqb = sb(st0, "gqb", [128, 64], F32)
            gkb = sb(st0, "gkb", [128, 64], F32)
            gq2 = sb(st0, "gq2", [128, 1], F32)
            gk2 = sb(st0, "gk2", [128, 1], F32)
            cTf = sb(st0, "cTf", [128, 8 * NSEQ * 128], F32)
            tb = Buf()
            S.dma("sp", gqb[:], gqb_d, writes=[tb])
            S.dma("sp", gkb[:], gkb_d, writes=[tb])
            S.dma("sp", gq2[:], gq2_d, writes=[tb])
            S.dma("sp", gk2[:], gk2_d, writes=[tb])
            S.dma("sp", cTf[:], cT_d.rearrange("p a b c -> p (a b c)"), writes=[tb])
            S.op("dve", lambda e: e.scalar_tensor_tensor(out=G2[:], in0=gq2[:], scalar=0.125, in1=gk2[:],
                                                          op0=ALU.mult, op1=ALU.mult), reads=[tb], writes=[cst])
            S.op("dve", lambda e: e.scalar_tensor_tensor(out=gqb[:], in0=gqb[:], scalar=0.125, in1=gkb[:],
                                                          op0=ALU.mult, op1=ALU.mult), reads=[tb], writes=[tb])
            S.op("dve", lambda e: e.tensor_reduce(out=negM[:], in_=gqb[:], axis=AX.X, op=ALU.max,
                                                  apply_absolute_value=True), reads=[tb], writes=[cst])
            S.op("dve", lambda e: e.tensor_scalar(out=negM[:], in0=negM[:], scalar1=-64.0, scalar2=None,
                                                  op0=ALU.mult), reads=[cst], writes=[cst])
            S.op("act", lambda e: e.activation(out=siluT[:].rearrange("p a b c -> p (a b c)"), in_=cTf[:],
                                               func=AF.Silu), reads=[tb], writes=[cst])
            S.barrier()

        chk('consts')
        def adaln_scratch(st):
            wst = [sb(st, "wst%d" % i, [128, 8, 512], BF16) for i in range(2)]
            wstb = [Buf(), Buf()]
            bst = [sb(st, "bst%d" % i, [128, 512], F32) for i in range(2)]
            gst = [sb(st, "gst%d" % i, [128, 512], F32) for i in range(2)]
            tmp = [sb(st, "adat%d" % i, [128, 512], F32) for i in range(2)]
            tmpb = [Buf(), Buf()]
            return wst, wstb, bst, gst, tmp, tmpb

        tabAll_d = nc.dram_tensor("tabAll_scr", [2 * NSEQ, 128, 3 * D], F32, kind="Internal").ap()
        tabAll_b = Buf()

        def adaln(tabs, tabs_b, seqs, first_j, ng_d, scr):
            wst, wstb, bst, gst, tmp, tmpb = scr
            it = 0
            for jj in range(3):
                for hc in range(2):
                    c0 = (first_j + jj) * D + hc * 512
                    b = it % 2
                    it += 1
                    S.dma("pool", wst[b][:], wada_d[:, :, c0:c0 + 512], writes=[wstb[b]])
                    S.dma("sp", bst[b][:], bada_d[:, c0:c0 + 512], writes=[wstb[b]])
                    if jj == 1:
                        S.dma("sp", gst[b][:], ng_d[:, hc * 512:(hc + 1) * 512], writes=[wstb[b]])
                    for n, s in enumerate(seqs):
                        p, pb = next_pw()
                        for kt in range(8):
                            S.op("pe", lambda e, p=p, b=b, kt=kt, s=s: e.matmul(
                                p[:], siluT[:, kt, s, :], wst[b][:, kt, :], start=(kt == 0), stop=(kt == 7)),
                                reads=[cst, wstb[b]], writes=[pb])
                        dst = tabs[n][:, jj, hc * 512:(hc + 1) * 512]
                        if jj == 1:
                            tb_ = tmpb[n]
                            S.op("dve", lambda e, p=p, b=b, n=n: e.tensor_tensor(
                                out=tmp[n][:], in0=p[:], in1=bst[b][:], op=ALU.add),
                                reads=[pb, wstb[b]], writes=[tb_])
                            S.op("dve", lambda e, b=b, n=n, dst=dst: e.scalar_tensor_tensor(
                                out=dst, in0=tmp[n][:], scalar=1.0, in1=gst[b][:], op0=ALU.add, op1=ALU.mult),
                                reads=[tb_, wstb[b]], writes=[tabs_b[n]])
                        else:
                            S.op("dve", lambda e, p=p, b=b, dst=dst: e.tensor_tensor(
                                out=dst, in0=p[:], in1=bst[b][:], op=ALU.add),
                                reads=[pb, wstb[b]], writes=[tabs_b[n]])

        with ExitStack() as stA:
            tabsA1 = sb(stA, "tabsA", [128, 3, D], F32)
            tabsA = [tabsA1, tabsA1]
            tabsA_b1 = Buf()
            tabsA_b = [tabsA_b1, tabsA_b1]

            ssmA_d = nc.dram_tensor("ssmA_scr", [128, 32, 128], BF16, kind="Internal").ap()
            ssmB_d = nc.dram_tensor("ssmB_scr", [128, 32, 128], BF16, kind="Internal").ap()
            ssmC_d = nc.dram_tensor("ssmC_scr", [128, 32, 2, 128], BF16, kind="Internal").ap()
            ssmd_b = Buf()
            ak = sb(stA, "ak", [128, 16, 8], F32)
            ck = sb(stA, "ck", [128, 16, 8], F32)
            nck = sb(stA, "nck", [128, 16, 8], F32)
            ssm_b = Buf()
            with ExitStack() as st1:
                extra_bank(st1, True)
                A_sb = sb(st1, "A_sb", [128, 32, 128], BF16)
                Bm_sb = sb(st1, "Bm_sb", [128, 32, 128], BF16)
                Cmz = sb(st1, "Cmz", [128, 32, 2, 128], BF16)

                def t3(name, n):
                    return sb(st1, name, [128, 16, n], F32)
                lre = t3("lre", 1); lim = t3("lim", 1); ldt = t3("ldt", 1)
                bre = t3("bre", 16); bim = t3("bim", 16); cre = t3("cre", 16); cim = t3("cim", 16)
                mtab = t3("mtab", NE)
                bmask = sb(st1, "bmask", [128, 128], F32)
                dmask = sb(st1, "dmask", [128, 128], F32)
                zmask = sb(st1, "zmask", [128, 2], F32)
                dsk = sb(st1, "dsk", [128, 32, 128], F32)
                ib = Buf()
                for t_, d_ in ((lre, lre_d), (lim, lim_d), (ldt, ldt_d)):
                    S.dma("sp", t_[:, :, 0], d_, writes=[ib])
                for t_, d_ in ((bre, bre_d), (bim, bim_d), (cre, cre_d), (cim, cim_d), (mtab, mtab_d),
                               (bmask, bmask_d), (dmask, dmask_d), (dsk, dsk_d), (zmask, zmask_d)):
                    S.dma("sp", t_[:], d_, writes=[ib])
                chk('ssm_a')
                dt_ = t3("dt_", 1); aa = t3("aa", 1); th = t3("th", 1)
                ang = t3("ang", NE); lmag = t3("lmag", NE); mag = t3("mag", NE)
                tq = t3("tq", NE); tqi = sb(st1, "tqi", [128, 16, NE], I32); tqf = t3("tqf", NE)
                wr = t3("wr", NE); sn = t3("sn", NE); cs = t3("cs", NE)
                pr_ = t3("pr_", NE); pi_ = t3("pi_", NE)
                wb = Buf()

                def V(fn, reads=(), writes=(wb,)):
                    S.op("dve", fn, reads=[ib, wb] + list(reads), writes=list(writes))

                def ACT(fn, reads=(), writes=(wb,)):
                    S.op("act", fn, reads=[ib, wb] + list(reads), writes=list(writes))

                ACT(lambda e: e.activation(out=dt_[:], in_=ldt[:], func=AF.Exp))
                V(lambda e: e.tensor_tensor(out=aa[:], in0=lre[:], in1=dt_[:], op=ALU.mult))
                V(lambda e: e.tensor_tensor(out=th[:], in0=lim[:], in1=dt_[:], op=ALU.mult))
                V(lambda e: e.tensor_tensor(out=lmag[:], in0=mtab[:], in1=aa[:].to_broadcast([128, 16, NE]), op=ALU.mult))
                V(lambda e: e.tensor_tensor(out=ang[:], in0=mtab[:], in1=th[:].to_broadcast([128, 16, NE]), op=ALU.mult))
                ACT(lambda e: e.activation(out=mag[:], in_=lmag[:], func=AF.Exp))
                V(lambda e: e.tensor_scalar(out=tq[:], in0=ang[:], scalar1=1.0 / TWO_PI, scalar2=None, op0=ALU.mult))
                V(lambda e: e.tensor_copy(out=tqi[:], in_=tq[:]))
                V(lambda e: e.tensor_copy(out=tqf[:], in_=tqi[:]))
                V(lambda e: e.scalar_tensor_tensor(out=wr[:], in0=tqf[:], scalar=-TWO_PI, in1=ang[:],
                                                   op0=ALU.mult, op1=ALU.add))
                wt_ = t3("wt_", NE)
                for t_, shift in ((sn, 0.0), (cs, math.pi / 2)):
                    V(lambda e, t_=t_, shift=shift: e.tensor_scalar(out=t_[:], in0=wr[:], scalar1=shift, scalar2=None, op0=ALU.add))
                    V(lambda e, t_=t_: e.tensor_scalar(out=wt_[:], in0=t_[:], scalar1=math.pi, scalar2=-TWO_PI,
                                                       op0=ALU.is_gt, op1=ALU.mult))
                    V(lambda e, t_=t_: e.tensor_scalar(out=tq[:], in0=t_[:], scalar1=-math.pi, scalar2=TWO_PI,
                                                       op0=ALU.is_lt, op1=ALU.mult))
                    V(lambda e, t_=t_: e.tensor_tensor(out=t_[:], in0=t_[:], in1=wt_[:], op=ALU.add))
                    V(lambda e, t_=t_: e.tensor_tensor(out=t_[:], in0=t_[:], in1=tq[:], op=ALU.add))
                for t_ in (sn, cs):
                    V(lambda e, t_=t_: e.tensor_scalar(out=t_[:], in0=t_[:], scalar1=3.14159, scalar2=-3.14159,
                                                       op0=ALU.min, op1=ALU.max))
                ACT(lambda e: e.activation(out=sn[:], in_=sn[:], func=AF.Sin))
                ACT(lambda e: e.activation(out=cs[:], in_=cs[:], func=AF.Sin))
                V(lambda e: e.tensor_tensor(out=pr_[:], in0=mag[:], in1=cs[:], op=ALU.mult))
                V(lambda e: e.tensor_tensor(out=pi_[:], in0=mag[:], in1=sn[:], op=ALU.mult))
                chk('ssm_b')
                i1 = EIDX[1]
                nr = t3("nr", 1); den = t3("den", 1); t1_ = t3("t1_", 1); gr = t3("gr", 1); gi = t3("gi", 1)
                V(lambda e: e.tensor_scalar(out=nr[:], in0=pr_[:, :, i1:i1 + 1], scalar1=-1.0, scalar2=None, op0=ALU.add))
                V(lambda e: e.tensor_tensor(out=den[:], in0=lre[:], in1=lre[:], op=ALU.mult))
                V(lambda e: e.tensor_tensor(out=t1_[:], in0=lim[:], in1=lim[:], op=ALU.mult))
                V(lambda e: e.tensor_tensor(out=den[:], in0=den[:], in1=t1_[:], op=ALU.add))
                V(lambda e: e.reciprocal(out=den[:], in_=den[:]))
                V(lambda e: e.tensor_tensor(out=gr[:], in0=nr[:], in1=lre[:], op=ALU.mult))
                V(lambda e: e.tensor_tensor(out=t1_[:], in0=pi_[:, :, i1:i1 + 1], in1=lim[:], op=ALU.mult))
                V(lambda e: e.tensor_tensor(out=gr[:], in0=gr[:], in1=t1_[:], op=ALU.add))
                V(lambda e: e.tensor_tensor(out=gr[:], in0=gr[:], in1=den[:], op=ALU.mult))
                V(lambda e: e.tensor_tensor(out=gi[:], in0=pi_[:, :, i1:i1 + 1], in1=lre[:], op=ALU.mult))
                V(lambda e: e.tensor_tensor(out=t1_[:], in0=nr[:], in1=lim[:], op=ALU.mult))
                V(lambda e: e.tensor_tensor(out=gi[:], in0=gi[:], in1=t1_[:], op=ALU.subtract))
                V(lambda e: e.tensor_tensor(out=gi[:], in0=gi[:], in1=den[:], op=ALU.mult))
                for k in range(8):
                    ii = EIDX[8 * (2 ** k)]
                    V(lambda e, k=k, ii=ii: e.tensor_copy(out=ak[:, :, k:k + 1], in_=pr_[:, :, ii:ii + 1]), writes=[wb, ssm_b])
                    V(lambda e, k=k, ii=ii: e.tensor_copy(out=ck[:, :, k:k + 1], in_=pi_[:, :, ii:ii + 1]), writes=[wb, ssm_b])
                    V(lambda e, k=k, ii=ii: e.tensor_scalar(out=nck[:, :, k:k + 1], in0=pi_[:, :, ii:ii + 1], scalar1=-1.0,
                                                            scalar2=None, op0=ALU.mult), writes=[wb, ssm_b])
                PBr = t3("PBr", 8); PBi = t3("PBi", 8); tt8 = t3("tt8", 8)
                e7 = EIDX[0]
                sl07 = slice(e7, e7 + 8)
                V(lambda e: e.tensor_tensor(out=PBr[:], in0=pr_[:, :, sl07], in1=gr[:].to_broadcast([128, 16, 8]), op=ALU.mult))
                V(lambda e: e.tensor_tensor(out=tt8[:], in0=pi_[:, :, sl07], in1=gi[:].to_broadcast([128, 16, 8]), op=ALU.mult))
                V(lambda e: e.tensor_tensor(out=PBr[:], in0=PBr[:], in1=tt8[:], op=ALU.subtract))
                V(lambda e: e.tensor_tensor(out=PBi[:], in0=pr_[:, :, sl07], in1=gi[:].to_broadcast([128, 16, 8]), op=ALU.mult))
                V(lambda e: e.tensor_tensor(out=tt8[:], in0=pi_[:, :, sl07], in1=gr[:].to_broadcast([128, 16, 8]), op=ALU.mult))
                V(lambda e: e.tensor_tensor(out=PBi[:], in0=PBi[:], in1=tt8[:], op=ALU.add))
                BmTr = sb(st1, "BmTr", [128, 16, 8, 16], F32)
                BmTi = sb(st1, "BmTi", [128, 16, 8, 16], F32)
                t816 = sb(st1, "t816", [128, 16, 8, 16], F32)
                for i in range(8):
                    m = 7 - i
                    def bc(t_, m=m):
                        return t_[:, :, m:m + 1].to_broadcast([128, 16, 16])
                    V(lambda e, i=i, bc=bc: e.tensor_tensor(out=BmTr[:, :, i, :], in0=bre[:], in1=bc(PBr), op=ALU.mult))
                    V(lambda e, i=i, bc=bc: e.tensor_tensor(out=t816[:, :, i, :], in0=bim[:], in1=bc(PBi), op=ALU.mult))
                    V(lambda e, i=i, bc=bc: e.tensor_tensor(out=BmTi[:, :, i, :], in0=bim[:], in1=bc(PBr), op=ALU.mult))
                V(lambda e: e.tensor_tensor(out=BmTr[:], in0=BmTr[:], in1=t816[:], op=ALU.subtract))
                for i in range(8):
                    m = 7 - i
                    V(lambda e, i=i, m=m: e.tensor_tensor(out=t816[:, :, i, :], in0=bre[:],
                                                          in1=PBi[:, :, m:m + 1].to_broadcast([128, 16, 16]), op=ALU.mult))
                V(lambda e: e.tensor_tensor(out=BmTi[:], in0=BmTi[:], in1=t816[:], op=ALU.add))
                Wcr = sb(st1, "Wcr", [128, 16, 8, 16], F32)
                Wci = sb(st1, "Wci", [128, 16, 8, 16], F32)
                Cmr = sb(st1, "Cmr", [128, 16, 8, 16], F32)
                Cmi = sb(st1, "Cmi", [128, 16, 8, 16], F32)
                for (dr, di, off) in ((Wcr, Wci, -7), (Cmr, Cmi, 1)):
                    for j in range(8):
                        ii = EIDX[j + off]
                        def bc2(t_, ii=ii):
                            return t_[:, :, ii:ii + 1].to_broadcast([128, 16, 16])
                        V(lambda e, j=j, bc2=bc2, dr=dr: e.tensor_tensor(out=dr[:, :, j, :], in0=cre[:], in1=bc2(pr_), op=ALU.mult))
                        V(lambda e, j=j, bc2=bc2: e.tensor_tensor(out=t816[:, :, j, :], in0=cim[:], in1=bc2(pi_), op=ALU.mult))
                        V(lambda e, j=j, bc2=bc2, di=di: e.tensor_tensor(out=di[:, :, j, :], in0=cre[:], in1=bc2(pi_), op=ALU.mult))
                    V(lambda e, dr=dr: e.tensor_tensor(out=dr[:], in0=dr[:], in1=t816[:], op=ALU.subtract))
                    for j in range(8):
                        ii = EIDX[j + off]
                        V(lambda e, j=j, ii=ii: e.tensor_tensor(out=t816[:, :, j, :], in0=cim[:],
                                                                in1=pr_[:, :, ii:ii + 1].to_broadcast([128, 16, 16]), op=ALU.mult))
                    V(lambda e, di=di: e.tensor_tensor(out=di[:], in0=di[:], in1=t816[:], op=ALU.add))
                    V(lambda e, di=di: e.tensor_scalar(out=di[:], in0=di[:], scalar1=-1.0, scalar2=None, op0=ALU.mult))
                chk('ssm_c')
                for pr in range(16):
                    for gp in range(2):
                        g = 2 * pr + gp
                        for ri, src in ((0, Cmr), (1, Cmi)):
                            V(lambda e, g=g, ri=ri, src=src, pr=pr, gp=gp: e.tensor_scalar(
                                out=Cmz[:, g, ri, :], in0=src[:, pr, :, :].rearrange("p a b -> p (a b)"),
                                scalar1=zmask[:, gp:gp + 1], scalar2=None, op0=ALU.mult), writes=[wb, ssm_b])
                chk('ssm_d')
                Bz = [sb(st1, "Bz%d" % i, [128, 2, 128], BF16) for i in range(2)]
                Wz = [sb(st1, "Wz%d" % i, [128, 2, 128], BF16) for i in range(2)]
                Bzb = [Buf(), Buf()]
                At = [sb(st1, "At%d" % i, [128, 128], F32) for i in range(2)]
                Atb = [Buf(), Buf()]
                for pr in range(16):
                    for gp in range(2):
                        g = 2 * pr + gp
                        b = g % 2
                        for ri, src in ((0, BmTr), (1, BmTi)):
                            V(lambda e, b=b, ri=ri, src=src, pr=pr, gp=gp: e.tensor_scalar(
                                out=Bz[b][:, ri, :], in0=src[:, pr, :, :].rearrange("p a b -> p (a b)"),
                                scalar1=zmask[:, gp:gp + 1], scalar2=None, op0=ALU.mult), writes=[wb, Bzb[b]])
                        for ri, src in ((0, Wcr), (1, Wci)):
                            V(lambda e, b=b, ri=ri, src=src, pr=pr, gp=gp: e.tensor_scalar(
                                out=Wz[b][:, ri, :], in0=src[:, pr, :, :].rearrange("p a b -> p (a b)"),
                                scalar1=zmask[:, gp:gp + 1], scalar2=None, op0=ALU.mult), writes=[wb, Bzb[b]])
                        ptt, ptb_ = next_pt()
                        for ri in range(2):
                            S.op("pe", lambda e, ptt=ptt, b=b, ri=ri: e.transpose(
                                out=ptt[:, ri * 128:(ri + 1) * 128], in_=Bz[b][:, ri, :], identity=ident_bf[:]),
                                reads=[Bzb[b], cst], writes=[ptb_])
                        for ri in range(2):
                            S.op("act", lambda e, ptt=ptt, g=g, ri=ri, gp=gp: e.copy(
                                out=Bm_sb[:, g, ri * 64:(ri + 1) * 64],
                                in_=ptt[:, ri * 128 + gp * 64: ri * 128 + gp * 64 + 64]),
                                reads=[ptb_], writes=[ssm_b])
                        p, pb = next_pw()
                        for ri in range(2):
                            S.op("pe", lambda e, p=p, b=b, ri=ri: e.matmul(
                                p[:, 0:128], Bz[b][:, ri, :], Wz[b][:, ri, :], start=(ri == 0), stop=(ri == 1)),
                                reads=[Bzb[b]], writes=[pb])
                        S.op("dve", lambda e, p=p, b=b: e.tensor_tensor(out=At[b][:], in0=p[:, 0:128], in1=bmask[:], op=ALU.mult),
                             reads=[pb, ib], writes=[Atb[b]])
                        S.op("dve", lambda e, b=b, g=g: e.tensor_tensor(out=dsk[:, g, :], in0=dsk[:, g, :], in1=dmask[:], op=ALU.mult),
                             reads=[ib], writes=[ib])
                        S.op("dve", lambda e, b=b, g=g: e.tensor_tensor(out=A_sb[:, g, :], in0=At[b][:], in1=dsk[:, g, :], op=ALU.add),
                             reads=[Atb[b], ib], writes=[ssm_b])
                chk('ssm_e')
                S.dma("sp", ssmA_d, A_sb[:], reads=[ssm_b], writes=[ssmd_b])
                S.dma("sp", ssmB_d, Bm_sb[:], reads=[ssm_b], writes=[ssmd_b])
                S.dma("sp", ssmC_d, Cmz[:], reads=[ssm_b], writes=[ssmd_b])
                scr = adaln_scratch(st1)
                tl = [tabsA1, sb(st1, "tabt1", [128, 3, D], F32)]
                tlb = [tabsA_b1, Buf()]
                for half, ngd in ((0, n1g_d), (1, n2g_d)):
                    adaln(tl, tlb, list(range(NSEQ)), 3 * half, ngd, scr)
                    for q in range(NSEQ):
                        S.dma("sp", tabAll_d[NSEQ * half + q], tl[q][:].rearrange("p a b -> p (a b)"),
                              reads=[tlb[q]], writes=[tabAll_b])
                S.barrier()

            chk('ssmsetup')
            for s in range(NSEQ):
                r0 = s * SEQ
                S.dma("sp", tabsA1[:].rearrange("p a b -> p (a b)"), tabAll_d[s], reads=[tabAll_b], writes=[tabsA_b1])
                chk('adaln')
                with ExitStack() as stS:
                    qT = sb(stS, "qT", [128, 4, SEQ], BF16)
                    kTz = sb(stS, "kTz", [128, 2, 2, SEQ], BF16)
                    qiT = sb(stS, "qiT", [128, 4, SEQ], BF16)
                    kiTz = sb(stS, "kiTz", [128, 2, SEQ], BF16)
                    Vp = sb(stS, "Vp", [128, 16, 2, 65], BF16)
                    wi = sb(stS, "wi", [128, 16, 8], F32)
                    mixT_s = sb(stS, "mixT_s", [128, 4, SEQ], BF16)
                    qT_b, kT_b, qiT_b, kiT_b, Vp_b, wi_b, mixT_sb = (Buf() for _ in range(7))
                    S.op("pool", lambda e: e.memset(kTz[:].rearrange("p a b c -> p (a b c)"), 0.0), writes=[kT_b])
                    S.op("pool", lambda e: e.memset(kiTz[:].rearrange("p a c -> p (a c)"), 0.0), writes=[kiT_b])
                    S.op("pool", lambda e: e.memset(Vp[:].rearrange("p a b c -> p (a b c)"), 1.0), writes=[Vp_b])

                    with ExitStack() as st12:
                        U8 = sb(st12, "U8", [128, 2, 32, 8, 16], BF16)
                        U8_b = Buf()
                        with ExitStack() as st1:
                            extra_bank(st1, True)
                            w_in = sb(st1, "w_in", [128, 8, DIN], BF16)
                            w_in_b = Buf()
                            for kt in range(8):
                                S.dma("pool", w_in[:, kt, :], win_d[:, kt, :], writes=[w_in_b])
                            hT = sb(st1, "hT", [128, 8, SEQ], BF16)
                            hT_b = Buf()
                            st1a = ExitStack()
                            xt = [sb(st1a, "xt%d" % i, [128, D], F32) for i in range(2)]
                            xt_b = [Buf(), Buf()]
                            junk = sb(st1a, "junk", [128, D], BF16)
                            junk_b = Buf()
                            t1 = sb(st1a, "t1", [128, D], F32)
                            hb = [sb(st1a, "hb%d" % i, [128, D], BF16) for i in range(2)]
                            hb_b = [Buf(), Buf()]
                            st_ = [sb(st1a, "st%d" % i, [128, 4], F32) for i in range(2)]
                            st_b = [Buf(), Buf()]
                            t1_b = Buf()
                            for t in range(16):
                                b = t % 2
                                S.dma("sp", xt[b][:], x_d[r0 + t * 128: r0 + (t + 1) * 128, :], writes=[xt_b[b]])
                                S.op("act", lambda e, b=b: e.activation(out=junk[:], in_=xt[b][:], func=AF.Square,
                                                                        accum_out=st_[b][:, 0:1]),
                                     reads=[xt_b[b]], writes=[junk_b, st_b[b]])
                                S.op("act", lambda e, b=b: e.activation(out=st_[b][:, 1:2], in_=st_[b][:, 0:1], func=AF.Sqrt,
                                                                        bias=EPS, scale=1.0 / D),
                                     reads=[st_b[b]], writes=[st_b[b]])
                                S.op("dve", lambda e, b=b: e.reciprocal(out=st_[b][:, 2:3], in_=st_[b][:, 1:2]),
                                     reads=[st_b[b]], writes=[st_b[b]])
                                S.op("dve", lambda e, b=b: e.scalar_tensor_tensor(
                                    out=t1[:], in0=xt[b][:], scalar=st_[b][:, 2:3], in1=tabsA[s][:, 1, :],
                                    op0=ALU.mult, op1=ALU.mult), reads=[xt_b[b], st_b[b], tabsA_b[s]], writes=[t1_b])
                                S.op("dve", lambda e, b=b: e.tensor_tensor(out=hb[b][:], in0=t1[:], in1=tabsA[s][:, 0, :], op=ALU.add),
                                     reads=[t1_b, tabsA_b[s]], writes=[hb_b[b]])
                                ptt, ptb_ = next_pt()
                                for kt in range(8):
                                    S.op("pe", lambda e, ptt=ptt, b=b, kt=kt: e.transpose(
                                        out=ptt[:, kt * 128:(kt + 1) * 128], in_=hb[b][:, kt * 128:(kt + 1) * 128],
                                        identity=ident_bf[:]), reads=[hb_b[b], cst], writes=[ptb_])
                                S.op("act", lambda e, ptt=ptt, t=t: e.copy(
                                    out=hT[:, :, t * 128:(t + 1) * 128], in_=ptt[:].rearrange("p (a b) -> p a b", b=128)),
                                    reads=[ptb_], writes=[hT_b])
                            S.barrier()
                            st1a.close()
                            sq = [sb(st1, "sq%d" % i, [128, 512], BF16) for i in range(2)]
                            sq_b = [Buf(), Buf()]
                            sd = [sb(st1, "sd%d" % i, [128, 512], F32) for i in range(2)]
                            sd_b = [Buf(), Buf()]
                            groups = []
                            for j in range(4):
                                groups.append(("q", j, [(512 + j * 128, 128, 0)]))
                            groups.append(("kA", 0, [(1024, 128, 0)]))
                            groups.append(("kB", 0, [(1088, 64, 0), (1024, 64, 64)]))
                            for j in range(4):
                                groups.append(("qi", j, [(1280 + j * 128, 128, 0)]))
                            groups.append(("ki", 0, [(1792, 64, 0), (1792, 64, 64)]))
                            it = 0
                            for kind, j, parts in groups:
                                for c in range(4):
                                    cs_ = slice(c * 512, (c + 1) * 512)
                                    p, pb = next_pw()
                                    for (c0, m, po) in parts:
                                        for kt in range(8):
                                            S.op("pe", lambda e, p=p, c0=c0, m=m, po=po, kt=kt, cs_=cs_: e.matmul(
                                                p[po:po + m, :], w_in[:, kt, c0:c0 + m], hT[:, kt, cs_],
                                                start=(kt == 0), stop=(kt == 7)),
                                                reads=[w_in_b, hT_b], writes=[pb])
                                    if kind == "qi":
                                        S.op("act", lambda e, p=p, j=j, cs_=cs_: e.copy(out=qiT[:, j, cs_], in_=p[:]),
                                             reads=[pb], writes=[qiT_b])
                                    elif kind == "ki":
                                        for half in range(2):
                                            rs = slice(half * 64, half * 64 + 64)
                                            S.op("act", lambda e, p=p, half=half, rs=rs, cs_=cs_: e.copy(
                                                out=kiTz[rs, half, cs_], in_=p[rs, :]), reads=[pb], writes=[kiT_b])
                                    else:
                                        b = it % 2
                                        it += 1
                                        S.op("act", lambda e, p=p, b=b: e.activation(out=sq[b][:], in_=p[:], func=AF.Square),
                                             reads=[pb], writes=[sq_b[b]])
                                        p2, p2b = next_pw()
                                        S.op("pe", lambda e, p2=p2, b=b: e.matmul(p2[:], onesblk[:], sq[b][:], start=True, stop=True),
                                             reads=[sq_b[b], cst], writes=[p2b])
                                        S.op("act", lambda e, p2=p2, b=b: e.activation(out=sd[b][:], in_=p2[:], func=AF.Sqrt,
                                                                                      bias=EPS, scale=1.0 / 64),
                                             reads=[p2b], writes=[sd_b[b]])
                                        S.op("dve", lambda e, b=b: e.reciprocal(out=sd[b][:], in_=sd[b][:]),
                                             reads=[sd_b[b]], writes=[sd_b[b]])
                                        if kind == "q":
                                            S.op("dve", lambda e, p=p, b=b, j=j, cs_=cs_: e.scalar_tensor_tensor(
                                                out=qT[:, j, cs_], in0=p[:], scalar=G2[:, 0:1], in1=sd[b][:],
                                                op0=ALU.mult, op1=ALU.mult), reads=[pb, sd_b[b], cst], writes=[qT_b])
                                        else:
                                            kvs = (0, 1) if kind == "kA" else (1, 0)
                                            for half in range(2):
                                                rs = slice(half * 64, half * 64 + 64)
                                                kv = kvs[half]
                                                S.op("dve", lambda e, p=p, b=b, rs=rs, kv=kv, half=half, cs_=cs_: e.tensor_tensor(
                                                    out=kTz[rs, kv, half, cs_], in0=p[rs, :], in1=sd[b][rs, :], op=ALU.mult),
                                                    reads=[pb, sd_b[b]], writes=[kT_b])
                            for t in range(16):
                                ts_ = slice(t * 128, (t + 1) * 128)
                                p, pb = next_pw()
                                for kt in range(8):
                                    S.op("pe", lambda e, p=p, kt=kt, ts_=ts_: e.matmul(
                                        p[:, 0:128], hT[:, kt, ts_], w_in[:, kt, 1152:1280], start=(kt == 0), stop=(kt == 7)),
                                        reads=[w_in_b, hT_b], writes=[pb])
                                for kt in range(8):
                                    S.op("pe", lambda e, p=p, kt=kt, ts_=ts_: e.matmul(
                                        p[:, 128:136], hT[:, kt, ts_], w_in[:, kt, 1856:1864], start=(kt == 0), stop=(kt == 7)),
                                        reads=[w_in_b, hT_b], writes=[pb])
                                S.op("act", lambda e, p=p, t=t: e.copy(
                                    out=Vp[:, t, :, 0:64], in_=p[:, 0:128].rearrange("p (a b) -> p a b", b=64)),
                                    reads=[pb], writes=[Vp_b])
                                S.op("act", lambda e, p=p, t=t: e.mul(out=wi[:, t, :], in_=p[:, 128:136], mul=IDX_SCALE),
                                     reads=[pb], writes=[wi_b])
                            for sp in range(2):
                                for i in range(8):
                                    p, pb = next_pw()
                                    for kt in range(8):
                                        lhs = hT[:, kt, sp * 1024:(sp + 1) * 1024].rearrange("p (b i) -> p i b", i=8)[:, i, :]
                                        S.op("pe", lambda e, p=p, lhs=lhs, kt=kt: e.matmul(
                                            p[:], lhs, w_in[:, kt, 0:512], start=(kt == 0), stop=(kt == 7)),
                                            reads=[w_in_b, hT_b], writes=[pb])
                                    S.op("act", lambda e, p=p, sp=sp, i=i: e.copy(
                                        out=U8[:, sp, :, i, :], in_=p[:].rearrange("p (g c) -> p g c", c=16)),
                                         reads=[pb], writes=[U8_b])
                            S.barrier()

                        chk('s1')
                        with ExitStack() as st2:
                            extra_bank(st2, True)
                            w_glu = sb(st2, "w_glu", [128, 4, 512], BF16)
                            w_glu_b = Buf()
                            S.dma("pool", w_glu[:], wglu_d, writes=[w_glu_b])
                            Ytok = sb(st2, "Ytok", [128, 2, 8, 512], F32)
                            Ytok_b = Buf()
                            U8T = [sb(st2, "U8T%d" % i, [128, 2, 256], BF16) for i in range(2)]
                            U8T_b = [Buf(), Buf()]
                            XA = [sb(st2, "XA%d" % i, [128, 2, 384], F32) for i in range(2)]
                            XB = [sb(st2, "XB%d" % i, [128, 2, 384], F32) for i in range(2)]
                            XA_b, XB_b = [Buf(), Buf()], [Buf(), Buf()]
                            TM = [sb(st2, "TM%d" % i, [128, 2, 256], F32) for i in range(2)]
                            TM_b = [Buf(), Buf()]
                            Xst = [sb(st2, "Xst%d" % i, [128, 2, 258], BF16) for i in range(2)]
                            Xst_b = [Buf(), Buf()]
                            Ysb = [sb(st2, "Ysb%d" % i, [128, 256], F32) for i in range(2)]
                            Ysb_b = [Buf(), Buf()]
                            for i in range(2):
                                S.op("pool", lambda e, i=i: e.memset(XA[i][:].rearrange("p a b -> p (a b)"), 0.0), writes=[XA_b[i]])
                                S.op("pool", lambda e, i=i: e.memset(XB[i][:].rearrange("p a b -> p (a b)"), 0.0), writes=[XB_b[i]])
                                S.op("pool", lambda e, i=i: e.memset(Xst[i][:].rearrange("p a b -> p (a b)"), 0.0), writes=[Xst_b[i]])
                            PAD = 128
                            A2 = [sb(st2, "A2_%d" % i, [128, 2, 128], BF16) for i in range(2)]
                            B2 = [sb(st2, "B2_%d" % i, [128, 2, 128], BF16) for i in range(2)]
                            C2 = [sb(st2, "C2_%d" % i, [128, 2, 2, 128], BF16) for i in range(2)]
                            M2_b = [Buf(), Buf()]

                            def gen_pair(pr):
                                ub = pr % 2
                                S.dma("sp", A2[ub][:], ssmA_d[:, 2 * pr:2 * pr + 2, :], reads=[ssmd_b], writes=[M2_b[ub]])
                                S.dma("sp", B2[ub][:], ssmB_d[:, 2 * pr:2 * pr + 2, :], reads=[ssmd_b], writes=[M2_b[ub]])
                                S.dma("sp", C2[ub][:], ssmC_d[:, 2 * pr:2 * pr + 2, :, :], reads=[ssmd_b], writes=[M2_b[ub]])
                                ptt, ptb_ = next_pt()
                                for gp in range(2):
                                    g = 2 * pr + gp
                                    for sp in range(2):
                                        S.op("pe", lambda e, ptt=ptt, gp=gp, sp=sp, g=g: e.transpose(
                                            out=ptt[:, (gp * 2 + sp) * 128:(gp * 2 + sp + 1) * 128],
                                            in_=U8[:, sp, g, :, :].rearrange("p a b -> p (a b)"), identity=ident_bf[:]),
                                            reads=[U8_b, cst], writes=[ptb_])
                                S.op("act", lambda e, ptt=ptt, ub=ub: e.copy(
                                    out=U8T[ub][:].rearrange("p a b -> p (a b)"), in_=ptt[:, 0:512]),
                                    reads=[ptb_], writes=[U8T_b[ub]])
                                p, pb = next_pw()
                                for gp in range(2):
                                    g = 2 * pr + gp
                                    for ri in range(2):
                                        S.op("pe", lambda e, p=p, gp=gp, g=g, ri=ri, ub=ub: e.matmul(
                                            p[gp * 64:(gp + 1) * 64, ri * 256:(ri + 1) * 256],
                                            B2[ub][:, gp, ri * 64:(ri + 1) * 64], U8T[ub][:, gp, :], start=True, stop=True),
                                            reads=[M2_b[ub], U8T_b[ub]], writes=[pb])
                                S.op("act", lambda e, p=p: e.copy(out=XA[ub][:, :, PAD:PAD + 256],
                                                                  in_=p[:].rearrange("p (a b) -> p a b", b=256)),
                                     reads=[pb], writes=[XA_b[ub]])
                                yield
                                cur, curb, nxt, nxtb = XA[ub], XA_b[ub], XB[ub], XB_b[ub]
                                xs = Xst[ub]
                                tm, tmb = TM[ub], TM_b[ub]
                                for k in range(8):
                                    sft = 2 ** k
                                    last = (k == 7)
                                    a_ = ak[:, pr, k:k + 1]
                                    c_ = ck[:, pr, k:k + 1]
                                    nc_ = nck[:, pr, k:k + 1]
                                    sh = slice(PAD - sft, PAD - sft + 256)
                                    ce = slice(PAD, PAD + 256)
                                    outr = xs[:, 0, 1:257] if last else nxt[:, 0, ce]
                                    outi = xs[:, 1, 1:257] if last else nxt[:, 1, ce]
                                    ob = Xst_b[ub] if last else nxtb
                                    S.op("dve", lambda e, cur=cur, a_=a_, sh=sh, ce=ce: e.scalar_tensor_tensor(
                                        out=tm[:, 0, :], in0=cur[:, 0, sh], scalar=a_, in1=cur[:, 0, ce], op0=ALU.mult, op1=ALU.add),
                                        reads=[curb, ssm_b], writes=[tmb])
                                    S.op("dve", lambda e, cur=cur, a_=a_, sh=sh, ce=ce: e.scalar_tensor_tensor(
                                        out=tm[:, 1, :], in0=cur[:, 1, sh], scalar=a_, in1=cur[:, 1, ce], op0=ALU.mult, op1=ALU.add),
                                        reads=[curb, ssm_b], writes=[tmb])
                                    yield
                                    S.op("dve", lambda e, cur=cur, nc_=nc_, sh=sh, outr=outr: e.scalar_tensor_tensor(
                                        out=outr, in0=cur[:, 1, sh], scalar=nc_, in1=tm[:, 0, :], op0=ALU.mult, op1=ALU.add),
                                        reads=[curb, tmb, ssm_b], writes=[ob])
                                    S.op("dve", lambda e, cur=cur, c_=c_, sh=sh, outi=outi: e.scalar_tensor_tensor(
                                        out=outi, in0=cur[:, 0, sh], scalar=c_, in1=tm[:, 1, :], op0=ALU.mult, op1=ALU.add),
                                        reads=[curb, tmb, ssm_b], writes=[ob])
                                    yield
                                    cur, curb, nxt, nxtb = nxt, nxtb, cur, curb
                                for gp in range(2):
                                    g = 2 * pr + gp
                                    yb = g % 2
                                    p, pb = next_pw()
                                    S.op("pe", lambda e, p=p, g=g, gp=gp, ub=ub: e.matmul(
                                        p[:, 0:256], A2[ub][:, gp, :], U8T[ub][:, gp, :], start=True, stop=False),
                                        reads=[M2_b[ub], U8T_b[ub]], writes=[pb])
                                    for ri in range(2):
                                        S.op("pe", lambda e, p=p, g=g, ri=ri, xs=xs: e.matmul(
                                            p[:, 0:256], C2[ub][:, gp, ri, :], xs[:, ri, 0:256], start=False, stop=(ri == 1)),
                                            reads=[M2_b[ub], Xst_b[ub]], writes=[pb])
                                    S.op("act", lambda e, p=p, yb=yb: e.copy(out=Ysb[yb][:], in_=p[:, 0:256]),
                                         reads=[pb], writes=[Ysb_b[yb]])
                                    p2, p2b = next_pw()
                                    for sp in range(2):
                                        S.op("pe", lambda e, p2=p2, sp=sp, yb=yb: e.transpose(
                                            out=p2[:, sp * 128:(sp + 1) * 128], in_=Ysb[yb][:, sp * 128:(sp + 1) * 128],
                                            identity=ident_f[:]), reads=[Ysb_b[yb], cst], writes=[p2b])
                                    for sp in range(2):
                                        S.op("act", lambda e, p2=p2, sp=sp, g=g: e.copy(
                                            out=Ytok[:, sp, :, g * 16:(g + 1) * 16],
                                            in_=p2[:, sp * 128:(sp + 1) * 128].rearrange("p (a b) -> p a b", b=16)),
                                            reads=[p2b], writes=[Ytok_b])
                                    yield

                            act_p = []
                            nxt_p = 0
                            steps = 0
                            while nxt_p < 16 or act_p:
                                if nxt_p < 16 and len(act_p) < 2 and (nxt_p == 0 or steps >= 6):
                                    if not act_p or act_p[-1][0] % 2 != nxt_p % 2:
                                        act_p.append((nxt_p, gen_pair(nxt_p)))
                                        nxt_p += 1
                                for item in list(act_p):
                                    try:
                                        next(item[1])
                                    except StopIteration:
                                        act_p.remove(item)
                                steps += 1
                            g1 = [sb(st2, "g1_%d" % i, [128, 512], F32) for i in range(2)]
                            g2_ = [sb(st2, "g2_%d" % i, [128, 512], F32) for i in range(2)]
                            zf = [sb(st2, "zf%d" % i, [128, 512], F32) for i in range(2)]
                            zb = [sb(st2, "zb%d" % i, [128, 512], BF16) for i in range(2)]
                            zT = [sb(st2, "zT%d" % i, [128, 4, 128], BF16) for i in range(2)]
                            sg = [sb(st2, "sg%d" % i, [128, 512], F32) for i in range(2)]
                            mb_ = [sb(st2, "mb%d" % i, [128, 512], BF16) for i in range(2)]
                            sst = [sb(st2, "sst%d" % i, [128, 4], F32) for i in range(2)]
                            gb = [[Buf() for _ in range(8)] for _ in range(2)]
                            KG = 2.0 * math.sqrt(2.0 / math.pi)
                            for sp in range(2):
                                for i in range(8):
                                    b = i % 2
                                    B = gb[b]
                                    y = Ytok[:, sp, i, :]
                                    S.op("act", lambda e, b=b, y=y: e.activation(out=g1[b][:], in_=y, func=AF.Square),
                                         reads=[Ytok_b], writes=[B[0]])
                                    S.op("dve", lambda e, b=b: e.tensor_scalar(out=g1[b][:], in0=g1[b][:], scalar1=0.044715,
                                                                               scalar2=1.0, op0=ALU.mult, op1=ALU.add),
                                         reads=[B[0]], writes=[B[0]])
                                    S.op("dve", lambda e, b=b, y=y: e.tensor_tensor(out=g2_[b][:], in0=g1[b][:], in1=y, op=ALU.mult),
                                         reads=[B[0], Ytok_b], writes=[B[1]])
                                    S.op("act", lambda e, b=b: e.activation(out=g2_[b][:], in_=g2_[b][:], func=AF.Sigmoid, scale=KG),
                                         reads=[B[1]], writes=[B[1]])
                                    S.op("dve", lambda e, b=b, y=y: e.tensor_tensor(out=zf[b][:], in0=g2_[b][:], in1=y, op=ALU.mult),
                                         reads=[B[1], Ytok_b], writes=[B[2]])
                                    S.op("act", lambda e, b=b: e.copy(out=zb[b][:], in_=zf[b][:]), reads=[B[2]], writes=[B[3]])
                                    ptt, ptb_ = next_pt()
                                    for ft in range(4):
                                        S.op("pe", lambda e, ptt=ptt, b=b, ft=ft: e.transpose(
                                            out=ptt[:, ft * 128:(ft + 1) * 128], in_=zb[b][:, ft * 128:(ft + 1) * 128],
                                            identity=ident_bf[:]), reads=[B[3], cst], writes=[ptb_])
                                    S.op("act", lambda e, ptt=ptt, b=b: e.copy(out=zT[b][:].rearrange("p a b -> p (a b)"),
                                                                               in_=ptt[:, 0:512]), reads=[ptb_], writes=[B[4]])
                                    p, pb = next_pw()
                                    for ft in range(4):
                                        S.op("pe", lambda e, p=p, b=b, ft=ft: e.matmul(
                                            p[:], zT[b][:, ft, :], w_glu[:, ft, :], start=(ft == 0), stop=(ft == 3)),
                                            reads=[B[4], w_glu_b], writes=[pb])
                                    S.op("dve", lambda e, p=p, b=b: e.tensor_tensor(out=sg[b][:], in0=p[:], in1=bglu_b[:], op=ALU.add),
                                         reads=[pb, cst], writes=[B[5]])
                                    S.op("act", lambda e, b=b: e.activation(out=sg[b][:], in_=sg[b][:], func=AF.Sigmoid),
                                         reads=[B[5]], writes=[B[5]])
                                    S.op("dve", lambda e, b=b: e.tensor_tensor(out=sg[b][:], in0=sg[b][:], in1=zf[b][:], op=ALU.mult),
                                         reads=[B[5], B[2]], writes=[B[5]])
                                    S.op("act", lambda e, b=b: e.activation(out=g1[b][:], in_=sg[b][:], func=AF.Square,
                                                                            accum_out=sst[b][:, 0:1]),
                                         reads=[B[5], B[0]], writes=[B[0], B[6]])
                                    S.op("dve", lambda e, b=b: e.tensor_scalar(out=sst[b][:, 1:2], in0=sst[b][:, 0:1], scalar1=1.0 / 512,
                                                                               scalar2=EPS, op0=ALU.mult, op1=ALU.add),
                                         reads=[B[6]], writes=[B[6]])
                                    S.op("pool", lambda e, b=b: e.tensor_tensor(out=sst[b][:, 2:3], in0=sst[b][:, 1:2], in1=neghalf[:, 0:1],
                                                                                op=ALU.pow), reads=[B[6], cst], writes=[B[6]])
                                    S.op("dve", lambda e, b=b: e.scalar_tensor_tensor(
                                        out=mb_[b][:], in0=sg[b][:], scalar=sst[b][:, 2:3], in1=gns_b[:], op0=ALU.mult, op1=ALU.mult),
                                        reads=[B[5], B[6], cst], writes=[B[7]])
                                    ptt, ptb_ = next_pt()
                                    for ft in range(4):
                                        S.op("pe", lambda e, ptt=ptt, b=b, ft=ft: e.transpose(
                                            out=ptt[:, ft * 128:(ft + 1) * 128], in_=mb_[b][:, ft * 128:(ft + 1) * 128],
                                            identity=ident_bf[:]), reads=[B[7], cst], writes=[ptb_])
                                    dst = mixT_s[:, :, sp * 1024:(sp + 1) * 1024].rearrange("p a (b i) -> p a i b", i=8)[:, :, i, :]
                                    S.op("act", lambda e, ptt=ptt, dst=dst: e.copy(
                                        out=dst, in_=ptt[:, 0:512].rearrange("p (a b) -> p a b", b=128)),
                                        reads=[ptb_], writes=[mixT_sb])
                            S.barrier()

                    chk('s2')
                    with ExitStack() as st3:
                        extra_bank(st3, False)
                        w_out = sb(st3, "w_out", [128, 8, D], BF16)
                        w_out_b = Buf()
                        S.dma("pool", w_out[:], wout_d, writes=[w_out_b])
                        score = [sb(st3, "score%d" % i, [128, SEQ], F32) for i in range(2)]
                        score_b = [Buf(), Buf()]
                        nm = [sb(st3, "nm%d" % i, [128, SEQ], BF16) for i in range(3)]
                        nm_b = [Buf() for _ in range(3)]
                        rbuf = [sb(st3, "rbuf%d" % i, [128, 8, 512], BF16) for i in range(2)]
                        rbuf_b = [Buf(), Buf()]
                        dg = [sb(st3, "dg%d" % i, [128, 8, 128], BF16) for i in range(2)]
                        dg_b = [Buf(), Buf()]
                        bs = [sb(st3, "bs%d" % i, [128, 16 + 3 * NITER], F32) for i in range(2)]
                        bs_b = [Buf(), Buf()]
                        PT = [sb(st3, "PT%d" % i, [128, 512], BF16) for i in range(3)]
                        PT_b = [Buf() for _ in range(3)]
                        yatt = sb(st3, "yatt", [128, 512], F32)
                        yatt_b = Buf()
                        rden = sb(st3, "rden", [128, 8], F32)
                        ajunk = sb(st3, "ajunk", [128, 512], BF16)
                        ast = sb(st3, "ast", [128, 4], F32)
                        mixa = sb(st3, "mixa", [128, 512], BF16)
                        mixa_b = Buf()
                        mixTa = sb(st3, "mixTa", [128, 4, 128], BF16)
                        mixTa_b = Buf()
                        xr = [sb(st3, "xr%d" % i, [128, D], F32) for i in range(2)]
                        xr_b = [Buf(), Buf()]
                        x1t = [sb(st3, "x1t%d" % i, [128, 512], F32) for i in range(2)]
                        x1t_b = [Buf(), Buf()]
                        prot = {"I": 0, "A": 0, "pt": 0}

                        def pwI():
                            i = prot["I"] % 3
                            prot["I"] += 1
                            if i == 2:
                                return xbank["t"], xbank["b"]
                            return pf[i], pfb[i]

                        def pwA():
                            i = 2 + prot["A"] % 2
                            prot["A"] += 1
                            return pf[i], pfb[i]

                        def gen_I(qt):
                            sl = qt % 2
                            nsl = qt % 3
                            nk = qt + 1
                            SK = nk * 128
                            qs = slice(qt * 128, (qt + 1) * 128)
                            sc, scb = score[sl], score_b[sl]
                            BS, BSb = bs[sl], bs_b[sl]
                            S.op("dve", lambda e: e.tensor_tensor(
                                out=dg[sl][:], in0=ident_bf[:].unsqueeze(1).to_broadcast([128, 8, 128]),
                                in1=wi[:, qt, :].unsqueeze(2).to_broadcast([128, 8, 128]), op=ALU.mult),
                                reads=[cst, wi_b], writes=[dg_b[sl]])
                            nch = (SK + 511) // 512
                            for c in range(nch):
                                cw = min(512, SK - c * 512)
                                ks = slice(c * 512, c * 512 + cw)
                                for h in range(8):
                                    j, half = h // 2, h % 2
                                    p, pb = pwI()
                                    S.op("pe", lambda e, p=p, j=j, half=half: e.matmul(
                                        p[:, 0:cw], qiT[:, j, qs], kiTz[:, half, ks], start=True, stop=True),
                                        reads=[qiT_b, kiT_b], writes=[pb])
                                    if False:
                                        S.op("act", lambda e, p=p, h=h: e.activation(
                                            out=rbuf[sl][:, h, 0:cw], in_=p[:, 0:cw], func=AF.Relu),
                                            reads=[pb], writes=[rbuf_b[sl]])
                                    else:
                                        S.op("dve", lambda e, p=p, h=h: e.tensor_scalar(
                                            out=rbuf[sl][:, h, 0:cw], in0=p[:, 0:cw], scalar1=0.0, scalar2=None, op0=ALU.max),
                                            reads=[pb], writes=[rbuf_b[sl]])
                                p, pb = pwI()
                                for h in range(8):
                                    S.op("pe", lambda e, p=p, h=h: e.matmul(
                                        p[:, 0:cw], dg[sl][:, h, :], rbuf[sl][:, h, 0:cw], start=(h == 0), stop=(h == 7)),
                                        reads=[dg_b[sl], rbuf_b[sl]], writes=[pb])
                                S.op("act", lambda e, p=p: e.copy(out=sc[:, ks], in_=p[:, 0:cw]),
                                     reads=[pb], writes=[scb])
                                S.op("dve", lambda e, c=c: e.tensor_reduce(
                                    out=BS[:, c:c + 1], in_=sc[:, ks], axis=AX.X, op=ALU.max, apply_absolute_value=True),
                                    reads=[scb], writes=[BSb])
                                yield
                            S.op("dve", lambda e: e.tensor_tensor(out=sc[:, qs], in0=sc[:, qs], in1=causal[:], op=ALU.add),
                                 reads=[scb, cst], writes=[scb])
                            if DVE_BISECT and qt >= 2 and qt % 2 == 1:
                                S.op("dve", lambda e: e.tensor_reduce(
                                    out=BS[:, 4:5], in_=BS[:, 0:nch], axis=AX.X, op=ALU.max), reads=[BSb], writes=[BSb])
                                S.op("dve", lambda e: e.tensor_scalar(
                                    out=BS[:, 8:8 + NITER + 1], in0=pow2[:, 0:NITER + 1], scalar1=BS[:, 4:5], scalar2=None,
                                    op0=ALU.mult), reads=[BSb, cst], writes=[BSb])
                                S.op("dve", lambda e: e.memset(BS[:, 5:6], 0.0), reads=[BSb], writes=[BSb])
                                for n in range(NITER):
                                    S.op("dve", lambda e: e.tensor_scalar(
                                        out=nm[nsl][:, 0:SK], in0=sc[:, 0:SK], scalar1=BS[:, 5:6], scalar2=None,
                                        op0=ALU.is_ge, op1=ALU.add, accum_out=BS[:, 6:7]),
                                        reads=[scb, BSb], writes=[nm_b[nsl], BSb])
                                    yield
                                    S.op("dve", lambda e, n=n: e.tensor_scalar(
                                        out=BS[:, 7:8], in0=BS[:, 6:7], scalar1=float(TOPK) - 0.5, scalar2=BS[:, 8 + n:9 + n],
                                        op0=ALU.is_ge, op1=ALU.mult), reads=[BSb], writes=[BSb])
                                    yield
                                    S.op("dve", lambda e, n=n: e.scalar_tensor_tensor(
                                        out=BS[:, 5:6], in0=BS[:, 7:8], scalar=BS[:, 9 + n:10 + n], in1=BS[:, 5:6],
                                        op0=ALU.subtract, op1=ALU.add), reads=[BSb], writes=[BSb])
                                    yield
                                S.op("dve", lambda e: e.tensor_tensor(
                                    out=BS[:, 5:6], in0=BS[:, 5:6], in1=BS[:, 8 + NITER:9 + NITER], op=ALU.subtract),
                                    reads=[BSb], writes=[BSb])
                                S.op("dve", lambda e: e.tensor_scalar(
                                    out=nm[nsl][:, 0:SK], in0=sc[:, 0:SK], scalar1=BS[:, 5:6], scalar2=None, op0=ALU.is_lt),
                                    reads=[scb, BSb], writes=[nm_b[nsl]])
                                yield
                                return
                            if qt >= 2:
                                S.op("dve", lambda e: e.tensor_reduce(
                                    out=BS[:, 4:5], in_=BS[:, 0:nch], axis=AX.X, op=ALU.max), reads=[BSb], writes=[BSb])
                                S.op("dve", lambda e: e.tensor_scalar(
                                    out=BS[:, 8:8 + NITER + 1], in0=pow2[:, 0:NITER + 1], scalar1=BS[:, 4:5], scalar2=None,
                                    op0=ALU.mult), reads=[BSb, cst], writes=[BSb])
                                S.op("dve", lambda e: e.memset(BS[:, 5:6], 0.0), reads=[BSb], writes=[BSb])
                                thr = float(2 * TOPK - SK) - 0.5
                                S.op("dve", lambda e: e.memset(BS[:, 3:4], -thr), reads=[BSb], writes=[BSb])
                                S.op("dve", lambda e: e.tensor_scalar(
                                    out=BS[:, 9 + NITER:10 + 2 * NITER], in0=BS[:, 8:9 + NITER], scalar1=-1.0, scalar2=None,
                                    op0=ALU.mult), reads=[BSb], writes=[BSb])
                                for n in range(NITER):
                                    S.op("act", lambda e: e.activation(
                                        out=nm[nsl][:, 0:SK], in_=sc[:, 0:SK], func=AF.Sign, bias=BS[:, 5:6], scale=1.0,
                                        accum_out=BS[:, 6:7]), reads=[scb, BSb], writes=[nm_b[nsl], BSb])
                                    yield
                                    S.op("act", lambda e: e.activation(
                                        out=BS[:, 7:8], in_=BS[:, 6:7], func=AF.Sign, bias=BS[:, 3:4], scale=1.0),
                                        reads=[BSb], writes=[BSb])
                                    yield
                                    S.op("act", lambda e, n=n: e.activation(
                                        out=BS[:, 5:6], in_=BS[:, 7:8], func=AF.Identity, bias=BS[:, 5:6],
                                        scale=BS[:, 9 + NITER + 1 + n:10 + NITER + 1 + n]),
                                        reads=[BSb], writes=[BSb])
                                    yield
                                S.op("dve", lambda e: e.tensor_tensor(
                                    out=BS[:, 5:6], in0=BS[:, 5:6], in1=BS[:, 8 + NITER:9 + NITER], op=ALU.add),
                                    reads=[BSb], writes=[BSb])
                                ntau = BS[:, 5:6]
                            else:
                                ntau = taufix[:, 0:1]
                            S.op("dve", lambda e: e.tensor_scalar(
                                out=nm[nsl][:, 0:SK], in0=sc[:, 0:SK], scalar1=ntau, scalar2=0.0, op0=ALU.add, op1=ALU.is_lt),
                                reads=[scb, BSb, cst], writes=[nm_b[nsl]])
                            yield

                        def gen_A(qt):
                            nsl = qt % 3
                            nk = qt + 1
                            qs = slice(qt * 128, (qt + 1) * 128)
                            O = [pf[4], pf[5]]
                            Ob = [pfb[4], pfb[5]]
                            for kv in range(2):
                                S.op("pe", lambda e, kv=kv: e.matmul(O[kv][:, 0:260], zeros_bf[:, 0:128], zeros_bf[:, 0:260],
                                                                     start=True, stop=True), reads=[cst], writes=[Ob[kv]])
                            steps = [(kt, kv) for kt in range(nk) for kv in range(2)]

                            def emit_L(kt, kv):
                                ksl = slice(kt * 128, (kt + 1) * 128)
                                p, pb = pwA()
                                S.op("pe", lambda e, p=p: e.matmul(
                                    p[:], nm[nsl][:, ksl], negi4[:], start=True, stop=True),
                                    reads=[nm_b[nsl], cst], writes=[pb])
                                for hh in range(4):
                                    head = kv * 4 + hh
                                    j, half = head // 2, head % 2
                                    S.op("pe", lambda e, p=p, hh=hh, half=half, j=j: e.matmul(
                                        p[:, hh * 128:(hh + 1) * 128], kTz[:, kv, half, ksl], qT[:, j, qs],
                                        start=False, stop=True, skip_group_check=True),
                                        reads=[kT_b, qT_b], writes=[pb])
                                return p, pb

                            def emit_PV(kt, kv, p, pb):
                                pi3 = prot["pt"] % 3
                                prot["pt"] += 1
                                S.op("act", lambda e: e.activation(
                                    out=PT[pi3][:], in_=p[:], func=AF.Exp, bias=negM[:, 0:1], scale=1.0),
                                    reads=[pb, cst], writes=[PT_b[pi3]])
                                for hh in range(4):
                                    S.op("pe", lambda e, hh=hh: e.matmul(
                                        O[kv][:, hh * 65:(hh + 1) * 65], PT[pi3][:, hh * 128:(hh + 1) * 128], Vp[:, kt, kv, :],
                                        start=False, stop=True, skip_group_check=True),
                                        reads=[PT_b[pi3], Vp_b], writes=[Ob[kv]])

                            Lcur = emit_L(*steps[0])
                            for i, (kt, kv) in enumerate(steps):
                                Lnext = emit_L(*steps[i + 1]) if i + 1 < len(steps) else None
                                emit_PV(kt, kv, *Lcur)
                                Lcur = Lnext
                                yield
                            for kv in range(2):
                                ov = O[kv][:, 0:260].rearrange("p (h e) -> p h e", e=65)
                                S.op("dve", lambda e, kv=kv, ov=ov: e.reciprocal(out=rden[:, kv * 4:(kv + 1) * 4], in_=ov[:, :, 64]),
                                     reads=[Ob[kv]], writes=[yatt_b])
                                S.op("dve", lambda e, kv=kv, ov=ov: e.tensor_tensor(
                                    out=yatt[:, kv * 256:(kv + 1) * 256].rearrange("p (h d) -> p h d", d=64), in0=ov[:, :, 0:64],
                                    in1=rden[:, kv * 4:(kv + 1) * 4].unsqueeze(2).to_broadcast([128, 4, 64]), op=ALU.mult),
                                    reads=[Ob[kv], yatt_b], writes=[yatt_b])
                            S.op("act", lambda e: e.activation(out=ajunk[:], in_=yatt[:], func=AF.Square, accum_out=ast[:, 0:1]),
                                 reads=[yatt_b], writes=[yatt_b])
                            S.op("dve", lambda e: e.tensor_scalar(out=ast[:, 1:2], in0=ast[:, 0:1], scalar1=1.0 / 512, scalar2=EPS,
                                                                  op0=ALU.mult, op1=ALU.add), reads=[yatt_b], writes=[yatt_b])
                            S.op("pool", lambda e: e.tensor_tensor(out=ast[:, 2:3], in0=ast[:, 1:2], in1=neghalf[:, 0:1], op=ALU.pow),
                                 reads=[yatt_b, cst], writes=[yatt_b])
                            S.op("dve", lambda e: e.scalar_tensor_tensor(out=mixa[:], in0=yatt[:], scalar=ast[:, 2:3], in1=gna_b[:],
                                                                          op0=ALU.mult, op1=ALU.mult),
                                 reads=[yatt_b, cst], writes=[mixa_b])
                            ptt, ptb_ = next_pt()
                            for ft in range(4):
                                S.op("pe", lambda e, ptt=ptt, ft=ft: e.transpose(
                                    out=ptt[:, ft * 128:(ft + 1) * 128], in_=mixa[:, ft * 128:(ft + 1) * 128], identity=ident_bf[:]),
                                    reads=[mixa_b, cst], writes=[ptb_])
                            S.op("act", lambda e, ptt=ptt: e.copy(out=mixTa[:].rearrange("p a b -> p (a b)"), in_=ptt[:, 0:512]),
                                 reads=[ptb_], writes=[mixTa_b])
                            yield
                            xb = qt % 2
                            rows = slice(r0 + qt * 128, r0 + (qt + 1) * 128)
                            S.dma("sp", xr[xb][:], x_d[rows, :], writes=[xr_b[xb]])
                            for hf in range(2):
                                p, pb = pwA()
                                for k8 in range(8):
                                    lhs = mixT_s[:, k8, qs] if k8 < 4 else mixTa[:, k8 - 4, :]
                                    S.op("pe", lambda e, p=p, lhs=lhs, k8=k8, hf=hf: e.matmul(
                                        p[:], lhs, w_out[:, k8, hf * 512:(hf + 1) * 512], start=(k8 == 0), stop=(k8 == 7)),
                                        reads=[mixT_sb, mixTa_b, w_out_b], writes=[pb])
                                hs = slice(hf * 512, (hf + 1) * 512)
                                S.op("dve", lambda e, p=p, hf=hf, hs=hs: e.tensor_tensor(
                                    out=x1t[hf][:], in0=p[:], in1=tabsA[s][:, 2, hs], op=ALU.mult),
                                    reads=[pb, tabsA_b[s]], writes=[x1t_b[hf]])
                                S.op("dve", lambda e, xb=xb, hf=hf, hs=hs: e.tensor_tensor(
                                    out=xr[xb][:, hs], in0=x1t[hf][:], in1=xr[xb][:, hs], op=ALU.add),
                                    reads=[x1t_b[hf], xr_b[xb]], writes=[xr_b[xb]])
                            S.dma("sp", out_d[rows, :], xr[xb][:], reads=[xr_b[xb]])
                            yield

                        NQ = 16
                        actI = []
                        actA = None
                        nextI, nextA = 0, 0
                        doneI = set()
                        while nextA < NQ or actA is not None:
                            if actA is None and nextA in doneI:
                                actA = (nextA, gen_A(nextA))
                                nextA += 1
                            firstA = actA[0] if actA is not None else nextA
                            while (nextI < NQ and len(actI) < 2 and nextI - 3 < firstA
                                   and (nextI < 2 or (nextI - 2) in doneI)):
                                actI.append((nextI, gen_I(nextI)))
                                nextI += 1
                            for item in list(actI):
                                try:
                                    next(item[1])
                                except StopIteration:
                                    actI.remove(item)
                                    doneI.add(item[0])
                            if actA is not None:
                                try:
                                    next(actA[1])
                                except StopIteration:
                                    actA = None
                        S.barrier()

        chk('s3')
        stC.close()
        NB = 512
        NTT = NB // 128
        with ExitStack() as stB:
            tabsB1 = sb(stB, "tabsB", [128, 3, D], F32)
            tabsB_b1 = Buf()
            extra_bank(stB, True)
            W1 = sb(stB, "W1", [128, 8, DFF], BF16)
            W2 = sb(stB, "W2", [128, 32, D], BF16)
            W_b = Buf()
            with ExitStack() as stW:
                NSTG = 4
                stg = [sb(stW, "stg%d" % i, [128, 2048], F32) for i in range(NSTG)]
                stg_b = [Buf() for _ in range(NSTG)]
                ci = 0
                chunks = []
                for kt in range(8):
                    for hc in range(2):
                        chunks.append((W1[:, kt, hc * 2048:(hc + 1) * 2048], wff1_d[:, kt, hc * 2048:(hc + 1) * 2048]))
                for ft in range(0, 32, 2):
                    chunks.append((W2[:, ft:ft + 2, :].rearrange("p a b -> p (a b)"),
                                   wff2_d[:, ft:ft + 2, :].rearrange("p a b -> p (a b)")))
                for dst, src in chunks:
                    b = ci % NSTG
                    S.dma("sp", stg[b][:], src, writes=[stg_b[b]])
                    eng = ("act", "pool", "dve")[ci % 3]
                    if eng == "act":
                        S.op("act", lambda e, dst=dst, b=b: e.copy(out=dst, in_=stg[b][:]), reads=[stg_b[b]], writes=[W_b])
                    else:
                        S.op(eng, lambda e, dst=dst, b=b: e.tensor_copy(out=dst, in_=stg[b][:]), reads=[stg_b[b]], writes=[W_b])
                    ci += 1
                S.barrier()
            hidT = sb(stB, "hidT", [128, 32, NB], BF16)
            hid_b = [Buf() for _ in range(32)]
            rl = [sb(stB, "rl%d" % i, [128, NB], BF16) for i in range(2)]
            rl_b = [Buf(), Buf()]
            h2T = sb(stB, "h2T", [128, 8, NB], BF16)
            h2T_b = Buf()
            x1 = [sb(stB, "x1_%d" % i, [128, D], F32) for i in range(2)]
            x1_b = [Buf() for _ in range(2)]
            tB = sb(stB, "tB", [128, D], F32)
            tB_b = Buf()
            h2 = [sb(stB, "h2_%d" % i, [128, D], BF16) for i in range(2)]
            h2_b = [Buf(), Buf()]
            stb = [sb(stB, "stb%d" % i, [128, 4], F32) for i in range(2)]
            stb_b = [Buf(), Buf()]
            ot = [sb(stB, "ot%d" % i, [128, D], F32) for i in range(2)]
            ot_b = [Buf(), Buf()]
            oc = 0
            nblk = NSEQ * SEQ // NB
            for blk in range(nblk):
                s = (blk * NB) // SEQ
                if (blk * NB) % SEQ == 0:
                    S.dma("sp", tabsB1[:].rearrange("p a b -> p (a b)"), tabAll_d[NSEQ + s], reads=[tabAll_b], writes=[tabsB_b1])
                for tt in range(NTT):
                    rows = slice(blk * NB + tt * 128, blk * NB + (tt + 1) * 128)
                    b = tt % 2
                    S.dma("sp", x1[b][:], out_d[rows, :], writes=[x1_b[b]])
                    S.op("act", lambda e, b=b: e.activation(out=h2[b][:], in_=x1[b][:], func=AF.Square,
                                                            accum_out=stb[b][:, 0:1]),
                         reads=[x1_b[b]], writes=[h2_b[b], stb_b[b]])
                    S.op("act", lambda e, b=b: e.activation(out=stb[b][:, 1:2], in_=stb[b][:, 0:1], func=AF.Sqrt,
                                                            bias=EPS, scale=1.0 / D), reads=[stb_b[b]], writes=[stb_b[b]])
                    S.op("dve", lambda e, b=b: e.reciprocal(out=stb[b][:, 2:3], in_=stb[b][:, 1:2]),
                         reads=[stb_b[b]], writes=[stb_b[b]])
                    S.op("dve", lambda e, b=b: e.scalar_tensor_tensor(
                        out=tB[:], in0=x1[b][:], scalar=stb[b][:, 2:3], in1=tabsB1[:, 1, :], op0=ALU.mult, op1=ALU.mult),
                        reads=[x1_b[b], stb_b[b], tabsB_b1], writes=[tB_b])
                    S.op("dve", lambda e, b=b: e.tensor_tensor(out=h2[b][:], in0=tB[:], in1=tabsB1[:, 0, :], op=ALU.add),
                         reads=[tB_b, tabsB_b1], writes=[h2_b[b]])
                    ptt, ptb_ = next_pt()
                    for kt in range(8):
                        S.op("pe", lambda e, ptt=ptt, b=b, kt=kt: e.transpose(
                            out=ptt[:, kt * 128:(kt + 1) * 128], in_=h2[b][:, kt * 128:(kt + 1) * 128], identity=ident_bf[:]),
                            reads=[h2_b[b], cst], writes=[ptb_])
                    S.op("act", lambda e, ptt=ptt, tt=tt: e.copy(
                        out=h2T[:, :, tt * 128:(tt + 1) * 128], in_=ptt[:].rearrange("p (a b) -> p a b", b=128)),
                        reads=[ptb_], writes=[h2T_b])
                for ft in range(32):
                    p, pb = next_pw(2)
                    for kt in range(8):
                        S.op("pe", lambda e, p=p, kt=kt, ft=ft: e.matmul(
                            p[:, 0:NB], W1[:, kt, ft * 128:(ft + 1) * 128], h2T[:, kt, :], start=(kt == 0), stop=(kt == 7)),
                            reads=[W_b, h2T_b], writes=[pb])
                    b = ft % 2
                    S.op("act", lambda e, p=p, b=b: e.activation(out=rl[b][:], in_=p[:, 0:NB], func=AF.Relu),
                         reads=[pb], writes=[rl_b[b]])
                    S.op("pool", lambda e, b=b, ft=ft: e.tensor_tensor(out=hidT[:, ft, :], in0=rl[b][:], in1=rl[b][:], op=ALU.mult),
                         reads=[rl_b[b]], writes=[hid_b[ft]])
                for tt in range(NTT):
                    rows = slice(blk * NB + tt * 128, blk * NB + (tt + 1) * 128)
                    ob = oc % 2
                    oc += 1
                    S.dma("sp", ot[ob][:], out_d[rows, :], writes=[ot_b[ob]])
                    for hf in range(2):
                        bi = 2 + (2 * tt + hf) % 4
                        p, pb = pf[bi], pfb[bi]
                        for ft in range(32):
                            S.op("pe", lambda e, p=p, ft=ft, tt=tt, hf=hf: e.matmul(
                                p[:], hidT[:, ft, tt * 128:(tt + 1) * 128], W2[:, ft, hf * 512:(hf + 1) * 512],
                                start=(ft == 0), stop=(ft == 31)), reads=[hid_b[ft], W_b], writes=[pb])
                        hs = slice(hf * 512, (hf + 1) * 512)
                        S.op("dve", lambda e, p=p, hs=hs: e.tensor_tensor(
                            out=tB[:, hs], in0=p[:], in1=tabsB1[:, 2, hs], op=ALU.mult),
                            reads=[pb, tabsB_b1], writes=[tB_b])
                        S.op("dve", lambda e, ob=ob, hs=hs: e.tensor_tensor(
                            out=ot[ob][:, hs], in0=tB[:, hs], in1=ot[ob][:, hs], op=ALU.add),
                            reads=[tB_b, ot_b[ob]], writes=[ot_b[ob]])
                    S.dma("sp", out_d[rows, :], ot[ob][:], reads=[ot_b[ob]])
            S.barrier()
    except _Stop:
        pass
    return nc


_PROGRAM = None


def _prep_shared(inp):
    f = np.float32
    def kt_layout(w):
        K, N = w.shape
        return np.ascontiguousarray(w.reshape(K // 128, 128, N).transpose(1, 0, 2)).astype(f)
    def bc(v, n=128):
        return np.ascontiguousarray(np.broadcast_to(np.asarray(v, f).reshape(1, -1), (n, np.asarray(v).size)))
    def qlay(a):
        a = np.asarray(a, f)
        rest = a.shape[2:]
        return np.ascontiguousarray(a.reshape((16, 2, 64) + rest).transpose((1, 2, 0) + tuple(range(3, 3 + len(rest))))
                                    .reshape((128, 16) + rest))
    sh = {}
    sh["w_ada"] = kt_layout(inp["w_ada"][0])
    sh["b_ada_b"] = bc(inp["b_ada"][0])
    sh["w_in"] = kt_layout(inp["w_in"][0])
    sh["w_out"] = kt_layout(inp["w_out"][0])
    sh["w_glu"] = kt_layout(inp["w_glu"][0])
    sh["w_ff1"] = kt_layout(inp["w_ff1"][0])
    sh["w_ff2"] = kt_layout(inp["w_ff2"][0])
    sh["norm1_g_b"] = bc(inp["norm1_g"][0])
    sh["norm2_g_b"] = bc(inp["norm2_g"][0])
    sh["b_glu_b"] = bc(inp["b_glu"][0])
    sh["gn_ssm_b"] = bc(inp["gn_ssm"][0])
    sh["gn_attn_b"] = bc(inp["gn_attn"][0])
    sh["gq_b"] = bc(inp["q_gain"][0])
    sh["gk_b"] = bc(inp["k_gain"][0])
    sh["gq2"] = np.ascontiguousarray(np.tile(np.asarray(inp["q_gain"][0], f), 2).reshape(128, 1))
    sh["gk2"] = np.ascontiguousarray(np.tile(np.asarray(inp["k_gain"][0], f), 2).reshape(128, 1))
    sh["lamre_q"] = qlay(inp["lam_re"][0])
    sh["lamim_q"] = qlay(inp["lam_im"][0])
    sh["logdt_q"] = qlay(np.broadcast_to(np.asarray(inp["log_dt"][0], f)[:, None], (32, 64)))
    sh["bre_q"] = qlay(inp["ssm_b_re"][0])
    sh["bim_q"] = qlay(inp["ssm_b_im"][0])
    sh["cre_q"] = qlay(np.asarray(inp["ssm_c_re"][0]).transpose(0, 2, 1))
    sh["cim_q"] = qlay(np.asarray(inp["ssm_c_im"][0]).transpose(0, 2, 1))
    dsk = np.asarray(inp["d_skip"][0], f).reshape(32, 16)
    sh["dsk_b"] = np.ascontiguousarray(np.broadcast_to(np.tile(dsk, (1, 8))[None], (128, 32, 128))).astype(f)
    sh["ident"] = np.eye(128, dtype=f)
    r = np.arange(128)
    sh["causal"] = np.where(r[None, :] <= r[:, None], 0.0, -1.0e30).astype(f)
    sh["negi4"] = np.tile(-BIG * np.eye(128, dtype=f), (1, 4)).astype(f)
    ib, jb = r // 16, r // 16
    sh["bmask"] = (jb[None, :] >= ib[:, None]).astype(f)
    sh["dmask"] = (r[None, :] == r[:, None]).astype(f)
    sh["onesblk"] = ((r[None, :] // 64) == (r[:, None] // 64)).astype(f)
    sh["mtab"] = np.ascontiguousarray(np.broadcast_to(np.asarray(EXPS, f)[None, None, :], (128, 16, NE)))
    sh["pow2"] = np.ascontiguousarray(np.broadcast_to((2.0 ** -np.arange(NITER + 2)).astype(f)[None], (128, NITER + 2)))
    zm = np.zeros((128, 2), f)
    zm[:64, 0] = 1.0
    zm[64:, 1] = 1.0
    sh["zmask"] = zm
    return sh


def kernel(**inputs):
    global _PROGRAM
    inp = {k: np.asarray(v) for k, v in inputs.items()}
    if _PROGRAM is None:
        _PROGRAM = build_program()
    nc = _PROGRAM
    shared = _prep_shared(inp)
    x = np.asarray(inp["x"], np.float32)
    c = np.asarray(inp["c"], np.float32)
    in_maps = []
    for core in range(NCORES):
        m = dict(shared)
        m["x"] = np.ascontiguousarray(x[NSEQ * core: NSEQ * (core + 1)].reshape(NSEQ * SEQ, D))
        cc = c[NSEQ * core: NSEQ * (core + 1)]
        cT = cc.reshape(NSEQ, 8, 128).transpose(2, 1, 0)
        m["cT"] = np.ascontiguousarray(np.broadcast_to(cT[:, :, :, None], (128, 8, NSEQ, 128))).astype(np.float32)
        in_maps.append(m)
    res = run_bass_kernel_spmd(nc, in_maps, core_ids=list(range(NCORES)))
    outs = [np.asarray(r["out"], np.float32).reshape(NSEQ, SEQ, D) for r in res.results]
    return np.concatenate(outs, axis=0)
```
